# Optimizing a Trainium2 kernel written in Bass

```python
import math
import jax
import jax.numpy as jnp
from jax import lax
import numpy as np

D_MODEL = 1024
BATCH = 1
SEQ = 16384
DEPTH = 2

GRID_W = 64
CTX_LEN = 256
N_EVEN = (DEPTH + 1) // 2
N_ODD = DEPTH // 2
MIX_W = D_MODEL
EPS = 1e-6
CONV_W = 3

HY_W = MIX_W // 2
HY_ORDER = 2
HY_EMB = 33
HY_FFN = 64
HY_TARGET = 1e-2
HY_SHORT_PCT = 0.3
HY_LONG_PCT = 1.5

HEAD_DIM = 64
ATT_HEADS = (MIX_W // 2) // HEAD_DIM
ATT_KV_HEADS = 2
ATT_GROUP = ATT_HEADS // ATT_KV_HEADS
ATT_WINDOW = 128
ATT_BLOCK = 128
ROPE_BASE = 10000.0
ATT_Q_W = ATT_HEADS * HEAD_DIM
ATT_KV_W = ATT_KV_HEADS * HEAD_DIM

SSD_W = MIX_W // 2
SSD_HEAD_DIM = 64
SSD_HEADS = SSD_W // SSD_HEAD_DIM
SSD_GROUPS = 2
SSD_STATE = 128
SSD_CHUNK = 128
SSD_CONV_CH = SSD_W + 2 * SSD_GROUPS * SSD_STATE

HG_W = MIX_W // 2
HG_EXPAND = 128
HG_HEADS = HG_W // HG_EXPAND
HG_VDIM = HG_W // HG_HEADS
HG_CHUNK = 64

N_EXPERTS = 32
TOP_K = 4
D_EXPERT = D_MODEL
SWIGLU_ALPHA = 1.702
SWIGLU_LIMIT = 7.0
MOE_BLOCK = 128

EVEN_IN = 2 * ATT_KV_W + ATT_Q_W + 3 * HY_W
ODD_STATE_COLS = SSD_CONV_CH + 2 * SSD_HEADS + 3 * HG_W
ODD_IN = ODD_STATE_COLS + SSD_W + 2 * HG_W

kernel_name = 'hybrid_hyena_swa_ssd_hgrn2_moe_dit'


def rms_norm(x, g):
    xf = x.astype(jnp.float32)
    y = xf * lax.rsqrt(jnp.mean(xf * xf, axis=-1, keepdims=True) + EPS)
    return (y * g.astype(jnp.float32)).astype(x.dtype)


def modulate(h, shift, scale):
    return h * (1.0 + scale) + shift


def adaln(cond, w, b, j):
    lo, hi = 3 * j * D_MODEL, 3 * (j + 1) * D_MODEL
    m = jax.nn.silu(cond) @ w[:, lo:hi] + b[lo:hi]
    return jnp.split(m, 3, axis=-1)


def dwconv_centred(u, w, b):
    ch = u.shape[-1]
    y = lax.conv_general_dilated(u, w[:, None, :].astype(u.dtype), window_strides=(1,),
                                 padding=[(CONV_W // 2, CONV_W // 2)],
                                 dimension_numbers=('NWC', 'WIO', 'NWC'), feature_group_count=ch)
    return y + b.astype(u.dtype)


def axial_rope_tables(length):
    rows = length // GRID_W
    n_pairs = HEAD_DIM // 4
    inv = ROPE_BASE ** (-jnp.arange(n_pairs, dtype=jnp.float32) / n_pairs)
    row_ang = jnp.arange(rows, dtype=jnp.float32)[:, None] * inv
    col_ang = jnp.arange(GRID_W, dtype=jnp.float32)[:, None] * inv
    ang_r = jnp.broadcast_to(row_ang[:, None], (rows, GRID_W, n_pairs)).reshape(length, n_pairs)
    ang_c = jnp.broadcast_to(col_ang[None], (rows, GRID_W, n_pairs)).reshape(length, n_pairs)
    return jnp.cos(ang_r), jnp.sin(ang_r), jnp.cos(ang_c), jnp.sin(ang_c)


def _rotate(u, cos, sin):
    n = u.shape[-1] // 2
    u1, u2 = u[..., :n], u[..., n:]
    cos = cos[None, :, None, :]
    sin = sin[None, :, None, :]
    return jnp.concatenate([u1 * cos - u2 * sin, u1 * sin + u2 * cos], axis=-1)


def apply_axial_rope(u, tables):
    cr, sr, cc, sc = tables
    half = HEAD_DIM // 2
    return jnp.concatenate([_rotate(u[..., :half], cr, sr), _rotate(u[..., half:], cc, sc)], axis=-1)


def hyena_filters(length, w1, b1, w2, b2, w3, b3, w4, freq):
    f32 = jnp.float32
    t = jnp.linspace(0.0, 1.0, length, dtype=f32)[:, None]
    bands = (HY_EMB - 1) // 2
    w_ang = 2.0 * math.pi * jnp.arange(length, dtype=f32)[:, None] / length
    fr = jnp.linspace(1e-4, bands - 1, bands, dtype=f32)[None]
    z = jnp.concatenate([t, jnp.cos(fr * w_ang), -jnp.sin(fr * w_ang)], axis=-1)
    fq = freq.astype(f32)
    hdn = jnp.sin(fq * (z @ w1.astype(f32) + b1.astype(f32)))
    hdn = jnp.sin(fq * (hdn @ w2.astype(f32) + b2.astype(f32)))
    hdn = jnp.sin(fq * (hdn @ w3.astype(f32) + b3.astype(f32)))
    h = (hdn @ w4.astype(f32)).reshape(length, HY_ORDER, 2, HY_W)
    max_decay = math.log(HY_TARGET) / HY_SHORT_PCT
    min_decay = math.log(HY_TARGET) / HY_LONG_PCT
    deltas = jnp.abs(jnp.linspace(min_decay, max_decay, HY_W, dtype=f32))
    h = h * jnp.exp(-t * deltas)[:, None, None, :]
    h2 = jnp.concatenate([h[:, :, 0], jnp.zeros((1, HY_ORDER, HY_W), f32), h[:0:-1, :, 1]], axis=0)
    h2 = h2 / jnp.sum(jnp.abs(h2), axis=0, keepdims=True)
    return jnp.fft.rfft(h2, axis=0)


def hyena_mix(u, hf, filter_bias, conv_w, conv_b):
    length = u.shape[1]
    u = dwconv_centred(u.astype(jnp.float32), conv_w, conv_b)
    v, x1, x2 = jnp.split(u, 3, axis=-1)
    z = v
    for o, gate in enumerate((x1, x2)):
        zf = jnp.fft.rfft(z, n=2 * length, axis=1)
        zc = jnp.fft.irfft(zf * hf[None, :, o], n=2 * length, axis=1)[:, :length]
        z = gate * (zc + z * filter_bias[o].astype(jnp.float32))
    return z


def window_attention(q, k, v, k_c, v_c, sink):
    bsz, length = q.shape[:2]
    nb = length // ATT_BLOCK
    scale = HEAD_DIM ** -0.5
    qb = q.reshape(bsz, nb, ATT_BLOCK, ATT_KV_HEADS, ATT_GROUP, HEAD_DIM)
    pad = ((0, 0), (ATT_BLOCK, ATT_BLOCK), (0, 0), (0, 0))

    def band(a):
        ap = jnp.pad(a, pad).reshape(bsz, nb + 2, ATT_BLOCK, ATT_KV_HEADS, HEAD_DIM)
        return jnp.concatenate([ap[:, :-2], ap[:, 1:-1], ap[:, 2:]], axis=2)

    kw, vw = band(k), band(v)
    s_loc = jnp.einsum('bnqhgd,bnkhd->bnhgqk', qb, kw) * scale
    s_ctx = jnp.einsum('bnqhgd,bchd->bnhgqc', qb, k_c) * scale
    qpos = jnp.arange(nb)[:, None] * ATT_BLOCK + jnp.arange(ATT_BLOCK)[None]
    kpos = (jnp.arange(nb)[:, None] - 1) * ATT_BLOCK + jnp.arange(3 * ATT_BLOCK)[None]
    rel = kpos[:, None, :] - qpos[:, :, None]
    valid = (jnp.abs(rel) <= ATT_WINDOW) & (kpos[:, None, :] >= 0) & (kpos[:, None, :] < length)
    s_loc = jnp.where(valid[None, :, None, None], s_loc, -jnp.inf)
    sink_l = jnp.broadcast_to(sink.astype(jnp.float32).reshape(1, 1, ATT_KV_HEADS, ATT_GROUP, 1, 1),
                              s_loc.shape[:-1] + (1,))
    p = jax.nn.softmax(jnp.concatenate([s_loc, s_ctx, sink_l], axis=-1), axis=-1)
    n_loc = 3 * ATT_BLOCK
    n_ctx = k_c.shape[1]
    o = (jnp.einsum('bnhgqk,bnkhd->bnqhgd', p[..., :n_loc], vw)
         + jnp.einsum('bnhgqc,bchd->bnqhgd', p[..., n_loc:n_loc + n_ctx], v_c))
    return o.reshape(bsz, length, ATT_Q_W)


def context_attention(q_c, k_c, v_c, sink):
    bsz, n_ctx = q_c.shape[:2]
    s = jnp.einsum('bqhgd,bkhd->bhgqk', q_c, k_c) * HEAD_DIM ** -0.5
    sink_l = jnp.broadcast_to(sink.astype(jnp.float32).reshape(1, ATT_KV_HEADS, ATT_GROUP, 1, 1),
                              s.shape[:-1] + (1,))
    p = jax.nn.softmax(jnp.concatenate([s, sink_l], axis=-1), axis=-1)[..., :-1]
    return jnp.einsum('bhgqk,bkhd->bqhgd', p, v_c).reshape(bsz, n_ctx, ATT_Q_W)


def ssd_scan(x, dt, a, bm, cm, d_skip, init, need_y):
    bsz, length, n_heads, hd = x.shape
    nc = length // SSD_CHUNK
    hpg = n_heads // SSD_GROUPS
    da = (dt * a).reshape(bsz, nc, SSD_CHUNK, SSD_GROUPS, hpg)
    cs = jnp.cumsum(da, axis=2)
    xdt = (x * dt[..., None]).reshape(bsz, nc, SSD_CHUNK, SSD_GROUPS, hpg, hd)
    bc = bm.reshape(bsz, nc, SSD_CHUNK, SSD_GROUPS, SSD_STATE)
    cc = cm.reshape(bsz, nc, SSD_CHUNK, SSD_GROUPS, SSD_STATE)
    to_end = jnp.exp(cs[:, :, -1:] - cs)
    states = jnp.einsum('bcsgn,bcsgh,bcsghp->bcghpn', bc, to_end, xdt)
    chunk_decay = jnp.exp(cs[:, :, -1])

    def step(s, inp):
        st, dec = inp
        return s * dec[..., None, None] + st, s

    s_final, s_in = lax.scan(step, init, (jnp.moveaxis(states, 1, 0), jnp.moveaxis(chunk_decay, 1, 0)))
    if not need_y:
        return None, s_final
    s_in = jnp.moveaxis(s_in, 0, 1)
    cs_t = jnp.moveaxis(cs, 2, -1)
    diff = cs_t[..., :, None] - cs_t[..., None, :]
    lower = jnp.tril(jnp.ones((SSD_CHUNK, SSD_CHUNK), bool))
    decay = jnp.where(lower, jnp.exp(jnp.where(lower, diff, 0.0)), 0.0)
    scores = jnp.einsum('bclgn,bcsgn->bcgls', cc, bc)
    y_diag = jnp.einsum('bcgls,bcghls,bcsghp->bclghp', scores, decay, xdt)
    y_off = jnp.einsum('bclgn,bcghpn,bclgh->bclghp', cc, s_in, jnp.exp(cs))
    y = (y_diag + y_off).reshape(bsz, length, n_heads, hd) + d_skip[:, None] * x
    return y, s_final


def hgrn2_scan(q, k, v, g, init, need_o):
    bsz, length, n_heads, _ = k.shape
    nc = length // HG_CHUNK

    def chunks(a):
        return a.reshape(bsz, nc, HG_CHUNK, n_heads, a.shape[-1]).transpose(1, 0, 3, 2, 4)

    lower = jnp.tril(jnp.ones((HG_CHUNK, HG_CHUNK), bool))[:, :, None]

    def update(s, kc, vc, cum):
        last = cum[:, :, -1]
        return (s * jnp.exp(last)[..., None]
                + jnp.einsum('bhsk,bhsv->bhkv', kc * jnp.exp(last[:, :, None] - cum), vc))

    if not need_o:
        def step_state(s, inp):
            kc, vc, gc = inp
            return update(s, kc, vc, jnp.cumsum(gc, axis=2)), None
        s_final, _ = lax.scan(step_state, init, (chunks(k), chunks(v), chunks(g)))
        return None, s_final

    def step(s, inp):
        qc, kc, vc, gc = inp
        cum = jnp.cumsum(gc, axis=2)
        diff = cum[:, :, :, None, :] - cum[:, :, None, :, :]
        decay = jnp.where(lower, jnp.exp(jnp.where(lower, diff, 0.0)), 0.0)
        att = jnp.einsum('bhtk,bhsk,bhtsk->bhts', qc, kc, decay)
        o = (jnp.einsum('bhtk,bhkv->bhtv', qc * jnp.exp(cum), s)
             + jnp.einsum('bhts,bhsv->bhtv', att, vc))
        return update(s, kc, vc, cum), o

    s_final, o = lax.scan(step, init, (chunks(q), chunks(k), chunks(v), chunks(g)))
    return o.transpose(1, 0, 3, 2, 4).reshape(bsz, length, n_heads, v.shape[-1]), s_final


def even_mixer(h, hc, w_in, conv_w, conv_b, f_w1, f_b1, f_w2, f_b2, f_w3, f_b3, f_w4, f_freq, f_bias,
               q_norm, k_norm, sink, ctx_out):
    f32 = jnp.float32
    bsz, length, _ = h.shape
    n_ctx = hc.shape[1]
    filt = (f_w1, f_b1, f_w2, f_b2, f_w3, f_b3, f_w4, f_freq)
    o_v = ATT_KV_W
    o_q = 2 * ATT_KV_W
    o_hy = o_q + ATT_Q_W
    p = (h @ w_in).astype(f32)
    pc = (hc @ (w_in if ctx_out else w_in[:, :o_q])).astype(f32)

    def heads(a, n_heads):
        return a.reshape(a.shape[0], a.shape[1], n_heads, HEAD_DIM)

    k_c = rms_norm(heads(pc[..., :o_v], ATT_KV_HEADS), k_norm)
    v_c = heads(pc[..., o_v:o_q], ATT_KV_HEADS)
    rope = axial_rope_tables(length)
    k = apply_axial_rope(rms_norm(heads(p[..., :o_v], ATT_KV_HEADS), k_norm), rope)
    v = heads(p[..., o_v:o_q], ATT_KV_HEADS)
    q = apply_axial_rope(rms_norm(heads(p[..., o_q:o_hy], ATT_HEADS), q_norm), rope)
    att = window_attention(q.reshape(bsz, length, ATT_KV_HEADS, ATT_GROUP, HEAD_DIM), k, v, k_c, v_c, sink)
    hy = hyena_mix(p[..., o_hy:], hyena_filters(length, *filt), f_bias, conv_w, conv_b)
    out = jnp.concatenate([hy, att], axis=-1).astype(h.dtype)
    if not ctx_out:
        return out, None
    q_c = rms_norm(heads(pc[..., o_q:o_hy], ATT_HEADS), q_norm).reshape(bsz, n_ctx, ATT_KV_HEADS, ATT_GROUP, HEAD_DIM)
    att_c = context_attention(q_c, k_c, v_c, sink)
    hy_c = hyena_mix(pc[..., o_hy:], hyena_filters(n_ctx, *filt), f_bias, conv_w, conv_b)
    return out, jnp.concatenate([hy_c, att_c], axis=-1).astype(hc.dtype)


def odd_mixer(h, hc, lb, w_in, conv_w, conv_b, dt_bias, a_log, d_skip, ssd_norm, hg_norm, ctx_out):
    f32 = jnp.float32
    bsz, length, _ = h.shape
    n_ctx = hc.shape[1]
    p = (h @ w_in).astype(f32)
    pc = (hc @ (w_in if ctx_out else w_in[:, :ODD_STATE_COLS])).astype(f32)
    o_dt = SSD_CONV_CH
    o_f = SSD_CONV_CH + 2 * SSD_HEADS
    o_i = o_f + 2 * HG_W
    o_z = ODD_STATE_COLS
    o_q = o_z + SSD_W
    o_g = o_q + HG_W
    gn = SSD_GROUPS * SSD_STATE

    def streams(pp):
        n = pp.shape[1]
        xbc = jax.nn.silu(dwconv_centred(pp[..., :SSD_CONV_CH], conv_w, conv_b))
        xs = xbc[..., :SSD_W].reshape(bsz, n, SSD_HEADS, SSD_HEAD_DIM)
        bm = xbc[..., SSD_W:SSD_W + gn].reshape(bsz, n, SSD_GROUPS, SSD_STATE)
        cm = xbc[..., SSD_W + gn:].reshape(bsz, n, SSD_GROUPS, SSD_STATE)
        dt_raw = pp[..., o_dt:o_f].reshape(bsz, n, 2, SSD_HEADS)
        f_raw = pp[..., o_f:o_i].reshape(bsz, n, 2, HG_HEADS, HG_EXPAND)
        iv = pp[..., o_i:o_i + HG_W].reshape(bsz, n, HG_HEADS, HG_VDIM)
        return xs, bm, cm, dt_raw, f_raw, iv

    xs, bm, cm, dt_raw, f_raw, iv = streams(p)
    xs_c, bm_c, cm_c, dt_raw_c, f_raw_c, iv_c = streams(pc)
    q = jax.nn.silu(p[..., o_q:o_g]).reshape(bsz, length, HG_HEADS, HG_EXPAND)
    q_c = jax.nn.silu(pc[..., o_q:o_g]).reshape(bsz, n_ctx, HG_HEADS, HG_EXPAND) if ctx_out else None
    lb = lb.astype(f32).reshape(HG_HEADS, HG_EXPAND)
    ssd0 = jnp.zeros((bsz, SSD_GROUPS, SSD_HEADS // SSD_GROUPS, SSD_HEAD_DIM, SSD_STATE), f32)
    hg0 = jnp.zeros((bsz, HG_HEADS, HG_EXPAND, HG_VDIM), f32)
    y_dirs, o_dirs, yc_dirs, oc_dirs = [], [], [], []
    for d in range(2):
        fl = (lambda a: jnp.flip(a, axis=1)) if d == 1 else (lambda a: a)
        a = -jnp.exp(a_log[d].astype(f32))
        dsk = d_skip[d].astype(f32)
        dtb = dt_bias[d].astype(f32)
        dt_l = jax.nn.softplus(dt_raw[:, :, d] + dtb)
        dt_c = jax.nn.softplus(dt_raw_c[:, :, d] + dtb)
        yc, s_ctx = ssd_scan(fl(xs_c), fl(dt_c), a, fl(bm_c), fl(cm_c), dsk, ssd0, ctx_out)
        yl, _ = ssd_scan(fl(xs), fl(dt_l), a, fl(bm), fl(cm), dsk, s_ctx, True)
        y_dirs.append(fl(yl))
        f_l = lb + (1.0 - lb) * jax.nn.sigmoid(f_raw[:, :, d])
        f_c = lb + (1.0 - lb) * jax.nn.sigmoid(f_raw_c[:, :, d])
        oc, s_hg = hgrn2_scan(fl(q_c) if ctx_out else None, fl(1.0 - f_c), fl(iv_c), fl(jnp.log(f_c)), hg0, ctx_out)
        ol, _ = hgrn2_scan(fl(q), fl(1.0 - f_l), fl(iv), fl(jnp.log(f_l)), s_hg, True)
        o_dirs.append(fl(ol))
        if ctx_out:
            yc_dirs.append(fl(yc))
            oc_dirs.append(fl(oc))

    def merge(yy, oo, pp, n):
        z = pp[..., o_z:o_q]
        g = pp[..., o_g:]
        ys = (yy.reshape(bsz, n, SSD_W) * jax.nn.silu(z)).reshape(bsz, n, SSD_GROUPS, SSD_W // SSD_GROUPS)
        ys = rms_norm(ys, ssd_norm.reshape(SSD_GROUPS, SSD_W // SSD_GROUPS)).reshape(bsz, n, SSD_W)
        hs = rms_norm(oo, hg_norm.reshape(HG_HEADS, HG_VDIM)).reshape(bsz, n, HG_W) * jax.nn.silu(g)
        return jnp.concatenate([ys, hs], axis=-1)

    out = merge(y_dirs[0] + y_dirs[1], o_dirs[0] + o_dirs[1], p, length).astype(h.dtype)
    if not ctx_out:
        return out, None
    out_c = merge(yc_dirs[0] + yc_dirs[1], oc_dirs[0] + oc_dirs[1], pc, n_ctx).astype(hc.dtype)
    return out, out_c


def moe_ffn(t, router_w, router_b, w_gu, b_gu, w_dn, b_dn):
    n_tok, d = t.shape
    logits = (t @ router_w).astype(jnp.float32) + router_b.astype(jnp.float32)
    top_val, top_idx = lax.top_k(logits, TOP_K)
    gates = jax.nn.softmax(top_val, axis=-1)
    flat_e = top_idx.reshape(-1)
    order = jnp.argsort(flat_e)
    e_sorted = flat_e[order]
    counts = jnp.bincount(flat_e, length=N_EXPERTS)
    padded = (counts + MOE_BLOCK - 1) // MOE_BLOCK * MOE_BLOCK
    pad_end = jnp.cumsum(padded)
    first = jnp.cumsum(counts) - counts
    dest = pad_end[e_sorted] - padded[e_sorted] + jnp.arange(n_tok * TOP_K) - first[e_sorted]
    n_blk = -(-(n_tok * TOP_K) // MOE_BLOCK) + N_EXPERTS
    n_rows = n_blk * MOE_BLOCK
    row_tok = jnp.full((n_rows,), n_tok, jnp.int32).at[dest].set((order // TOP_K).astype(jnp.int32))
    row_gate = jnp.zeros((n_rows,), jnp.float32).at[dest].set(gates.reshape(-1)[order])
    blk_e = jnp.minimum(jnp.searchsorted(pad_end, jnp.arange(n_blk) * MOE_BLOCK, side='right'), N_EXPERTS - 1)
    x_rows = jnp.concatenate([t, jnp.zeros((1, d), t.dtype)], axis=0)[row_tok].reshape(n_blk, MOE_BLOCK, d)

    def expert_block(args):
        xb, e = args
        gu = xb @ w_gu[e] + b_gu[e]
        gate = jnp.minimum(gu[:, :D_EXPERT], SWIGLU_LIMIT)
        up = jnp.clip(gu[:, D_EXPERT:], -SWIGLU_LIMIT, SWIGLU_LIMIT)
        act = (up + 1.0) * gate * jax.nn.sigmoid(SWIGLU_ALPHA * gate)
        return act @ w_dn[e] + b_dn[e]

    y_rows = lax.map(expert_block, (x_rows, blk_e)).reshape(n_rows, d)
    y = jax.ops.segment_sum(y_rows * row_gate[:, None].astype(y_rows.dtype), row_tok, num_segments=n_tok + 1)
    return y[:n_tok]


def setup_inputs(seed: int = 0) -> dict:
    key = jax.random.key(seed)
    keys = iter(jax.random.split(key, 48))
    f32 = jnp.float32

    def nrm(shape, scale):
        return jax.random.normal(next(keys), shape, f32) * scale

    def gain(shape):
        return 1.0 + nrm(shape, 0.05)

    dt0 = jnp.exp(jax.random.uniform(next(keys), (N_ODD, 2, SSD_HEADS), f32, math.log(1e-3), math.log(1e-1)))
    a0 = jax.random.uniform(next(keys), (N_ODD, 2, SSD_HEADS), f32, 1.0, 16.0)
    return {
        'x': nrm((BATCH, SEQ, D_MODEL), 1.0),
        'c': nrm((BATCH, D_MODEL), 1.0),
        'ctx': nrm((BATCH, CTX_LEN, D_MODEL), 1.0),
        'c_ctx': nrm((D_MODEL,), 1.0),
        'norm_g': gain((DEPTH, 2, D_MODEL)),
        'ada_w': nrm((DEPTH, D_MODEL, 6 * D_MODEL), 0.5 * D_MODEL ** -0.5),
        'ada_b': nrm((DEPTH, 6 * D_MODEL), 0.02),
        'w_out': nrm((DEPTH, MIX_W, D_MODEL), MIX_W ** -0.5),
        'w_in_even': nrm((N_EVEN, D_MODEL, EVEN_IN), D_MODEL ** -0.5),
        'hy_conv_w': nrm((N_EVEN, CONV_W, 3 * HY_W), CONV_W ** -0.5),
        'hy_conv_b': nrm((N_EVEN, 3 * HY_W), 0.02),
        'hy_w1': nrm((N_EVEN, HY_EMB, HY_FFN), HY_EMB ** -0.5),
        'hy_b1': nrm((N_EVEN, HY_FFN), 0.1),
        'hy_w2': nrm((N_EVEN, HY_FFN, HY_FFN), HY_FFN ** -0.5),
        'hy_b2': nrm((N_EVEN, HY_FFN), 0.1),
        'hy_w3': nrm((N_EVEN, HY_FFN, HY_FFN), HY_FFN ** -0.5),
        'hy_b3': nrm((N_EVEN, HY_FFN), 0.1),
        'hy_w4': nrm((N_EVEN, HY_FFN, HY_ORDER * 2 * HY_W), HY_FFN ** -0.5),
        'hy_freq': gain((N_EVEN, HY_FFN)),
        'hy_filter_bias': nrm((N_EVEN, HY_ORDER, HY_W), 0.5),
        'att_q_norm': gain((N_EVEN, HEAD_DIM)),
        'att_k_norm': gain((N_EVEN, HEAD_DIM)),
        'att_sink': nrm((N_EVEN, ATT_HEADS), 0.5),
        'w_in_odd': nrm((N_ODD, D_MODEL, ODD_IN), D_MODEL ** -0.5),
        'ssd_conv_w': nrm((N_ODD, CONV_W, SSD_CONV_CH), CONV_W ** -0.5),
        'ssd_conv_b': nrm((N_ODD, SSD_CONV_CH), 0.02),
        'ssd_dt_bias': dt0 + jnp.log(-jnp.expm1(-dt0)),
        'ssd_A_log': jnp.log(a0),
        'ssd_D': 1.0 + nrm((N_ODD, 2, SSD_HEADS), 0.1),
        'ssd_norm': gain((N_ODD, SSD_W)),
        'hg_lower_bounds': nrm((DEPTH, HG_W), 0.5),
        'hg_norm': gain((N_ODD, HG_W)),
        'router_w': nrm((DEPTH, D_MODEL, N_EXPERTS), D_MODEL ** -0.5),
        'router_b': nrm((DEPTH, N_EXPERTS), 0.01),
        'moe_w_gu': nrm((DEPTH, N_EXPERTS, D_MODEL, 2 * D_EXPERT), D_MODEL ** -0.5),
        'moe_b_gu': nrm((DEPTH, N_EXPERTS, 2 * D_EXPERT), 0.02),
        'moe_w_dn': nrm((DEPTH, N_EXPERTS, D_EXPERT, D_MODEL), D_EXPERT ** -0.5),
        'moe_b_dn': nrm((DEPTH, N_EXPERTS, D_MODEL), 0.02),
    }


def reference(x, c, ctx, c_ctx, norm_g, ada_w, ada_b, w_out, w_in_even, hy_conv_w, hy_conv_b,
              hy_w1, hy_b1, hy_w2, hy_b2, hy_w3, hy_b3, hy_w4, hy_freq, hy_filter_bias,
              att_q_norm, att_k_norm, att_sink, w_in_odd, ssd_conv_w, ssd_conv_b, ssd_dt_bias,
              ssd_A_log, ssd_D, ssd_norm, hg_lower_bounds, hg_norm, router_w, router_b,
              moe_w_gu, moe_b_gu, moe_w_dn, moe_b_dn):
    lbs = jax.nn.softmax(hg_lower_bounds.astype(jnp.float32), axis=0)
    lbs = jnp.cumsum(lbs, axis=0) - lbs[0]
    xc = ctx
    for layer in range(DEPTH):
        ctx_out = layer < DEPTH - 1
        i = layer // 2
        sh, sc, gt = adaln(c, ada_w[layer], ada_b[layer], 0)
        sh_c, sc_c, gt_c = adaln(c_ctx, ada_w[layer], ada_b[layer], 0)
        h = modulate(rms_norm(x, norm_g[layer, 0]), sh[:, None], sc[:, None])
        hc = modulate(rms_norm(xc, norm_g[layer, 0]), sh_c, sc_c)
        if layer % 2 == 0:
            m, m_c = even_mixer(h, hc, w_in_even[i], hy_conv_w[i], hy_conv_b[i], hy_w1[i], hy_b1[i],
                                hy_w2[i], hy_b2[i], hy_w3[i], hy_b3[i], hy_w4[i], hy_freq[i],
                                hy_filter_bias[i], att_q_norm[i], att_k_norm[i], att_sink[i], ctx_out)
        else:
            m, m_c = odd_mixer(h, hc, lbs[layer], w_in_odd[i], ssd_conv_w[i], ssd_conv_b[i], ssd_dt_bias[i],
                               ssd_A_log[i], ssd_D[i], ssd_norm[i], hg_norm[i], ctx_out)
        x = x + gt[:, None] * (m @ w_out[layer])
        if ctx_out:
            xc = xc + gt_c * (m_c @ w_out[layer])
        sh, sc, gt = adaln(c, ada_w[layer], ada_b[layer], 1)
        h = modulate(rms_norm(x, norm_g[layer, 1]), sh[:, None], sc[:, None])
        moe_params = (router_w[layer], router_b[layer], moe_w_gu[layer], moe_b_gu[layer],
                      moe_w_dn[layer], moe_b_dn[layer])
        n_lat = x.shape[0] * x.shape[1]
        if ctx_out:
            sh_c, sc_c, gt_c = adaln(c_ctx, ada_w[layer], ada_b[layer], 1)
            hc = modulate(rms_norm(xc, norm_g[layer, 1]), sh_c, sc_c)
            y = moe_ffn(jnp.concatenate([h.reshape(n_lat, D_MODEL), hc.reshape(-1, D_MODEL)], axis=0), *moe_params)
            x = x + gt[:, None] * y[:n_lat].reshape(x.shape)
            xc = xc + gt_c * y[n_lat:].reshape(xc.shape)
        else:
            x = x + gt[:, None] * moe_ffn(h.reshape(n_lat, D_MODEL), *moe_params).reshape(x.shape)
    return x
```

```python
import contextlib
import numpy as np
import concourse.bass as bass
import concourse.mybir as mybir
from concourse.bass_utils import run_bass_kernel_spmd

F32 = mybir.dt.float32
BF16 = mybir.dt.bfloat16
I32 = mybir.dt.int32
AF = mybir.ActivationFunctionType
ALU = mybir.AluOpType
AX = mybir.AxisListType

PE, DVE, ACT, POOL, SP = "tensor", "vector", "scalar", "gpsimd", "sync"
COMPUTE = (PE, DVE, ACT, POOL)
NDMASEM = 8
EPOCH_LEN = 20000


class Prog:
    def __init__(self):
        self.nc = bass.Bass("TRN2", target_bir_lowering=False)
        self.stack = contextlib.ExitStack()
        self.streams = {e: [] for e in (PE, DVE, ACT, POOL, SP)}
        self.cnt = {e: 0 for e in COMPUTE}
        self.dcnt = {e: 0 for e in (SP, ACT, POOL)}
        self.sem = {}
        self.dsem = {}
        self.waited = {}
        self.lastw = {}
        self.reads = {}
        self.ntens = 0
        self.out_tokens = []
        self.epoch = {e: 0 for e in COMPUTE}
        self.root_stack = self.stack
        for e in COMPUTE:
            self.sem[(e, 0)] = self.stack.enter_context(self.nc.semaphore("s_" + e + "_0"))
        for q in (SP, ACT, POOL):
            self.dsem[q] = [self.stack.enter_context(self.nc.semaphore(f"d_{q}_{i}")) for i in range(NDMASEM)]

    @contextlib.contextmanager
    def scope(self):
        outer = self.stack
        self.stack = contextlib.ExitStack()
        try:
            yield
        finally:
            self.barrier()
            self.stack.close()
            self.stack = outer

    def barrier(self):
        toks = [("c", e, (self.epoch[e], self.cnt[e])) for e in COMPUTE if self.cnt[e] > 0]
        for q in (SP, ACT, POOL):
            for k in range(max(0, self.dcnt[q] - NDMASEM), self.dcnt[q]):
                toks.append(("d", q, k))
        for st in (PE, DVE, ACT, POOL, SP):
            self._emit_waits(st, [t for t in toks if not (t[0] == "c" and t[1] == st)])

    def dram(self, name, shape, dtype, kind):
        return self.nc.dram_tensor(name, list(shape), dtype, kind=kind).ap()

    def sb(self, shape, dtype, name=None):
        self.ntens += 1
        name = "sb_" + (name or f"t{self.ntens}")
        return self.stack.enter_context(self.nc.sbuf_tensor(name, list(shape), dtype))

    def ps(self, shape, dtype=F32, name=None):
        self.ntens += 1
        name = "ps_" + (name or f"p{self.ntens}")
        return self.stack.enter_context(self.nc.psum_tensor(name, list(shape), dtype))

    def _key(self, t):
        if isinstance(t, str):
            return t
        if isinstance(t, tuple):
            return t
        th = getattr(t, "tensor", t)
        return getattr(th, "name", None) or id(th)

    def _tok_sem_val(self, tok):
        kind, e, i = tok
        if kind == "c":
            ep, idx = i
            return self.sem[(e, ep)], idx, ("c", e, ep)
        return self.dsem[e][i % NDMASEM], 16 * (i // NDMASEM + 1), ("d", e, i % NDMASEM)

    def _emit_waits(self, stream, toks):
        need = {}
        for tok in toks:
            if tok is None:
                continue
            s, v, k = self._tok_sem_val(tok)
            if tok[0] == "c" and tok[1] == stream and stream == PE:
                continue
            if k not in need or need[k][1] < v:
                need[k] = (s, v)
        for k, (s, v) in need.items():
            if self.waited.get((stream, k), 0) >= v:
                continue
            self.waited[(stream, k)] = v
            self.streams[stream].append(("wait", s, v))

    def _deps(self, reads, writes):
        toks = []
        for t in reads:
            k = self._key(t)
            toks.append(self.lastw.get(k))
        for t in writes:
            k = self._key(t)
            toks.append(self.lastw.get(k))
            toks.extend(self.reads.get(k, []))
        return toks

    def _commit(self, tok, reads, writes):
        for t in reads:
            k = self._key(t)
            self.reads.setdefault(k, []).append(tok)
            if len(self.reads[k]) > 24:
                best = {}
                for tk in self.reads[k]:
                    kk = (tk[0], tk[1], tk[2][0]) if tk[0] == "c" else tk
                    if kk not in best or best[kk][2] < tk[2]:
                        best[kk] = tk
                self.reads[k] = list(best.values())
        for t in writes:
            k = self._key(t)
            self.lastw[k] = tok
            self.reads[k] = []

    def op(self, eng, fn, reads=(), writes=()):
        self._emit_waits(eng, self._deps(reads, writes))
        if self.cnt[eng] >= EPOCH_LEN:
            self.epoch[eng] += 1
            self.cnt[eng] = 0
            self.sem[(eng, self.epoch[eng])] = self.root_stack.enter_context(self.nc.semaphore(f"s_{eng}_{self.epoch[eng]}"))
        self.cnt[eng] += 1
        tok = ("c", eng, (self.epoch[eng], self.cnt[eng]))
        self.streams[eng].append(("op", fn, self.sem[(eng, self.epoch[eng])], 1))
        self._commit(tok, reads, writes)
        return tok

    def I(self, eng, mname, reads, writes, *args, **kw):
        return self.op(eng, lambda e: getattr(e, mname)(*args, **kw), reads=reads, writes=writes)

    def dma(self, q, out, in_, reads=(), writes=(), **kw):
        k = self.dcnt[q]
        toks = self._deps(reads, writes)
        if k >= NDMASEM:
            toks.append(("d", q, k - NDMASEM))
        self._emit_waits(q, toks)
        self.dcnt[q] += 1
        tok = ("d", q, k)
        self.streams[q].append(("op", lambda e: e.dma_start(out=out, in_=in_, **kw), self.dsem[q][k % NDMASEM], 16))
        self._commit(tok, reads, writes)
        return tok

    def finish(self, final_toks):
        self._emit_waits(SP, final_toks)
        nc = self.nc
        streams = self.streams

        def run(engine, lst):
            for it in lst:
                if it[0] == "wait":
                    engine.wait_ge(it[1], it[2])
                else:
                    it[1](engine).then_inc(it[2], it[3])

        with nc.Block() as block:
            @block.sync
            def _(e):
                run(e, streams[SP])

            @block.tensor
            def _(e):
                run(e, streams[PE])

            @block.vector
            def _(e):
                run(e, streams[DVE])

            @block.scalar
            def _(e):
                run(e, streams[ACT])

            @block.gpsimd
            def _(e):
                run(e, streams[POOL])
        self.stack.close()
        return nc


D = 1024
SEQ = 16384
NCORE = 8
TLOC = SEQ // NCORE
HALO = 128
TEXT = TLOC + 2 * HALO
NCTX = 256
EPS = 1e-6
MASKNEG = -30000.0


def _bcast_free(ap2d, n):
    return ap2d.unsqueeze(2).broadcast_to([ap2d.shape[0], ap2d.shape[1], n])


class Blob:
    def __init__(self, items):
        self.items = {}
        o = 0
        for name, rows, cols in items:
            self.items[name] = (rows, o, cols); o += cols
        self.total = o
        self.t = None

    def dram(self, P):
        self.t = P.dram("blob", [128, self.total], F32, "ExternalInput")
        return self

    def ap(self, name):
        rows, o, cols = self.items[name]
        return self.t[0:rows, o:o + cols]

    def pack(self, d):
        out = np.zeros((128, self.total), np.float32)
        for name, (rows, o, cols) in self.items.items():
            out[0:rows, o:o + cols] = np.asarray(d[name], np.float32).reshape(rows, cols)
        return out


BLOB_A = [("cvec", 128, 16), ("adab", 128, 24), ("normg", 128, 8), ("gains", 128, 640), ("masks", 128, 512), ("sinkrow", 128, 1024),
          ("ident", 128, 128), ("onesd", 128, 128), ("convw", 128, 36), ("convb", 128, 12), ("edge", 128, 2)]
BLOB_C = [("cvec", 128, 16), ("adab", 128, 32), ("normg", 128, 8), ("rw", 128, 256), ("rb", 128, 32), ("bgu", 128, 512),
          ("ident", 128, 128), ("onesd", 128, 128)]
BLOB_B = [("w1", 33, 64), ("w2", 64, 64), ("w3", 64, 64), ("w4s", 64, 256), ("fqb", 64, 4), ("negd", 128, 1), ("fbias", 128, 128),
          ("ident", 128, 128), ("onesd", 128, 128), ("jmat", 128, 128)]
BLOB_D = [("cvec", 128, 16), ("adab", 128, 24), ("normg", 128, 8), ("onesd", 128, 128), ("convw", 128, 24), ("convb", 128, 8),
          ("edge", 128, 2), ("hglb", 128, 8), ("dtb", 16, 1)]
BLOB_E = [("ssdp", 128, 4), ("U", 128, 128), ("U4", 128, 128), ("Mneg", 128, 128), ("ident", 128, 128), ("onesd", 128, 128)]
BLOB_M = [("nrm", 128, 8), ("onesd", 128, 128)]


def blobify(maps, spec):
    bl = Blob(spec)
    out = []
    for m in maps:
        m2 = {k: v for k, v in m.items() if k not in bl.items}
        m2['blob'] = bl.pack(m)
        out.append(m2)
    return out


class Banks:
    def __init__(self, P, nb16=1):
        self.f = [P.ps([128, 512], F32, name=f"bank{i}") for i in range(8 - nb16)]
        self.h = [P.ps([128, 1024], BF16, name=f"bankh{i}") for i in range(nb16)]


def emit_adaln(P, B, cv_sb, adaw_dram, adab_sb, wbuf, out_sb, ncol, nparts=3):
    sc = P.sb([128, 8, ncol], F32, name="ada_silu")
    P.op(ACT, lambda e: e.activation(out=sc[:], in_=cv_sb[:], func=AF.Silu), reads=[cv_sb], writes=[sc])
    ps = B.f[0]
    for part in range(nparts):
        for kc in range(8):
            P.dma(SP if kc % 2 == 0 else ACT, wbuf[:, kc, :], adaw_dram[kc * 128:(kc + 1) * 128, part * 1024:(part + 1) * 1024],
                  writes=[wbuf])
        for o in range(8):
            oc = part * 8 + o
            for kc in range(8):
                P.op(PE, lambda e, o=o, kc=kc, oc=oc: e.matmul(ps[:, oc * ncol:(oc + 1) * ncol], wbuf[:, kc, o * 128:(o + 1) * 128],
                                                               sc[:, kc, :], start=(kc == 0), stop=(kc == 7)),
                     reads=[wbuf, sc], writes=[ps])
    P.op(DVE, lambda e: e.tensor_tensor(out=out_sb[:], in0=ps[:, 0:8 * nparts * ncol].rearrange("p (o n) -> p o n", n=ncol),
                                        in1=_bcast_free(adab_sb[:], ncol), op=ALU.add),
         reads=[ps, adab_sb], writes=[out_sb])


def emit_norm_mod(P, B, x_sb, ones_sb, A_col, B_col, h_out, ntok, tmp_sq, tmp_rs, hcol0=0):
    ps = B.f[1]
    for kc in range(8):
        P.op(ACT, lambda e, kc=kc: e.activation(out=tmp_sq[:, kc, 0:ntok], in_=x_sb[:, kc, 0:ntok], func=AF.Square),
             reads=[x_sb], writes=[tmp_sq])
    for kc in range(8):
        P.op(PE, lambda e, kc=kc: e.matmul(ps[:, 0:ntok], ones_sb[:], tmp_sq[:, kc, 0:ntok], start=(kc == 0), stop=(kc == 7)),
             reads=[ones_sb, tmp_sq], writes=[ps])
    P.op(DVE, lambda e: e.tensor_scalar(out=tmp_rs[:, 0:ntok], in0=ps[:, 0:ntok], scalar1=1.0 / D, scalar2=EPS,
                                        op0=ALU.mult, op1=ALU.add), reads=[ps], writes=[tmp_rs])
    P.op(ACT, lambda e: e.activation(out=tmp_rs[:, 0:ntok], in_=tmp_rs[:, 0:ntok], func=AF.Ln), reads=[tmp_rs], writes=[tmp_rs])
    P.op(ACT, lambda e: e.activation(out=tmp_rs[:, 0:ntok], in_=tmp_rs[:, 0:ntok], func=AF.Exp, scale=-0.5), reads=[tmp_rs], writes=[tmp_rs])
    for kc in range(8):
        P.op(DVE, lambda e, kc=kc: e.tensor_tensor(out=tmp_sq[:, kc, 0:ntok], in0=x_sb[:, kc, 0:ntok], in1=tmp_rs[:, 0:ntok],
                                                   op=ALU.mult), reads=[x_sb, tmp_rs, tmp_sq], writes=[tmp_sq])
        P.op(ACT, lambda e, kc=kc: e.activation(out=h_out[:, kc, hcol0:hcol0 + ntok], in_=tmp_sq[:, kc, 0:ntok], func=AF.Identity,
                                                bias=B_col[:, kc:kc + 1], scale=A_col[:, kc:kc + 1]),
             reads=[tmp_sq, A_col, B_col], writes=[h_out])


TT = 384
NTT = TEXT // TT


def build_A():
    P = Prog()
    Bk = Banks(P)
    di = lambda n, s, dt=F32: P.dram(n, s, dt, "ExternalInput")
    do = lambda n, s, dt=F32: P.dram(n, s, dt, "ExternalOutput")
    xT = di("xT", [D, TEXT]); ctxT = di("ctxT", [D, NCTX])
    bl = Blob(BLOB_A).dram(P)
    cvec = bl.ap("cvec"); adaw = di("adaw", [D, 3072]); adab = bl.ap("adab")
    normg = bl.ap("normg"); w_in = di("w_in", [D, 2304])
    gains = bl.ap("gains"); ctab = di("ctab", [TEXT, 64]); stab = di("stab", [TEXT, 64])
    masks = bl.ap("masks"); sinkrow = bl.ap("sinkrow")
    ident = bl.ap("ident"); onesd = bl.ap("onesd")
    convw = bl.ap("convw"); convb = bl.ap("convb"); edge = bl.ap("edge")
    uT = do("uT", [1536, TLOC]); ucT = do("ucT", [1536, NCTX])
    attT = do("attT", [512, TLOC]); attcT = do("attcT", [512, NCTX])

    ones_sb = P.sb([128, 128], F32, name="ones"); P.dma(SP, ones_sb[:], onesd[:, :], writes=[ones_sb])
    id_f = P.sb([128, 128], F32, name="idf"); P.dma(SP, id_f[:], ident[:, :], writes=[id_f])
    id_b = P.sb([128, 128], BF16, name="idb"); P.dma(POOL, id_b[:], ident[:, :], writes=[id_b])
    cv = P.sb([128, 8, 2], F32, name="cv"); P.dma(SP, cv[:], cvec.rearrange("p (k n) -> p k n", n=2), writes=[cv])
    adab_sb = P.sb([128, 24], F32, name="adab"); P.dma(SP, adab_sb[:], adab[:, :], writes=[adab_sb])
    g_sb = P.sb([128, 8], F32, name="normg"); P.dma(SP, g_sb[:], normg[:, :], writes=[g_sb])
    gains_sb = P.sb([128, 640], F32, name="gains"); P.dma(SP, gains_sb[:], gains[:, :], writes=[gains_sb])
    mask_sb = P.sb([128, 4, 128], BF16, name="masks")
    P.dma(POOL, mask_sb[:], masks.rearrange("k (m q) -> k m q", m=4), writes=[mask_sb])
    sink_sb = P.sb([128, 1024], F32, name="sink"); P.dma(SP, sink_sb[:], sinkrow[:, :], writes=[sink_sb])
    P.op(ACT, lambda e: e.activation(out=sink_sb[:], in_=sink_sb[:], func=AF.Exp), reads=[sink_sb], writes=[sink_sb])
    w_sb = P.sb([128, 8, 2304], BF16, name="w_in")
    for kc in range(8):
        P.dma(POOL, w_sb[:, kc, :], w_in[kc * 128:(kc + 1) * 128, :], writes=[w_sb])

    ada = P.sb([128, 24, 2], F32, name="ada")
    with P.scope():
        wbuf = P.sb([128, 8, 1024], F32, name="adawbuf")
        emit_adaln(P, Bk, cv, adaw, adab_sb, wbuf, ada, 2)
    Acol = [P.sb([128, 8], F32, name=f"Acol{j}") for j in range(2)]
    Bcol = [P.sb([128, 8], F32, name=f"Bcol{j}") for j in range(2)]
    for j in range(2):
        P.op(DVE, lambda e, j=j: e.scalar_tensor_tensor(out=Acol[j][:], in0=ada[:, 8:16, j], scalar=1.0, in1=g_sb[:],
                                                         op0=ALU.add, op1=ALU.mult), reads=[ada, g_sb], writes=[Acol[j]])
        P.op(DVE, lambda e, j=j: e.tensor_copy(out=Bcol[j][:], in_=ada[:, 0:8, j]), reads=[ada], writes=[Bcol[j]])

    kqT = P.sb([64, 10, TEXT + NCTX], BF16, name="kqT")
    vaug = P.sb([128, (TEXT + NCTX) // 128, 2, 65], BF16, name="vaug")
    P.op(POOL, lambda e: e.memset(vaug[:], 1.0), writes=[vaug])

    x_sb = P.sb([128, 8, TT], F32, name="x_sb")
    sq_sb = P.sb([128, 8, TT], F32, name="sq_sb")
    rs_sb = P.sb([128, TT], F32, name="rs_sb")
    h_all = P.sb([128, 8, TEXT + NCTX], BF16, name="h_all")
    cw_sb = P.sb([128, 12, 3], F32, name="cw"); P.dma(SP, cw_sb[:], convw.rearrange("p (o t) -> p o t", t=3), writes=[cw_sb])
    cb_sb = P.sb([128, 12], F32, name="cb"); P.dma(SP, cb_sb[:], convb[:, :], writes=[cb_sb])
    edge_sb = P.sb([128, 2], F32, name="edge"); P.dma(SP, edge_sb[:], edge[:, :], writes=[edge_sb])
    kqv = P.sb([128, 768], F32, name="kqv")
    sq2 = P.sb([128, 640], F32, name="sq2")
    ss = P.sb([128, 10], F32, name="ss")
    tmpr = P.sb([128, 640], F32, name="tmpr")
    kqb = P.sb([128, 640], BF16, name="kqb")
    ct_sb = P.sb([128, 64], F32, name="ct"); st_sb = P.sb([128, 64], F32, name="st")
    u_sb = [P.sb([128, 512], F32, name=f"u_sb{i}") for i in range(2)]
    acc_sb = [P.sb([128, 512], F32, name=f"acc_sb{i}") for i in range(2)]

    def proj_tile(src_dram, col0, ntok, j, tok_base, is_ctx):
        for kc in range(8):
            P.dma(SP if kc % 2 == 0 else ACT, x_sb[:, kc, 0:ntok], src_dram[kc * 128:(kc + 1) * 128, col0:col0 + ntok], writes=[x_sb])
        emit_norm_mod(P, Bk, x_sb, ones_sb, Acol[j], Bcol[j], h_all, ntok, sq_sb, rs_sb, hcol0=tok_base)
        for s in range(ntok // 128):
            pa, pb = Bk.f[2], Bk.f[3]
            for kc in range(8):
                P.op(PE, lambda e, kc=kc, s=s: e.matmul(pa[:, 0:512], h_all[:, kc, tok_base + s * 128:tok_base + (s + 1) * 128], w_sb[:, kc, 0:512],
                                                        start=(kc == 0), stop=(kc == 7)), reads=[h_all, w_sb], writes=[pa])
            for kc in range(8):
                P.op(PE, lambda e, kc=kc, s=s: e.matmul(pb[:, 0:256], h_all[:, kc, tok_base + s * 128:tok_base + (s + 1) * 128], w_sb[:, kc, 512:768],
                                                        start=(kc == 0), stop=(kc == 7)), reads=[h_all, w_sb], writes=[pb])
            P.op(ACT, lambda e: e.activation(out=kqv[:, 0:512], in_=pa[:, 0:512], func=AF.Copy), reads=[pa], writes=[kqv])
            P.op(ACT, lambda e: e.activation(out=kqv[:, 512:768], in_=pb[:, 0:256], func=AF.Copy), reads=[pb], writes=[kqv])
            tile_idx = (tok_base + s * 128) // 128
            P.op(POOL, lambda e, ti=tile_idx: e.tensor_copy(out=vaug[:, ti, :, 0:64], in_=kqv[:, 640:768].rearrange("p (g d) -> p g d", d=64)),
                 reads=[kqv], writes=[vaug])
            P.op(DVE, lambda e: e.tensor_tensor(out=sq2[:], in0=kqv[:, 0:640], in1=kqv[:, 0:640], op=ALU.mult), reads=[kqv], writes=[sq2])
            P.op(DVE, lambda e: e.tensor_reduce(out=ss[:], in_=sq2[:].rearrange("p (h d) -> p h d", d=64), axis=AX.X, op=ALU.add),
                 reads=[sq2], writes=[ss])
            P.op(DVE, lambda e: e.tensor_scalar(out=ss[:], in0=ss[:], scalar1=1.0 / 64, scalar2=EPS, op0=ALU.mult, op1=ALU.add),
                 reads=[ss], writes=[ss])
            P.op(ACT, lambda e: e.activation(out=ss[:], in_=ss[:], func=AF.Ln), reads=[ss], writes=[ss])
            P.op(ACT, lambda e: e.activation(out=ss[:], in_=ss[:], func=AF.Exp, scale=-0.5), reads=[ss], writes=[ss])
            P.op(DVE, lambda e: e.tensor_tensor(out=sq2[:].rearrange("p (h d) -> p h d", d=64), in0=kqv[:, 0:640].rearrange("p (h d) -> p h d", d=64),
                                                in1=_bcast_free(ss[:], 64), op=ALU.mult), reads=[kqv, ss], writes=[sq2])
            P.op(DVE, lambda e: e.tensor_tensor(out=sq2[:], in0=sq2[:], in1=gains_sb[:], op=ALU.mult), reads=[sq2, gains_sb], writes=[sq2])
            if not is_ctx:
                r0 = col0 + s * 128
                P.dma(SP, ct_sb[:], ctab[r0:r0 + 128, :], writes=[ct_sb])
                P.dma(ACT, st_sb[:], stab[r0:r0 + 128, :], writes=[st_sb])
                v5 = lambda t: t[:].rearrange("p (h b two s) -> p (h b) two s", b=2, two=2, s=16)
                stv = st_sb[:].rearrange("p (b two s) -> p b two s", two=2, s=16)
                ctv = ct_sb[:].rearrange("p (b two s) -> p b two s", two=2, s=16)

                def bc(tv, two):
                    a = tv[:, :, two, :]
                    return a.unsqueeze(1).broadcast_to([128, 10, 2, 16])
                u4 = sq2[:].rearrange("p (h b two s) -> p h b two s", b=2, two=2, s=16)
                t4 = tmpr[:].rearrange("p (h b two s) -> p h b two s", b=2, two=2, s=16)
                for two in range(2):
                    P.op(DVE, lambda e, two=two: e.tensor_tensor(out=t4[:, :, :, two, :], in0=u4[:, :, :, 1 - two, :], in1=bc(stv, two), op=ALU.mult),
                         reads=[sq2, st_sb], writes=[tmpr])
                for two in range(2):
                    P.op(DVE, lambda e, two=two: e.tensor_tensor(out=u4[:, :, :, two, :], in0=u4[:, :, :, two, :], in1=bc(ctv, two), op=ALU.mult),
                         reads=[sq2, ct_sb], writes=[sq2])
                P.op(DVE, lambda e: e.tensor_tensor(out=kqb[:], in0=sq2[:], in1=tmpr[:], op=ALU.add), reads=[sq2, tmpr], writes=[kqb])
            else:
                P.op(DVE, lambda e: e.tensor_copy(out=kqb[:], in_=sq2[:]), reads=[sq2], writes=[kqb])
            pt = Bk.h[0]
            for hh in range(10):
                P.op(PE, lambda e, hh=hh: e.transpose(pt[0:64, hh * 128:(hh + 1) * 128][:, 0:128] if False else pt[0:64, hh * 128 % 1024:(hh * 128 % 1024) + 128],
                                                      kqb[:, hh * 64:(hh + 1) * 64], id_b[:]),
                     reads=[kqb, id_b], writes=[pt])
                if hh == 7 or hh == 9:
                    h0 = 0 if hh == 7 else 8
                    nh = hh - h0 + 1
                    t0 = tok_base + s * 128
                    P.op(ACT, lambda e, h0=h0, nh=nh, t0=t0: e.activation(
                        out=kqT[:, h0:h0 + nh, t0:t0 + 128],
                        in_=pt[0:64, (h0 * 128) % 1024:(h0 * 128) % 1024 + nh * 128].rearrange("p (h t) -> p h t", t=128), func=AF.Copy),
                        reads=[pt], writes=[kqT])

    def hyena_tile(h0, n, out_dram, ocol0, zl, zr, el, er):
        a0 = h0 - (0 if zl else 1); a1 = h0 + n + (0 if zr else 1)
        w = a1 - a0
        off = 1 if zl else 0
        for oc in range(12):
            pu = Bk.f[4 + oc % 2]
            ub = u_sb[oc % 2]; ac = acc_sb[oc % 2]
            for kc in range(8):
                P.I(PE, "matmul", [w_sb, h_all], [pu], pu[:, off:off + w], w_sb[:, kc, 768 + oc * 128:768 + (oc + 1) * 128], h_all[:, kc, a0:a1],
                    start=(kc == 0), stop=(kc == 7))
            if zl:
                P.I(POOL, "memset", [], [ub], ub[:, 0:1], 0.0)
            if zr:
                P.I(POOL, "memset", [], [ub], ub[:, n + 1:n + 2], 0.0)
            P.I(ACT, "activation", [pu], [ub], out=ub[:, off:off + w], in_=pu[:, off:off + w], func=AF.Copy)
            if el:
                P.I(DVE, "tensor_scalar", [ub, edge_sb], [ub], out=ub[:, 0:1], in0=ub[:, 0:1], scalar1=edge_sb[:, 0:1], scalar2=None, op0=ALU.mult)
            if er:
                P.I(DVE, "tensor_scalar", [ub, edge_sb], [ub], out=ub[:, n + 1:n + 2], in0=ub[:, n + 1:n + 2], scalar1=edge_sb[:, 1:2], scalar2=None, op0=ALU.mult)
            P.I(DVE, "tensor_scalar", [ub, cw_sb, cb_sb], [ac], out=ac[:, 0:n], in0=ub[:, 1:n + 1], scalar1=cw_sb[:, oc, 1:2], scalar2=cb_sb[:, oc:oc + 1], op0=ALU.mult, op1=ALU.add)
            P.I(DVE, "scalar_tensor_tensor", [ub, cw_sb, ac], [ac], out=ac[:, 0:n], in0=ub[:, 0:n], scalar=cw_sb[:, oc, 0:1], in1=ac[:, 0:n], op0=ALU.mult, op1=ALU.add)
            P.I(DVE, "scalar_tensor_tensor", [ub, cw_sb, ac], [ac], out=ac[:, 0:n], in0=ub[:, 2:n + 2], scalar=cw_sb[:, oc, 2:3], in1=ac[:, 0:n], op0=ALU.mult, op1=ALU.add)
            outs.append(P.dma(POOL, out_dram[oc * 128:(oc + 1) * 128, ocol0:ocol0 + n], ac[:, 0:n], reads=[ac]))

    outs = []
    for t in range(NTT):
        proj_tile(xT, t * TT, TT, 0, t * TT, False)
    proj_tile(ctxT, 0, NCTX, 1, TEXT, True)
    lo = 0
    while lo < TLOC:
        n = min(510, TLOC - lo)
        hyena_tile(HALO + lo, n, uT, lo, False, False, lo == 0, lo + n == TLOC)
        lo += n
    hyena_tile(TEXT, NCTX, ucT, 0, True, True, False, False)

    NB = TLOC // 128
    ctx_tiles = [TEXT // 128, TEXT // 128 + 1]
    pT = [P.sb([128, 512], BF16, name=f"pT{i}") for i in range(2)]
    o_sb = P.sb([64, 512], F32, name="o_sb"); rden = P.sb([65, 512], F32, name="rden")
    of_sb = P.sb([64, 512], F32, name="of_sb")
    ones_b = P.sb([128, 64], F32, name="ones_b")
    P.op(POOL, lambda e: e.memset(ones_b[:], 1.0), writes=[ones_b])

    def attend(qtile, key_tiles, key_masks, out_dram, out_col0):
        for g in range(2):
            po = Bk.f[2]
            nkt = len(key_tiles)
            for ci, (kt, mk) in enumerate(zip(key_tiles, key_masks)):
                psc = Bk.f[ci % 2]
                pTb = pT[ci % 2]
                qv = kqT[:, 2 + 4 * g:2 + 4 * g + 4, qtile * 128:(qtile + 1) * 128]
                P.op(PE, lambda e, psc=psc, kt=kt, qv=qv, mk=mk, g=g: e.matmul(psc[:, 0:512], kqT[:, g, kt * 128:(kt + 1) * 128], qv,
                                                                         start=True, stop=(mk is None)), reads=[kqT], writes=[psc])
                if mk is not None:
                    for hh in range(4):
                        P.op(PE, lambda e, psc=psc, hh=hh, mk=mk: e.matmul(psc[:, hh * 128:(hh + 1) * 128], id_b[:], mask_sb[:, mk, :],
                                                                           start=False, stop=(hh == 3)), reads=[id_b, mask_sb], writes=[psc])
                P.op(ACT, lambda e, psc=psc, pTb=pTb: e.activation(out=pTb[:], in_=psc[:, 0:512], func=AF.Exp, scale=0.125),
                     reads=[psc], writes=[pTb])
                P.op(PE, lambda e, pTb=pTb, kt=kt, ci=ci, g=g: e.matmul(po[0:65, 0:512], vaug[:, kt, g, :], pTb[:], start=(ci == 0), stop=(ci == nkt - 1)),
                     reads=[vaug, pTb], writes=[po])
            P.op(DVE, lambda e, g=g: e.tensor_tensor(out=rden[64:65, :], in0=po[64:65, 0:512], in1=sink_sb[64:65, g * 512:(g + 1) * 512], op=ALU.add),
                 reads=[po, sink_sb], writes=[rden])
            P.op(DVE, lambda e: e.reciprocal(out=rden[64:65, :], in_=rden[64:65, :]), reads=[rden], writes=[rden])
            P.op(ACT, lambda e: e.activation(out=o_sb[:], in_=po[0:64, 0:512], func=AF.Copy), reads=[po], writes=[o_sb])
            pb = Bk.f[3]
            P.op(PE, lambda e: e.matmul(pb[0:64, 0:512], ones_b[64:65, :], rden[64:65, :], start=True, stop=True), reads=[ones_b, rden], writes=[pb])
            P.op(DVE, lambda e: e.tensor_tensor(out=of_sb[:], in0=o_sb[:], in1=pb[0:64, 0:512], op=ALU.mult), reads=[o_sb, pb], writes=[of_sb])
            for hh in range(4):
                r0 = (4 * g + hh) * 64
                outs.append(P.dma(POOL if hh % 2 == 0 else SP, out_dram[r0:r0 + 64, out_col0:out_col0 + 128], of_sb[:, hh * 128:(hh + 1) * 128], reads=[of_sb]))

    for b in range(NB):
        qt = b + 1
        mprev = 0 if b == 0 else 1
        mnext = 3 if b == NB - 1 else 2
        attend(qt, [qt - 1, qt, qt + 1] + ctx_tiles, [mprev, None, mnext, None, None], attT, b * 128)
    for cb in range(2):
        attend(ctx_tiles[cb], ctx_tiles, [None, None], attcT, cb * 128)
    return P.finish(outs)


def col_layout(v, nchunk):
    return np.ascontiguousarray(np.asarray(v, np.float32).reshape(nchunk, 128).T)


def rope_tables(tok_idx):
    inv = (10000.0 ** (-np.arange(16, dtype=np.float32) / 16)).astype(np.float32)
    t = np.asarray(tok_idx)
    row = (t // 64).astype(np.float32)[:, None] * inv
    col = (t % 64).astype(np.float32)[:, None] * inv
    cr, sr, cc, sc_ = np.cos(row), np.sin(row), np.cos(col), np.sin(col)
    C = np.concatenate([cr, cr, cc, cc], axis=1).astype(np.float32)
    S = np.concatenate([-sr, sr, -sc_, sc_], axis=1).astype(np.float32)
    return C, S


def band_masks(core):
    j = np.arange(128)[:, None]; i = np.arange(128)[None, :]
    prev = np.where(j >= i, 0.0, MASKNEG).astype(np.float32)
    nxt = np.where(j <= i, 0.0, MASKNEG).astype(np.float32)
    allneg = np.full((128, 128), MASKNEG, np.float32)
    return np.stack([allneg if core == 0 else prev, prev, nxt, allneg if core == NCORE - 1 else nxt])


def prep_A(inp, layer=0):
    x = inp['x'][0]; ctx = inp['ctx'][0]
    xT = np.ascontiguousarray(x.T)
    xTp = np.concatenate([np.zeros((D, HALO), np.float32), xT, np.zeros((D, HALO), np.float32)], axis=1)
    w = inp['w_in_even'][0]
    w_perm = np.ascontiguousarray(np.concatenate([w[:, 0:128], w[:, 256:768], w[:, 128:256], w[:, 768:]], axis=1))
    gains = np.concatenate([np.tile(inp['att_k_norm'][0], 2), np.tile(inp['att_q_norm'][0], 8)])
    gains = np.ascontiguousarray(np.broadcast_to(gains[None, :], (128, 640))).astype(np.float32)
    sink = inp['att_sink'][0]
    sinkrow = np.ascontiguousarray(np.broadcast_to(np.repeat(sink, 128)[None, :], (128, 1024))).astype(np.float32)
    cvec = np.stack([col_layout(inp['c'][0], 8), col_layout(inp['c_ctx'], 8)], axis=2).reshape(128, 16)
    common = dict(
        ctxT=np.ascontiguousarray(ctx.T), cvec=np.ascontiguousarray(cvec),
        adaw=np.ascontiguousarray(inp['ada_w'][layer][:, 0:3072]), adab=col_layout(inp['ada_b'][layer][0:3072], 24),
        normg=col_layout(inp['norm_g'][layer, 0], 8), w_in=w_perm, gains=gains, sinkrow=sinkrow,
        ident=np.eye(128, dtype=np.float32), onesd=np.ones((128, 128), np.float32),
        convw=np.ascontiguousarray(inp['hy_conv_w'][0].reshape(3, 12, 128).transpose(2, 1, 0).reshape(128, 36)),
        convb=col_layout(inp['hy_conv_b'][0], 12))
    maps = []
    for c in range(NCORE):
        s0 = c * TLOC
        C, S = rope_tables(np.arange(s0 - HALO, s0 + TLOC + HALO).clip(0, SEQ - 1))
        m = dict(common)
        edge = np.ones((128, 2), np.float32); edge[:, 0] = 0.0 if c == 0 else 1.0; edge[:, 1] = 0.0 if c == NCORE - 1 else 1.0
        m.update(xT=np.ascontiguousarray(xTp[:, s0:s0 + TEXT]), ctab=C, stab=S, edge=edge,
                 masks=np.ascontiguousarray(band_masks(c).transpose(1, 0, 2).reshape(128, 512)))
        maps.append(m)
    return blobify(maps, BLOB_A)


NEXP = 32
NC_MOE = 1
NH_MOE = 16


def build_C(nlat, nctx, nhalf=2):
    TH = nlat + nctx
    tiles = []
    c0 = 0
    while c0 < nlat:
        n = min(512, nlat - c0); tiles.append((c0, n, 0)); c0 += n
    if nctx:
        tiles.append((nlat, nctx, 1))
    P = Prog()
    Bk = Banks(P)
    di = lambda n, s, dt=F32: P.dram(n, s, dt, "ExternalInput")
    mT = di("mT", [nhalf, D, TH]); xT = di("xT", [nhalf, D, TH])
    bl = Blob(BLOB_C).dram(P)
    cvec = bl.ap("cvec"); adaw = di("adaw", [D, 4096]); adab = bl.ap("adab")
    normg = bl.ap("normg"); w_out = di("w_out", [D, D])
    rw = bl.ap("rw"); rb = bl.ap("rb")
    w_gu = di("w_gu", [NEXP, D, 2048]); bgu = bl.ap("bgu")
    w_dn = di("w_dn", [NEXP, D, D]); bdn = di("bdn", [NEXP, D])
    ident = bl.ap("ident"); onesd = bl.ap("onesd")
    outT = P.dram("outT", [nhalf, D, TH], F32, "ExternalOutput")
    gt_dram = P.dram("gt_scratch", [nhalf, NEXP, TH], F32, "Internal")

    ones_sb = P.sb([128, 128], F32, name="ones"); P.dma(SP, ones_sb[:], onesd[:, :], writes=[ones_sb])
    id_f = P.sb([128, 128], F32, name="idf"); P.dma(SP, id_f[:], ident[:, :], writes=[id_f])
    cv = P.sb([128, 8, 2], F32, name="cv"); P.dma(SP, cv[:], cvec.rearrange("p (k n) -> p k n", n=2), writes=[cv])
    adab_sb = P.sb([128, 32], F32, name="adab"); P.dma(SP, adab_sb[:], adab[:, :], writes=[adab_sb])
    g_sb = P.sb([128, 8], F32, name="normg"); P.dma(SP, g_sb[:], normg[:, :], writes=[g_sb])
    rw_sb = P.sb([128, 8, NEXP], F32, name="rw"); P.dma(SP, rw_sb[:], rw.rearrange("p (k e) -> p k e", e=NEXP), writes=[rw_sb])
    rb_sb = P.sb([128, NEXP], F32, name="rb"); P.dma(SP, rb_sb[:], rb[:, :], writes=[rb_sb])
    bgu_sb = P.sb([128, NEXP, 16], F32, name="bgu"); P.dma(SP, bgu_sb[:], bgu.rearrange("p (e o) -> p e o", o=16), writes=[bgu_sb])
    bdn_sb = P.sb([NEXP, D], F32, name="bdn"); P.dma(SP, bdn_sb[:], bdn[:, :], writes=[bdn_sb])
    ada = P.sb([128, 32, 2], F32, name="ada")
    with P.scope():
        wbuf = P.sb([128, 8, 1024], F32, name="adawbuf")
        emit_adaln(P, Bk, cv, adaw, adab_sb, wbuf, ada, 2, nparts=4)
    Acol = [P.sb([128, 8], F32, name=f"Acol{j}") for j in range(2)]
    Bcol = [P.sb([128, 8], F32, name=f"Bcol{j}") for j in range(2)]
    G0 = [P.sb([128, 8], F32, name=f"G0{j}") for j in range(2)]
    G1 = [P.sb([128, 8], F32, name=f"G1{j}") for j in range(2)]
    for j in range(2):
        P.I(DVE, "scalar_tensor_tensor", [ada, g_sb], [Acol[j]], out=Acol[j][:], in0=ada[:, 16:24, j], scalar=1.0, in1=g_sb[:], op0=ALU.add, op1=ALU.mult)
        P.I(DVE, "tensor_copy", [ada], [Bcol[j]], out=Bcol[j][:], in_=ada[:, 8:16, j])
        P.I(DVE, "tensor_copy", [ada], [G0[j]], out=G0[j][:], in_=ada[:, 0:8, j])
        P.I(DVE, "tensor_copy", [ada], [G1[j]], out=G1[j][:], in_=ada[:, 24:32, j])

    x1 = P.sb([128, 8, TH], F32, name="x1")
    hT = P.sb([128, 8, TH], BF16, name="hT")
    GT = P.sb([NEXP, TH], F32, name="GT")
    outs = []
    gu_cnt = [0]; dn_cnt = [0]; ch_cnt = [0]

    for hf in range(nhalf):
        with P.scope():
            mb = P.sb([128, 8, TH], BF16, name=f"mb{hf}")
            wo = P.sb([128, 8, D], BF16, name=f"wo{hf}")
            for kc in range(8):
                P.dma(POOL, mb[:, kc, :], mT[hf, kc * 128:(kc + 1) * 128, :], writes=[mb])
                P.dma(POOL, wo[:, kc, :], w_out[kc * 128:(kc + 1) * 128, :], writes=[wo])
                P.dma(SP if kc % 2 == 0 else ACT, x1[:, kc, :], xT[hf, kc * 128:(kc + 1) * 128, :], writes=[x1])
            for (c0, n, j) in tiles:
                for oc in range(8):
                    ps = Bk.f[oc % 2]
                    for kc in range(8):
                        P.I(PE, "matmul", [wo, mb], [ps], ps[:, 0:n], wo[:, kc, oc * 128:(oc + 1) * 128], mb[:, kc, c0:c0 + n], start=(kc == 0), stop=(kc == 7))
                    P.I(DVE, "scalar_tensor_tensor", [ps, G0[j], x1], [x1], out=x1[:, oc, c0:c0 + n], in0=ps[:, 0:n], scalar=G0[j][:, oc:oc + 1],
                        in1=x1[:, oc, c0:c0 + n], op0=ALU.mult, op1=ALU.add)
        with P.scope():
            sq_sb = P.sb([128, 8, 512], F32, name=f"sq{hf}")
            rs_sb = P.sb([128, 512], F32, name=f"rs{hf}")
            hf32 = P.sb([128, 8, 512], F32, name=f"hf32{hf}")
            lg = P.sb([128, NEXP], F32, name=f"lg{hf}"); mx = P.sb([128, 8], F32, name=f"mx{hf}")
            msk = P.sb([128, NEXP], F32, name=f"msk{hf}"); ex = P.sb([128, NEXP], F32, name=f"ex{hf}")
            sm = P.sb([128, 1], F32, name=f"sm{hf}"); nm = P.sb([128, 1], F32, name=f"nm{hf}")
            for (c0, n, j) in tiles:
                ps = Bk.f[6]
                for kc in range(8):
                    P.I(ACT, "activation", [x1], [sq_sb], out=sq_sb[:, kc, 0:n], in_=x1[:, kc, c0:c0 + n], func=AF.Square)
                for kc in range(8):
                    P.I(PE, "matmul", [ones_sb, sq_sb], [ps], ps[:, 0:n], ones_sb[:], sq_sb[:, kc, 0:n], start=(kc == 0), stop=(kc == 7))
                P.I(DVE, "tensor_scalar", [ps], [rs_sb], out=rs_sb[:, 0:n], in0=ps[:, 0:n], scalar1=1.0 / D, scalar2=EPS, op0=ALU.mult, op1=ALU.add)
                P.I(ACT, "activation", [rs_sb], [rs_sb], out=rs_sb[:, 0:n], in_=rs_sb[:, 0:n], func=AF.Ln)
                P.I(ACT, "activation", [rs_sb], [rs_sb], out=rs_sb[:, 0:n], in_=rs_sb[:, 0:n], func=AF.Exp, scale=-0.5)
                for kc in range(8):
                    P.I(DVE, "tensor_tensor", [x1, rs_sb, sq_sb], [sq_sb], out=sq_sb[:, kc, 0:n], in0=x1[:, kc, c0:c0 + n], in1=rs_sb[:, 0:n], op=ALU.mult)
                    P.I(ACT, "activation", [sq_sb, Acol[j], Bcol[j]], [hf32], out=hf32[:, kc, 0:n], in_=sq_sb[:, kc, 0:n], func=AF.Identity,
                        bias=Bcol[j][:, kc:kc + 1], scale=Acol[j][:, kc:kc + 1])
                    P.I(POOL, "tensor_copy", [hf32], [hT], out=hT[:, kc, c0:c0 + n], in_=hf32[:, kc, 0:n])
                s0 = 0
                while s0 < n:
                    m = min(128, n - s0)
                    pl = Bk.f[5]
                    for kc in range(8):
                        P.I(PE, "matmul", [hf32, rw_sb], [pl], pl[0:m, 0:NEXP], hf32[:, kc, s0:s0 + m], rw_sb[:, kc, :], start=(kc == 0), stop=(kc == 7))
                    P.I(DVE, "tensor_tensor", [pl, rb_sb], [lg], out=lg[0:m, :], in0=pl[0:m, 0:NEXP], in1=rb_sb[0:m, :], op=ALU.add)
                    P.I(DVE, "max", [lg], [mx], out=mx[0:m, :], in_=lg[0:m, :])
                    P.I(DVE, "tensor_scalar", [lg, mx], [msk], out=msk[0:m, :], in0=lg[0:m, :], scalar1=mx[0:m, 3:4], scalar2=None, op0=ALU.is_ge)
                    P.I(DVE, "tensor_scalar", [mx], [nm], out=nm[0:m, :], in0=mx[0:m, 0:1], scalar1=-1.0, scalar2=None, op0=ALU.mult)
                    P.I(ACT, "activation", [lg, nm], [ex], out=ex[0:m, :], in_=lg[0:m, :], func=AF.Exp, bias=nm[0:m, 0:1], scale=1.0)
                    P.I(DVE, "tensor_tensor", [ex, msk], [ex], out=ex[0:m, :], in0=ex[0:m, :], in1=msk[0:m, :], op=ALU.mult)
                    P.I(DVE, "reduce_sum", [ex], [sm], out=sm[0:m, :], in_=ex[0:m, :], axis=AX.X)
                    P.I(DVE, "reciprocal", [sm], [sm], out=sm[0:m, :], in_=sm[0:m, :])
                    P.I(DVE, "tensor_scalar", [ex, sm], [ex], out=ex[0:m, :], in0=ex[0:m, :], scalar1=sm[0:m, 0:1], scalar2=None, op0=ALU.mult)
                    pt = Bk.f[4]
                    P.I(PE, "transpose", [ex, id_f], [pt], pt[0:NEXP, 0:m], ex[0:m, :], id_f[0:m, 0:m])
                    P.I(ACT, "activation", [pt], [GT], out=GT[:, c0 + s0:c0 + s0 + m], in_=pt[0:NEXP, 0:m], func=AF.Copy)
                    s0 += m
        P.dma(SP, gt_dram[hf], GT[:, :], reads=[GT], writes=[("gtd", hf)])
        p3 = P.scope(); p3.__enter__()
        yT = P.sb([128, 8, TH], F32, name=f"yT{hf}")
        actT = P.sb([128, 8, TH], BF16, name=f"actT{hf}")
        gu_ring = [P.sb([128, 8, 512], BF16, name=f"gu{hf}_{i}") for i in range(3)]
        dn_ring = [P.sb([128, 8, 256], BF16, name=f"dn{hf}_{i}") for i in range(3)]
        gbs = [P.sb([128, TH], F32, name=f"gb{hf}_{i}") for i in range(2)]
        g1 = [P.sb([128, 512], F32, name=f"g1{hf}_{i}") for i in range(2)]
        tt = [P.sb([128, 512], F32, name=f"tt{hf}_{i}") for i in range(2)]
        u1 = [P.sb([128, 512], F32, name=f"u1{hf}_{i}") for i in range(2)]
        for e in range(NEXP):
            gb_sb = gbs[e % 2]
            P.dma(SP, gb_sb[:, :], gt_dram[hf, e:e + 1, :].partition_broadcast(128), reads=[("gtd", hf)], writes=[gb_sb])
            for q in range(4):
                wb = gu_ring[gu_cnt[0] % 3]; gu_cnt[0] += 1
                src = w_gu[e].rearrange("(k p) n -> p k n", p=128)
                P.dma(POOL, wb[:, :, 0:256], src[:, :, q * 256:(q + 1) * 256], writes=[wb])
                P.dma(POOL, wb[:, :, 256:512], src[:, :, 1024 + q * 256:1024 + (q + 1) * 256], writes=[wb])
                for o2 in range(2):
                    oc = q * 2 + o2
                    for (c0, n, j) in tiles:
                        i = ch_cnt[0] % 2; ch_cnt[0] += 1
                        pgt, put = Bk.f[i], Bk.f[2 + i]
                        for kc in range(8):
                            P.I(PE, "matmul", [wb, hT], [pgt], pgt[:, 0:n], wb[:, kc, o2 * 128:(o2 + 1) * 128], hT[:, kc, c0:c0 + n], start=(kc == 0), stop=(kc == 7))
                        for kc in range(8):
                            P.I(PE, "matmul", [wb, hT], [put], put[:, 0:n], wb[:, kc, 256 + o2 * 128:256 + (o2 + 1) * 128], hT[:, kc, c0:c0 + n], start=(kc == 0), stop=(kc == 7))
                        P.I(DVE, "tensor_scalar", [pgt, bgu_sb], [g1[i]], out=g1[i][:, 0:n], in0=pgt[:, 0:n], scalar1=bgu_sb[:, e, oc:oc + 1], scalar2=7.0, op0=ALU.add, op1=ALU.min)
                        P.I(ACT, "activation", [g1[i]], [tt[i]], out=tt[i][:, 0:n], in_=g1[i][:, 0:n], func=AF.Silu, scale=1.702)
                        P.I(DVE, "tensor_scalar", [put, bgu_sb], [u1[i]], out=u1[i][:, 0:n], in0=put[:, 0:n], scalar1=bgu_sb[:, e, 8 + oc:8 + oc + 1], scalar2=7.0, op0=ALU.add, op1=ALU.min)
                        P.I(POOL, "tensor_scalar", [u1[i]], [u1[i]], out=u1[i][:, 0:n], in0=u1[i][:, 0:n], scalar1=-7.0, scalar2=1.0, op0=ALU.max, op1=ALU.add)
                        P.I(POOL, "tensor_tensor", [tt[i], u1[i]], [tt[i]], out=tt[i][:, 0:n], in0=tt[i][:, 0:n], in1=u1[i][:, 0:n], op=ALU.mult)
                        P.I(DVE, "scalar_tensor_tensor", [tt[i], gb_sb], [actT], out=actT[:, oc, c0:c0 + n], in0=tt[i][:, 0:n], scalar=1.0 / 1.702, in1=gb_sb[:, c0:c0 + n],
                            op0=ALU.mult, op1=ALU.mult)
            for q in range(4):
                wd = dn_ring[dn_cnt[0] % 3]; dn_cnt[0] += 1
                P.dma(POOL, wd[:], w_dn[e].rearrange("(k p) n -> p k n", p=128)[:, :, q * 256:(q + 1) * 256], writes=[wd])
                for o2 in range(2):
                    dc = q * 2 + o2
                    for (c0, n, j) in tiles:
                        i = ch_cnt[0] % 2; ch_cnt[0] += 1
                        pd = Bk.f[4 + i]
                        for kc in range(8):
                            P.I(PE, "matmul", [wd, actT], [pd], pd[:, 0:n], wd[:, kc, o2 * 128:(o2 + 1) * 128], actT[:, kc, c0:c0 + n], start=(kc == 0), stop=(kc == 7))
                        if e == 0:
                            P.I(DVE, "tensor_copy", [pd], [yT], out=yT[:, dc, c0:c0 + n], in_=pd[:, 0:n])
                        else:
                            P.I(DVE, "tensor_tensor", [pd, yT], [yT], out=yT[:, dc, c0:c0 + n], in0=pd[:, 0:n], in1=yT[:, dc, c0:c0 + n], op=ALU.add)
        for (c0, n, j) in tiles:
            for dc in range(8):
                pd = Bk.f[4 + dc % 2]
                P.I(PE, "matmul", [bdn_sb, GT], [pd], pd[:, 0:n], bdn_sb[:, dc * 128:(dc + 1) * 128], GT[:, c0:c0 + n], start=True, stop=True)
                P.I(DVE, "tensor_tensor", [pd, yT], [yT], out=yT[:, dc, c0:c0 + n], in0=pd[:, 0:n], in1=yT[:, dc, c0:c0 + n], op=ALU.add)
                P.I(DVE, "scalar_tensor_tensor", [yT, G1[j], x1], [x1], out=x1[:, dc, c0:c0 + n], in0=yT[:, dc, c0:c0 + n], scalar=G1[j][:, dc:dc + 1],
                    in1=x1[:, dc, c0:c0 + n], op0=ALU.mult, op1=ALU.add)
        for kc in range(8):
            outs.append(P.dma(SP if kc % 2 == 0 else ACT, outT[hf, kc * 128:(kc + 1) * 128, :], x1[:, kc, :], reads=[x1]))
        p3.__exit__(None, None, None)
    return P.finish(outs)


def prep_C(inp, layer, mT_full, xT_full, mcT=None, xcT=None):
    nhalf = NH_MOE
    nlat = SEQ // NC_MOE // nhalf
    nctx = (NCTX // NC_MOE // nhalf) if mcT is not None else 0
    aw = inp['ada_w'][layer]; ab = inp['ada_b'][layer]
    adaw = np.ascontiguousarray(np.concatenate([aw[:, 2048:3072], aw[:, 3072:6144]], axis=1))
    adab = col_layout(np.concatenate([ab[2048:3072], ab[3072:6144]]), 32)
    cvec = np.stack([col_layout(inp['c'][0], 8), col_layout(inp['c_ctx'], 8)], axis=2).reshape(128, 16)
    bgu = inp['moe_b_gu'][layer]
    bgu_l = np.ascontiguousarray(bgu.reshape(NEXP, 16, 128).transpose(2, 0, 1).reshape(128, NEXP * 16))
    common = dict(
        cvec=np.ascontiguousarray(cvec), adaw=adaw, adab=adab, normg=col_layout(inp['norm_g'][layer, 1], 8),
        w_out=np.ascontiguousarray(inp['w_out'][layer]),
        rw=np.ascontiguousarray(inp['router_w'][layer].reshape(8, 128, NEXP).transpose(1, 0, 2).reshape(128, 8 * NEXP)),
        rb=np.ascontiguousarray(np.broadcast_to(inp['router_b'][layer][None, :], (128, NEXP))).astype(np.float32),
        w_gu=inp['moe_w_gu'][layer], bgu=bgu_l, w_dn=inp['moe_w_dn'][layer], bdn=np.ascontiguousarray(inp['moe_b_dn'][layer]),
        ident=np.eye(128, dtype=np.float32), onesd=np.ones((128, 128), np.float32))
    maps = []
    for c in range(NC_MOE):
        ms, xs = [], []
        for hf in range(nhalf):
            t0 = (c * nhalf + hf) * nlat
            m_ = mT_full[:, t0:t0 + nlat]; x_ = xT_full[:, t0:t0 + nlat]
            if nctx:
                k0 = (c * nhalf + hf) * nctx
                m_ = np.concatenate([m_, mcT[:, k0:k0 + nctx]], axis=1); x_ = np.concatenate([x_, xcT[:, k0:k0 + nctx]], axis=1)
            ms.append(m_); xs.append(x_)
        d = dict(common); d.update(mT=np.ascontiguousarray(np.stack(ms)), xT=np.ascontiguousarray(np.stack(xs)))
        maps.append(d)
    return blobify(maps, BLOB_C), nlat, nctx


def gather_C(results, nlat, nctx):
    nhalf = NH_MOE
    xT = np.zeros((D, SEQ), np.float32); xcT = np.zeros((D, NCTX), np.float32) if nctx else None
    for c in range(NC_MOE):
        o = results[c]['outT']
        for hf in range(nhalf):
            t0 = (c * nhalf + hf) * nlat
            xT[:, t0:t0 + nlat] = o[hf][:, 0:nlat]
            if nctx:
                k0 = (c * nhalf + hf) * nctx
                xcT[:, k0:k0 + nctx] = o[hf][:, nlat:nlat + nctx]
    return xT, xcT


PI = float(np.pi)


def build_B(L):
    NBK = L // 128
    NK = 2 * NBK - 1
    HROW = 2 * L
    P = Prog()
    Bk = Banks(P)
    di = lambda n, s, dt=F32: P.dram(n, s, dt, "ExternalInput")
    vB = di("vB", [128, 64, NBK]); x1B = di("x1B", [128, 64, NBK]); x2B = di("x2B", [128, 64, NBK])
    zt = di("zt", [2, 33, L]); tn = di("tn", [2, 1, L])
    bl = Blob(BLOB_B).dram(P)
    w1 = bl.ap("w1"); w2 = bl.ap("w2"); w3 = bl.ap("w3"); w4s = bl.ap("w4s")
    fqb = bl.ap("fqb")
    negd = bl.ap("negd"); fbias = bl.ap("fbias")
    ident = bl.ap("ident"); onesd = bl.ap("onesd"); jmat = bl.ap("jmat")
    hyB = P.dram("hyB", [128, 64, NBK], F32, "ExternalOutput")
    Hd = P.dram("Hd_scratch", [128, HROW], BF16, "Internal")

    ones_sb = P.sb([128, 128], F32, name="ones"); P.dma(SP, ones_sb[:], onesd[:, :], writes=[ones_sb])
    id_f = P.sb([128, 128], F32, name="idf"); P.dma(SP, id_f[:], ident[:, :], writes=[id_f])
    j_b = P.sb([128, 128], BF16, name="jb"); P.dma(POOL, j_b[:], jmat[:, :], writes=[j_b])
    w1_sb = P.sb([33, 64], F32, name="w1"); P.dma(SP, w1_sb[:], w1[:, :], writes=[w1_sb])
    w2_sb = P.sb([64, 64], F32, name="w2"); P.dma(SP, w2_sb[:], w2[:, :], writes=[w2_sb])
    w3_sb = P.sb([64, 64], F32, name="w3"); P.dma(SP, w3_sb[:], w3[:, :], writes=[w3_sb])
    w4_sb = P.sb([64, 2, 128], F32, name="w4"); P.dma(SP, w4_sb[:], w4s.rearrange("k (s m) -> k s m", s=2), writes=[w4_sb])
    fq_sb = P.sb([64, 4], F32, name="fq"); P.dma(SP, fq_sb[:], fqb[:, :], writes=[fq_sb])
    fb_sb = P.sb([64, 3], F32, name="fqbias")
    P.I(DVE, "tensor_tensor", [fq_sb], [fb_sb], out=fb_sb[:], in0=fq_sb[:, 1:4], in1=fq_sb[:, 0:1].broadcast_to([64, 3]), op=ALU.mult)
    negd_sb = P.sb([128, 1], F32, name="negd"); P.dma(SP, negd_sb[:], negd[:, :], writes=[negd_sb], allow_slow_non_contiguous=True)
    fbias_sb = P.sb([128, 128], F32, name="fbias"); P.dma(SP, fbias_sb[:], fbias[:, :], writes=[fbias_sb])

    CH = min(512, L)
    NCH = L // CH
    abss = P.sb([128, 2 * NCH], F32, name="abss")
    with P.scope():
        z_sb = [P.sb([33, CH], F32, name=f"z{i}") for i in range(2)]
        tn_sb = [P.sb([128, CH], F32, name=f"tn{i}") for i in range(2)]
        a_sb = P.sb([64, CH], F32, name="a_sb"); t_sb = P.sb([64, CH], F32, name="t_sb")
        hd_sb = P.sb([64, CH], F32, name="hd_sb")
        dec_sb = P.sb([128, CH], F32, name="dec_sb"); hf_sb = P.sb([128, CH], F32, name="hf_sb")
        hb_sb = [P.sb([128, CH], BF16, name=f"hb{i}") for i in range(2)]
        it = 0
        for side in range(2):
            for c in range(NCH):
                zb = z_sb[it % 2]; tb = tn_sb[it % 2]; hb = hb_sb[it % 2]; it += 1
                P.dma(SP, zb[:], zt[side, :, c * CH:(c + 1) * CH], writes=[zb])
                P.dma(ACT, tb[:], tn[side, :, c * CH:(c + 1) * CH].partition_broadcast(128), writes=[tb])
                src, Kd, wl = zb, 33, [w1_sb, w2_sb, w3_sb]
                for l in range(3):
                    ps = Bk.f[l % 2]
                    P.I(PE, "matmul", [wl[l], src], [ps], ps[0:64, 0:CH], wl[l][:], src[0:Kd, :], start=True, stop=True)
                    P.I(DVE, "tensor_scalar", [ps, fq_sb, fb_sb], [a_sb], out=a_sb[:], in0=ps[0:64, 0:CH], scalar1=fq_sb[:, 0:1], scalar2=fb_sb[:, l:l + 1], op0=ALU.mult, op1=ALU.add)
                    for rep in range(2):
                        P.I(POOL, "tensor_scalar", [a_sb], [t_sb], out=t_sb[:], in0=a_sb[:], scalar1=PI, scalar2=-2 * PI, op0=ALU.is_gt, op1=ALU.mult)
                        P.I(DVE, "tensor_tensor", [a_sb, t_sb], [hd_sb], out=hd_sb[:], in0=a_sb[:], in1=t_sb[:], op=ALU.add)
                        P.I(POOL, "tensor_scalar", [a_sb], [t_sb], out=t_sb[:], in0=a_sb[:], scalar1=-PI, scalar2=2 * PI, op0=ALU.is_lt, op1=ALU.mult)
                        P.I(DVE, "tensor_tensor", [hd_sb, t_sb], [a_sb], out=a_sb[:], in0=hd_sb[:], in1=t_sb[:], op=ALU.add)
                    P.I(ACT, "activation", [a_sb], [hd_sb], out=hd_sb[:], in_=a_sb[:], func=AF.Sin)
                    src, Kd = hd_sb, 64
                p4 = Bk.f[2]
                P.I(PE, "matmul", [w4_sb, hd_sb], [p4], p4[:, 0:CH], w4_sb[:, side, :], hd_sb[:], start=True, stop=True)
                P.I(ACT, "activation", [tb, negd_sb], [dec_sb], out=dec_sb[:], in_=tb[:], func=AF.Exp, scale=negd_sb[:, 0:1])
                P.I(DVE, "tensor_tensor", [p4, dec_sb], [hf_sb], out=hf_sb[:], in0=p4[:, 0:CH], in1=dec_sb[:], op=ALU.mult)
                if side == 1 and c == NCH - 1:
                    P.I(DVE, "memset", [], [hf_sb], hf_sb[:, CH - 1:CH], 0.0)
                P.I(DVE, "tensor_reduce", [hf_sb], [abss], out=abss[:, side * NCH + c:side * NCH + c + 1], in_=hf_sb[:], axis=AX.X, op=ALU.add, apply_absolute_value=True)
                P.I(ACT, "activation", [hf_sb], [hb], out=hb[:], in_=hf_sb[:], func=AF.Copy)
                if side == 1:
                    n = CH - 1 if c == NCH - 1 else CH
                    P.dma(POOL, Hd[:, c * CH:c * CH + n], hb[:, 0:n], reads=[hb], writes=["Hd"])
                else:
                    P.dma(POOL, Hd[:, L - 1 + c * CH:L - 1 + (c + 1) * CH], hb[:], reads=[hb], writes=["Hd"])
        zpad = P.sb([128, 1], BF16, name="zpad"); P.I(DVE, "memset", [], [zpad], zpad[:], 0.0)
        P.dma(POOL, Hd[:, 2 * L - 1:2 * L], zpad[:], reads=[zpad], writes=["Hd"], allow_slow_non_contiguous=True)
    rn = P.sb([128, 1], F32, name="rn"); rnb = P.sb([128, 128], F32, name="rnb"); dg = P.sb([128, 128], F32, name="dg")
    P.I(DVE, "reduce_sum", [abss], [rn], out=rn[:], in_=abss[:], axis=AX.X)
    P.I(DVE, "reciprocal", [rn], [rn], out=rn[:], in_=rn[:])
    P.I(DVE, "tensor_scalar", [id_f, rn], [dg], out=dg[:], in0=id_f[:], scalar1=rn[:, 0:1], scalar2=None, op0=ALU.mult)
    pr = Bk.f[0]
    P.I(PE, "matmul", [ones_sb, dg], [pr], pr[:, 0:128], ones_sb[:], dg[:], start=True, stop=True)
    P.I(ACT, "activation", [pr], [rnb], out=rnb[:], in_=pr[:, 0:128], func=AF.Copy)

    v_sb = P.sb([128, 64, NBK], F32, name="v_sb"); x1_sb = P.sb([128, 64, NBK], F32, name="x1_sb"); x2_sb = P.sb([128, 64, NBK], F32, name="x2_sb")
    P.dma(SP, v_sb[:], vB[:, :, :], writes=[v_sb]); P.dma(ACT, x1_sb[:], x1B[:, :, :], writes=[x1_sb]); P.dma(SP, x2_sb[:], x2B[:, :, :], writes=[x2_sb])
    zb16 = P.sb([128, 64 * NBK], BF16, name="zb16")
    zrev = P.sb([128, 64, NBK], BF16, name="zrev")
    z1_sb = P.sb([128, 64, NBK], F32, name="z1_sb")
    t0_sb = [P.sb([128, NBK], F32, name=f"t0_{i}") for i in range(2)]
    t1_sb = [P.sb([128, NBK], F32, name=f"t1_{i}") for i in range(2)]
    KP = min(51, NK)
    NPIECE = (NK + KP - 1) // KP
    hs_ring = [P.sb([128, KP * 128], BF16, name=f"hs{i}") for i in range(3)]
    outs = []
    hs_cnt = [0]

    def make_zrev(src_f32):
        flat = src_f32[:].rearrange("p c j -> p (c j)")
        P.I(ACT, "activation", [src_f32], [zb16], out=zb16[:], in_=flat, func=AF.Copy)
        tot = 64 * NBK
        c0 = 0
        zr_flat = zrev[:].rearrange("p c j -> p (c j)")
        i = 0
        while c0 < tot:
            n = min(512, tot - c0)
            pz = Bk.f[4 + i % 2]; i += 1
            P.I(PE, "matmul", [j_b, zb16], [pz], pz[:, 0:n], j_b[:], zb16[:, c0:c0 + n], start=True, stop=True)
            P.I(ACT if i % 2 else DVE, "activation" if i % 2 else "tensor_copy", [pz], [zrev],
                **(dict(out=zr_flat[:, c0:c0 + n], in_=pz[:, 0:n], func=AF.Copy) if i % 2 else dict(out=zr_flat[:, c0:c0 + n], in_=pz[:, 0:n])))
            c0 += n

    mid = (NK // 2) // KP
    piece_order = [mid] + [p for p in range(NPIECE) if p != mid]

    def conv(o, zin_f32, gate_sb, dst_sb):
        for ch in range(64):
            row = o * 64 + ch
            py = Bk.f[ch % 4][:, 0:NBK] if False else Bk.f[ch % 2]
            first = True
            for pi in piece_order:
                kk0 = pi * KP; kk1 = min(NK, kk0 + KP)
                hs = hs_ring[hs_cnt[0] % 3]; hs_cnt[0] += 1
                ncol = (kk1 - kk0) * 128
                src = bass.AP(Hd.tensor, row * HROW + kk0 * 128, [[1, 128], [1, ncol]])
                P.dma(SP if hs_cnt[0] % 2 else ACT, hs[:, 0:ncol], src, reads=["Hd"], writes=[hs])
                ks = list(range(kk0, kk1))
                if pi == mid:
                    ks.remove(NBK - 1); ks = [NBK - 1] + ks
                for kk in ks:
                    k = kk - (NBK - 1)
                    a_lo = max(0, k); a_hi = min(NBK - 1, NBK - 1 + k)
                    last = (pi == piece_order[-1] and kk == ks[-1])
                    P.I(PE, "matmul", [hs, zrev], [py], py[:, a_lo:a_hi + 1], hs[:, (kk - kk0) * 128:(kk - kk0 + 1) * 128], zrev[:, ch, a_lo - k:a_hi - k + 1],
                        start=first, stop=last)
                    first = False
            i = ch % 2
            P.I(POOL, "tensor_scalar", [zin_f32, fbias_sb], [t0_sb[i]], out=t0_sb[i][:], in0=zin_f32[:, ch, :], scalar1=fbias_sb[:, row:row + 1], scalar2=None, op0=ALU.mult)
            P.I(DVE, "scalar_tensor_tensor", [py, rnb, t0_sb[i]], [t1_sb[i]], out=t1_sb[i][:], in0=py[:, 0:NBK], scalar=rnb[:, row:row + 1], in1=t0_sb[i][:], op0=ALU.mult, op1=ALU.add)
            P.I(POOL, "tensor_tensor", [t1_sb[i], gate_sb], [dst_sb], out=dst_sb[:, ch, :], in0=t1_sb[i][:], in1=gate_sb[:, ch, :], op=ALU.mult)

    make_zrev(v_sb)
    conv(0, v_sb, x1_sb, z1_sb)
    make_zrev(z1_sb)
    conv(1, z1_sb, x2_sb, v_sb)
    outs.append(P.dma(SP, hyB[:, :, :], v_sb[:], reads=[v_sb]))
    return P.finish(outs)


def hyena_tables(L):
    t = np.linspace(0.0, 1.0, L, dtype=np.float32)[:, None]
    bands = 16
    w_ang = (2.0 * np.pi * np.arange(L, dtype=np.float32)[:, None] / L).astype(np.float32)
    fr = np.linspace(1e-4, bands - 1, bands, dtype=np.float32)[None]
    z = np.concatenate([t, np.cos(fr * w_ang), -np.sin(fr * w_ang)], axis=-1).astype(np.float32)
    zt = np.stack([z.T, z[::-1].T]).astype(np.float32)
    tn = np.stack([t.T, t[::-1].T]).astype(np.float32)
    return np.ascontiguousarray(zt), np.ascontiguousarray(tn)


def prep_B(inp, uT_full, L):
    NBK = L // 128
    zt, tn = hyena_tables(L)
    max_decay = np.log(1e-2) / 0.3; min_decay = np.log(1e-2) / 1.5
    deltas = np.abs(np.linspace(min_decay, max_decay, 512, dtype=np.float32)).astype(np.float32)
    w4 = inp['hy_w4'][0].reshape(64, 2, 2, 512)
    fqb = np.stack([inp['hy_freq'][0], inp['hy_b1'][0], inp['hy_b2'][0], inp['hy_b3'][0]], axis=1).astype(np.float32)
    jm = np.eye(128, dtype=np.float32)[::-1].copy()
    maps = []
    for c in range(NCORE):
        chs = slice(64 * c, 64 * c + 64)

        def blk(rows):
            return np.ascontiguousarray(rows.reshape(64, NBK, 128).transpose(2, 0, 1))
        w4s = np.concatenate([w4[:, :, s, chs].reshape(64, 128) for s in range(2)], axis=1)
        fb = inp['hy_filter_bias'][0][:, chs].reshape(128)
        maps.append(dict(
            vB=blk(uT_full[0:512][chs]), x1B=blk(uT_full[512:1024][chs]), x2B=blk(uT_full[1024:1536][chs]),
            zt=zt, tn=tn, w1=np.ascontiguousarray(inp['hy_w1'][0]), w2=np.ascontiguousarray(inp['hy_w2'][0]),
            w3=np.ascontiguousarray(inp['hy_w3'][0]), w4s=np.ascontiguousarray(w4s), fqb=np.ascontiguousarray(fqb),
            negd=np.ascontiguousarray(-np.tile(deltas[chs], 2)[:, None]).astype(np.float32),
            fbias=np.ascontiguousarray(np.broadcast_to(fb[None, :], (128, 128))).astype(np.float32),
            ident=np.eye(128, dtype=np.float32), onesd=np.ones((128, 128), np.float32), jmat=jm))
    return blobify(maps, BLOB_B)


def gather_B(results, L):
    NBK = L // 128
    hyT = np.zeros((512, L), np.float32)
    for c in range(NCORE):
        hb = results[c]['hyB']
        hyT[64 * c:64 * c + 64] = hb.transpose(1, 2, 0).reshape(64, L)
    return hyT


DROWS = 5136


def build_D():
    P = Prog()
    Bk = Banks(P)
    di = lambda n, s, dt=F32: P.dram(n, s, dt, "ExternalInput")
    xT = di("xT", [D, TEXT]); ctxT = di("ctxT", [D, NCTX])
    bl = Blob(BLOB_D).dram(P)
    cvec = bl.ap("cvec"); adaw = di("adaw", [D, 3072]); adab = bl.ap("adab")
    normg = bl.ap("normg"); w_in = di("w_in", [D, 4112])
    onesd = bl.ap("onesd")
    convw = bl.ap("convw"); convb = bl.ap("convb"); edge = bl.ap("edge")
    hglb = bl.ap("hglb")
    dtb = bl.ap("dtb")
    outT = P.dram("outT", [DROWS, TLOC], F32, "ExternalOutput"); outcT = P.dram("outcT", [DROWS, NCTX], F32, "ExternalOutput")

    ones_sb = P.sb([128, 128], F32, name="ones"); P.dma(SP, ones_sb[:], onesd[:, :], writes=[ones_sb])
    cv = P.sb([128, 8, 2], F32, name="cv"); P.dma(SP, cv[:], cvec.rearrange("p (k n) -> p k n", n=2), writes=[cv])
    adab_sb = P.sb([128, 24], F32, name="adab"); P.dma(SP, adab_sb[:], adab[:, :], writes=[adab_sb])
    g_sb = P.sb([128, 8], F32, name="normg"); P.dma(SP, g_sb[:], normg[:, :], writes=[g_sb])
    cw_sb = P.sb([128, 8, 3], F32, name="cw"); P.dma(SP, cw_sb[:], convw.rearrange("p (o t) -> p o t", t=3), writes=[cw_sb])
    cb_sb = P.sb([128, 8], F32, name="cb"); P.dma(SP, cb_sb[:], convb[:, :], writes=[cb_sb])
    edge_sb = P.sb([128, 2], F32, name="edge"); P.dma(SP, edge_sb[:], edge[:, :], writes=[edge_sb])
    lb_sb = P.sb([128, 8], F32, name="hglb"); P.dma(SP, lb_sb[:], hglb[:, :], writes=[lb_sb])
    dtb_sb = P.sb([16, 1], F32, name="dtb"); P.dma(SP, dtb_sb[:], dtb[:, :], writes=[dtb_sb], allow_slow_non_contiguous=True)
    lbc = P.sb([128, 4], F32, name="lbc"); oml = P.sb([128, 4], F32, name="oml")
    P.I(DVE, "tensor_tensor", [lb_sb], [lbc], out=lbc[:], in0=lb_sb[:, 4:8], in1=lb_sb[:, 0:4], op=ALU.subtract)
    P.I(ACT, "activation", [lbc], [lbc], out=lbc[:], in_=lbc[:], func=AF.Sigmoid)
    P.I(DVE, "tensor_scalar", [lbc], [oml], out=oml[:], in0=lbc[:], scalar1=-1.0, scalar2=1.0, op0=ALU.mult, op1=ALU.add)
    w_sb = P.sb([128, 8, 4112], BF16, name="w_in")
    for kc in range(8):
        P.dma(POOL, w_sb[:, kc, :], w_in[kc * 128:(kc + 1) * 128, :], writes=[w_sb])
    ada = P.sb([128, 24, 2], F32, name="ada")
    with P.scope():
        wbuf = P.sb([128, 8, 1024], F32, name="adawbuf")
        emit_adaln(P, Bk, cv, adaw, adab_sb, wbuf, ada, 2)
    Acol = [P.sb([128, 8], F32, name=f"Acol{j}") for j in range(2)]
    Bcol = [P.sb([128, 8], F32, name=f"Bcol{j}") for j in range(2)]
    for j in range(2):
        P.I(DVE, "scalar_tensor_tensor", [ada, g_sb], [Acol[j]], out=Acol[j][:], in0=ada[:, 8:16, j], scalar=1.0, in1=g_sb[:], op0=ALU.add, op1=ALU.mult)
        P.I(DVE, "tensor_copy", [ada], [Bcol[j]], out=Bcol[j][:], in_=ada[:, 0:8, j])
    h_all = P.sb([128, 8, TEXT + NCTX], BF16, name="h_all")
    x_sb = P.sb([128, 8, TT], F32, name="x_sb"); sq_sb = P.sb([128, 8, TT], F32, name="sq_sb"); rs_sb = P.sb([128, TT], F32, name="rs_sb")
    for t in range(NTT):
        for kc in range(8):
            P.dma(SP if kc % 2 == 0 else ACT, x_sb[:, kc, :], xT[kc * 128:(kc + 1) * 128, t * TT:(t + 1) * TT], writes=[x_sb])
        emit_norm_mod(P, Bk, x_sb, ones_sb, Acol[0], Bcol[0], h_all, TT, sq_sb, rs_sb, hcol0=t * TT)
    for kc in range(8):
        P.dma(SP if kc % 2 == 0 else ACT, x_sb[:, kc, 0:NCTX], ctxT[kc * 128:(kc + 1) * 128, :], writes=[x_sb])
    emit_norm_mod(P, Bk, x_sb, ones_sb, Acol[1], Bcol[1], h_all, NCTX, sq_sb, rs_sb, hcol0=TEXT)

    u_sb = [P.sb([128, 512], F32, name=f"u_sb{i}") for i in range(2)]
    a1_sb = [P.sb([128, 512], F32, name=f"a1_sb{i}") for i in range(2)]
    a2_sb = [P.sb([128, 512], F32, name=f"a2_sb{i}") for i in range(2)]
    outs = []

    def tile(h0, n, out_dram, ocol0, zl, zr, el, er):
        a0 = h0 - (0 if zl else 1); a1 = h0 + n + (0 if zr else 1)
        w = a1 - a0
        off = 1 if zl else 0
        for oc in range(33):
            M = 128 if oc < 32 else 16
            i = oc % 2
            pu = Bk.f[2 + i]; ub = u_sb[i]; r1 = a1_sb[i]; r2 = a2_sb[i]
            for kc in range(8):
                P.I(PE, "matmul", [w_sb, h_all], [pu], pu[0:M, off:off + w], w_sb[:, kc, oc * 128:oc * 128 + M], h_all[:, kc, a0:a1], start=(kc == 0), stop=(kc == 7))
            ctr = pu[0:M, 1:n + 1]
            dq = SP if oc % 2 == 0 else POOL
            if oc < 8:
                if zl:
                    P.I(POOL, "memset", [], [ub], ub[:, 0:1], 0.0)
                if zr:
                    P.I(POOL, "memset", [], [ub], ub[:, n + 1:n + 2], 0.0)
                P.I(ACT, "activation", [pu], [ub], out=ub[:, off:off + w], in_=pu[:, off:off + w], func=AF.Copy)
                if el:
                    P.I(DVE, "tensor_scalar", [ub, edge_sb], [ub], out=ub[:, 0:1], in0=ub[:, 0:1], scalar1=edge_sb[:, 0:1], scalar2=None, op0=ALU.mult)
                if er:
                    P.I(DVE, "tensor_scalar", [ub, edge_sb], [ub], out=ub[:, n + 1:n + 2], in0=ub[:, n + 1:n + 2], scalar1=edge_sb[:, 1:2], scalar2=None, op0=ALU.mult)
                P.I(DVE, "tensor_scalar", [ub, cw_sb, cb_sb], [r1], out=r1[:, 0:n], in0=ub[:, 1:n + 1], scalar1=cw_sb[:, oc, 1:2], scalar2=cb_sb[:, oc:oc + 1], op0=ALU.mult, op1=ALU.add)
                P.I(DVE, "scalar_tensor_tensor", [ub, cw_sb, r1], [r1], out=r1[:, 0:n], in0=ub[:, 0:n], scalar=cw_sb[:, oc, 0:1], in1=r1[:, 0:n], op0=ALU.mult, op1=ALU.add)
                P.I(DVE, "scalar_tensor_tensor", [ub, cw_sb, r1], [r1], out=r1[:, 0:n], in0=ub[:, 2:n + 2], scalar=cw_sb[:, oc, 2:3], in1=r1[:, 0:n], op0=ALU.mult, op1=ALU.add)
                P.I(ACT, "activation", [r1], [r2], out=r2[:, 0:n], in_=r1[:, 0:n], func=AF.Silu)
                outs.append(P.dma(dq, out_dram[oc * 128:(oc + 1) * 128, ocol0:ocol0 + n], r2[:, 0:n], reads=[r2]))
            elif oc < 16:
                hd = (oc - 8) % 4
                P.I(ACT, "activation", [pu], [r1], out=r1[:, 0:n], in_=ctr, func=AF.Sigmoid)
                P.I(DVE, "tensor_scalar", [r1, oml, lbc], [r1], out=r1[:, 0:n], in0=r1[:, 0:n], scalar1=oml[:, hd:hd + 1], scalar2=lbc[:, hd:hd + 1], op0=ALU.mult, op1=ALU.add)
                P.I(DVE, "tensor_scalar", [r1], [r2], out=r2[:, 0:n], in0=r1[:, 0:n], scalar1=-1.0, scalar2=1.0, op0=ALU.mult, op1=ALU.add)
                outs.append(P.dma(dq, out_dram[1024 + (oc - 8) * 128:1024 + (oc - 7) * 128, ocol0:ocol0 + n], r2[:, 0:n], reads=[r2]))
                P.I(ACT, "activation", [r1], [ub], out=ub[:, 0:n], in_=r1[:, 0:n], func=AF.Ln)
                outs.append(P.dma(dq, out_dram[2048 + (oc - 8) * 128:2048 + (oc - 7) * 128, ocol0:ocol0 + n], ub[:, 0:n], reads=[ub]))
            elif oc < 20:
                P.I(ACT, "activation", [pu], [r1], out=r1[:, 0:n], in_=ctr, func=AF.Copy)
                outs.append(P.dma(dq, out_dram[3072 + (oc - 16) * 128:3072 + (oc - 15) * 128, ocol0:ocol0 + n], r1[:, 0:n], reads=[r1]))
            elif oc < 32:
                P.I(ACT, "activation", [pu], [r1], out=r1[:, 0:n], in_=ctr, func=AF.Silu)
                outs.append(P.dma(dq, out_dram[3584 + (oc - 20) * 128:3584 + (oc - 19) * 128, ocol0:ocol0 + n], r1[:, 0:n], reads=[r1]))
            else:
                P.I(ACT, "activation", [pu, dtb_sb], [r1], out=r1[0:16, 0:n], in_=pu[0:16, 1:n + 1], func=AF.Exp, bias=dtb_sb[:, 0:1], scale=1.0)
                P.I(ACT, "activation", [r1], [r2], out=r2[0:16, 0:n], in_=r1[0:16, 0:n], func=AF.Ln, bias=1.0, scale=1.0)
                outs.append(P.dma(dq, out_dram[5120:5136, ocol0:ocol0 + n], r2[0:16, 0:n], reads=[r2]))

    lo = 0
    while lo < TLOC:
        n = min(510, TLOC - lo)
        tile(HALO + lo, n, outT, lo, False, False, lo == 0, lo + n == TLOC)
        lo += n
    tile(TEXT, NCTX, outcT, 0, True, True, False, False)
    return P.finish(outs)


def prep_D(inp, xT_full, xcT):
    layer = 1
    xTp = np.concatenate([np.zeros((D, HALO), np.float32), xT_full, np.zeros((D, HALO), np.float32)], axis=1)
    w = inp['w_in_odd'][0]
    w_perm = np.ascontiguousarray(np.concatenate([w[:, 0:1024], w[:, 1040:2064], w[:, 2064:2576], w[:, 2576:3088], w[:, 3088:3600],
                                                   w[:, 3600:4112], w[:, 1024:1040]], axis=1))
    cvec = np.stack([col_layout(inp['c'][0], 8), col_layout(inp['c_ctx'], 8)], axis=2).reshape(128, 16)
    hl = inp['hg_lower_bounds']
    common = dict(
        ctxT=np.ascontiguousarray(xcT), cvec=np.ascontiguousarray(cvec),
        adaw=np.ascontiguousarray(inp['ada_w'][layer][:, 0:3072]), adab=col_layout(inp['ada_b'][layer][0:3072], 24),
        normg=col_layout(inp['norm_g'][layer, 0], 8), w_in=w_perm, onesd=np.ones((128, 128), np.float32),
        convw=np.ascontiguousarray(inp['ssd_conv_w'][0].reshape(3, 8, 128).transpose(2, 1, 0).reshape(128, 24)),
        convb=col_layout(inp['ssd_conv_b'][0], 8),
        hglb=np.ascontiguousarray(np.concatenate([col_layout(hl[0], 4), col_layout(hl[1], 4)], axis=1)),
        dtb=np.ascontiguousarray(inp['ssd_dt_bias'][0].reshape(16, 1)))
    maps = []
    for c in range(NCORE):
        s0 = c * TLOC
        edge = np.ones((128, 2), np.float32); edge[:, 0] = 0.0 if c == 0 else 1.0; edge[:, 1] = 0.0 if c == NCORE - 1 else 1.0
        m = dict(common); m.update(xT=np.ascontiguousarray(xTp[:, s0:s0 + TEXT]), edge=edge)
        maps.append(m)
    return blobify(maps, BLOB_D)


LS = NCTX + SEQ
NBLK = LS // 128
GB = 10


def build_E():
    P = Prog()
    Bk = Banks(P)
    di = lambda n, s, dt=F32: P.dram(n, s, dt, "ExternalInput")
    xtok = di("xtok", [2, 128, NBLK, 64]); Btok = di("Btok", [2, 128, NBLK, 128])
    BT = di("BT", [2, 128, LS]); CT = di("CT", [2, 128, LS]); dttok = di("dttok", [2, 128, NBLK])
    bl = Blob(BLOB_E).dram(P)
    ssdp = bl.ap("ssdp")
    qT = di("qT", [2, 128, LS]); kT = di("kT", [2, 128, LS]); gtok = di("gtok", [2, 128, NBLK, 128])
    vZ = di("vZ", [2, 128, NBLK, 5, 64])
    Ud = bl.ap("U"); U4d = bl.ap("U4"); Mnegd = bl.ap("Mneg")
    ident = bl.ap("ident"); onesd = bl.ap("onesd")
    ytok = P.dram("ytok", [2, 128, NBLK, 64], F32, "ExternalOutput"); otok = P.dram("otok", [2, 128, NBLK, 64], F32, "ExternalOutput")

    ones_sb = P.sb([128, 128], F32, name="ones"); P.dma(SP, ones_sb[:], onesd[:, :], writes=[ones_sb])
    U_sb = P.sb([128, 128], F32, name="U"); P.dma(SP, U_sb[:], Ud[:, :], writes=[U_sb])
    U4_sb = P.sb([128, 128], F32, name="U4"); P.dma(SP, U4_sb[:], U4d[:, :], writes=[U4_sb])
    Mn_sb = P.sb([128, 128], F32, name="Mneg"); P.dma(SP, Mn_sb[:], Mnegd[:, :], writes=[Mn_sb])
    id_b = P.sb([128, 128], BF16, name="idb"); P.dma(POOL, id_b[:], ident[:, :], writes=[id_b])
    sp_sb = P.sb([128, 4], F32, name="ssdp"); P.dma(SP, sp_sb[:], ssdp[:, :], writes=[sp_sb])
    acol = P.sb([128, 2], F32, name="acol")
    P.I(ACT, "activation", [sp_sb], [acol], out=acol[:], in_=sp_sb[:, 0:2], func=AF.Exp)
    P.I(DVE, "tensor_scalar", [acol], [acol], out=acol[:], in0=acol[:], scalar1=-1.0, scalar2=None, op0=ALU.mult)
    outs = []

    with P.scope():
        dt_sb = P.sb([128, 2, NBLK], F32, name="dt_sb")
        for d in range(2):
            P.dma(SP, dt_sb[:, d, :], dttok[d], writes=[dt_sb])
        xg = [P.sb([128, GB, 64], F32, name=f"xg{i}") for i in range(2)]
        Bg = [P.sb([128, GB, 128], BF16, name=f"Bg{i}") for i in range(2)]
        BTg = [P.sb([128, GB * 128], BF16, name=f"BTg{i}") for i in range(2)]
        CTg = [P.sb([128, GB * 128], F32, name=f"CTg{i}") for i in range(2)]
        CTh = [P.sb([128, GB * 128], BF16, name=f"CTh{i}") for i in range(2)]
        yg = [P.sb([128, GB, 64], F32, name=f"yg{i}") for i in range(2)]
        da = P.sb([128, 1], F32, name="da"); dab = P.sb([128, 128], F32, name="dab")
        cs_sb = P.sb([128, 1], F32, name="cs_sb"); tot_sb = P.sb([128, 1], F32, name="tot_sb")
        te = P.sb([128, 1], F32, name="te"); dec = P.sb([128, 1], F32, name="dec"); wcol = P.sb([128, 1], F32, name="wcol")
        xdt = P.sb([128, 64], BF16, name="xdt"); xw = P.sb([128, 64], BF16, name="xw")
        em = P.sb([128, 128], F32, name="em"); gt = P.sb([128, 128], BF16, name="gt")
        ecs = P.sb([128, 128], F32, name="ecs"); cp = P.sb([128, 128], BF16, name="cp")
        S = P.sb([128, 64], F32, name="S_ssd"); Sbf = P.sb([128, 64], BF16, name="Sbf_ssd")
        gi = 0
        for d in range(2):
            P.I(DVE, "memset", [], [S], S[:], 0.0)
            P.I(POOL, "memset", [], [Sbf], Sbf[:], 0.0)
            for g0 in range(0, NBLK, GB):
                i = gi % 2; gi += 1
                P.dma(SP, xg[i][:], xtok[d, :, g0:g0 + GB, :], writes=[xg[i]])
                P.dma(POOL, Bg[i][:], Btok[d, :, g0:g0 + GB, :], writes=[Bg[i]])
                P.dma(POOL, BTg[i][:], BT[d, :, g0 * 128:(g0 + GB) * 128], writes=[BTg[i]])
                P.dma(ACT, CTg[i][:], CT[d, :, g0 * 128:(g0 + GB) * 128], writes=[CTg[i]])
                P.dma(POOL, CTh[i][:], CT[d, :, g0 * 128:(g0 + GB) * 128], writes=[CTh[i]])
                for bb in range(GB):
                    b = g0 + bb
                    cols = slice(bb * 128, (bb + 1) * 128)
                    P.I(DVE, "tensor_scalar", [dt_sb, acol], [da], out=da[:], in0=dt_sb[:, d, b:b + 1], scalar1=acol[:, d:d + 1], scalar2=None, op0=ALU.mult)
                    P.I(DVE, "tensor_scalar", [ones_sb, da], [dab], out=dab[:], in0=ones_sb[:], scalar1=da[:, 0:1], scalar2=None, op0=ALU.mult)
                    pc, pr = Bk.f[0], Bk.f[1]
                    P.I(PE, "matmul", [U_sb, da], [pc], pc[:, 0:1], U_sb[:], da[:], start=True, stop=True)
                    P.I(PE, "matmul", [dab, U_sb], [pr], pr[:, 0:128], dab[:], U_sb[:], start=True, stop=True)
                    P.I(ACT, "activation", [pc], [cs_sb], out=cs_sb[:], in_=pc[:, 0:1], func=AF.Copy)
                    P.I(ACT, "activation", [pr], [tot_sb], out=tot_sb[:], in_=pr[:, 127:128], func=AF.Copy)
                    P.I(ACT, "activation", [cs_sb, tot_sb], [te], out=te[:], in_=cs_sb[:], func=AF.Exp, scale=-1.0, bias=tot_sb[:, 0:1])
                    P.I(ACT, "activation", [tot_sb], [dec], out=dec[:], in_=tot_sb[:], func=AF.Exp)
                    P.I(DVE, "tensor_scalar", [xg[i], dt_sb], [xdt], out=xdt[:], in0=xg[i][:, bb, :], scalar1=dt_sb[:, d, b:b + 1], scalar2=None, op0=ALU.mult)
                    P.I(DVE, "tensor_tensor", [dt_sb, te], [wcol], out=wcol[:], in0=dt_sb[:, d, b:b + 1], in1=te[:], op=ALU.mult)
                    P.I(DVE, "tensor_scalar", [xg[i], wcol], [xw], out=xw[:], in0=xg[i][:, bb, :], scalar1=wcol[:, 0:1], scalar2=None, op0=ALU.mult)
                    psc = Bk.f[2]
                    P.I(PE, "matmul", [Bg[i], xw], [psc], psc[:, 0:64], Bg[i][:, bb, :], xw[:], start=True, stop=True)
                    pss = Bk.f[3]
                    P.I(PE, "matmul", [BTg[i], CTh[i]], [pss], pss[:, 0:128], BTg[i][:, cols], CTh[i][:, cols], start=True, stop=True)
                    P.I(DVE, "scalar_tensor_tensor", [pr, cs_sb, Mn_sb], [em], out=em[:], in0=pr[:, 0:128], scalar=cs_sb[:, 0:1], in1=Mn_sb[:], op0=ALU.subtract, op1=ALU.add)
                    P.I(ACT, "activation", [em], [em], out=em[:], in_=em[:], func=AF.Exp)
                    P.I(DVE, "tensor_tensor", [pss, em], [gt], out=gt[:], in0=pss[:, 0:128], in1=em[:], op=ALU.mult)
                    P.I(ACT, "activation", [pr], [ecs], out=ecs[:], in_=pr[:, 0:128], func=AF.Exp)
                    P.I(DVE, "tensor_tensor", [CTg[i], ecs], [cp], out=cp[:], in0=CTg[i][:, cols], in1=ecs[:], op=ALU.mult)
                    py = Bk.f[4]
                    P.I(PE, "matmul", [gt, xdt], [py], py[:, 0:64], gt[:], xdt[:], start=True, stop=False)
                    P.I(PE, "matmul", [cp, Sbf], [py], py[:, 0:64], cp[:], Sbf[:], start=False, stop=True)
                    P.I(DVE, "scalar_tensor_tensor", [xg[i], sp_sb, py], [yg[i]], out=yg[i][:, bb, :], in0=xg[i][:, bb, :], scalar=sp_sb[:, 2 + d:3 + d], in1=py[:, 0:64], op0=ALU.mult, op1=ALU.add)
                    P.I(DVE, "scalar_tensor_tensor", [S, dec, psc], [S], out=S[:], in0=S[:], scalar=dec[:, 0:1], in1=psc[:, 0:64], op0=ALU.mult, op1=ALU.add)
                    P.I(ACT, "activation", [S], [Sbf], out=Sbf[:], in_=S[:], func=AF.Copy)
                outs.append(P.dma(SP, ytok[d, :, g0:g0 + GB, :], yg[i][:], reads=[yg[i]]))

    with P.scope():
        qg = [P.sb([128, GB * 128], F32, name=f"qg{i}") for i in range(2)]
        kg = [P.sb([128, GB * 128], F32, name=f"kg{i}") for i in range(2)]
        gg = [P.sb([128, GB, 128], F32, name=f"gg{i}") for i in range(2)]
        vg = [P.sb([128, GB, 5, 64], BF16, name=f"vg{i}") for i in range(2)]
        og = [P.sb([128, GB, 64], F32, name=f"og{i}") for i in range(2)]
        cum = P.sb([128, 128], F32, name="cum"); d1 = P.sb([128, 128], F32, name="d1"); d2 = P.sb([128, 128], F32, name="d2")
        e1 = P.sb([128, 128], F32, name="e1"); e2 = P.sb([128, 128], F32, name="e2"); e3 = P.sb([128, 128], F32, name="e3"); e4 = P.sb([128, 128], F32, name="e4")
        dec4 = P.sb([128, 4], F32, name="dec4")
        QiZ = P.sb([128, 4, 128], BF16, name="QiZ"); P.I(POOL, "memset", [], [QiZ], QiZ[:], 0.0)
        Qp = P.sb([128, 128], BF16, name="Qp"); Kp = P.sb([128, 128], BF16, name="Kp"); Kpp = P.sb([128, 128], BF16, name="Kpp")
        am = P.sb([128, 128], F32, name="am"); amb = P.sb([128, 128], BF16, name="amb"); Ktok = P.sb([128, 128], BF16, name="Ktok")
        S = P.sb([128, 64], F32, name="S_hg"); Sst = [P.sb([128, 4, 64], BF16, name=f"Sst{i}") for i in range(2)]
        c3 = lambda t: t[:].rearrange("p (c t) -> p c t", t=32)
        QiZd = bass.AP(QiZ[:].tensor, 0, [[QiZ[:].ap[0][0], 128], [128 + 32, 4], [1, 32]])
        gi = 0; blk = 0
        for d in range(2):
            P.I(DVE, "memset", [], [S], S[:], 0.0)
            P.I(POOL, "memset", [], [Sst[blk % 2]], Sst[blk % 2][:], 0.0)
            for g0 in range(0, NBLK, GB):
                i = gi % 2; gi += 1
                P.dma(SP, qg[i][:], qT[d, :, g0 * 128:(g0 + GB) * 128], writes=[qg[i]])
                P.dma(ACT, kg[i][:], kT[d, :, g0 * 128:(g0 + GB) * 128], writes=[kg[i]])
                P.dma(SP, gg[i][:], gtok[d, :, g0:g0 + GB, :], writes=[gg[i]])
                P.dma(POOL, vg[i][:], vZ[d, :, g0:g0 + GB, :, :], writes=[vg[i]])
                for bb in range(GB):
                    cols = slice(bb * 128, (bb + 1) * 128)
                    cur, nxt = Sst[blk % 2], Sst[(blk + 1) % 2]; blk += 1
                    pcum = Bk.f[0]
                    P.I(PE, "matmul", [gg[i], U4_sb], [pcum], pcum[:, 0:128], gg[i][:, bb, :], U4_sb[:], start=True, stop=True)
                    P.I(ACT, "activation", [pcum], [cum], out=cum[:], in_=pcum[:, 0:128], func=AF.Copy)
                    rmid = c3(cum)[:, :, 15:16].broadcast_to([128, 4, 32]); cend = c3(cum)[:, :, 31:32].broadcast_to([128, 4, 32])
                    P.I(DVE, "tensor_tensor", [cum], [d1], out=c3(d1), in0=c3(cum), in1=rmid, op=ALU.subtract)
                    P.I(DVE, "tensor_tensor", [cum], [d2], out=c3(d2), in0=c3(cum), in1=cend, op=ALU.subtract)
                    P.I(ACT, "activation", [cum], [e1], out=e1[:], in_=cum[:], func=AF.Exp)
                    P.I(ACT, "activation", [d1], [e2], out=e2[:], in_=d1[:], func=AF.Exp)
                    P.I(ACT, "activation", [d1], [e3], out=e3[:], in_=d1[:], func=AF.Exp, scale=-1.0)
                    P.I(ACT, "activation", [d2], [e4], out=e4[:], in_=d2[:], func=AF.Exp, scale=-1.0)
                    P.I(ACT, "activation", [cum], [dec4], out=dec4[:], in_=c3(cum)[:, :, 31], func=AF.Exp)
                    P.I(DVE, "tensor_tensor", [qg[i], e1], [QiZ], out=QiZd, in0=qg[i][:, cols].rearrange("p (c t) -> p c t", t=32), in1=c3(e1), op=ALU.mult)
                    P.I(DVE, "tensor_tensor", [qg[i], e2], [Qp], out=Qp[:], in0=qg[i][:, cols], in1=e2[:], op=ALU.mult)
                    P.I(DVE, "tensor_tensor", [kg[i], e3], [Kp], out=Kp[:], in0=kg[i][:, cols], in1=e3[:], op=ALU.mult)
                    P.I(DVE, "tensor_tensor", [kg[i], e4], [Kpp], out=Kpp[:], in0=kg[i][:, cols], in1=e4[:], op=ALU.mult)
                    pa = Bk.f[1]
                    P.I(PE, "matmul", [Kp, Qp], [pa], pa[:, 0:128], Kp[:], Qp[:], start=True, stop=True)
                    P.I(DVE, "tensor_scalar", [pa], [am], out=am[:], in0=pa[:, 0:128], scalar1=1e30, scalar2=-1e30, op0=ALU.min, op1=ALU.max)
                    P.I(DVE, "tensor_tensor", [am, U4_sb], [amb], out=amb[:], in0=am[:], in1=U4_sb[:], op=ALU.mult)
                    pt = Bk.h[0]
                    P.I(PE, "transpose", [Kpp, id_b], [pt], pt[:, 0:128], Kpp[:], id_b[:])
                    P.I(ACT, "activation", [pt], [Ktok], out=Ktok[:], in_=pt[:, 0:128], func=AF.Copy)
                    po = Bk.f[2]
                    P.I(PE, "matmul", [amb, vg[i]], [po], po[:, 0:64], amb[:], vg[i][:, bb, 4, :], start=True, stop=False)
                    for ci in range(4):
                        P.I(PE, "matmul", [QiZ, cur], [po], po[:, 0:64], QiZ[:, ci, :], cur[:, ci, :], start=False, stop=(ci == 3))
                        if True:
                            psc = Bk.f[3 + ci % 2]
                            P.I(PE, "matmul", [Ktok, vg[i]], [psc], psc[:, 0:64], Ktok[:], vg[i][:, bb, ci, :], start=True, stop=True)
                            P.I(DVE, "scalar_tensor_tensor", [S, dec4, psc], [S], out=S[:], in0=S[:], scalar=dec4[:, ci:ci + 1], in1=psc[:, 0:64], op0=ALU.mult, op1=ALU.add)
                            if ci < 3:
                                P.I(ACT, "activation", [S], [cur], out=cur[:, ci + 1, :], in_=S[:], func=AF.Copy)
                            else:
                                P.I(ACT, "activation", [S], [nxt], out=nxt[:, 0, :], in_=S[:], func=AF.Copy)
                    P.I(ACT, "activation", [po], [og[i]], out=og[i][:, bb, :], in_=po[:, 0:64], func=AF.Copy)
                outs.append(P.dma(SP, otok[d, :, g0:g0 + GB, :], og[i][:], reads=[og[i]]))
    return P.finish(outs)


def gather_D(results):
    full = np.concatenate([results[c]['outT'] for c in range(NCORE)], axis=1)
    return full, results[0]['outcT']


def prep_E(inp, dfull, dctx):
    def seq(rows_lat, rows_ctx, d):
        if d == 0:
            return np.concatenate([rows_ctx, rows_lat], axis=1)
        return np.concatenate([rows_ctx[:, ::-1], rows_lat[:, ::-1]], axis=1)

    def blk(a):
        return np.ascontiguousarray(a.T.reshape(NBLK, 128, a.shape[0]).transpose(1, 0, 2))
    s_ = np.arange(128)[:, None]; l_ = np.arange(128)[None, :]
    U = (s_ <= l_).astype(np.float32)
    U4 = ((s_ // 32 == l_ // 32) & (s_ <= l_)).astype(np.float32)
    Mneg = np.where(s_ <= l_, 0.0, MASKNEG).astype(np.float32)
    maps = []
    for c in range(NCORE):
        h = c; g = c // 4; hh = c // 2; vh = c % 2
        R = lambda r0, n, d: seq(dfull[r0:r0 + n], dctx[r0:r0 + n], d)
        m = dict(U=U, U4=U4, Mneg=Mneg, ident=np.eye(128, dtype=np.float32), onesd=np.ones((128, 128), np.float32))
        m['xtok'] = np.stack([blk(R(64 * h, 64, d)) for d in range(2)])
        m['Btok'] = np.stack([blk(R(512 + 128 * g, 128, d)) for d in range(2)])
        m['BT'] = np.stack([np.ascontiguousarray(R(512 + 128 * g, 128, d)) for d in range(2)])
        m['CT'] = np.stack([np.ascontiguousarray(R(768 + 128 * g, 128, d)) for d in range(2)])
        m['dttok'] = np.stack([np.ascontiguousarray(R(5120 + 8 * d + h, 1, d)[0].reshape(NBLK, 128).T) for d in range(2)])
        al = inp['ssd_A_log'][0]; dd = inp['ssd_D'][0]
        m['ssdp'] = np.ascontiguousarray(np.broadcast_to(np.array([al[0, h], al[1, h], dd[0, h], dd[1, h]], np.float32)[None, :], (128, 4)))
        m['qT'] = np.stack([np.ascontiguousarray(R(4096 + 128 * hh, 128, d)) for d in range(2)])
        m['kT'] = np.stack([np.ascontiguousarray(R(1024 + 512 * d + 128 * hh, 128, d)) for d in range(2)])
        m['gtok'] = np.stack([blk(R(2048 + 512 * d + 128 * hh, 128, d)) for d in range(2)])
        vz = []
        for d in range(2):
            v = blk(R(3072 + 128 * hh + 64 * vh, 64, d))
            z5 = np.zeros((128, NBLK, 5, 64), np.float32)
            z5[:, :, 4, :] = v
            for i in range(4):
                z5[32 * i:32 * i + 32, :, i, :] = v[32 * i:32 * i + 32]
            vz.append(z5)
        m['vZ'] = np.stack(vz)
        maps.append(m)
    return blobify(maps, BLOB_E)


def gather_E(results):
    yT = np.zeros((2, 512, SEQ), np.float32); oT = np.zeros((2, 512, SEQ), np.float32)
    for c in range(NCORE):
        h = c; hh = c // 2; vh = c % 2
        for d in range(2):
            for name, dst, r0 in (("ytok", yT, 64 * h), ("otok", oT, 128 * hh + 64 * vh)):
                a = results[c][name][d].transpose(1, 0, 2).reshape(LS, 64)[NCTX:]
                if d == 1:
                    a = a[::-1]
                dst[d, r0:r0 + 64] = a.T
    return yT, oT


def build_M():
    P = Prog()
    Bk = Banks(P)
    di = lambda n, s, dt=F32: P.dram(n, s, dt, "ExternalInput")
    yT = di("yT", [2, 512, TLOC]); oT = di("oT", [2, 512, TLOC]); szT = di("szT", [512, TLOC]); sgT = di("sgT", [512, TLOC])
    bl = Blob(BLOB_M).dram(P)
    nrm = bl.ap("nrm"); onesd = bl.ap("onesd")
    mT = P.dram("mT", [D, TLOC], F32, "ExternalOutput")
    ones_sb = P.sb([128, 128], F32, name="ones"); P.dma(SP, ones_sb[:], onesd[:, :], writes=[ones_sb])
    nrm_sb = P.sb([128, 8], F32, name="nrm"); P.dma(SP, nrm_sb[:], nrm[:, :], writes=[nrm_sb])
    a = [P.sb([128, 4, 512], F32, name=f"ma{i}") for i in range(2)]
    b = [P.sb([128, 4, 512], F32, name=f"mb{i}") for i in range(2)]
    g = [P.sb([128, 4, 512], F32, name=f"mg{i}") for i in range(2)]
    sq = P.sb([128, 4, 512], F32, name="msq"); rs = P.sb([128, 512], F32, name="mrs")
    outs = []
    it = 0
    for part in range(2):
        src = yT if part == 0 else oT
        gsrc = szT if part == 0 else sgT
        for t0 in range(0, TLOC, 512):
            i = it % 2; it += 1
            P.dma(SP, a[i][:], src[0, :, t0:t0 + 512].rearrange("(k p) t -> p k t", p=128), writes=[a[i]])
            P.dma(ACT, b[i][:], src[1, :, t0:t0 + 512].rearrange("(k p) t -> p k t", p=128), writes=[b[i]])
            P.dma(SP, g[i][:], gsrc[:, t0:t0 + 512].rearrange("(k p) t -> p k t", p=128), writes=[g[i]])
            P.I(DVE, "tensor_tensor", [a[i], b[i]], [a[i]], out=a[i][:], in0=a[i][:], in1=b[i][:], op=ALU.add)
            if part == 0:
                P.I(DVE, "tensor_tensor", [a[i], g[i]], [a[i]], out=a[i][:], in0=a[i][:], in1=g[i][:], op=ALU.mult)
            P.I(ACT, "activation", [a[i]], [sq], out=sq[:], in_=a[i][:], func=AF.Square)
            groups = [(0, 2), (2, 4)] if part == 0 else [(0, 1), (1, 2), (2, 3), (3, 4)]
            for (k0, k1) in groups:
                ps = Bk.f[k0 % 2]
                for kc in range(k0, k1):
                    P.I(PE, "matmul", [ones_sb, sq], [ps], ps[:, 0:512], ones_sb[:], sq[:, kc, :], start=(kc == k0), stop=(kc == k1 - 1))
                P.I(DVE, "tensor_scalar", [ps], [rs], out=rs[:], in0=ps[:, 0:512], scalar1=1.0 / (128 * (k1 - k0)), scalar2=EPS, op0=ALU.mult, op1=ALU.add)
                P.I(ACT, "activation", [rs], [rs], out=rs[:], in_=rs[:], func=AF.Ln)
                P.I(ACT, "activation", [rs], [rs], out=rs[:], in_=rs[:], func=AF.Exp, scale=-0.5)
                for kc in range(k0, k1):
                    P.I(DVE, "scalar_tensor_tensor", [a[i], nrm_sb, rs], [b[i]], out=b[i][:, kc, :], in0=a[i][:, kc, :], scalar=nrm_sb[:, part * 4 + kc:part * 4 + kc + 1],
                        in1=rs[:], op0=ALU.mult, op1=ALU.mult)
            if part == 1:
                P.I(DVE, "tensor_tensor", [b[i], g[i]], [b[i]], out=b[i][:], in0=b[i][:], in1=g[i][:], op=ALU.mult)
            outs.append(P.dma(POOL, mT[part * 512:(part + 1) * 512, t0:t0 + 512].rearrange("(k p) t -> p k t", p=128), b[i][:], reads=[b[i]]))
    return P.finish(outs)


def prep_M(inp, yT, oT, dfull):
    nrm = np.concatenate([col_layout(inp['ssd_norm'][0], 4), col_layout(inp['hg_norm'][0], 4)], axis=1)
    maps = []
    for c in range(NCORE):
        sl = slice(c * TLOC, (c + 1) * TLOC)
        maps.append(dict(yT=np.ascontiguousarray(yT[:, :, sl]), oT=np.ascontiguousarray(oT[:, :, sl]),
                         szT=np.ascontiguousarray(dfull[3584:4096, sl]), sgT=np.ascontiguousarray(dfull[4608:5120, sl]),
                         nrm=np.ascontiguousarray(nrm), onesd=np.ones((128, 128), np.float32)))
    return blobify(maps, BLOB_M)


def _run(nc, maps, tag=""):
    import time, sys
    t0 = time.time()
    r = run_bass_kernel_spmd(nc, maps, core_ids=list(range(len(maps)))).results
    print(f"[kernel] launch {tag}: {time.time() - t0:.1f}s", file=sys.stderr, flush=True)
    return r


def kernel(**inp):
    inp = {k: np.asarray(v) for k, v in inp.items()}
    x = inp['x'][0]; ctx = inp['ctx'][0]
    xT = np.ascontiguousarray(x.T); xcT = np.ascontiguousarray(ctx.T)
    rA = _run(build_A(), prep_A(inp), 'A')
    uT = np.concatenate([rA[c]['uT'] for c in range(NCORE)], axis=1)
    attT = np.concatenate([rA[c]['attT'] for c in range(NCORE)], axis=1)
    ucT = rA[0]['ucT']; attcT = rA[0]['attcT']
    hyT = gather_B(_run(build_B(SEQ), prep_B(inp, uT, SEQ), 'B'), SEQ)
    hycT = gather_B(_run(build_B(NCTX), prep_B(inp, ucT, NCTX), 'Bc'), NCTX)
    mT = np.concatenate([hyT, attT], axis=0); mcT = np.concatenate([hycT, attcT], axis=0)
    maps, nlat, nctx = prep_C(inp, 0, mT, xT, mcT, xcT)
    xT, xcT = gather_C(_run(build_C(nlat, nctx, NH_MOE), maps, 'C0'), nlat, nctx)
    dfull, dctx = gather_D(_run(build_D(), prep_D(inp, xT, xcT), 'D'))
    yT, oT = gather_E(_run(build_E(), prep_E(inp, dfull, dctx), 'E'))
    rM = _run(build_M(), prep_M(inp, yT, oT, dfull), 'M')
    mT = np.concatenate([rM[c]['mT'] for c in range(NCORE)], axis=1)
    maps, nlat, nctx = prep_C(inp, 1, mT, xT, None, None)
    xT, _ = gather_C(_run(build_C(nlat, nctx, NH_MOE), maps, 'C1'), nlat, nctx)
    return np.ascontiguousarray(xT.T)[None].astype(np.float32)
```

```python
import contextlib
import numpy as np
import concourse.bass as bass
import concourse.mybir as mybir
from concourse.bass_utils import run_bass_kernel_spmd

F32 = mybir.dt.float32
BF16 = mybir.dt.bfloat16
I32 = mybir.dt.int32
AF = mybir.ActivationFunctionType
ALU = mybir.AluOpType
AX = mybir.AxisListType

PE, DVE, ACT, POOL, SP = "tensor", "vector", "scalar", "gpsimd", "sync"
COMPUTE = (PE, DVE, ACT, POOL)
NDMASEM = 8
EPOCH_LEN = 20000


class Prog:
    def __init__(self):
        self.nc = bass.Bass("TRN2", target_bir_lowering=False)
        self.stack = contextlib.ExitStack()
        self.streams = {e: [] for e in (PE, DVE, ACT, POOL, SP)}
        self.cnt = {e: 0 for e in COMPUTE}
        self.dcnt = {e: 0 for e in (SP, ACT, POOL)}
        self.sem = {}
        self.dsem = {}
        self.waited = {}
        self.lastw = {}
        self.reads = {}
        self.ntens = 0
        self.out_tokens = []
        self.epoch = {e: 0 for e in COMPUTE}
        self.root_stack = self.stack
        for e in COMPUTE:
            self.sem[(e, 0)] = self.stack.enter_context(self.nc.semaphore("s_" + e + "_0"))
        for q in (SP, ACT, POOL):
            self.dsem[q] = [self.stack.enter_context(self.nc.semaphore(f"d_{q}_{i}")) for i in range(NDMASEM)]

    @contextlib.contextmanager
    def scope(self):
        outer = self.stack
        self.stack = contextlib.ExitStack()
        try:
            yield
        finally:
            self.barrier()
            self.stack.close()
            self.stack = outer

    def barrier(self):
        toks = [("c", e, (self.epoch[e], self.cnt[e])) for e in COMPUTE if self.cnt[e] > 0]
        for q in (SP, ACT, POOL):
            for k in range(max(0, self.dcnt[q] - NDMASEM), self.dcnt[q]):
                toks.append(("d", q, k))
        for st in (PE, DVE, ACT, POOL, SP):
            self._emit_waits(st, [t for t in toks if not (t[0] == "c" and t[1] == st)])

    def dram(self, name, shape, dtype, kind):
        return self.nc.dram_tensor(name, list(shape), dtype, kind=kind).ap()

    def sb(self, shape, dtype, name=None):
        self.ntens += 1
        name = "sb_" + (name or f"t{self.ntens}")
        return self.stack.enter_context(self.nc.sbuf_tensor(name, list(shape), dtype))

    def ps(self, shape, dtype=F32, name=None):
        self.ntens += 1
        name = "ps_" + (name or f"p{self.ntens}")
        return self.stack.enter_context(self.nc.psum_tensor(name, list(shape), dtype))

    def _key(self, t):
        if isinstance(t, str):
            return t
        if isinstance(t, tuple):
            return t
        th = getattr(t, "tensor", t)
        return getattr(th, "name", None) or id(th)

    def _tok_sem_val(self, tok):
        kind, e, i = tok
        if kind == "c":
            ep, idx = i
            return self.sem[(e, ep)], idx, ("c", e, ep)
        return self.dsem[e][i % NDMASEM], 16 * (i // NDMASEM + 1), ("d", e, i % NDMASEM)

    def _emit_waits(self, stream, toks):
        need = {}
        for tok in toks:
            if tok is None:
                continue
            s, v, k = self._tok_sem_val(tok)
            if tok[0] == "c" and tok[1] == stream and stream == PE:
                continue
            if k not in need or need[k][1] < v:
                need[k] = (s, v)
        for k, (s, v) in need.items():
            if self.waited.get((stream, k), 0) >= v:
                continue
            self.waited[(stream, k)] = v
            self.streams[stream].append(("wait", s, v))

    def _deps(self, reads, writes):
        toks = []
        for t in reads:
            k = self._key(t)
            toks.append(self.lastw.get(k))
        for t in writes:
            k = self._key(t)
            toks.append(self.lastw.get(k))
            toks.extend(self.reads.get(k, []))
        return toks

    def _commit(self, tok, reads, writes):
        for t in reads:
            k = self._key(t)
            self.reads.setdefault(k, []).append(tok)
            if len(self.reads[k]) > 24:
                best = {}
                for tk in self.reads[k]:
                    kk = (tk[0], tk[1], tk[2][0]) if tk[0] == "c" else tk
                    if kk not in best or best[kk][2] < tk[2]:
                        best[kk] = tk
                self.reads[k] = list(best.values())
        for t in writes:
            k = self._key(t)
            self.lastw[k] = tok
            self.reads[k] = []

    def op(self, eng, fn, reads=(), writes=()):
        self._emit_waits(eng, self._deps(reads, writes))
        if self.cnt[eng] >= EPOCH_LEN:
            self.epoch[eng] += 1
            self.cnt[eng] = 0
            self.sem[(eng, self.epoch[eng])] = self.root_stack.enter_context(self.nc.semaphore(f"s_{eng}_{self.epoch[eng]}"))
        self.cnt[eng] += 1
        tok = ("c", eng, (self.epoch[eng], self.cnt[eng]))
        self.streams[eng].append(("op", fn, self.sem[(eng, self.epoch[eng])], 1))
        self._commit(tok, reads, writes)
        return tok

    def I(self, eng, mname, reads, writes, *args, **kw):
        return self.op(eng, lambda e: getattr(e, mname)(*args, **kw), reads=reads, writes=writes)

    def dma(self, q, out, in_, reads=(), writes=(), **kw):
        k = self.dcnt[q]
        toks = self._deps(reads, writes)
        if k >= NDMASEM:
            toks.append(("d", q, k - NDMASEM))
        self._emit_waits(q, toks)
        self.dcnt[q] += 1
        tok = ("d", q, k)
        self.streams[q].append(("op", lambda e: e.dma_start(out=out, in_=in_, **kw), self.dsem[q][k % NDMASEM], 16))
        self._commit(tok, reads, writes)
        return tok

    def finish(self, final_toks):
        self._emit_waits(SP, final_toks)
        nc = self.nc
        streams = self.streams

        def run(engine, lst):
            for it in lst:
                if it[0] == "wait":
                    engine.wait_ge(it[1], it[2])
                else:
                    it[1](engine).then_inc(it[2], it[3])

        with nc.Block() as block:
            @block.sync
            def _(e):
                run(e, streams[SP])

            @block.tensor
            def _(e):
                run(e, streams[PE])

            @block.vector
            def _(e):
                run(e, streams[DVE])

            @block.scalar
            def _(e):
                run(e, streams[ACT])

            @block.gpsimd
            def _(e):
                run(e, streams[POOL])
        self.stack.close()
        return nc


D = 1024
SEQ = 16384
NCORE = 8
TLOC = SEQ // NCORE
HALO = 128
TEXT = TLOC + 2 * HALO
NCTX = 256
EPS = 1e-6
MASKNEG = -30000.0


def _bcast_free(ap2d, n):
    return ap2d.unsqueeze(2).broadcast_to([ap2d.shape[0], ap2d.shape[1], n])


class Blob:
    def __init__(self, items):
        self.items = {}
        o = 0
        for name, rows, cols in items:
            self.items[name] = (rows, o, cols); o += cols
        self.total = o
        self.t = None

    def dram(self, P):
        self.t = P.dram("blob", [128, self.total], F32, "ExternalInput")
        return self

    def ap(self, name):
        rows, o, cols = self.items[name]
        return self.t[0:rows, o:o + cols]

    def pack(self, d):
        out = np.zeros((128, self.total), np.float32)
        for name, (rows, o, cols) in self.items.items():
            out[0:rows, o:o + cols] = np.asarray(d[name], np.float32).reshape(rows, cols)
        return out


BLOB_A = [("cvec", 128, 16), ("adab", 128, 24), ("normg", 128, 8), ("gains", 128, 640), ("masks", 128, 512), ("sinkrow", 128, 1024),
          ("ident", 128, 128), ("onesd", 128, 128), ("convw", 128, 36), ("convb", 128, 12), ("edge", 128, 2)]
BLOB_C = [("cvec", 128, 16), ("adab", 128, 32), ("normg", 128, 8), ("rw", 128, 256), ("rb", 128, 32), ("bgu", 128, 512),
          ("ident", 128, 128), ("onesd", 128, 128)]
BLOB_B = [("w1", 33, 64), ("w2", 64, 64), ("w3", 64, 64), ("w4s", 64, 256), ("fqb", 64, 4), ("negd", 128, 1), ("fbias", 128, 128),
          ("ident", 128, 128), ("onesd", 128, 128), ("jmat", 128, 128)]
BLOB_D = [("cvec", 128, 16), ("adab", 128, 24), ("normg", 128, 8), ("onesd", 128, 128), ("convw", 128, 24), ("convb", 128, 8),
          ("edge", 128, 2), ("hglb", 128, 8), ("dtb", 16, 1)]
BLOB_E = [("ssdp", 128, 4), ("U", 128, 128), ("U4", 128, 128), ("Mneg", 128, 128), ("ident", 128, 128), ("onesd", 128, 128)]
BLOB_M = [("nrm", 128, 8), ("onesd", 128, 128)]


def blobify(maps, spec):
    bl = Blob(spec)
    out = []
    for m in maps:
        m2 = {k: v for k, v in m.items() if k not in bl.items}
        m2['blob'] = bl.pack(m)
        out.append(m2)
    return out


class Banks:
    def __init__(self, P, nb16=1):
        self.f = [P.ps([128, 512], F32, name=f"bank{i}") for i in range(8 - nb16)]
        self.h = [P.ps([128, 1024], BF16, name=f"bankh{i}") for i in range(nb16)]


def emit_adaln(P, B, cv_sb, adaw_dram, adab_sb, wbuf, out_sb, ncol, nparts=3):
    sc = P.sb([128, 8, ncol], F32, name="ada_silu")
    P.op(ACT, lambda e: e.activation(out=sc[:], in_=cv_sb[:], func=AF.Silu), reads=[cv_sb], writes=[sc])
    ps = B.f[0]
    for part in range(nparts):
        for kc in range(8):
            P.dma(SP if kc % 2 == 0 else ACT, wbuf[:, kc, :], adaw_dram[kc * 128:(kc + 1) * 128, part * 1024:(part + 1) * 1024],
                  writes=[wbuf])
        for o in range(8):
            oc = part * 8 + o
            for kc in range(8):
                P.op(PE, lambda e, o=o, kc=kc, oc=oc: e.matmul(ps[:, oc * ncol:(oc + 1) * ncol], wbuf[:, kc, o * 128:(o + 1) * 128],
                                                               sc[:, kc, :], start=(kc == 0), stop=(kc == 7)),
                     reads=[wbuf, sc], writes=[ps])
    P.op(DVE, lambda e: e.tensor_tensor(out=out_sb[:], in0=ps[:, 0:8 * nparts * ncol].rearrange("p (o n) -> p o n", n=ncol),
                                        in1=_bcast_free(adab_sb[:], ncol), op=ALU.add),
         reads=[ps, adab_sb], writes=[out_sb])


def emit_norm_mod(P, B, x_sb, ones_sb, A_col, B_col, h_out, ntok, tmp_sq, tmp_rs, hcol0=0):
    ps = B.f[1]
    for kc in range(8):
        P.op(ACT, lambda e, kc=kc: e.activation(out=tmp_sq[:, kc, 0:ntok], in_=x_sb[:, kc, 0:ntok], func=AF.Square),
             reads=[x_sb], writes=[tmp_sq])
    for kc in range(8):
        P.op(PE, lambda e, kc=kc: e.matmul(ps[:, 0:ntok], ones_sb[:], tmp_sq[:, kc, 0:ntok], start=(kc == 0), stop=(kc == 7)),
             reads=[ones_sb, tmp_sq], writes=[ps])
    P.op(DVE, lambda e: e.tensor_scalar(out=tmp_rs[:, 0:ntok], in0=ps[:, 0:ntok], scalar1=1.0 / D, scalar2=EPS,
                                        op0=ALU.mult, op1=ALU.add), reads=[ps], writes=[tmp_rs])
    P.op(ACT, lambda e: e.activation(out=tmp_rs[:, 0:ntok], in_=tmp_rs[:, 0:ntok], func=AF.Ln), reads=[tmp_rs], writes=[tmp_rs])
    P.op(ACT, lambda e: e.activation(out=tmp_rs[:, 0:ntok], in_=tmp_rs[:, 0:ntok], func=AF.Exp, scale=-0.5), reads=[tmp_rs], writes=[tmp_rs])
    for kc in range(8):
        P.op(DVE, lambda e, kc=kc: e.tensor_tensor(out=tmp_sq[:, kc, 0:ntok], in0=x_sb[:, kc, 0:ntok], in1=tmp_rs[:, 0:ntok],
                                                   op=ALU.mult), reads=[x_sb, tmp_rs, tmp_sq], writes=[tmp_sq])
        P.op(ACT, lambda e, kc=kc: e.activation(out=h_out[:, kc, hcol0:hcol0 + ntok], in_=tmp_sq[:, kc, 0:ntok], func=AF.Identity,
                                                bias=B_col[:, kc:kc + 1], scale=A_col[:, kc:kc + 1]),
             reads=[tmp_sq, A_col, B_col], writes=[h_out])


TT = 384
NTT = TEXT // TT


def build_A():
    P = Prog()
    Bk = Banks(P)
    di = lambda n, s, dt=F32: P.dram(n, s, dt, "ExternalInput")
    do = lambda n, s, dt=F32: P.dram(n, s, dt, "ExternalOutput")
    xT = di("xT", [D, TEXT]); ctxT = di("ctxT", [D, NCTX])
    bl = Blob(BLOB_A).dram(P)
    cvec = bl.ap("cvec"); adaw = di("adaw", [D, 3072]); adab = bl.ap("adab")
    normg = bl.ap("normg"); w_in = di("w_in", [D, 2304])
    gains = bl.ap("gains"); ctab = di("ctab", [TEXT, 64]); stab = di("stab", [TEXT, 64])
    masks = bl.ap("masks"); sinkrow = bl.ap("sinkrow")
    ident = bl.ap("ident"); onesd = bl.ap("onesd")
    convw = bl.ap("convw"); convb = bl.ap("convb"); edge = bl.ap("edge")
    uT = do("uT", [1536, TLOC]); ucT = do("ucT", [1536, NCTX])
    attT = do("attT", [512, TLOC]); attcT = do("attcT", [512, NCTX])

    ones_sb = P.sb([128, 128], F32, name="ones"); P.dma(SP, ones_sb[:], onesd[:, :], writes=[ones_sb])
    id_f = P.sb([128, 128], F32, name="idf"); P.dma(SP, id_f[:], ident[:, :], writes=[id_f])
    id_b = P.sb([128, 128], BF16, name="idb"); P.dma(POOL, id_b[:], ident[:, :], writes=[id_b])
    cv = P.sb([128, 8, 2], F32, name="cv"); P.dma(SP, cv[:], cvec.rearrange("p (k n) -> p k n", n=2), writes=[cv])
    adab_sb = P.sb([128, 24], F32, name="adab"); P.dma(SP, adab_sb[:], adab[:, :], writes=[adab_sb])
    g_sb = P.sb([128, 8], F32, name="normg"); P.dma(SP, g_sb[:], normg[:, :], writes=[g_sb])
    gains_sb = P.sb([128, 640], F32, name="gains"); P.dma(SP, gains_sb[:], gains[:, :], writes=[gains_sb])
    mask_sb = P.sb([128, 4, 128], BF16, name="masks")
    P.dma(POOL, mask_sb[:], masks.rearrange("k (m q) -> k m q", m=4), writes=[mask_sb])
    sink_sb = P.sb([128, 1024], F32, name="sink"); P.dma(SP, sink_sb[:], sinkrow[:, :], writes=[sink_sb])
    P.op(ACT, lambda e: e.activation(out=sink_sb[:], in_=sink_sb[:], func=AF.Exp), reads=[sink_sb], writes=[sink_sb])
    w_sb = P.sb([128, 8, 2304], BF16, name="w_in")
    for kc in range(8):
        P.dma(POOL, w_sb[:, kc, :], w_in[kc * 128:(kc + 1) * 128, :], writes=[w_sb])

    ada = P.sb([128, 24, 2], F32, name="ada")
    with P.scope():
        wbuf = P.sb([128, 8, 1024], F32, name="adawbuf")
        emit_adaln(P, Bk, cv, adaw, adab_sb, wbuf, ada, 2)
    Acol = [P.sb([128, 8], F32, name=f"Acol{j}") for j in range(2)]
    Bcol = [P.sb([128, 8], F32, name=f"Bcol{j}") for j in range(2)]
    for j in range(2):
        P.op(DVE, lambda e, j=j: e.scalar_tensor_tensor(out=Acol[j][:], in0=ada[:, 8:16, j], scalar=1.0, in1=g_sb[:],
                                                         op0=ALU.add, op1=ALU.mult), reads=[ada, g_sb], writes=[Acol[j]])
        P.op(DVE, lambda e, j=j: e.tensor_copy(out=Bcol[j][:], in_=ada[:, 0:8, j]), reads=[ada], writes=[Bcol[j]])

    kqT = P.sb([64, 10, TEXT + NCTX], BF16, name="kqT")
    vaug = P.sb([128, (TEXT + NCTX) // 128, 2, 65], BF16, name="vaug")
    P.op(POOL, lambda e: e.memset(vaug[:], 1.0), writes=[vaug])

    x_sb = P.sb([128, 8, TT], F32, name="x_sb")
    sq_sb = P.sb([128, 8, TT], F32, name="sq_sb")
    rs_sb = P.sb([128, TT], F32, name="rs_sb")
    h_all = P.sb([128, 8, TEXT + NCTX], BF16, name="h_all")
    cw_sb = P.sb([128, 12, 3], F32, name="cw"); P.dma(SP, cw_sb[:], convw.rearrange("p (o t) -> p o t", t=3), writes=[cw_sb])
    cb_sb = P.sb([128, 12], F32, name="cb"); P.dma(SP, cb_sb[:], convb[:, :], writes=[cb_sb])
    edge_sb = P.sb([128, 2], F32, name="edge"); P.dma(SP, edge_sb[:], edge[:, :], writes=[edge_sb])
    kqv = P.sb([128, 768], F32, name="kqv")
    sq2 = P.sb([128, 640], F32, name="sq2")
    ss = P.sb([128, 10], F32, name="ss")
    tmpr = P.sb([128, 640], F32, name="tmpr")
    kqb = P.sb([128, 640], BF16, name="kqb")
    ct_sb = P.sb([128, 64], F32, name="ct"); st_sb = P.sb([128, 64], F32, name="st")
    u_sb = [P.sb([128, 512], F32, name=f"u_sb{i}") for i in range(2)]
    acc_sb = [P.sb([128, 512], F32, name=f"acc_sb{i}") for i in range(2)]

    def proj_tile(src_dram, col0, ntok, j, tok_base, is_ctx):
        for kc in range(8):
            P.dma(SP if kc % 2 == 0 else ACT, x_sb[:, kc, 0:ntok], src_dram[kc * 128:(kc + 1) * 128, col0:col0 + ntok], writes=[x_sb])
        emit_norm_mod(P, Bk, x_sb, ones_sb, Acol[j], Bcol[j], h_all, ntok, sq_sb, rs_sb, hcol0=tok_base)
        for s in range(ntok // 128):
            pa, pb = Bk.f[2], Bk.f[3]
            for kc in range(8):
                P.op(PE, lambda e, kc=kc, s=s: e.matmul(pa[:, 0:512], h_all[:, kc, tok_base + s * 128:tok_base + (s + 1) * 128], w_sb[:, kc, 0:512],
                                                        start=(kc == 0), stop=(kc == 7)), reads=[h_all, w_sb], writes=[pa])
            for kc in range(8):
                P.op(PE, lambda e, kc=kc, s=s: e.matmul(pb[:, 0:256], h_all[:, kc, tok_base + s * 128:tok_base + (s + 1) * 128], w_sb[:, kc, 512:768],
                                                        start=(kc == 0), stop=(kc == 7)), reads=[h_all, w_sb], writes=[pb])
            P.op(ACT, lambda e: e.activation(out=kqv[:, 0:512], in_=pa[:, 0:512], func=AF.Copy), reads=[pa], writes=[kqv])
            P.op(ACT, lambda e: e.activation(out=kqv[:, 512:768], in_=pb[:, 0:256], func=AF.Copy), reads=[pb], writes=[kqv])
            tile_idx = (tok_base + s * 128) // 128
            P.op(POOL, lambda e, ti=tile_idx: e.tensor_copy(out=vaug[:, ti, :, 0:64], in_=kqv[:, 640:768].rearrange("p (g d) -> p g d", d=64)),
                 reads=[kqv], writes=[vaug])
            P.op(DVE, lambda e: e.tensor_tensor(out=sq2[:], in0=kqv[:, 0:640], in1=kqv[:, 0:640], op=ALU.mult), reads=[kqv], writes=[sq2])
            P.op(DVE, lambda e: e.tensor_reduce(out=ss[:], in_=sq2[:].rearrange("p (h d) -> p h d", d=64), axis=AX.X, op=ALU.add),
                 reads=[sq2], writes=[ss])
            P.op(DVE, lambda e: e.tensor_scalar(out=ss[:], in0=ss[:], scalar1=1.0 / 64, scalar2=EPS, op0=ALU.mult, op1=ALU.add),
                 reads=[ss], writes=[ss])
            P.op(ACT, lambda e: e.activation(out=ss[:], in_=ss[:], func=AF.Ln), reads=[ss], writes=[ss])
            P.op(ACT, lambda e: e.activation(out=ss[:], in_=ss[:], func=AF.Exp, scale=-0.5), reads=[ss], writes=[ss])
            P.op(DVE, lambda e: e.tensor_tensor(out=sq2[:].rearrange("p (h d) -> p h d", d=64), in0=kqv[:, 0:640].rearrange("p (h d) -> p h d", d=64),
                                                in1=_bcast_free(ss[:], 64), op=ALU.mult), reads=[kqv, ss], writes=[sq2])
            P.op(DVE, lambda e: e.tensor_tensor(out=sq2[:], in0=sq2[:], in1=gains_sb[:], op=ALU.mult), reads=[sq2, gains_sb], writes=[sq2])
            if not is_ctx:
                r0 = col0 + s * 128
                P.dma(SP, ct_sb[:], ctab[r0:r0 + 128, :], writes=[ct_sb])
                P.dma(ACT, st_sb[:], stab[r0:r0 + 128, :], writes=[st_sb])
                v5 = lambda t: t[:].rearrange("p (h b two s) -> p (h b) two s", b=2, two=2, s=16)
                stv = st_sb[:].rearrange("p (b two s) -> p b two s", two=2, s=16)
                ctv = ct_sb[:].rearrange("p (b two s) -> p b two s", two=2, s=16)

                def bc(tv, two):
                    a = tv[:, :, two, :]
                    return a.unsqueeze(1).broadcast_to([128, 10, 2, 16])
                u4 = sq2[:].rearrange("p (h b two s) -> p h b two s", b=2, two=2, s=16)
                t4 = tmpr[:].rearrange("p (h b two s) -> p h b two s", b=2, two=2, s=16)
                for two in range(2):
                    P.op(DVE, lambda e, two=two: e.tensor_tensor(out=t4[:, :, :, two, :], in0=u4[:, :, :, 1 - two, :], in1=bc(stv, two), op=ALU.mult),
                         reads=[sq2, st_sb], writes=[tmpr])
                for two in range(2):
                    P.op(DVE, lambda e, two=two: e.tensor_tensor(out=u4[:, :, :, two, :], in0=u4[:, :, :, two, :], in1=bc(ctv, two), op=ALU.mult),
                         reads=[sq2, ct_sb], writes=[sq2])
                P.op(DVE, lambda e: e.tensor_tensor(out=kqb[:], in0=sq2[:], in1=tmpr[:], op=ALU.add), reads=[sq2, tmpr], writes=[kqb])
            else:
                P.op(DVE, lambda e: e.tensor_copy(out=kqb[:], in_=sq2[:]), reads=[sq2], writes=[kqb])
            pt = Bk.h[0]
            for hh in range(10):
                P.op(PE, lambda e, hh=hh: e.transpose(pt[0:64, hh * 128:(hh + 1) * 128][:, 0:128] if False else pt[0:64, hh * 128 % 1024:(hh * 128 % 1024) + 128],
                                                      kqb[:, hh * 64:(hh + 1) * 64], id_b[:]),
                     reads=[kqb, id_b], writes=[pt])
                if hh == 7 or hh == 9:
                    h0 = 0 if hh == 7 else 8
                    nh = hh - h0 + 1
                    t0 = tok_base + s * 128
                    P.op(ACT, lambda e, h0=h0, nh=nh, t0=t0: e.activation(
                        out=kqT[:, h0:h0 + nh, t0:t0 + 128],
                        in_=pt[0:64, (h0 * 128) % 1024:(h0 * 128) % 1024 + nh * 128].rearrange("p (h t) -> p h t", t=128), func=AF.Copy),
                        reads=[pt], writes=[kqT])

    def hyena_tile(h0, n, out_dram, ocol0, zl, zr, el, er):
        a0 = h0 - (0 if zl else 1); a1 = h0 + n + (0 if zr else 1)
        w = a1 - a0
        off = 1 if zl else 0
        for oc in range(12):
            pu = Bk.f[4 + oc % 2]
            ub = u_sb[oc % 2]; ac = acc_sb[oc % 2]
            for kc in range(8):
                P.I(PE, "matmul", [w_sb, h_all], [pu], pu[:, off:off + w], w_sb[:, kc, 768 + oc * 128:768 + (oc + 1) * 128], h_all[:, kc, a0:a1],
                    start=(kc == 0), stop=(kc == 7))
            if zl:
                P.I(POOL, "memset", [], [ub], ub[:, 0:1], 0.0)
            if zr:
                P.I(POOL, "memset", [], [ub], ub[:, n + 1:n + 2], 0.0)
            P.I(ACT, "activation", [pu], [ub], out=ub[:, off:off + w], in_=pu[:, off:off + w], func=AF.Copy)
            if el:
                P.I(DVE, "tensor_scalar", [ub, edge_sb], [ub], out=ub[:, 0:1], in0=ub[:, 0:1], scalar1=edge_sb[:, 0:1], scalar2=None, op0=ALU.mult)
            if er:
                P.I(DVE, "tensor_scalar", [ub, edge_sb], [ub], out=ub[:, n + 1:n + 2], in0=ub[:, n + 1:n + 2], scalar1=edge_sb[:, 1:2], scalar2=None, op0=ALU.mult)
            P.I(DVE, "tensor_scalar", [ub, cw_sb, cb_sb], [ac], out=ac[:, 0:n], in0=ub[:, 1:n + 1], scalar1=cw_sb[:, oc, 1:2], scalar2=cb_sb[:, oc:oc + 1], op0=ALU.mult, op1=ALU.add)
            P.I(DVE, "scalar_tensor_tensor", [ub, cw_sb, ac], [ac], out=ac[:, 0:n], in0=ub[:, 0:n], scalar=cw_sb[:, oc, 0:1], in1=ac[:, 0:n], op0=ALU.mult, op1=ALU.add)
            P.I(DVE, "scalar_tensor_tensor", [ub, cw_sb, ac], [ac], out=ac[:, 0:n], in0=ub[:, 2:n + 2], scalar=cw_sb[:, oc, 2:3], in1=ac[:, 0:n], op0=ALU.mult, op1=ALU.add)
            outs.append(P.dma(POOL, out_dram[oc * 128:(oc + 1) * 128, ocol0:ocol0 + n], ac[:, 0:n], reads=[ac]))

    outs = []
    for t in range(NTT):
        proj_tile(xT, t * TT, TT, 0, t * TT, False)
    proj_tile(ctxT, 0, NCTX, 1, TEXT, True)
    lo = 0
    while lo < TLOC:
        n = min(510, TLOC - lo)
        hyena_tile(HALO + lo, n, uT, lo, False, False, lo == 0, lo + n == TLOC)
        lo += n
    hyena_tile(TEXT, NCTX, ucT, 0, True, True, False, False)

    NB = TLOC // 128
    ctx_tiles = [TEXT // 128, TEXT // 128 + 1]
    pT = [P.sb([128, 512], BF16, name=f"pT{i}") for i in range(2)]
    o_sb = P.sb([64, 512], F32, name="o_sb"); rden = P.sb([65, 512], F32, name="rden")
    of_sb = P.sb([64, 512], F32, name="of_sb")
    ones_b = P.sb([128, 64], F32, name="ones_b")
    P.op(POOL, lambda e: e.memset(ones_b[:], 1.0), writes=[ones_b])

    def attend(qtile, key_tiles, key_masks, out_dram, out_col0):
        for g in range(2):
            po = Bk.f[2]
            nkt = len(key_tiles)
            for ci, (kt, mk) in enumerate(zip(key_tiles, key_masks)):
                psc = Bk.f[ci % 2]
                pTb = pT[ci % 2]
                qv = kqT[:, 2 + 4 * g:2 + 4 * g + 4, qtile * 128:(qtile + 1) * 128]
                P.op(PE, lambda e, psc=psc, kt=kt, qv=qv, mk=mk, g=g: e.matmul(psc[:, 0:512], kqT[:, g, kt * 128:(kt + 1) * 128], qv,
                                                                         start=True, stop=(mk is None)), reads=[kqT], writes=[psc])
                if mk is not None:
                    for hh in range(4):
                        P.op(PE, lambda e, psc=psc, hh=hh, mk=mk: e.matmul(psc[:, hh * 128:(hh + 1) * 128], id_b[:], mask_sb[:, mk, :],
                                                                           start=False, stop=(hh == 3)), reads=[id_b, mask_sb], writes=[psc])
                P.op(ACT, lambda e, psc=psc, pTb=pTb: e.activation(out=pTb[:], in_=psc[:, 0:512], func=AF.Exp, scale=0.125),
                     reads=[psc], writes=[pTb])
                P.op(PE, lambda e, pTb=pTb, kt=kt, ci=ci, g=g: e.matmul(po[0:65, 0:512], vaug[:, kt, g, :], pTb[:], start=(ci == 0), stop=(ci == nkt - 1)),
                     reads=[vaug, pTb], writes=[po])
            P.op(DVE, lambda e, g=g: e.tensor_tensor(out=rden[64:65, :], in0=po[64:65, 0:512], in1=sink_sb[64:65, g * 512:(g + 1) * 512], op=ALU.add),
                 reads=[po, sink_sb], writes=[rden])
            P.op(DVE, lambda e: e.reciprocal(out=rden[64:65, :], in_=rden[64:65, :]), reads=[rden], writes=[rden])
            P.op(ACT, lambda e: e.activation(out=o_sb[:], in_=po[0:64, 0:512], func=AF.Copy), reads=[po], writes=[o_sb])
            pb = Bk.f[3]
            P.op(PE, lambda e: e.matmul(pb[0:64, 0:512], ones_b[64:65, :], rden[64:65, :], start=True, stop=True), reads=[ones_b, rden], writes=[pb])
            P.op(DVE, lambda e: e.tensor_tensor(out=of_sb[:], in0=o_sb[:], in1=pb[0:64, 0:512], op=ALU.mult), reads=[o_sb, pb], writes=[of_sb])
            for hh in range(4):
                r0 = (4 * g + hh) * 64
                outs.append(P.dma(POOL if hh % 2 == 0 else SP, out_dram[r0:r0 + 64, out_col0:out_col0 + 128], of_sb[:, hh * 128:(hh + 1) * 128], reads=[of_sb]))

    for b in range(NB):
        qt = b + 1
        mprev = 0 if b == 0 else 1
        mnext = 3 if b == NB - 1 else 2
        attend(qt, [qt - 1, qt, qt + 1] + ctx_tiles, [mprev, None, mnext, None, None], attT, b * 128)
    for cb in range(2):
        attend(ctx_tiles[cb], ctx_tiles, [None, None], attcT, cb * 128)
    return P.finish(outs)


def col_layout(v, nchunk):
    return np.ascontiguousarray(np.asarray(v, np.float32).reshape(nchunk, 128).T)


def rope_tables(tok_idx):
    inv = (10000.0 ** (-np.arange(16, dtype=np.float32) / 16)).astype(np.float32)
    t = np.asarray(tok_idx)
    row = (t // 64).astype(np.float32)[:, None] * inv
    col = (t % 64).astype(np.float32)[:, None] * inv
    cr, sr, cc, sc_ = np.cos(row), np.sin(row), np.cos(col), np.sin(col)
    C = np.concatenate([cr, cr, cc, cc], axis=1).astype(np.float32)
    S = np.concatenate([-sr, sr, -sc_, sc_], axis=1).astype(np.float32)
    return C, S


def band_masks(core):
    j = np.arange(128)[:, None]; i = np.arange(128)[None, :]
    prev = np.where(j >= i, 0.0, MASKNEG).astype(np.float32)
    nxt = np.where(j <= i, 0.0, MASKNEG).astype(np.float32)
    allneg = np.full((128, 128), MASKNEG, np.float32)
    return np.stack([allneg if core == 0 else prev, prev, nxt, allneg if core == NCORE - 1 else nxt])


def prep_A(inp, layer=0):
    x = inp['x'][0]; ctx = inp['ctx'][0]
    xT = np.ascontiguousarray(x.T)
    xTp = np.concatenate([np.zeros((D, HALO), np.float32), xT, np.zeros((D, HALO), np.float32)], axis=1)
    w = inp['w_in_even'][0]
    w_perm = np.ascontiguousarray(np.concatenate([w[:, 0:128], w[:, 256:768], w[:, 128:256], w[:, 768:]], axis=1))
    gains = np.concatenate([np.tile(inp['att_k_norm'][0], 2), np.tile(inp['att_q_norm'][0], 8)])
    gains = np.ascontiguousarray(np.broadcast_to(gains[None, :], (128, 640))).astype(np.float32)
    sink = inp['att_sink'][0]
    sinkrow = np.ascontiguousarray(np.broadcast_to(np.repeat(sink, 128)[None, :], (128, 1024))).astype(np.float32)
    cvec = np.stack([col_layout(inp['c'][0], 8), col_layout(inp['c_ctx'], 8)], axis=2).reshape(128, 16)
    common = dict(
        ctxT=np.ascontiguousarray(ctx.T), cvec=np.ascontiguousarray(cvec),
        adaw=np.ascontiguousarray(inp['ada_w'][layer][:, 0:3072]), adab=col_layout(inp['ada_b'][layer][0:3072], 24),
        normg=col_layout(inp['norm_g'][layer, 0], 8), w_in=w_perm, gains=gains, sinkrow=sinkrow,
        ident=np.eye(128, dtype=np.float32), onesd=np.ones((128, 128), np.float32),
        convw=np.ascontiguousarray(inp['hy_conv_w'][0].reshape(3, 12, 128).transpose(2, 1, 0).reshape(128, 36)),
        convb=col_layout(inp['hy_conv_b'][0], 12))
    maps = []
    for c in range(NCORE):
        s0 = c * TLOC
        C, S = rope_tables(np.arange(s0 - HALO, s0 + TLOC + HALO).clip(0, SEQ - 1))
        m = dict(common)
        edge = np.ones((128, 2), np.float32); edge[:, 0] = 0.0 if c == 0 else 1.0; edge[:, 1] = 0.0 if c == NCORE - 1 else 1.0
        m.update(xT=np.ascontiguousarray(xTp[:, s0:s0 + TEXT]), ctab=C, stab=S, edge=edge,
                 masks=np.ascontiguousarray(band_masks(c).transpose(1, 0, 2).reshape(128, 512)))
        maps.append(m)
    return blobify(maps, BLOB_A)


NEXP = 32
NC_MOE = 4
NH_MOE = 16


def moe_passes(ncore, has_ctx):
    nlat_pass = (SEQ // ncore) // 1024
    passes = [[(0, 512, 0), (512, 512, 0)] for _ in range(nlat_pass)]
    if has_ctx:
        passes.append([(0, NCTX // ncore, 1)])
    return passes


def build_C(passes):
    TH = 1024
    nhalf = len(passes)
    P = Prog()
    Bk = Banks(P)
    di = lambda n, s, dt=F32: P.dram(n, s, dt, "ExternalInput")
    mT = di("mT", [nhalf, D, TH]); xT = di("xT", [nhalf, D, TH])
    bl = Blob(BLOB_C).dram(P)
    cvec = bl.ap("cvec"); adaw = di("adaw", [D, 4096]); adab = bl.ap("adab")
    normg = bl.ap("normg"); w_out = di("w_out", [D, D])
    rw = bl.ap("rw"); rb = bl.ap("rb")
    w_gu = di("w_gu", [NEXP, D, 2048]); bgu = bl.ap("bgu")
    w_dn = di("w_dn", [NEXP, D, D]); bdn = di("bdn", [NEXP, D])
    ident = bl.ap("ident"); onesd = bl.ap("onesd")
    outT = P.dram("outT", [nhalf, D, TH], F32, "ExternalOutput")
    gt_dram = P.dram("gt_scratch", [nhalf, NEXP, TH], F32, "Internal")

    ones_sb = P.sb([128, 128], F32, name="ones"); P.dma(SP, ones_sb[:], onesd[:, :], writes=[ones_sb])
    id_f = P.sb([128, 128], F32, name="idf"); P.dma(SP, id_f[:], ident[:, :], writes=[id_f])
    cv = P.sb([128, 8, 2], F32, name="cv"); P.dma(SP, cv[:], cvec.rearrange("p (k n) -> p k n", n=2), writes=[cv])
    adab_sb = P.sb([128, 32], F32, name="adab"); P.dma(SP, adab_sb[:], adab[:, :], writes=[adab_sb])
    g_sb = P.sb([128, 8], F32, name="normg"); P.dma(SP, g_sb[:], normg[:, :], writes=[g_sb])
    rw_sb = P.sb([128, 8, NEXP], F32, name="rw"); P.dma(SP, rw_sb[:], rw.rearrange("p (k e) -> p k e", e=NEXP), writes=[rw_sb])
    rb_sb = P.sb([128, NEXP], F32, name="rb"); P.dma(SP, rb_sb[:], rb[:, :], writes=[rb_sb])
    bgu_sb = P.sb([128, NEXP, 16], F32, name="bgu"); P.dma(SP, bgu_sb[:], bgu.rearrange("p (e o) -> p e o", o=16), writes=[bgu_sb])
    bdn_sb = P.sb([NEXP, D], F32, name="bdn"); P.dma(SP, bdn_sb[:], bdn[:, :], writes=[bdn_sb])
    ada = P.sb([128, 32, 2], F32, name="ada")
    with P.scope():
        wbuf = P.sb([128, 8, 1024], F32, name="adawbuf")
        emit_adaln(P, Bk, cv, adaw, adab_sb, wbuf, ada, 2, nparts=4)
    Acol = [P.sb([128, 8], F32, name=f"Acol{j}") for j in range(2)]
    Bcol = [P.sb([128, 8], F32, name=f"Bcol{j}") for j in range(2)]
    G0 = [P.sb([128, 8], F32, name=f"G0{j}") for j in range(2)]
    G1 = [P.sb([128, 8], F32, name=f"G1{j}") for j in range(2)]
    for j in range(2):
        P.I(DVE, "scalar_tensor_tensor", [ada, g_sb], [Acol[j]], out=Acol[j][:], in0=ada[:, 16:24, j], scalar=1.0, in1=g_sb[:], op0=ALU.add, op1=ALU.mult)
        P.I(DVE, "tensor_copy", [ada], [Bcol[j]], out=Bcol[j][:], in_=ada[:, 8:16, j])
        P.I(DVE, "tensor_copy", [ada], [G0[j]], out=G0[j][:], in_=ada[:, 0:8, j])
        P.I(DVE, "tensor_copy", [ada], [G1[j]], out=G1[j][:], in_=ada[:, 24:32, j])

    x1 = P.sb([128, 8, TH], F32, name="x1")
    hT = P.sb([128, 8, TH], BF16, name="hT")
    GT = P.sb([NEXP, TH], F32, name="GT")
    outs = []
    gu_cnt = [0]; dn_cnt = [0]; ch_cnt = [0]

    for hf in range(nhalf):
        tiles = passes[hf]
        with P.scope():
            mb = P.sb([128, 8, TH], BF16, name=f"mb{hf}")
            wo = P.sb([128, 8, D], BF16, name=f"wo{hf}")
            for kc in range(8):
                P.dma(POOL, mb[:, kc, :], mT[hf, kc * 128:(kc + 1) * 128, :], writes=[mb])
                P.dma(POOL, wo[:, kc, :], w_out[kc * 128:(kc + 1) * 128, :], writes=[wo])
                P.dma(SP if kc % 2 == 0 else ACT, x1[:, kc, :], xT[hf, kc * 128:(kc + 1) * 128, :], writes=[x1])
            for (c0, n, j) in tiles:
                for oc in range(8):
                    ps = Bk.f[oc % 2]
                    for kc in range(8):
                        P.I(PE, "matmul", [wo, mb], [ps], ps[:, 0:n], wo[:, kc, oc * 128:(oc + 1) * 128], mb[:, kc, c0:c0 + n], start=(kc == 0), stop=(kc == 7))
                    P.I(DVE, "scalar_tensor_tensor", [ps, G0[j], x1], [x1], out=x1[:, oc, c0:c0 + n], in0=ps[:, 0:n], scalar=G0[j][:, oc:oc + 1],
                        in1=x1[:, oc, c0:c0 + n], op0=ALU.mult, op1=ALU.add)
        with P.scope():
            sq_sb = P.sb([128, 8, 512], F32, name=f"sq{hf}")
            rs_sb = P.sb([128, 512], F32, name=f"rs{hf}")
            hf32 = P.sb([128, 8, 512], F32, name=f"hf32{hf}")
            lg = P.sb([128, NEXP], F32, name=f"lg{hf}"); mx = P.sb([128, 8], F32, name=f"mx{hf}")
            msk = P.sb([128, NEXP], F32, name=f"msk{hf}"); ex = P.sb([128, NEXP], F32, name=f"ex{hf}")
            sm = P.sb([128, 1], F32, name=f"sm{hf}"); nm = P.sb([128, 1], F32, name=f"nm{hf}")
            for (c0, n, j) in tiles:
                ps = Bk.f[6]
                for kc in range(8):
                    P.I(ACT, "activation", [x1], [sq_sb], out=sq_sb[:, kc, 0:n], in_=x1[:, kc, c0:c0 + n], func=AF.Square)
                for kc in range(8):
                    P.I(PE, "matmul", [ones_sb, sq_sb], [ps], ps[:, 0:n], ones_sb[:], sq_sb[:, kc, 0:n], start=(kc == 0), stop=(kc == 7))
                P.I(DVE, "tensor_scalar", [ps], [rs_sb], out=rs_sb[:, 0:n], in0=ps[:, 0:n], scalar1=1.0 / D, scalar2=EPS, op0=ALU.mult, op1=ALU.add)
                P.I(ACT, "activation", [rs_sb], [rs_sb], out=rs_sb[:, 0:n], in_=rs_sb[:, 0:n], func=AF.Ln)
                P.I(ACT, "activation", [rs_sb], [rs_sb], out=rs_sb[:, 0:n], in_=rs_sb[:, 0:n], func=AF.Exp, scale=-0.5)
                for kc in range(8):
                    P.I(DVE, "tensor_tensor", [x1, rs_sb, sq_sb], [sq_sb], out=sq_sb[:, kc, 0:n], in0=x1[:, kc, c0:c0 + n], in1=rs_sb[:, 0:n], op=ALU.mult)
                    P.I(ACT, "activation", [sq_sb, Acol[j], Bcol[j]], [hf32], out=hf32[:, kc, 0:n], in_=sq_sb[:, kc, 0:n], func=AF.Identity,
                        bias=Bcol[j][:, kc:kc + 1], scale=Acol[j][:, kc:kc + 1])
                    P.I(POOL, "tensor_copy", [hf32], [hT], out=hT[:, kc, c0:c0 + n], in_=hf32[:, kc, 0:n])
                s0 = 0
                while s0 < n:
                    m = min(128, n - s0)
                    pl = Bk.f[5]
                    for kc in range(8):
                        P.I(PE, "matmul", [hf32, rw_sb], [pl], pl[0:m, 0:NEXP], hf32[:, kc, s0:s0 + m], rw_sb[:, kc, :], start=(kc == 0), stop=(kc == 7))
                    P.I(DVE, "tensor_tensor", [pl, rb_sb], [lg], out=lg[0:m, :], in0=pl[0:m, 0:NEXP], in1=rb_sb[0:m, :], op=ALU.add)
                    P.I(DVE, "max", [lg], [mx], out=mx[0:m, :], in_=lg[0:m, :])
                    P.I(DVE, "tensor_scalar", [lg, mx], [msk], out=msk[0:m, :], in0=lg[0:m, :], scalar1=mx[0:m, 3:4], scalar2=None, op0=ALU.is_ge)
                    P.I(DVE, "tensor_scalar", [mx], [nm], out=nm[0:m, :], in0=mx[0:m, 0:1], scalar1=-1.0, scalar2=None, op0=ALU.mult)
                    P.I(ACT, "activation", [lg, nm], [ex], out=ex[0:m, :], in_=lg[0:m, :], func=AF.Exp, bias=nm[0:m, 0:1], scale=1.0)
                    P.I(DVE, "tensor_tensor", [ex, msk], [ex], out=ex[0:m, :], in0=ex[0:m, :], in1=msk[0:m, :], op=ALU.mult)
                    P.I(DVE, "reduce_sum", [ex], [sm], out=sm[0:m, :], in_=ex[0:m, :], axis=AX.X)
                    P.I(DVE, "reciprocal", [sm], [sm], out=sm[0:m, :], in_=sm[0:m, :])
                    P.I(DVE, "tensor_scalar", [ex, sm], [ex], out=ex[0:m, :], in0=ex[0:m, :], scalar1=sm[0:m, 0:1], scalar2=None, op0=ALU.mult)
                    pt = Bk.f[4]
                    P.I(PE, "transpose", [ex, id_f], [pt], pt[0:NEXP, 0:m], ex[0:m, :], id_f[0:m, 0:m])
                    P.I(ACT, "activation", [pt], [GT], out=GT[:, c0 + s0:c0 + s0 + m], in_=pt[0:NEXP, 0:m], func=AF.Copy)
                    s0 += m
        P.dma(SP, gt_dram[hf], GT[:, :], reads=[GT], writes=[("gtd", hf)])
        p3 = P.scope(); p3.__enter__()
        yT = P.sb([128, 8, TH], F32, name=f"yT{hf}")
        actT = P.sb([128, 8, TH], BF16, name=f"actT{hf}")
        gu_ring = [P.sb([128, 8, 512], BF16, name=f"gu{hf}_{i}") for i in range(3)]
        dn_ring = [P.sb([128, 8, 256], BF16, name=f"dn{hf}_{i}") for i in range(3)]
        gbs = [P.sb([128, TH], F32, name=f"gb{hf}_{i}") for i in range(2)]
        g1 = [P.sb([128, 512], F32, name=f"g1{hf}_{i}") for i in range(2)]
        tt = [P.sb([128, 512], F32, name=f"tt{hf}_{i}") for i in range(2)]
        u1 = [P.sb([128, 512], F32, name=f"u1{hf}_{i}") for i in range(2)]
        for e in range(NEXP):
            gb_sb = gbs[e % 2]
            P.dma(SP, gb_sb[:, :], gt_dram[hf, e:e + 1, :].partition_broadcast(128), reads=[("gtd", hf)], writes=[gb_sb])
            for q in range(4):
                wb = gu_ring[gu_cnt[0] % 3]; gu_cnt[0] += 1
                src = w_gu[e].rearrange("(k p) n -> p k n", p=128)
                P.dma(POOL, wb[:, :, 0:256], src[:, :, q * 256:(q + 1) * 256], writes=[wb])
                P.dma(POOL, wb[:, :, 256:512], src[:, :, 1024 + q * 256:1024 + (q + 1) * 256], writes=[wb])
                for o2 in range(2):
                    oc = q * 2 + o2
                    for (c0, n, j) in tiles:
                        i = ch_cnt[0] % 2; ch_cnt[0] += 1
                        pgt, put = Bk.f[i], Bk.f[2 + i]
                        for kc in range(8):
                            P.I(PE, "matmul", [wb, hT], [pgt], pgt[:, 0:n], wb[:, kc, o2 * 128:(o2 + 1) * 128], hT[:, kc, c0:c0 + n], start=(kc == 0), stop=(kc == 7))
                        for kc in range(8):
                            P.I(PE, "matmul", [wb, hT], [put], put[:, 0:n], wb[:, kc, 256 + o2 * 128:256 + (o2 + 1) * 128], hT[:, kc, c0:c0 + n], start=(kc == 0), stop=(kc == 7))
                        P.I(DVE, "tensor_scalar", [pgt, bgu_sb], [g1[i]], out=g1[i][:, 0:n], in0=pgt[:, 0:n], scalar1=bgu_sb[:, e, oc:oc + 1], scalar2=7.0, op0=ALU.add, op1=ALU.min)
                        P.I(ACT, "activation", [g1[i]], [tt[i]], out=tt[i][:, 0:n], in_=g1[i][:, 0:n], func=AF.Silu, scale=1.702)
                        P.I(DVE, "tensor_scalar", [put, bgu_sb], [u1[i]], out=u1[i][:, 0:n], in0=put[:, 0:n], scalar1=bgu_sb[:, e, 8 + oc:8 + oc + 1], scalar2=7.0, op0=ALU.add, op1=ALU.min)
                        P.I(POOL, "tensor_scalar", [u1[i]], [u1[i]], out=u1[i][:, 0:n], in0=u1[i][:, 0:n], scalar1=-7.0, scalar2=1.0, op0=ALU.max, op1=ALU.add)
                        P.I(POOL, "tensor_tensor", [tt[i], u1[i]], [tt[i]], out=tt[i][:, 0:n], in0=tt[i][:, 0:n], in1=u1[i][:, 0:n], op=ALU.mult)
                        P.I(DVE, "scalar_tensor_tensor", [tt[i], gb_sb], [actT], out=actT[:, oc, c0:c0 + n], in0=tt[i][:, 0:n], scalar=1.0 / 1.702, in1=gb_sb[:, c0:c0 + n],
                            op0=ALU.mult, op1=ALU.mult)
            for q in range(4):
                wd = dn_ring[dn_cnt[0] % 3]; dn_cnt[0] += 1
                P.dma(POOL, wd[:], w_dn[e].rearrange("(k p) n -> p k n", p=128)[:, :, q * 256:(q + 1) * 256], writes=[wd])
                for o2 in range(2):
                    dc = q * 2 + o2
                    for (c0, n, j) in tiles:
                        i = ch_cnt[0] % 2; ch_cnt[0] += 1
                        pd = Bk.f[4 + i]
                        for kc in range(8):
                            P.I(PE, "matmul", [wd, actT], [pd], pd[:, 0:n], wd[:, kc, o2 * 128:(o2 + 1) * 128], actT[:, kc, c0:c0 + n], start=(kc == 0), stop=(kc == 7))
                        if e == 0:
                            P.I(DVE, "tensor_copy", [pd], [yT], out=yT[:, dc, c0:c0 + n], in_=pd[:, 0:n])
                        else:
                            P.I(DVE, "tensor_tensor", [pd, yT], [yT], out=yT[:, dc, c0:c0 + n], in0=pd[:, 0:n], in1=yT[:, dc, c0:c0 + n], op=ALU.add)
        for (c0, n, j) in tiles:
            for dc in range(8):
                pd = Bk.f[4 + dc % 2]
                P.I(PE, "matmul", [bdn_sb, GT], [pd], pd[:, 0:n], bdn_sb[:, dc * 128:(dc + 1) * 128], GT[:, c0:c0 + n], start=True, stop=True)
                P.I(DVE, "tensor_tensor", [pd, yT], [yT], out=yT[:, dc, c0:c0 + n], in0=pd[:, 0:n], in1=yT[:, dc, c0:c0 + n], op=ALU.add)
                P.I(DVE, "scalar_tensor_tensor", [yT, G1[j], x1], [x1], out=x1[:, dc, c0:c0 + n], in0=yT[:, dc, c0:c0 + n], scalar=G1[j][:, dc:dc + 1],
                    in1=x1[:, dc, c0:c0 + n], op0=ALU.mult, op1=ALU.add)
        for kc in range(8):
            outs.append(P.dma(SP if kc % 2 == 0 else ACT, outT[hf, kc * 128:(kc + 1) * 128, :], x1[:, kc, :], reads=[x1]))
        p3.__exit__(None, None, None)
    return P.finish(outs)


def prep_C(inp, layer, mT_full, xT_full, mcT=None, xcT=None):
    passes = moe_passes(NC_MOE, mcT is not None)
    nlp = (SEQ // NC_MOE) // 1024
    ncx = NCTX // NC_MOE
    aw = inp['ada_w'][layer]; ab = inp['ada_b'][layer]
    adaw = np.ascontiguousarray(np.concatenate([aw[:, 2048:3072], aw[:, 3072:6144]], axis=1))
    adab = col_layout(np.concatenate([ab[2048:3072], ab[3072:6144]]), 32)
    cvec = np.stack([col_layout(inp['c'][0], 8), col_layout(inp['c_ctx'], 8)], axis=2).reshape(128, 16)
    bgu = inp['moe_b_gu'][layer]
    bgu_l = np.ascontiguousarray(bgu.reshape(NEXP, 16, 128).transpose(2, 0, 1).reshape(128, NEXP * 16))
    common = dict(
        cvec=np.ascontiguousarray(cvec), adaw=adaw, adab=adab, normg=col_layout(inp['norm_g'][layer, 1], 8),
        w_out=np.ascontiguousarray(inp['w_out'][layer]),
        rw=np.ascontiguousarray(inp['router_w'][layer].reshape(8, 128, NEXP).transpose(1, 0, 2).reshape(128, 8 * NEXP)),
        rb=np.ascontiguousarray(np.broadcast_to(inp['router_b'][layer][None, :], (128, NEXP))).astype(np.float32),
        w_gu=inp['moe_w_gu'][layer], bgu=bgu_l, w_dn=inp['moe_w_dn'][layer], bdn=np.ascontiguousarray(inp['moe_b_dn'][layer]),
        ident=np.eye(128, dtype=np.float32), onesd=np.ones((128, 128), np.float32))
    maps = []
    for c in range(NC_MOE):
        ms = np.zeros((len(passes), D, 1024), np.float32); xs = np.zeros((len(passes), D, 1024), np.float32)
        for hf in range(nlp):
            t0 = (c * nlp + hf) * 1024
            ms[hf] = mT_full[:, t0:t0 + 1024]; xs[hf] = xT_full[:, t0:t0 + 1024]
        if mcT is not None:
            ms[nlp, :, 0:ncx] = mcT[:, c * ncx:(c + 1) * ncx]; xs[nlp, :, 0:ncx] = xcT[:, c * ncx:(c + 1) * ncx]
        d = dict(common); d.update(mT=ms, xT=xs)
        maps.append(d)
    return blobify(maps, BLOB_C), passes


def gather_C(results, has_ctx):
    nlp = (SEQ // NC_MOE) // 1024
    ncx = NCTX // NC_MOE
    xT = np.zeros((D, SEQ), np.float32); xcT = np.zeros((D, NCTX), np.float32) if has_ctx else None
    for c in range(NC_MOE):
        o = results[c]['outT']
        for hf in range(nlp):
            t0 = (c * nlp + hf) * 1024
            xT[:, t0:t0 + 1024] = o[hf]
        if has_ctx:
            xcT[:, c * ncx:(c + 1) * ncx] = o[nlp][:, 0:ncx]
    return xT, xcT


PI = float(np.pi)


def build_B(L):
    NBK = L // 128
    NK = 2 * NBK - 1
    HROW = 2 * L
    P = Prog()
    Bk = Banks(P)
    di = lambda n, s, dt=F32: P.dram(n, s, dt, "ExternalInput")
    vB = di("vB", [128, 64, NBK]); x1B = di("x1B", [128, 64, NBK]); x2B = di("x2B", [128, 64, NBK])
    zt = di("zt", [2, 33, L]); tn = di("tn", [2, 1, L])
    bl = Blob(BLOB_B).dram(P)
    w1 = bl.ap("w1"); w2 = bl.ap("w2"); w3 = bl.ap("w3"); w4s = bl.ap("w4s")
    fqb = bl.ap("fqb")
    negd = bl.ap("negd"); fbias = bl.ap("fbias")
    ident = bl.ap("ident"); onesd = bl.ap("onesd"); jmat = bl.ap("jmat")
    hyB = P.dram("hyB", [128, 64, NBK], F32, "ExternalOutput")
    Hd = P.dram("Hd_scratch", [128, HROW], BF16, "Internal")

    ones_sb = P.sb([128, 128], F32, name="ones"); P.dma(SP, ones_sb[:], onesd[:, :], writes=[ones_sb])
    id_f = P.sb([128, 128], F32, name="idf"); P.dma(SP, id_f[:], ident[:, :], writes=[id_f])
    j_b = P.sb([128, 128], BF16, name="jb"); P.dma(POOL, j_b[:], jmat[:, :], writes=[j_b])
    w1_sb = P.sb([33, 64], F32, name="w1"); P.dma(SP, w1_sb[:], w1[:, :], writes=[w1_sb])
    w2_sb = P.sb([64, 64], F32, name="w2"); P.dma(SP, w2_sb[:], w2[:, :], writes=[w2_sb])
    w3_sb = P.sb([64, 64], F32, name="w3"); P.dma(SP, w3_sb[:], w3[:, :], writes=[w3_sb])
    w4_sb = P.sb([64, 2, 128], F32, name="w4"); P.dma(SP, w4_sb[:], w4s.rearrange("k (s m) -> k s m", s=2), writes=[w4_sb])
    fq_sb = P.sb([64, 4], F32, name="fq"); P.dma(SP, fq_sb[:], fqb[:, :], writes=[fq_sb])
    fb_sb = P.sb([64, 3], F32, name="fqbias")
    P.I(DVE, "tensor_tensor", [fq_sb], [fb_sb], out=fb_sb[:], in0=fq_sb[:, 1:4], in1=fq_sb[:, 0:1].broadcast_to([64, 3]), op=ALU.mult)
    negd_sb = P.sb([128, 1], F32, name="negd"); P.dma(SP, negd_sb[:], negd[:, :], writes=[negd_sb], allow_slow_non_contiguous=True)
    fbias_sb = P.sb([128, 128], F32, name="fbias"); P.dma(SP, fbias_sb[:], fbias[:, :], writes=[fbias_sb])

    CH = min(512, L)
    NCH = L // CH
    abss = P.sb([128, 2 * NCH], F32, name="abss")
    with P.scope():
        z_sb = [P.sb([33, CH], F32, name=f"z{i}") for i in range(2)]
        tn_sb = [P.sb([128, CH], F32, name=f"tn{i}") for i in range(2)]
        a_sb = P.sb([64, CH], F32, name="a_sb"); t_sb = P.sb([64, CH], F32, name="t_sb")
        hd_sb = P.sb([64, CH], F32, name="hd_sb")
        dec_sb = P.sb([128, CH], F32, name="dec_sb"); hf_sb = P.sb([128, CH], F32, name="hf_sb")
        hb_sb = [P.sb([128, CH], BF16, name=f"hb{i}") for i in range(2)]
        it = 0
        for side in range(2):
            for c in range(NCH):
                zb = z_sb[it % 2]; tb = tn_sb[it % 2]; hb = hb_sb[it % 2]; it += 1
                P.dma(SP, zb[:], zt[side, :, c * CH:(c + 1) * CH], writes=[zb])
                P.dma(ACT, tb[:], tn[side, :, c * CH:(c + 1) * CH].partition_broadcast(128), writes=[tb])
                src, Kd, wl = zb, 33, [w1_sb, w2_sb, w3_sb]
                for l in range(3):
                    ps = Bk.f[l % 2]
                    P.I(PE, "matmul", [wl[l], src], [ps], ps[0:64, 0:CH], wl[l][:], src[0:Kd, :], start=True, stop=True)
                    P.I(DVE, "tensor_scalar", [ps, fq_sb, fb_sb], [a_sb], out=a_sb[:], in0=ps[0:64, 0:CH], scalar1=fq_sb[:, 0:1], scalar2=fb_sb[:, l:l + 1], op0=ALU.mult, op1=ALU.add)
                    for rep in range(2):
                        P.I(POOL, "tensor_scalar", [a_sb], [t_sb], out=t_sb[:], in0=a_sb[:], scalar1=PI, scalar2=-2 * PI, op0=ALU.is_gt, op1=ALU.mult)
                        P.I(DVE, "tensor_tensor", [a_sb, t_sb], [hd_sb], out=hd_sb[:], in0=a_sb[:], in1=t_sb[:], op=ALU.add)
                        P.I(POOL, "tensor_scalar", [a_sb], [t_sb], out=t_sb[:], in0=a_sb[:], scalar1=-PI, scalar2=2 * PI, op0=ALU.is_lt, op1=ALU.mult)
                        P.I(DVE, "tensor_tensor", [hd_sb, t_sb], [a_sb], out=a_sb[:], in0=hd_sb[:], in1=t_sb[:], op=ALU.add)
                    P.I(ACT, "activation", [a_sb], [hd_sb], out=hd_sb[:], in_=a_sb[:], func=AF.Sin)
                    src, Kd = hd_sb, 64
                p4 = Bk.f[2]
                P.I(PE, "matmul", [w4_sb, hd_sb], [p4], p4[:, 0:CH], w4_sb[:, side, :], hd_sb[:], start=True, stop=True)
                P.I(ACT, "activation", [tb, negd_sb], [dec_sb], out=dec_sb[:], in_=tb[:], func=AF.Exp, scale=negd_sb[:, 0:1])
                P.I(DVE, "tensor_tensor", [p4, dec_sb], [hf_sb], out=hf_sb[:], in0=p4[:, 0:CH], in1=dec_sb[:], op=ALU.mult)
                if side == 1 and c == NCH - 1:
                    P.I(DVE, "memset", [], [hf_sb], hf_sb[:, CH - 1:CH], 0.0)
                P.I(DVE, "tensor_reduce", [hf_sb], [abss], out=abss[:, side * NCH + c:side * NCH + c + 1], in_=hf_sb[:], axis=AX.X, op=ALU.add, apply_absolute_value=True)
                P.I(ACT, "activation", [hf_sb], [hb], out=hb[:], in_=hf_sb[:], func=AF.Copy)
                if side == 1:
                    n = CH - 1 if c == NCH - 1 else CH
                    P.dma(POOL, Hd[:, c * CH:c * CH + n], hb[:, 0:n], reads=[hb], writes=["Hd"])
                else:
                    P.dma(POOL, Hd[:, L - 1 + c * CH:L - 1 + (c + 1) * CH], hb[:], reads=[hb], writes=["Hd"])
        zpad = P.sb([128, 1], BF16, name="zpad"); P.I(DVE, "memset", [], [zpad], zpad[:], 0.0)
        P.dma(POOL, Hd[:, 2 * L - 1:2 * L], zpad[:], reads=[zpad], writes=["Hd"], allow_slow_non_contiguous=True)
    rn = P.sb([128, 1], F32, name="rn"); rnb = P.sb([128, 128], F32, name="rnb"); dg = P.sb([128, 128], F32, name="dg")
    P.I(DVE, "reduce_sum", [abss], [rn], out=rn[:], in_=abss[:], axis=AX.X)
    P.I(DVE, "reciprocal", [rn], [rn], out=rn[:], in_=rn[:])
    P.I(DVE, "tensor_scalar", [id_f, rn], [dg], out=dg[:], in0=id_f[:], scalar1=rn[:, 0:1], scalar2=None, op0=ALU.mult)
    pr = Bk.f[0]
    P.I(PE, "matmul", [ones_sb, dg], [pr], pr[:, 0:128], ones_sb[:], dg[:], start=True, stop=True)
    P.I(ACT, "activation", [pr], [rnb], out=rnb[:], in_=pr[:, 0:128], func=AF.Copy)

    v_sb = P.sb([128, 64, NBK], F32, name="v_sb"); x1_sb = P.sb([128, 64, NBK], F32, name="x1_sb"); x2_sb = P.sb([128, 64, NBK], F32, name="x2_sb")
    P.dma(SP, v_sb[:], vB[:, :, :], writes=[v_sb]); P.dma(ACT, x1_sb[:], x1B[:, :, :], writes=[x1_sb]); P.dma(SP, x2_sb[:], x2B[:, :, :], writes=[x2_sb])
    zb16 = P.sb([128, 64 * NBK], BF16, name="zb16")
    zrev = P.sb([128, 64, NBK], BF16, name="zrev")
    z1_sb = P.sb([128, 64, NBK], F32, name="z1_sb")
    t0_sb = [P.sb([128, NBK], F32, name=f"t0_{i}") for i in range(2)]
    t1_sb = [P.sb([128, NBK], F32, name=f"t1_{i}") for i in range(2)]
    KP = min(51, NK)
    NPIECE = (NK + KP - 1) // KP
    hs_ring = [P.sb([128, KP * 128], BF16, name=f"hs{i}") for i in range(3)]
    outs = []
    hs_cnt = [0]

    def make_zrev(src_f32):
        flat = src_f32[:].rearrange("p c j -> p (c j)")
        P.I(ACT, "activation", [src_f32], [zb16], out=zb16[:], in_=flat, func=AF.Copy)
        tot = 64 * NBK
        c0 = 0
        zr_flat = zrev[:].rearrange("p c j -> p (c j)")
        i = 0
        while c0 < tot:
            n = min(512, tot - c0)
            pz = Bk.f[4 + i % 2]; i += 1
            P.I(PE, "matmul", [j_b, zb16], [pz], pz[:, 0:n], j_b[:], zb16[:, c0:c0 + n], start=True, stop=True)
            P.I(ACT if i % 2 else DVE, "activation" if i % 2 else "tensor_copy", [pz], [zrev],
                **(dict(out=zr_flat[:, c0:c0 + n], in_=pz[:, 0:n], func=AF.Copy) if i % 2 else dict(out=zr_flat[:, c0:c0 + n], in_=pz[:, 0:n])))
            c0 += n

    mid = (NK // 2) // KP
    piece_order = [mid] + [p for p in range(NPIECE) if p != mid]

    def conv(o, zin_f32, gate_sb, dst_sb):
        for ch in range(64):
            row = o * 64 + ch
            py = Bk.f[ch % 4][:, 0:NBK] if False else Bk.f[ch % 2]
            first = True
            for pi in piece_order:
                kk0 = pi * KP; kk1 = min(NK, kk0 + KP)
                hs = hs_ring[hs_cnt[0] % 3]; hs_cnt[0] += 1
                ncol = (kk1 - kk0) * 128
                src = bass.AP(Hd.tensor, row * HROW + kk0 * 128, [[1, 128], [1, ncol]])
                P.dma(SP if hs_cnt[0] % 2 else ACT, hs[:, 0:ncol], src, reads=["Hd"], writes=[hs])
                ks = list(range(kk0, kk1))
                if pi == mid:
                    ks.remove(NBK - 1); ks = [NBK - 1] + ks
                for kk in ks:
                    k = kk - (NBK - 1)
                    a_lo = max(0, k); a_hi = min(NBK - 1, NBK - 1 + k)
                    last = (pi == piece_order[-1] and kk == ks[-1])
                    P.I(PE, "matmul", [hs, zrev], [py], py[:, a_lo:a_hi + 1], hs[:, (kk - kk0) * 128:(kk - kk0 + 1) * 128], zrev[:, ch, a_lo - k:a_hi - k + 1],
                        start=first, stop=last)
                    first = False
            i = ch % 2
            P.I(POOL, "tensor_scalar", [zin_f32, fbias_sb], [t0_sb[i]], out=t0_sb[i][:], in0=zin_f32[:, ch, :], scalar1=fbias_sb[:, row:row + 1], scalar2=None, op0=ALU.mult)
            P.I(DVE, "scalar_tensor_tensor", [py, rnb, t0_sb[i]], [t1_sb[i]], out=t1_sb[i][:], in0=py[:, 0:NBK], scalar=rnb[:, row:row + 1], in1=t0_sb[i][:], op0=ALU.mult, op1=ALU.add)
            P.I(POOL, "tensor_tensor", [t1_sb[i], gate_sb], [dst_sb], out=dst_sb[:, ch, :], in0=t1_sb[i][:], in1=gate_sb[:, ch, :], op=ALU.mult)

    make_zrev(v_sb)
    conv(0, v_sb, x1_sb, z1_sb)
    make_zrev(z1_sb)
    conv(1, z1_sb, x2_sb, v_sb)
    outs.append(P.dma(SP, hyB[:, :, :], v_sb[:], reads=[v_sb]))
    return P.finish(outs)


def hyena_tables(L):
    t = np.linspace(0.0, 1.0, L, dtype=np.float32)[:, None]
    bands = 16
    w_ang = (2.0 * np.pi * np.arange(L, dtype=np.float32)[:, None] / L).astype(np.float32)
    fr = np.linspace(1e-4, bands - 1, bands, dtype=np.float32)[None]
    z = np.concatenate([t, np.cos(fr * w_ang), -np.sin(fr * w_ang)], axis=-1).astype(np.float32)
    zt = np.stack([z.T, z[::-1].T]).astype(np.float32)
    tn = np.stack([t.T, t[::-1].T]).astype(np.float32)
    return np.ascontiguousarray(zt), np.ascontiguousarray(tn)


def prep_B(inp, uT_full, L):
    NBK = L // 128
    zt, tn = hyena_tables(L)
    max_decay = np.log(1e-2) / 0.3; min_decay = np.log(1e-2) / 1.5
    deltas = np.abs(np.linspace(min_decay, max_decay, 512, dtype=np.float32)).astype(np.float32)
    w4 = inp['hy_w4'][0].reshape(64, 2, 2, 512)
    fqb = np.stack([inp['hy_freq'][0], inp['hy_b1'][0], inp['hy_b2'][0], inp['hy_b3'][0]], axis=1).astype(np.float32)
    jm = np.eye(128, dtype=np.float32)[::-1].copy()
    maps = []
    for c in range(NCORE):
        chs = slice(64 * c, 64 * c + 64)

        def blk(rows):
            return np.ascontiguousarray(rows.reshape(64, NBK, 128).transpose(2, 0, 1))
        w4s = np.concatenate([w4[:, :, s, chs].reshape(64, 128) for s in range(2)], axis=1)
        fb = inp['hy_filter_bias'][0][:, chs].reshape(128)
        maps.append(dict(
            vB=blk(uT_full[0:512][chs]), x1B=blk(uT_full[512:1024][chs]), x2B=blk(uT_full[1024:1536][chs]),
            zt=zt, tn=tn, w1=np.ascontiguousarray(inp['hy_w1'][0]), w2=np.ascontiguousarray(inp['hy_w2'][0]),
            w3=np.ascontiguousarray(inp['hy_w3'][0]), w4s=np.ascontiguousarray(w4s), fqb=np.ascontiguousarray(fqb),
            negd=np.ascontiguousarray(-np.tile(deltas[chs], 2)[:, None]).astype(np.float32),
            fbias=np.ascontiguousarray(np.broadcast_to(fb[None, :], (128, 128))).astype(np.float32),
            ident=np.eye(128, dtype=np.float32), onesd=np.ones((128, 128), np.float32), jmat=jm))
    return blobify(maps, BLOB_B)


def gather_B(results, L):
    NBK = L // 128
    hyT = np.zeros((512, L), np.float32)
    for c in range(NCORE):
        hb = results[c]['hyB']
        hyT[64 * c:64 * c + 64] = hb.transpose(1, 2, 0).reshape(64, L)
    return hyT


DROWS = 5136


def build_D():
    P = Prog()
    Bk = Banks(P)
    di = lambda n, s, dt=F32: P.dram(n, s, dt, "ExternalInput")
    xT = di("xT", [D, TEXT]); ctxT = di("ctxT", [D, NCTX])
    bl = Blob(BLOB_D).dram(P)
    cvec = bl.ap("cvec"); adaw = di("adaw", [D, 3072]); adab = bl.ap("adab")
    normg = bl.ap("normg"); w_in = di("w_in", [D, 4112])
    onesd = bl.ap("onesd")
    convw = bl.ap("convw"); convb = bl.ap("convb"); edge = bl.ap("edge")
    hglb = bl.ap("hglb")
    dtb = bl.ap("dtb")
    outT = P.dram("outT", [DROWS, TLOC], F32, "ExternalOutput"); outcT = P.dram("outcT", [DROWS, NCTX], F32, "ExternalOutput")

    ones_sb = P.sb([128, 128], F32, name="ones"); P.dma(SP, ones_sb[:], onesd[:, :], writes=[ones_sb])
    cv = P.sb([128, 8, 2], F32, name="cv"); P.dma(SP, cv[:], cvec.rearrange("p (k n) -> p k n", n=2), writes=[cv])
    adab_sb = P.sb([128, 24], F32, name="adab"); P.dma(SP, adab_sb[:], adab[:, :], writes=[adab_sb])
    g_sb = P.sb([128, 8], F32, name="normg"); P.dma(SP, g_sb[:], normg[:, :], writes=[g_sb])
    cw_sb = P.sb([128, 8, 3], F32, name="cw"); P.dma(SP, cw_sb[:], convw.rearrange("p (o t) -> p o t", t=3), writes=[cw_sb])
    cb_sb = P.sb([128, 8], F32, name="cb"); P.dma(SP, cb_sb[:], convb[:, :], writes=[cb_sb])
    edge_sb = P.sb([128, 2], F32, name="edge"); P.dma(SP, edge_sb[:], edge[:, :], writes=[edge_sb])
    lb_sb = P.sb([128, 8], F32, name="hglb"); P.dma(SP, lb_sb[:], hglb[:, :], writes=[lb_sb])
    dtb_sb = P.sb([16, 1], F32, name="dtb"); P.dma(SP, dtb_sb[:], dtb[:, :], writes=[dtb_sb], allow_slow_non_contiguous=True)
    lbc = P.sb([128, 4], F32, name="lbc"); oml = P.sb([128, 4], F32, name="oml")
    P.I(DVE, "tensor_tensor", [lb_sb], [lbc], out=lbc[:], in0=lb_sb[:, 4:8], in1=lb_sb[:, 0:4], op=ALU.subtract)
    P.I(ACT, "activation", [lbc], [lbc], out=lbc[:], in_=lbc[:], func=AF.Sigmoid)
    P.I(DVE, "tensor_scalar", [lbc], [oml], out=oml[:], in0=lbc[:], scalar1=-1.0, scalar2=1.0, op0=ALU.mult, op1=ALU.add)
    w_sb = P.sb([128, 8, 4112], BF16, name="w_in")
    for kc in range(8):
        P.dma(POOL, w_sb[:, kc, :], w_in[kc * 128:(kc + 1) * 128, :], writes=[w_sb])
    ada = P.sb([128, 24, 2], F32, name="ada")
    with P.scope():
        wbuf = P.sb([128, 8, 1024], F32, name="adawbuf")
        emit_adaln(P, Bk, cv, adaw, adab_sb, wbuf, ada, 2)
    Acol = [P.sb([128, 8], F32, name=f"Acol{j}") for j in range(2)]
    Bcol = [P.sb([128, 8], F32, name=f"Bcol{j}") for j in range(2)]
    for j in range(2):
        P.I(DVE, "scalar_tensor_tensor", [ada, g_sb], [Acol[j]], out=Acol[j][:], in0=ada[:, 8:16, j], scalar=1.0, in1=g_sb[:], op0=ALU.add, op1=ALU.mult)
        P.I(DVE, "tensor_copy", [ada], [Bcol[j]], out=Bcol[j][:], in_=ada[:, 0:8, j])
    h_all = P.sb([128, 8, TEXT + NCTX], BF16, name="h_all")
    x_sb = P.sb([128, 8, TT], F32, name="x_sb"); sq_sb = P.sb([128, 8, TT], F32, name="sq_sb"); rs_sb = P.sb([128, TT], F32, name="rs_sb")
    for t in range(NTT):
        for kc in range(8):
            P.dma(SP if kc % 2 == 0 else ACT, x_sb[:, kc, :], xT[kc * 128:(kc + 1) * 128, t * TT:(t + 1) * TT], writes=[x_sb])
        emit_norm_mod(P, Bk, x_sb, ones_sb, Acol[0], Bcol[0], h_all, TT, sq_sb, rs_sb, hcol0=t * TT)
    for kc in range(8):
        P.dma(SP if kc % 2 == 0 else ACT, x_sb[:, kc, 0:NCTX], ctxT[kc * 128:(kc + 1) * 128, :], writes=[x_sb])
    emit_norm_mod(P, Bk, x_sb, ones_sb, Acol[1], Bcol[1], h_all, NCTX, sq_sb, rs_sb, hcol0=TEXT)

    u_sb = [P.sb([128, 512], F32, name=f"u_sb{i}") for i in range(2)]
    a1_sb = [P.sb([128, 512], F32, name=f"a1_sb{i}") for i in range(2)]
    a2_sb = [P.sb([128, 512], F32, name=f"a2_sb{i}") for i in range(2)]
    outs = []

    def tile(h0, n, out_dram, ocol0, zl, zr, el, er):
        a0 = h0 - (0 if zl else 1); a1 = h0 + n + (0 if zr else 1)
        w = a1 - a0
        off = 1 if zl else 0
        for oc in range(33):
            M = 128 if oc < 32 else 16
            i = oc % 2
            pu = Bk.f[2 + i]; ub = u_sb[i]; r1 = a1_sb[i]; r2 = a2_sb[i]
            for kc in range(8):
                P.I(PE, "matmul", [w_sb, h_all], [pu], pu[0:M, off:off + w], w_sb[:, kc, oc * 128:oc * 128 + M], h_all[:, kc, a0:a1], start=(kc == 0), stop=(kc == 7))
            ctr = pu[0:M, 1:n + 1]
            dq = SP if oc % 2 == 0 else POOL
            if oc < 8:
                if zl:
                    P.I(POOL, "memset", [], [ub], ub[:, 0:1], 0.0)
                if zr:
                    P.I(POOL, "memset", [], [ub], ub[:, n + 1:n + 2], 0.0)
                P.I(ACT, "activation", [pu], [ub], out=ub[:, off:off + w], in_=pu[:, off:off + w], func=AF.Copy)
                if el:
                    P.I(DVE, "tensor_scalar", [ub, edge_sb], [ub], out=ub[:, 0:1], in0=ub[:, 0:1], scalar1=edge_sb[:, 0:1], scalar2=None, op0=ALU.mult)
                if er:
                    P.I(DVE, "tensor_scalar", [ub, edge_sb], [ub], out=ub[:, n + 1:n + 2], in0=ub[:, n + 1:n + 2], scalar1=edge_sb[:, 1:2], scalar2=None, op0=ALU.mult)
                P.I(DVE, "tensor_scalar", [ub, cw_sb, cb_sb], [r1], out=r1[:, 0:n], in0=ub[:, 1:n + 1], scalar1=cw_sb[:, oc, 1:2], scalar2=cb_sb[:, oc:oc + 1], op0=ALU.mult, op1=ALU.add)
                P.I(DVE, "scalar_tensor_tensor", [ub, cw_sb, r1], [r1], out=r1[:, 0:n], in0=ub[:, 0:n], scalar=cw_sb[:, oc, 0:1], in1=r1[:, 0:n], op0=ALU.mult, op1=ALU.add)
                P.I(DVE, "scalar_tensor_tensor", [ub, cw_sb, r1], [r1], out=r1[:, 0:n], in0=ub[:, 2:n + 2], scalar=cw_sb[:, oc, 2:3], in1=r1[:, 0:n], op0=ALU.mult, op1=ALU.add)
                P.I(ACT, "activation", [r1], [r2], out=r2[:, 0:n], in_=r1[:, 0:n], func=AF.Silu)
                outs.append(P.dma(dq, out_dram[oc * 128:(oc + 1) * 128, ocol0:ocol0 + n], r2[:, 0:n], reads=[r2]))
            elif oc < 16:
                hd = (oc - 8) % 4
                P.I(ACT, "activation", [pu], [r1], out=r1[:, 0:n], in_=ctr, func=AF.Sigmoid)
                P.I(DVE, "tensor_scalar", [r1, oml, lbc], [r1], out=r1[:, 0:n], in0=r1[:, 0:n], scalar1=oml[:, hd:hd + 1], scalar2=lbc[:, hd:hd + 1], op0=ALU.mult, op1=ALU.add)
                P.I(DVE, "tensor_scalar", [r1], [r2], out=r2[:, 0:n], in0=r1[:, 0:n], scalar1=-1.0, scalar2=1.0, op0=ALU.mult, op1=ALU.add)
                outs.append(P.dma(dq, out_dram[1024 + (oc - 8) * 128:1024 + (oc - 7) * 128, ocol0:ocol0 + n], r2[:, 0:n], reads=[r2]))
                P.I(ACT, "activation", [r1], [ub], out=ub[:, 0:n], in_=r1[:, 0:n], func=AF.Ln)
                outs.append(P.dma(dq, out_dram[2048 + (oc - 8) * 128:2048 + (oc - 7) * 128, ocol0:ocol0 + n], ub[:, 0:n], reads=[ub]))
            elif oc < 20:
                P.I(ACT, "activation", [pu], [r1], out=r1[:, 0:n], in_=ctr, func=AF.Copy)
                outs.append(P.dma(dq, out_dram[3072 + (oc - 16) * 128:3072 + (oc - 15) * 128, ocol0:ocol0 + n], r1[:, 0:n], reads=[r1]))
            elif oc < 32:
                P.I(ACT, "activation", [pu], [r1], out=r1[:, 0:n], in_=ctr, func=AF.Silu)
                outs.append(P.dma(dq, out_dram[3584 + (oc - 20) * 128:3584 + (oc - 19) * 128, ocol0:ocol0 + n], r1[:, 0:n], reads=[r1]))
            else:
                P.I(ACT, "activation", [pu, dtb_sb], [r1], out=r1[0:16, 0:n], in_=pu[0:16, 1:n + 1], func=AF.Exp, bias=dtb_sb[:, 0:1], scale=1.0)
                P.I(ACT, "activation", [r1], [r2], out=r2[0:16, 0:n], in_=r1[0:16, 0:n], func=AF.Ln, bias=1.0, scale=1.0)
                outs.append(P.dma(dq, out_dram[5120:5136, ocol0:ocol0 + n], r2[0:16, 0:n], reads=[r2]))

    lo = 0
    while lo < TLOC:
        n = min(510, TLOC - lo)
        tile(HALO + lo, n, outT, lo, False, False, lo == 0, lo + n == TLOC)
        lo += n
    tile(TEXT, NCTX, outcT, 0, True, True, False, False)
    return P.finish(outs)


def prep_D(inp, xT_full, xcT):
    layer = 1
    xTp = np.concatenate([np.zeros((D, HALO), np.float32), xT_full, np.zeros((D, HALO), np.float32)], axis=1)
    w = inp['w_in_odd'][0]
    w_perm = np.ascontiguousarray(np.concatenate([w[:, 0:1024], w[:, 1040:2064], w[:, 2064:2576], w[:, 2576:3088], w[:, 3088:3600],
                                                   w[:, 3600:4112], w[:, 1024:1040]], axis=1))
    cvec = np.stack([col_layout(inp['c'][0], 8), col_layout(inp['c_ctx'], 8)], axis=2).reshape(128, 16)
    hl = inp['hg_lower_bounds']
    common = dict(
        ctxT=np.ascontiguousarray(xcT), cvec=np.ascontiguousarray(cvec),
        adaw=np.ascontiguousarray(inp['ada_w'][layer][:, 0:3072]), adab=col_layout(inp['ada_b'][layer][0:3072], 24),
        normg=col_layout(inp['norm_g'][layer, 0], 8), w_in=w_perm, onesd=np.ones((128, 128), np.float32),
        convw=np.ascontiguousarray(inp['ssd_conv_w'][0].reshape(3, 8, 128).transpose(2, 1, 0).reshape(128, 24)),
        convb=col_layout(inp['ssd_conv_b'][0], 8),
        hglb=np.ascontiguousarray(np.concatenate([col_layout(hl[0], 4), col_layout(hl[1], 4)], axis=1)),
        dtb=np.ascontiguousarray(inp['ssd_dt_bias'][0].reshape(16, 1)))
    maps = []
    for c in range(NCORE):
        s0 = c * TLOC
        edge = np.ones((128, 2), np.float32); edge[:, 0] = 0.0 if c == 0 else 1.0; edge[:, 1] = 0.0 if c == NCORE - 1 else 1.0
        m = dict(common); m.update(xT=np.ascontiguousarray(xTp[:, s0:s0 + TEXT]), edge=edge)
        maps.append(m)
    return blobify(maps, BLOB_D)


LS = NCTX + SEQ
NBLK = LS // 128
GB = 10


def build_E():
    P = Prog()
    Bk = Banks(P)
    di = lambda n, s, dt=F32: P.dram(n, s, dt, "ExternalInput")
    xtok = di("xtok", [2, 128, NBLK, 64]); Btok = di("Btok", [2, 128, NBLK, 128])
    BT = di("BT", [2, 128, LS]); CT = di("CT", [2, 128, LS]); dttok = di("dttok", [2, 128, NBLK])
    bl = Blob(BLOB_E).dram(P)
    ssdp = bl.ap("ssdp")
    qT = di("qT", [2, 128, LS]); kT = di("kT", [2, 128, LS]); gtok = di("gtok", [2, 128, NBLK, 128])
    vZ = di("vZ", [2, 128, NBLK, 5, 64])
    Ud = bl.ap("U"); U4d = bl.ap("U4"); Mnegd = bl.ap("Mneg")
    ident = bl.ap("ident"); onesd = bl.ap("onesd")
    ytok = P.dram("ytok", [2, 128, NBLK, 64], F32, "ExternalOutput"); otok = P.dram("otok", [2, 128, NBLK, 64], F32, "ExternalOutput")

    ones_sb = P.sb([128, 128], F32, name="ones"); P.dma(SP, ones_sb[:], onesd[:, :], writes=[ones_sb])
    U_sb = P.sb([128, 128], F32, name="U"); P.dma(SP, U_sb[:], Ud[:, :], writes=[U_sb])
    U4_sb = P.sb([128, 128], F32, name="U4"); P.dma(SP, U4_sb[:], U4d[:, :], writes=[U4_sb])
    Mn_sb = P.sb([128, 128], F32, name="Mneg"); P.dma(SP, Mn_sb[:], Mnegd[:, :], writes=[Mn_sb])
    id_b = P.sb([128, 128], BF16, name="idb"); P.dma(POOL, id_b[:], ident[:, :], writes=[id_b])
    sp_sb = P.sb([128, 4], F32, name="ssdp"); P.dma(SP, sp_sb[:], ssdp[:, :], writes=[sp_sb])
    acol = P.sb([128, 2], F32, name="acol")
    P.I(ACT, "activation", [sp_sb], [acol], out=acol[:], in_=sp_sb[:, 0:2], func=AF.Exp)
    P.I(DVE, "tensor_scalar", [acol], [acol], out=acol[:], in0=acol[:], scalar1=-1.0, scalar2=None, op0=ALU.mult)
    outs = []

    with P.scope():
        dt_sb = P.sb([128, 2, NBLK], F32, name="dt_sb")
        for d in range(2):
            P.dma(SP, dt_sb[:, d, :], dttok[d], writes=[dt_sb])
        xg = [P.sb([128, GB, 64], F32, name=f"xg{i}") for i in range(2)]
        Bg = [P.sb([128, GB, 128], BF16, name=f"Bg{i}") for i in range(2)]
        BTg = [P.sb([128, GB * 128], BF16, name=f"BTg{i}") for i in range(2)]
        CTg = [P.sb([128, GB * 128], F32, name=f"CTg{i}") for i in range(2)]
        CTh = [P.sb([128, GB * 128], BF16, name=f"CTh{i}") for i in range(2)]
        yg = [P.sb([128, GB, 64], F32, name=f"yg{i}") for i in range(2)]
        da = P.sb([128, 1], F32, name="da"); dab = P.sb([128, 128], F32, name="dab")
        cs_sb = P.sb([128, 1], F32, name="cs_sb"); tot_sb = P.sb([128, 1], F32, name="tot_sb")
        te = P.sb([128, 1], F32, name="te"); dec = P.sb([128, 1], F32, name="dec"); wcol = P.sb([128, 1], F32, name="wcol")
        xdt = P.sb([128, 64], BF16, name="xdt"); xw = P.sb([128, 64], BF16, name="xw")
        em = P.sb([128, 128], F32, name="em"); gt = P.sb([128, 128], BF16, name="gt")
        ecs = P.sb([128, 128], F32, name="ecs"); cp = P.sb([128, 128], BF16, name="cp")
        S = P.sb([128, 64], F32, name="S_ssd"); Sbf = P.sb([128, 64], BF16, name="Sbf_ssd")
        gi = 0
        for d in range(2):
            P.I(DVE, "memset", [], [S], S[:], 0.0)
            P.I(POOL, "memset", [], [Sbf], Sbf[:], 0.0)
            for g0 in range(0, NBLK, GB):
                i = gi % 2; gi += 1
                P.dma(SP, xg[i][:], xtok[d, :, g0:g0 + GB, :], writes=[xg[i]])
                P.dma(POOL, Bg[i][:], Btok[d, :, g0:g0 + GB, :], writes=[Bg[i]])
                P.dma(POOL, BTg[i][:], BT[d, :, g0 * 128:(g0 + GB) * 128], writes=[BTg[i]])
                P.dma(ACT, CTg[i][:], CT[d, :, g0 * 128:(g0 + GB) * 128], writes=[CTg[i]])
                P.dma(POOL, CTh[i][:], CT[d, :, g0 * 128:(g0 + GB) * 128], writes=[CTh[i]])
                for bb in range(GB):
                    b = g0 + bb
                    cols = slice(bb * 128, (bb + 1) * 128)
                    P.I(DVE, "tensor_scalar", [dt_sb, acol], [da], out=da[:], in0=dt_sb[:, d, b:b + 1], scalar1=acol[:, d:d + 1], scalar2=None, op0=ALU.mult)
                    P.I(DVE, "tensor_scalar", [ones_sb, da], [dab], out=dab[:], in0=ones_sb[:], scalar1=da[:, 0:1], scalar2=None, op0=ALU.mult)
                    pc, pr = Bk.f[0], Bk.f[1]
                    P.I(PE, "matmul", [U_sb, da], [pc], pc[:, 0:1], U_sb[:], da[:], start=True, stop=True)
                    P.I(PE, "matmul", [dab, U_sb], [pr], pr[:, 0:128], dab[:], U_sb[:], start=True, stop=True)
                    P.I(ACT, "activation", [pc], [cs_sb], out=cs_sb[:], in_=pc[:, 0:1], func=AF.Copy)
                    P.I(ACT, "activation", [pr], [tot_sb], out=tot_sb[:], in_=pr[:, 127:128], func=AF.Copy)
                    P.I(ACT, "activation", [cs_sb, tot_sb], [te], out=te[:], in_=cs_sb[:], func=AF.Exp, scale=-1.0, bias=tot_sb[:, 0:1])
                    P.I(ACT, "activation", [tot_sb], [dec], out=dec[:], in_=tot_sb[:], func=AF.Exp)
                    P.I(DVE, "tensor_scalar", [xg[i], dt_sb], [xdt], out=xdt[:], in0=xg[i][:, bb, :], scalar1=dt_sb[:, d, b:b + 1], scalar2=None, op0=ALU.mult)
                    P.I(DVE, "tensor_tensor", [dt_sb, te], [wcol], out=wcol[:], in0=dt_sb[:, d, b:b + 1], in1=te[:], op=ALU.mult)
                    P.I(DVE, "tensor_scalar", [xg[i], wcol], [xw], out=xw[:], in0=xg[i][:, bb, :], scalar1=wcol[:, 0:1], scalar2=None, op0=ALU.mult)
                    psc = Bk.f[2]
                    P.I(PE, "matmul", [Bg[i], xw], [psc], psc[:, 0:64], Bg[i][:, bb, :], xw[:], start=True, stop=True)
                    pss = Bk.f[3]
                    P.I(PE, "matmul", [BTg[i], CTh[i]], [pss], pss[:, 0:128], BTg[i][:, cols], CTh[i][:, cols], start=True, stop=True)
                    P.I(DVE, "scalar_tensor_tensor", [pr, cs_sb, Mn_sb], [em], out=em[:], in0=pr[:, 0:128], scalar=cs_sb[:, 0:1], in1=Mn_sb[:], op0=ALU.subtract, op1=ALU.add)
                    P.I(ACT, "activation", [em], [em], out=em[:], in_=em[:], func=AF.Exp)
                    P.I(DVE, "tensor_tensor", [pss, em], [gt], out=gt[:], in0=pss[:, 0:128], in1=em[:], op=ALU.mult)
                    P.I(ACT, "activation", [pr], [ecs], out=ecs[:], in_=pr[:, 0:128], func=AF.Exp)
                    P.I(DVE, "tensor_tensor", [CTg[i], ecs], [cp], out=cp[:], in0=CTg[i][:, cols], in1=ecs[:], op=ALU.mult)
                    py = Bk.f[4]
                    P.I(PE, "matmul", [gt, xdt], [py], py[:, 0:64], gt[:], xdt[:], start=True, stop=False)
                    P.I(PE, "matmul", [cp, Sbf], [py], py[:, 0:64], cp[:], Sbf[:], start=False, stop=True)
                    P.I(DVE, "scalar_tensor_tensor", [xg[i], sp_sb, py], [yg[i]], out=yg[i][:, bb, :], in0=xg[i][:, bb, :], scalar=sp_sb[:, 2 + d:3 + d], in1=py[:, 0:64], op0=ALU.mult, op1=ALU.add)
                    P.I(DVE, "scalar_tensor_tensor", [S, dec, psc], [S], out=S[:], in0=S[:], scalar=dec[:, 0:1], in1=psc[:, 0:64], op0=ALU.mult, op1=ALU.add)
                    P.I(ACT, "activation", [S], [Sbf], out=Sbf[:], in_=S[:], func=AF.Copy)
                outs.append(P.dma(SP, ytok[d, :, g0:g0 + GB, :], yg[i][:], reads=[yg[i]]))

    with P.scope():
        qg = [P.sb([128, GB * 128], F32, name=f"qg{i}") for i in range(2)]
        kg = [P.sb([128, GB * 128], F32, name=f"kg{i}") for i in range(2)]
        gg = [P.sb([128, GB, 128], F32, name=f"gg{i}") for i in range(2)]
        vg = [P.sb([128, GB, 5, 64], BF16, name=f"vg{i}") for i in range(2)]
        og = [P.sb([128, GB, 64], F32, name=f"og{i}") for i in range(2)]
        cum = P.sb([128, 128], F32, name="cum"); d1 = P.sb([128, 128], F32, name="d1"); d2 = P.sb([128, 128], F32, name="d2")
        e1 = P.sb([128, 128], F32, name="e1"); e2 = P.sb([128, 128], F32, name="e2"); e3 = P.sb([128, 128], F32, name="e3"); e4 = P.sb([128, 128], F32, name="e4")
        dec4 = P.sb([128, 4], F32, name="dec4")
        QiZ = P.sb([128, 4, 128], BF16, name="QiZ"); P.I(POOL, "memset", [], [QiZ], QiZ[:], 0.0)
        Qp = P.sb([128, 128], BF16, name="Qp"); Kp = P.sb([128, 128], BF16, name="Kp"); Kpp = P.sb([128, 128], BF16, name="Kpp")
        am = P.sb([128, 128], F32, name="am"); amb = P.sb([128, 128], BF16, name="amb"); Ktok = P.sb([128, 128], BF16, name="Ktok")
        S = P.sb([128, 64], F32, name="S_hg"); Sst = [P.sb([128, 4, 64], BF16, name=f"Sst{i}") for i in range(2)]
        c3 = lambda t: t[:].rearrange("p (c t) -> p c t", t=32)
        QiZd = bass.AP(QiZ[:].tensor, 0, [[QiZ[:].ap[0][0], 128], [128 + 32, 4], [1, 32]])
        gi = 0; blk = 0
        for d in range(2):
            P.I(DVE, "memset", [], [S], S[:], 0.0)
            P.I(POOL, "memset", [], [Sst[blk % 2]], Sst[blk % 2][:], 0.0)
            for g0 in range(0, NBLK, GB):
                i = gi % 2; gi += 1
                P.dma(SP, qg[i][:], qT[d, :, g0 * 128:(g0 + GB) * 128], writes=[qg[i]])
                P.dma(ACT, kg[i][:], kT[d, :, g0 * 128:(g0 + GB) * 128], writes=[kg[i]])
                P.dma(SP, gg[i][:], gtok[d, :, g0:g0 + GB, :], writes=[gg[i]])
                P.dma(POOL, vg[i][:], vZ[d, :, g0:g0 + GB, :, :], writes=[vg[i]])
                for bb in range(GB):
                    cols = slice(bb * 128, (bb + 1) * 128)
                    cur, nxt = Sst[blk % 2], Sst[(blk + 1) % 2]; blk += 1
                    pcum = Bk.f[0]
                    P.I(PE, "matmul", [gg[i], U4_sb], [pcum], pcum[:, 0:128], gg[i][:, bb, :], U4_sb[:], start=True, stop=True)
                    P.I(ACT, "activation", [pcum], [cum], out=cum[:], in_=pcum[:, 0:128], func=AF.Copy)
                    rmid = c3(cum)[:, :, 15:16].broadcast_to([128, 4, 32]); cend = c3(cum)[:, :, 31:32].broadcast_to([128, 4, 32])
                    P.I(DVE, "tensor_tensor", [cum], [d1], out=c3(d1), in0=c3(cum), in1=rmid, op=ALU.subtract)
                    P.I(DVE, "tensor_tensor", [cum], [d2], out=c3(d2), in0=c3(cum), in1=cend, op=ALU.subtract)
                    P.I(ACT, "activation", [cum], [e1], out=e1[:], in_=cum[:], func=AF.Exp)
                    P.I(ACT, "activation", [d1], [e2], out=e2[:], in_=d1[:], func=AF.Exp)
                    P.I(ACT, "activation", [d1], [e3], out=e3[:], in_=d1[:], func=AF.Exp, scale=-1.0)
                    P.I(ACT, "activation", [d2], [e4], out=e4[:], in_=d2[:], func=AF.Exp, scale=-1.0)
                    P.I(ACT, "activation", [cum], [dec4], out=dec4[:], in_=c3(cum)[:, :, 31], func=AF.Exp)
                    P.I(DVE, "tensor_tensor", [qg[i], e1], [QiZ], out=QiZd, in0=qg[i][:, cols].rearrange("p (c t) -> p c t", t=32), in1=c3(e1), op=ALU.mult)
                    P.I(DVE, "tensor_tensor", [qg[i], e2], [Qp], out=Qp[:], in0=qg[i][:, cols], in1=e2[:], op=ALU.mult)
                    P.I(DVE, "tensor_tensor", [kg[i], e3], [Kp], out=Kp[:], in0=kg[i][:, cols], in1=e3[:], op=ALU.mult)
                    P.I(DVE, "tensor_tensor", [kg[i], e4], [Kpp], out=Kpp[:], in0=kg[i][:, cols], in1=e4[:], op=ALU.mult)
                    pa = Bk.f[1]
                    P.I(PE, "matmul", [Kp, Qp], [pa], pa[:, 0:128], Kp[:], Qp[:], start=True, stop=True)
                    P.I(DVE, "tensor_scalar", [pa], [am], out=am[:], in0=pa[:, 0:128], scalar1=1e30, scalar2=-1e30, op0=ALU.min, op1=ALU.max)
                    P.I(DVE, "tensor_tensor", [am, U4_sb], [amb], out=amb[:], in0=am[:], in1=U4_sb[:], op=ALU.mult)
                    pt = Bk.h[0]
                    P.I(PE, "transpose", [Kpp, id_b], [pt], pt[:, 0:128], Kpp[:], id_b[:])
                    P.I(ACT, "activation", [pt], [Ktok], out=Ktok[:], in_=pt[:, 0:128], func=AF.Copy)
                    po = Bk.f[2]
                    P.I(PE, "matmul", [amb, vg[i]], [po], po[:, 0:64], amb[:], vg[i][:, bb, 4, :], start=True, stop=False)
                    for ci in range(4):
                        P.I(PE, "matmul", [QiZ, cur], [po], po[:, 0:64], QiZ[:, ci, :], cur[:, ci, :], start=False, stop=(ci == 3))
                        if True:
                            psc = Bk.f[3 + ci % 2]
                            P.I(PE, "matmul", [Ktok, vg[i]], [psc], psc[:, 0:64], Ktok[:], vg[i][:, bb, ci, :], start=True, stop=True)
                            P.I(DVE, "scalar_tensor_tensor", [S, dec4, psc], [S], out=S[:], in0=S[:], scalar=dec4[:, ci:ci + 1], in1=psc[:, 0:64], op0=ALU.mult, op1=ALU.add)
                            if ci < 3:
                                P.I(ACT, "activation", [S], [cur], out=cur[:, ci + 1, :], in_=S[:], func=AF.Copy)
                            else:
                                P.I(ACT, "activation", [S], [nxt], out=nxt[:, 0, :], in_=S[:], func=AF.Copy)
                    P.I(ACT, "activation", [po], [og[i]], out=og[i][:, bb, :], in_=po[:, 0:64], func=AF.Copy)
                outs.append(P.dma(SP, otok[d, :, g0:g0 + GB, :], og[i][:], reads=[og[i]]))
    return P.finish(outs)


def gather_D(results):
    full = np.concatenate([results[c]['outT'] for c in range(NCORE)], axis=1)
    return full, results[0]['outcT']


def prep_E(inp, dfull, dctx):
    def seq(rows_lat, rows_ctx, d):
        if d == 0:
            return np.concatenate([rows_ctx, rows_lat], axis=1)
        return np.concatenate([rows_ctx[:, ::-1], rows_lat[:, ::-1]], axis=1)

    def blk(a):
        return np.ascontiguousarray(a.T.reshape(NBLK, 128, a.shape[0]).transpose(1, 0, 2))
    s_ = np.arange(128)[:, None]; l_ = np.arange(128)[None, :]
    U = (s_ <= l_).astype(np.float32)
    U4 = ((s_ // 32 == l_ // 32) & (s_ <= l_)).astype(np.float32)
    Mneg = np.where(s_ <= l_, 0.0, MASKNEG).astype(np.float32)
    maps = []
    for c in range(NCORE):
        h = c; g = c // 4; hh = c // 2; vh = c % 2
        R = lambda r0, n, d: seq(dfull[r0:r0 + n], dctx[r0:r0 + n], d)
        m = dict(U=U, U4=U4, Mneg=Mneg, ident=np.eye(128, dtype=np.float32), onesd=np.ones((128, 128), np.float32))
        m['xtok'] = np.stack([blk(R(64 * h, 64, d)) for d in range(2)])
        m['Btok'] = np.stack([blk(R(512 + 128 * g, 128, d)) for d in range(2)])
        m['BT'] = np.stack([np.ascontiguousarray(R(512 + 128 * g, 128, d)) for d in range(2)])
        m['CT'] = np.stack([np.ascontiguousarray(R(768 + 128 * g, 128, d)) for d in range(2)])
        m['dttok'] = np.stack([np.ascontiguousarray(R(5120 + 8 * d + h, 1, d)[0].reshape(NBLK, 128).T) for d in range(2)])
        al = inp['ssd_A_log'][0]; dd = inp['ssd_D'][0]
        m['ssdp'] = np.ascontiguousarray(np.broadcast_to(np.array([al[0, h], al[1, h], dd[0, h], dd[1, h]], np.float32)[None, :], (128, 4)))
        m['qT'] = np.stack([np.ascontiguousarray(R(4096 + 128 * hh, 128, d)) for d in range(2)])
        m['kT'] = np.stack([np.ascontiguousarray(R(1024 + 512 * d + 128 * hh, 128, d)) for d in range(2)])
        m['gtok'] = np.stack([blk(R(2048 + 512 * d + 128 * hh, 128, d)) for d in range(2)])
        vz = []
        for d in range(2):
            v = blk(R(3072 + 128 * hh + 64 * vh, 64, d))
            z5 = np.zeros((128, NBLK, 5, 64), np.float32)
            z5[:, :, 4, :] = v
            for i in range(4):
                z5[32 * i:32 * i + 32, :, i, :] = v[32 * i:32 * i + 32]
            vz.append(z5)
        m['vZ'] = np.stack(vz)
        maps.append(m)
    return blobify(maps, BLOB_E)


def gather_E(results):
    yT = np.zeros((2, 512, SEQ), np.float32); oT = np.zeros((2, 512, SEQ), np.float32)
    for c in range(NCORE):
        h = c; hh = c // 2; vh = c % 2
        for d in range(2):
            for name, dst, r0 in (("ytok", yT, 64 * h), ("otok", oT, 128 * hh + 64 * vh)):
                a = results[c][name][d].transpose(1, 0, 2).reshape(LS, 64)[NCTX:]
                if d == 1:
                    a = a[::-1]
                dst[d, r0:r0 + 64] = a.T
    return yT, oT


def build_M():
    P = Prog()
    Bk = Banks(P)
    di = lambda n, s, dt=F32: P.dram(n, s, dt, "ExternalInput")
    yT = di("yT", [2, 512, TLOC]); oT = di("oT", [2, 512, TLOC]); szT = di("szT", [512, TLOC]); sgT = di("sgT", [512, TLOC])
    bl = Blob(BLOB_M).dram(P)
    nrm = bl.ap("nrm"); onesd = bl.ap("onesd")
    mT = P.dram("mT", [D, TLOC], F32, "ExternalOutput")
    ones_sb = P.sb([128, 128], F32, name="ones"); P.dma(SP, ones_sb[:], onesd[:, :], writes=[ones_sb])
    nrm_sb = P.sb([128, 8], F32, name="nrm"); P.dma(SP, nrm_sb[:], nrm[:, :], writes=[nrm_sb])
    a = [P.sb([128, 4, 512], F32, name=f"ma{i}") for i in range(2)]
    b = [P.sb([128, 4, 512], F32, name=f"mb{i}") for i in range(2)]
    g = [P.sb([128, 4, 512], F32, name=f"mg{i}") for i in range(2)]
    sq = P.sb([128, 4, 512], F32, name="msq"); rs = P.sb([128, 512], F32, name="mrs")
    outs = []
    it = 0
    for part in range(2):
        src = yT if part == 0 else oT
        gsrc = szT if part == 0 else sgT
        for t0 in range(0, TLOC, 512):
            i = it % 2; it += 1
            P.dma(SP, a[i][:], src[0, :, t0:t0 + 512].rearrange("(k p) t -> p k t", p=128), writes=[a[i]])
            P.dma(ACT, b[i][:], src[1, :, t0:t0 + 512].rearrange("(k p) t -> p k t", p=128), writes=[b[i]])
            P.dma(SP, g[i][:], gsrc[:, t0:t0 + 512].rearrange("(k p) t -> p k t", p=128), writes=[g[i]])
            P.I(DVE, "tensor_tensor", [a[i], b[i]], [a[i]], out=a[i][:], in0=a[i][:], in1=b[i][:], op=ALU.add)
            if part == 0:
                P.I(DVE, "tensor_tensor", [a[i], g[i]], [a[i]], out=a[i][:], in0=a[i][:], in1=g[i][:], op=ALU.mult)
            P.I(ACT, "activation", [a[i]], [sq], out=sq[:], in_=a[i][:], func=AF.Square)
            groups = [(0, 2), (2, 4)] if part == 0 else [(0, 1), (1, 2), (2, 3), (3, 4)]
            for (k0, k1) in groups:
                ps = Bk.f[k0 % 2]
                for kc in range(k0, k1):
                    P.I(PE, "matmul", [ones_sb, sq], [ps], ps[:, 0:512], ones_sb[:], sq[:, kc, :], start=(kc == k0), stop=(kc == k1 - 1))
                P.I(DVE, "tensor_scalar", [ps], [rs], out=rs[:], in0=ps[:, 0:512], scalar1=1.0 / (128 * (k1 - k0)), scalar2=EPS, op0=ALU.mult, op1=ALU.add)
                P.I(ACT, "activation", [rs], [rs], out=rs[:], in_=rs[:], func=AF.Ln)
                P.I(ACT, "activation", [rs], [rs], out=rs[:], in_=rs[:], func=AF.Exp, scale=-0.5)
                for kc in range(k0, k1):
                    P.I(DVE, "scalar_tensor_tensor", [a[i], nrm_sb, rs], [b[i]], out=b[i][:, kc, :], in0=a[i][:, kc, :], scalar=nrm_sb[:, part * 4 + kc:part * 4 + kc + 1],
                        in1=rs[:], op0=ALU.mult, op1=ALU.mult)
            if part == 1:
                P.I(DVE, "tensor_tensor", [b[i], g[i]], [b[i]], out=b[i][:], in0=b[i][:], in1=g[i][:], op=ALU.mult)
            outs.append(P.dma(POOL, mT[part * 512:(part + 1) * 512, t0:t0 + 512].rearrange("(k p) t -> p k t", p=128), b[i][:], reads=[b[i]]))
    return P.finish(outs)


def prep_M(inp, yT, oT, dfull):
    nrm = np.concatenate([col_layout(inp['ssd_norm'][0], 4), col_layout(inp['hg_norm'][0], 4)], axis=1)
    maps = []
    for c in range(NCORE):
        sl = slice(c * TLOC, (c + 1) * TLOC)
        maps.append(dict(yT=np.ascontiguousarray(yT[:, :, sl]), oT=np.ascontiguousarray(oT[:, :, sl]),
                         szT=np.ascontiguousarray(dfull[3584:4096, sl]), sgT=np.ascontiguousarray(dfull[4608:5120, sl]),
                         nrm=np.ascontiguousarray(nrm), onesd=np.ones((128, 128), np.float32)))
    return blobify(maps, BLOB_M)


def _run(nc, maps, tag=""):
    import time, sys
    t0 = time.time()
    r = run_bass_kernel_spmd(nc, maps, core_ids=list(range(len(maps)))).results
    print(f"[kernel] launch {tag}: {time.time() - t0:.1f}s", file=sys.stderr, flush=True)
    return r


def kernel(**inp):
    inp = {k: np.asarray(v) for k, v in inp.items()}
    x = inp['x'][0]; ctx = inp['ctx'][0]
    xT = np.ascontiguousarray(x.T); xcT = np.ascontiguousarray(ctx.T)
    rA = _run(build_A(), prep_A(inp), 'A')
    uT = np.concatenate([rA[c]['uT'] for c in range(NCORE)], axis=1)
    attT = np.concatenate([rA[c]['attT'] for c in range(NCORE)], axis=1)
    ucT = rA[0]['ucT']; attcT = rA[0]['attcT']
    hyT = gather_B(_run(build_B(SEQ), prep_B(inp, uT, SEQ), 'B'), SEQ)
    hycT = gather_B(_run(build_B(NCTX), prep_B(inp, ucT, NCTX), 'Bc'), NCTX)
    mT = np.concatenate([hyT, attT], axis=0); mcT = np.concatenate([hycT, attcT], axis=0)
    maps, passes = prep_C(inp, 0, mT, xT, mcT, xcT)
    xT, xcT = gather_C(_run(build_C(passes), maps, 'C0'), True)
    dfull, dctx = gather_D(_run(build_D(), prep_D(inp, xT, xcT), 'D'))
    yT, oT = gather_E(_run(build_E(), prep_E(inp, dfull, dctx), 'E'))
    rM = _run(build_M(), prep_M(inp, yT, oT, dfull), 'M')
    mT = np.concatenate([rM[c]['mT'] for c in range(NCORE)], axis=1)
    maps, passes = prep_C(inp, 1, mT, xT, None, None)
    xT, _ = gather_C(_run(build_C(passes), maps, 'C1'), False)
    return np.ascontiguousarray(xT.T)[None].astype(np.float32)
```

```python
import contextlib
import numpy as np
import concourse.bass as bass
import concourse.mybir as mybir
from concourse.bass_utils import run_bass_kernel_spmd

F32 = mybir.dt.float32
BF16 = mybir.dt.bfloat16
I32 = mybir.dt.int32
AF = mybir.ActivationFunctionType
ALU = mybir.AluOpType
AX = mybir.AxisListType

PE, DVE, ACT, POOL, SP = "tensor", "vector", "scalar", "gpsimd", "sync"
COMPUTE = (PE, DVE, ACT, POOL)
NDMASEM = 8
EPOCH_LEN = 20000


class Prog:
    def __init__(self):
        self.nc = bass.Bass("TRN2", target_bir_lowering=False)
        self.stack = contextlib.ExitStack()
        self.streams = {e: [] for e in (PE, DVE, ACT, POOL, SP)}
        self.cnt = {e: 0 for e in COMPUTE}
        self.dcnt = {e: 0 for e in (SP, ACT, POOL)}
        self.sem = {}
        self.dsem = {}
        self.waited = {}
        self.lastw = {}
        self.reads = {}
        self.ntens = 0
        self.out_tokens = []
        self.epoch = {e: 0 for e in COMPUTE}
        self.root_stack = self.stack
        for e in COMPUTE:
            self.sem[(e, 0)] = self.stack.enter_context(self.nc.semaphore("s_" + e + "_0"))
        for q in (SP, ACT, POOL):
            self.dsem[q] = [self.stack.enter_context(self.nc.semaphore(f"d_{q}_{i}")) for i in range(NDMASEM)]

    @contextlib.contextmanager
    def scope(self):
        outer = self.stack
        self.stack = contextlib.ExitStack()
        try:
            yield
        finally:
            self.barrier()
            self.stack.close()
            self.stack = outer

    def barrier(self):
        toks = [("c", e, (self.epoch[e], self.cnt[e])) for e in COMPUTE if self.cnt[e] > 0]
        for q in (SP, ACT, POOL):
            for k in range(max(0, self.dcnt[q] - NDMASEM), self.dcnt[q]):
                toks.append(("d", q, k))
        for st in (PE, DVE, ACT, POOL, SP):
            self._emit_waits(st, [t for t in toks if not (t[0] == "c" and t[1] == st)])

    def dram(self, name, shape, dtype, kind):
        return self.nc.dram_tensor(name, list(shape), dtype, kind=kind).ap()

    def sb(self, shape, dtype, name=None):
        self.ntens += 1
        name = "sb_" + (name or f"t{self.ntens}")
        return self.stack.enter_context(self.nc.sbuf_tensor(name, list(shape), dtype))

    def ps(self, shape, dtype=F32, name=None):
        self.ntens += 1
        name = "ps_" + (name or f"p{self.ntens}")
        return self.stack.enter_context(self.nc.psum_tensor(name, list(shape), dtype))

    def _key(self, t):
        if isinstance(t, str):
            return t
        if isinstance(t, tuple):
            return t
        th = getattr(t, "tensor", t)
        return getattr(th, "name", None) or id(th)

    def _tok_sem_val(self, tok):
        kind, e, i = tok
        if kind == "c":
            ep, idx = i
            return self.sem[(e, ep)], idx, ("c", e, ep)
        return self.dsem[e][i % NDMASEM], 16 * (i // NDMASEM + 1), ("d", e, i % NDMASEM)

    def _emit_waits(self, stream, toks):
        need = {}
        for tok in toks:
            if tok is None:
                continue
            s, v, k = self._tok_sem_val(tok)
            if tok[0] == "c" and tok[1] == stream and stream == PE:
                continue
            if k not in need or need[k][1] < v:
                need[k] = (s, v)
        for k, (s, v) in need.items():
            if self.waited.get((stream, k), 0) >= v:
                continue
            self.waited[(stream, k)] = v
            self.streams[stream].append(("wait", s, v))

    def _deps(self, reads, writes):
        toks = []
        for t in reads:
            k = self._key(t)
            toks.append(self.lastw.get(k))
        for t in writes:
            k = self._key(t)
            toks.append(self.lastw.get(k))
            toks.extend(self.reads.get(k, []))
        return toks

    def _commit(self, tok, reads, writes):
        for t in reads:
            k = self._key(t)
            self.reads.setdefault(k, []).append(tok)
            if len(self.reads[k]) > 24:
                best = {}
                for tk in self.reads[k]:
                    kk = (tk[0], tk[1], tk[2][0]) if tk[0] == "c" else tk
                    if kk not in best or best[kk][2] < tk[2]:
                        best[kk] = tk
                self.reads[k] = list(best.values())
        for t in writes:
            k = self._key(t)
            self.lastw[k] = tok
            self.reads[k] = []

    def op(self, eng, fn, reads=(), writes=()):
        self._emit_waits(eng, self._deps(reads, writes))
        if self.cnt[eng] >= EPOCH_LEN:
            self.epoch[eng] += 1
            self.cnt[eng] = 0
            self.sem[(eng, self.epoch[eng])] = self.root_stack.enter_context(self.nc.semaphore(f"s_{eng}_{self.epoch[eng]}"))
        self.cnt[eng] += 1
        tok = ("c", eng, (self.epoch[eng], self.cnt[eng]))
        self.streams[eng].append(("op", fn, self.sem[(eng, self.epoch[eng])], 1))
        self._commit(tok, reads, writes)
        return tok

    def I(self, eng, mname, reads, writes, *args, **kw):
        return self.op(eng, lambda e: getattr(e, mname)(*args, **kw), reads=reads, writes=writes)

    def dma(self, q, out, in_, reads=(), writes=(), **kw):
        k = self.dcnt[q]
        toks = self._deps(reads, writes)
        if k >= NDMASEM:
            toks.append(("d", q, k - NDMASEM))
        self._emit_waits(q, toks)
        self.dcnt[q] += 1
        tok = ("d", q, k)
        self.streams[q].append(("op", lambda e: e.dma_start(out=out, in_=in_, **kw), self.dsem[q][k % NDMASEM], 16))
        self._commit(tok, reads, writes)
        return tok

    def finish(self, final_toks):
        self._emit_waits(SP, final_toks)
        nc = self.nc
        streams = self.streams

        def run(engine, lst):
            for it in lst:
                if it[0] == "wait":
                    engine.wait_ge(it[1], it[2])
                else:
                    it[1](engine).then_inc(it[2], it[3])

        with nc.Block() as block:
            @block.sync
            def _(e):
                run(e, streams[SP])

            @block.tensor
            def _(e):
                run(e, streams[PE])

            @block.vector
            def _(e):
                run(e, streams[DVE])

            @block.scalar
            def _(e):
                run(e, streams[ACT])

            @block.gpsimd
            def _(e):
                run(e, streams[POOL])
        self.stack.close()
        return nc


D = 1024
SEQ = 16384
NCORE = 8
TLOC = SEQ // NCORE
HALO = 128
TEXT = TLOC + 2 * HALO
NCTX = 256
EPS = 1e-6
MASKNEG = -30000.0


def _bcast_free(ap2d, n):
    return ap2d.unsqueeze(2).broadcast_to([ap2d.shape[0], ap2d.shape[1], n])


class Blob:
    def __init__(self, items):
        self.items = {}
        o = 0
        for name, rows, cols in items:
            self.items[name] = (rows, o, cols); o += cols
        self.total = o
        self.t = None

    def dram(self, P):
        self.t = P.dram("blob", [128, self.total], F32, "ExternalInput")
        return self

    def ap(self, name):
        rows, o, cols = self.items[name]
        return self.t[0:rows, o:o + cols]

    def pack(self, d):
        out = np.zeros((128, self.total), np.float32)
        for name, (rows, o, cols) in self.items.items():
            out[0:rows, o:o + cols] = np.asarray(d[name], np.float32).reshape(rows, cols)
        return out


BLOB_A = [("cvec", 128, 16), ("adab", 128, 24), ("normg", 128, 8), ("gains", 128, 640), ("masks", 128, 512), ("sinkrow", 128, 1024),
          ("ident", 128, 128), ("onesd", 128, 128), ("convw", 128, 36), ("convb", 128, 12), ("edge", 128, 2)]
BLOB_C = [("cvec", 128, 16), ("adab", 128, 32), ("normg", 128, 8), ("rw", 128, 256), ("rb", 128, 32), ("bgu", 128, 512),
          ("ident", 128, 128), ("onesd", 128, 128)]
BLOB_B = [("w1", 33, 64), ("w2", 64, 64), ("w3", 64, 64), ("w4s", 64, 256), ("fqb", 64, 4), ("negd", 128, 1), ("fbias", 128, 128),
          ("ident", 128, 128), ("onesd", 128, 128), ("jmat", 128, 128)]
BLOB_D = [("cvec", 128, 16), ("adab", 128, 24), ("normg", 128, 8), ("onesd", 128, 128), ("convw", 128, 24), ("convb", 128, 8),
          ("edge", 128, 2), ("hglb", 128, 8), ("dtb", 16, 1)]
BLOB_E = [("ssdp", 128, 4), ("U", 128, 128), ("U4", 128, 128), ("Mneg", 128, 128), ("ident", 128, 128), ("onesd", 128, 128)]
BLOB_M = [("nrm", 128, 8), ("onesd", 128, 128)]


def blobify(maps, spec):
    bl = Blob(spec)
    out = []
    for m in maps:
        m2 = {k: v for k, v in m.items() if k not in bl.items}
        m2['blob'] = bl.pack(m)
        out.append(m2)
    return out


class Banks:
    def __init__(self, P, nb16=1):
        self.f = [P.ps([128, 512], F32, name=f"bank{i}") for i in range(8 - nb16)]
        self.h = [P.ps([128, 1024], BF16, name=f"bankh{i}") for i in range(nb16)]


def emit_adaln(P, B, cv_sb, adaw_dram, adab_sb, wbuf, out_sb, ncol, nparts=3):
    sc = P.sb([128, 8, ncol], F32, name="ada_silu")
    P.op(ACT, lambda e: e.activation(out=sc[:], in_=cv_sb[:], func=AF.Silu), reads=[cv_sb], writes=[sc])
    ps = B.f[0]
    for part in range(nparts):
        for kc in range(8):
            P.dma(SP if kc % 2 == 0 else ACT, wbuf[:, kc, :], adaw_dram[kc * 128:(kc + 1) * 128, part * 1024:(part + 1) * 1024],
                  writes=[wbuf])
        for o in range(8):
            oc = part * 8 + o
            for kc in range(8):
                P.op(PE, lambda e, o=o, kc=kc, oc=oc: e.matmul(ps[:, oc * ncol:(oc + 1) * ncol], wbuf[:, kc, o * 128:(o + 1) * 128],
                                                               sc[:, kc, :], start=(kc == 0), stop=(kc == 7)),
                     reads=[wbuf, sc], writes=[ps])
    P.op(DVE, lambda e: e.tensor_tensor(out=out_sb[:], in0=ps[:, 0:8 * nparts * ncol].rearrange("p (o n) -> p o n", n=ncol),
                                        in1=_bcast_free(adab_sb[:], ncol), op=ALU.add),
         reads=[ps, adab_sb], writes=[out_sb])


def emit_norm_mod(P, B, x_sb, ones_sb, A_col, B_col, h_out, ntok, tmp_sq, tmp_rs, hcol0=0):
    ps = B.f[1]
    for kc in range(8):
        P.op(ACT, lambda e, kc=kc: e.activation(out=tmp_sq[:, kc, 0:ntok], in_=x_sb[:, kc, 0:ntok], func=AF.Square),
             reads=[x_sb], writes=[tmp_sq])
    for kc in range(8):
        P.op(PE, lambda e, kc=kc: e.matmul(ps[:, 0:ntok], ones_sb[:], tmp_sq[:, kc, 0:ntok], start=(kc == 0), stop=(kc == 7)),
             reads=[ones_sb, tmp_sq], writes=[ps])
    P.op(DVE, lambda e: e.tensor_scalar(out=tmp_rs[:, 0:ntok], in0=ps[:, 0:ntok], scalar1=1.0 / D, scalar2=EPS,
                                        op0=ALU.mult, op1=ALU.add), reads=[ps], writes=[tmp_rs])
    P.op(ACT, lambda e: e.activation(out=tmp_rs[:, 0:ntok], in_=tmp_rs[:, 0:ntok], func=AF.Ln), reads=[tmp_rs], writes=[tmp_rs])
    P.op(ACT, lambda e: e.activation(out=tmp_rs[:, 0:ntok], in_=tmp_rs[:, 0:ntok], func=AF.Exp, scale=-0.5), reads=[tmp_rs], writes=[tmp_rs])
    for kc in range(8):
        P.op(DVE, lambda e, kc=kc: e.tensor_tensor(out=tmp_sq[:, kc, 0:ntok], in0=x_sb[:, kc, 0:ntok], in1=tmp_rs[:, 0:ntok],
                                                   op=ALU.mult), reads=[x_sb, tmp_rs, tmp_sq], writes=[tmp_sq])
        P.op(ACT, lambda e, kc=kc: e.activation(out=h_out[:, kc, hcol0:hcol0 + ntok], in_=tmp_sq[:, kc, 0:ntok], func=AF.Identity,
                                                bias=B_col[:, kc:kc + 1], scale=A_col[:, kc:kc + 1]),
             reads=[tmp_sq, A_col, B_col], writes=[h_out])


TT = 384
NTT = TEXT // TT


def build_A():
    P = Prog()
    Bk = Banks(P)
    di = lambda n, s, dt=F32: P.dram(n, s, dt, "ExternalInput")
    do = lambda n, s, dt=F32: P.dram(n, s, dt, "ExternalOutput")
    xT = di("xT", [D, TEXT]); ctxT = di("ctxT", [D, NCTX])
    bl = Blob(BLOB_A).dram(P)
    cvec = bl.ap("cvec"); adaw = di("adaw", [D, 3072]); adab = bl.ap("adab")
    normg = bl.ap("normg"); w_in = di("w_in", [D, 2304])
    gains = bl.ap("gains"); ctab = di("ctab", [TEXT, 64]); stab = di("stab", [TEXT, 64])
    masks = bl.ap("masks"); sinkrow = bl.ap("sinkrow")
    ident = bl.ap("ident"); onesd = bl.ap("onesd")
    convw = bl.ap("convw"); convb = bl.ap("convb"); edge = bl.ap("edge")
    uT = do("uT", [1536, TLOC]); ucT = do("ucT", [1536, NCTX])
    attT = do("attT", [512, TLOC]); attcT = do("attcT", [512, NCTX])

    ones_sb = P.sb([128, 128], F32, name="ones"); P.dma(SP, ones_sb[:], onesd[:, :], writes=[ones_sb])
    id_f = P.sb([128, 128], F32, name="idf"); P.dma(SP, id_f[:], ident[:, :], writes=[id_f])
    id_b = P.sb([128, 128], BF16, name="idb"); P.dma(POOL, id_b[:], ident[:, :], writes=[id_b])
    cv = P.sb([128, 8, 2], F32, name="cv"); P.dma(SP, cv[:], cvec.rearrange("p (k n) -> p k n", n=2), writes=[cv])
    adab_sb = P.sb([128, 24], F32, name="adab"); P.dma(SP, adab_sb[:], adab[:, :], writes=[adab_sb])
    g_sb = P.sb([128, 8], F32, name="normg"); P.dma(SP, g_sb[:], normg[:, :], writes=[g_sb])
    gains_sb = P.sb([128, 640], F32, name="gains"); P.dma(SP, gains_sb[:], gains[:, :], writes=[gains_sb])
    mask_sb = P.sb([128, 4, 128], BF16, name="masks")
    P.dma(POOL, mask_sb[:], masks.rearrange("k (m q) -> k m q", m=4), writes=[mask_sb])
    sink_sb = P.sb([128, 1024], F32, name="sink"); P.dma(SP, sink_sb[:], sinkrow[:, :], writes=[sink_sb])
    P.op(ACT, lambda e: e.activation(out=sink_sb[:], in_=sink_sb[:], func=AF.Exp), reads=[sink_sb], writes=[sink_sb])
    w_sb = P.sb([128, 8, 2304], BF16, name="w_in")
    for kc in range(8):
        P.dma(POOL, w_sb[:, kc, :], w_in[kc * 128:(kc + 1) * 128, :], writes=[w_sb])

    ada = P.sb([128, 24, 2], F32, name="ada")
    with P.scope():
        wbuf = P.sb([128, 8, 1024], F32, name="adawbuf")
        emit_adaln(P, Bk, cv, adaw, adab_sb, wbuf, ada, 2)
    Acol = [P.sb([128, 8], F32, name=f"Acol{j}") for j in range(2)]
    Bcol = [P.sb([128, 8], F32, name=f"Bcol{j}") for j in range(2)]
    for j in range(2):
        P.op(DVE, lambda e, j=j: e.scalar_tensor_tensor(out=Acol[j][:], in0=ada[:, 8:16, j], scalar=1.0, in1=g_sb[:],
                                                         op0=ALU.add, op1=ALU.mult), reads=[ada, g_sb], writes=[Acol[j]])
        P.op(DVE, lambda e, j=j: e.tensor_copy(out=Bcol[j][:], in_=ada[:, 0:8, j]), reads=[ada], writes=[Bcol[j]])

    kqT = P.sb([64, 10, TEXT + NCTX], BF16, name="kqT")
    vaug = P.sb([128, (TEXT + NCTX) // 128, 2, 65], BF16, name="vaug")
    P.op(POOL, lambda e: e.memset(vaug[:], 1.0), writes=[vaug])

    x_sb = P.sb([128, 8, TT], F32, name="x_sb")
    sq_sb = P.sb([128, 8, TT], F32, name="sq_sb")
    rs_sb = P.sb([128, TT], F32, name="rs_sb")
    h_all = P.sb([128, 8, TEXT + NCTX], BF16, name="h_all")
    cw_sb = P.sb([128, 12, 3], F32, name="cw"); P.dma(SP, cw_sb[:], convw.rearrange("p (o t) -> p o t", t=3), writes=[cw_sb])
    cb_sb = P.sb([128, 12], F32, name="cb"); P.dma(SP, cb_sb[:], convb[:, :], writes=[cb_sb])
    edge_sb = P.sb([128, 2], F32, name="edge"); P.dma(SP, edge_sb[:], edge[:, :], writes=[edge_sb])
    kqv = P.sb([128, 768], F32, name="kqv")
    sq2 = P.sb([128, 640], F32, name="sq2")
    ss = P.sb([128, 10], F32, name="ss")
    tmpr = P.sb([128, 640], F32, name="tmpr")
    kqb = P.sb([128, 640], BF16, name="kqb")
    ct_sb = P.sb([128, 64], F32, name="ct"); st_sb = P.sb([128, 64], F32, name="st")
    u_sb = [P.sb([128, 512], F32, name=f"u_sb{i}") for i in range(2)]
    acc_sb = [P.sb([128, 512], F32, name=f"acc_sb{i}") for i in range(2)]

    def proj_tile(src_dram, col0, ntok, j, tok_base, is_ctx):
        for kc in range(8):
            P.dma(SP if kc % 2 == 0 else ACT, x_sb[:, kc, 0:ntok], src_dram[kc * 128:(kc + 1) * 128, col0:col0 + ntok], writes=[x_sb])
        emit_norm_mod(P, Bk, x_sb, ones_sb, Acol[j], Bcol[j], h_all, ntok, sq_sb, rs_sb, hcol0=tok_base)
        for s in range(ntok // 128):
            pa, pb = Bk.f[2], Bk.f[3]
            for kc in range(8):
                P.op(PE, lambda e, kc=kc, s=s: e.matmul(pa[:, 0:512], h_all[:, kc, tok_base + s * 128:tok_base + (s + 1) * 128], w_sb[:, kc, 0:512],
                                                        start=(kc == 0), stop=(kc == 7)), reads=[h_all, w_sb], writes=[pa])
            for kc in range(8):
                P.op(PE, lambda e, kc=kc, s=s: e.matmul(pb[:, 0:256], h_all[:, kc, tok_base + s * 128:tok_base + (s + 1) * 128], w_sb[:, kc, 512:768],
                                                        start=(kc == 0), stop=(kc == 7)), reads=[h_all, w_sb], writes=[pb])
            P.op(ACT, lambda e: e.activation(out=kqv[:, 0:512], in_=pa[:, 0:512], func=AF.Copy), reads=[pa], writes=[kqv])
            P.op(ACT, lambda e: e.activation(out=kqv[:, 512:768], in_=pb[:, 0:256], func=AF.Copy), reads=[pb], writes=[kqv])
            tile_idx = (tok_base + s * 128) // 128
            P.op(POOL, lambda e, ti=tile_idx: e.tensor_copy(out=vaug[:, ti, :, 0:64], in_=kqv[:, 640:768].rearrange("p (g d) -> p g d", d=64)),
                 reads=[kqv], writes=[vaug])
            P.op(DVE, lambda e: e.tensor_tensor(out=sq2[:], in0=kqv[:, 0:640], in1=kqv[:, 0:640], op=ALU.mult), reads=[kqv], writes=[sq2])
            P.op(DVE, lambda e: e.tensor_reduce(out=ss[:], in_=sq2[:].rearrange("p (h d) -> p h d", d=64), axis=AX.X, op=ALU.add),
                 reads=[sq2], writes=[ss])
            P.op(DVE, lambda e: e.tensor_scalar(out=ss[:], in0=ss[:], scalar1=1.0 / 64, scalar2=EPS, op0=ALU.mult, op1=ALU.add),
                 reads=[ss], writes=[ss])
            P.op(ACT, lambda e: e.activation(out=ss[:], in_=ss[:], func=AF.Ln), reads=[ss], writes=[ss])
            P.op(ACT, lambda e: e.activation(out=ss[:], in_=ss[:], func=AF.Exp, scale=-0.5), reads=[ss], writes=[ss])
            P.op(DVE, lambda e: e.tensor_tensor(out=sq2[:].rearrange("p (h d) -> p h d", d=64), in0=kqv[:, 0:640].rearrange("p (h d) -> p h d", d=64),
                                                in1=_bcast_free(ss[:], 64), op=ALU.mult), reads=[kqv, ss], writes=[sq2])
            P.op(DVE, lambda e: e.tensor_tensor(out=sq2[:], in0=sq2[:], in1=gains_sb[:], op=ALU.mult), reads=[sq2, gains_sb], writes=[sq2])
            if not is_ctx:
                r0 = col0 + s * 128
                P.dma(SP, ct_sb[:], ctab[r0:r0 + 128, :], writes=[ct_sb])
                P.dma(ACT, st_sb[:], stab[r0:r0 + 128, :], writes=[st_sb])
                v5 = lambda t: t[:].rearrange("p (h b two s) -> p (h b) two s", b=2, two=2, s=16)
                stv = st_sb[:].rearrange("p (b two s) -> p b two s", two=2, s=16)
                ctv = ct_sb[:].rearrange("p (b two s) -> p b two s", two=2, s=16)

                def bc(tv, two):
                    a = tv[:, :, two, :]
                    return a.unsqueeze(1).broadcast_to([128, 10, 2, 16])
                u4 = sq2[:].rearrange("p (h b two s) -> p h b two s", b=2, two=2, s=16)
                t4 = tmpr[:].rearrange("p (h b two s) -> p h b two s", b=2, two=2, s=16)
                for two in range(2):
                    P.op(DVE, lambda e, two=two: e.tensor_tensor(out=t4[:, :, :, two, :], in0=u4[:, :, :, 1 - two, :], in1=bc(stv, two), op=ALU.mult),
                         reads=[sq2, st_sb], writes=[tmpr])
                for two in range(2):
                    P.op(DVE, lambda e, two=two: e.tensor_tensor(out=u4[:, :, :, two, :], in0=u4[:, :, :, two, :], in1=bc(ctv, two), op=ALU.mult),
                         reads=[sq2, ct_sb], writes=[sq2])
                P.op(DVE, lambda e: e.tensor_tensor(out=kqb[:], in0=sq2[:], in1=tmpr[:], op=ALU.add), reads=[sq2, tmpr], writes=[kqb])
            else:
                P.op(DVE, lambda e: e.tensor_copy(out=kqb[:], in_=sq2[:]), reads=[sq2], writes=[kqb])
            pt = Bk.h[0]
            for hh in range(10):
                P.op(PE, lambda e, hh=hh: e.transpose(pt[0:64, hh * 128:(hh + 1) * 128][:, 0:128] if False else pt[0:64, hh * 128 % 1024:(hh * 128 % 1024) + 128],
                                                      kqb[:, hh * 64:(hh + 1) * 64], id_b[:]),
                     reads=[kqb, id_b], writes=[pt])
                if hh == 7 or hh == 9:
                    h0 = 0 if hh == 7 else 8
                    nh = hh - h0 + 1
                    t0 = tok_base + s * 128
                    P.op(ACT, lambda e, h0=h0, nh=nh, t0=t0: e.activation(
                        out=kqT[:, h0:h0 + nh, t0:t0 + 128],
                        in_=pt[0:64, (h0 * 128) % 1024:(h0 * 128) % 1024 + nh * 128].rearrange("p (h t) -> p h t", t=128), func=AF.Copy),
                        reads=[pt], writes=[kqT])

    def hyena_tile(h0, n, out_dram, ocol0, zl, zr, el, er):
        a0 = h0 - (0 if zl else 1); a1 = h0 + n + (0 if zr else 1)
        w = a1 - a0
        off = 1 if zl else 0
        for oc in range(12):
            pu = Bk.f[4 + oc % 2]
            ub = u_sb[oc % 2]; ac = acc_sb[oc % 2]
            for kc in range(8):
                P.I(PE, "matmul", [w_sb, h_all], [pu], pu[:, off:off + w], w_sb[:, kc, 768 + oc * 128:768 + (oc + 1) * 128], h_all[:, kc, a0:a1],
                    start=(kc == 0), stop=(kc == 7))
            if zl:
                P.I(POOL, "memset", [], [ub], ub[:, 0:1], 0.0)
            if zr:
                P.I(POOL, "memset", [], [ub], ub[:, n + 1:n + 2], 0.0)
            P.I(ACT, "activation", [pu], [ub], out=ub[:, off:off + w], in_=pu[:, off:off + w], func=AF.Copy)
            if el:
                P.I(DVE, "tensor_scalar", [ub, edge_sb], [ub], out=ub[:, 0:1], in0=ub[:, 0:1], scalar1=edge_sb[:, 0:1], scalar2=None, op0=ALU.mult)
            if er:
                P.I(DVE, "tensor_scalar", [ub, edge_sb], [ub], out=ub[:, n + 1:n + 2], in0=ub[:, n + 1:n + 2], scalar1=edge_sb[:, 1:2], scalar2=None, op0=ALU.mult)
            P.I(DVE, "tensor_scalar", [ub, cw_sb, cb_sb], [ac], out=ac[:, 0:n], in0=ub[:, 1:n + 1], scalar1=cw_sb[:, oc, 1:2], scalar2=cb_sb[:, oc:oc + 1], op0=ALU.mult, op1=ALU.add)
            P.I(DVE, "scalar_tensor_tensor", [ub, cw_sb, ac], [ac], out=ac[:, 0:n], in0=ub[:, 0:n], scalar=cw_sb[:, oc, 0:1], in1=ac[:, 0:n], op0=ALU.mult, op1=ALU.add)
            P.I(DVE, "scalar_tensor_tensor", [ub, cw_sb, ac], [ac], out=ac[:, 0:n], in0=ub[:, 2:n + 2], scalar=cw_sb[:, oc, 2:3], in1=ac[:, 0:n], op0=ALU.mult, op1=ALU.add)
            outs.append(P.dma(POOL, out_dram[oc * 128:(oc + 1) * 128, ocol0:ocol0 + n], ac[:, 0:n], reads=[ac]))

    outs = []
    for t in range(NTT):
        proj_tile(xT, t * TT, TT, 0, t * TT, False)
    proj_tile(ctxT, 0, NCTX, 1, TEXT, True)
    lo = 0
    while lo < TLOC:
        n = min(510, TLOC - lo)
        hyena_tile(HALO + lo, n, uT, lo, False, False, lo == 0, lo + n == TLOC)
        lo += n
    hyena_tile(TEXT, NCTX, ucT, 0, True, True, False, False)

    NB = TLOC // 128
    ctx_tiles = [TEXT // 128, TEXT // 128 + 1]
    pT = [P.sb([128, 512], BF16, name=f"pT{i}") for i in range(2)]
    o_sb = P.sb([64, 512], F32, name="o_sb"); rden = P.sb([65, 512], F32, name="rden")
    of_sb = P.sb([64, 512], F32, name="of_sb")
    ones_b = P.sb([128, 64], F32, name="ones_b")
    P.op(POOL, lambda e: e.memset(ones_b[:], 1.0), writes=[ones_b])

    def attend(qtile, key_tiles, key_masks, out_dram, out_col0):
        for g in range(2):
            po = Bk.f[2]
            nkt = len(key_tiles)
            for ci, (kt, mk) in enumerate(zip(key_tiles, key_masks)):
                psc = Bk.f[ci % 2]
                pTb = pT[ci % 2]
                qv = kqT[:, 2 + 4 * g:2 + 4 * g + 4, qtile * 128:(qtile + 1) * 128]
                P.op(PE, lambda e, psc=psc, kt=kt, qv=qv, mk=mk, g=g: e.matmul(psc[:, 0:512], kqT[:, g, kt * 128:(kt + 1) * 128], qv,
                                                                         start=True, stop=(mk is None)), reads=[kqT], writes=[psc])
                if mk is not None:
                    for hh in range(4):
                        P.op(PE, lambda e, psc=psc, hh=hh, mk=mk: e.matmul(psc[:, hh * 128:(hh + 1) * 128], id_b[:], mask_sb[:, mk, :],
                                                                           start=False, stop=(hh == 3)), reads=[id_b, mask_sb], writes=[psc])
                P.op(ACT, lambda e, psc=psc, pTb=pTb: e.activation(out=pTb[:], in_=psc[:, 0:512], func=AF.Exp, scale=0.125),
                     reads=[psc], writes=[pTb])
                P.op(PE, lambda e, pTb=pTb, kt=kt, ci=ci, g=g: e.matmul(po[0:65, 0:512], vaug[:, kt, g, :], pTb[:], start=(ci == 0), stop=(ci == nkt - 1)),
                     reads=[vaug, pTb], writes=[po])
            P.op(DVE, lambda e, g=g: e.tensor_tensor(out=rden[64:65, :], in0=po[64:65, 0:512], in1=sink_sb[64:65, g * 512:(g + 1) * 512], op=ALU.add),
                 reads=[po, sink_sb], writes=[rden])
            P.op(DVE, lambda e: e.reciprocal(out=rden[64:65, :], in_=rden[64:65, :]), reads=[rden], writes=[rden])
            P.op(ACT, lambda e: e.activation(out=o_sb[:], in_=po[0:64, 0:512], func=AF.Copy), reads=[po], writes=[o_sb])
            pb = Bk.f[3]
            P.op(PE, lambda e: e.matmul(pb[0:64, 0:512], ones_b[64:65, :], rden[64:65, :], start=True, stop=True), reads=[ones_b, rden], writes=[pb])
            P.op(DVE, lambda e: e.tensor_tensor(out=of_sb[:], in0=o_sb[:], in1=pb[0:64, 0:512], op=ALU.mult), reads=[o_sb, pb], writes=[of_sb])
            for hh in range(4):
                r0 = (4 * g + hh) * 64
                outs.append(P.dma(POOL if hh % 2 == 0 else SP, out_dram[r0:r0 + 64, out_col0:out_col0 + 128], of_sb[:, hh * 128:(hh + 1) * 128], reads=[of_sb]))

    for b in range(NB):
        qt = b + 1
        mprev = 0 if b == 0 else 1
        mnext = 3 if b == NB - 1 else 2
        attend(qt, [qt - 1, qt, qt + 1] + ctx_tiles, [mprev, None, mnext, None, None], attT, b * 128)
    for cb in range(2):
        attend(ctx_tiles[cb], ctx_tiles, [None, None], attcT, cb * 128)
    return P.finish(outs)


def col_layout(v, nchunk):
    return np.ascontiguousarray(np.asarray(v, np.float32).reshape(nchunk, 128).T)


def rope_tables(tok_idx):
    inv = (10000.0 ** (-np.arange(16, dtype=np.float32) / 16)).astype(np.float32)
    t = np.asarray(tok_idx)
    row = (t // 64).astype(np.float32)[:, None] * inv
    col = (t % 64).astype(np.float32)[:, None] * inv
    cr, sr, cc, sc_ = np.cos(row), np.sin(row), np.cos(col), np.sin(col)
    C = np.concatenate([cr, cr, cc, cc], axis=1).astype(np.float32)
    S = np.concatenate([-sr, sr, -sc_, sc_], axis=1).astype(np.float32)
    return C, S


def band_masks(core):
    j = np.arange(128)[:, None]; i = np.arange(128)[None, :]
    prev = np.where(j >= i, 0.0, MASKNEG).astype(np.float32)
    nxt = np.where(j <= i, 0.0, MASKNEG).astype(np.float32)
    allneg = np.full((128, 128), MASKNEG, np.float32)
    return np.stack([allneg if core == 0 else prev, prev, nxt, allneg if core == NCORE - 1 else nxt])


def prep_A(inp, layer=0):
    x = inp['x'][0]; ctx = inp['ctx'][0]
    xT = np.ascontiguousarray(x.T)
    xTp = np.concatenate([np.zeros((D, HALO), np.float32), xT, np.zeros((D, HALO), np.float32)], axis=1)
    w = inp['w_in_even'][0]
    w_perm = np.ascontiguousarray(np.concatenate([w[:, 0:128], w[:, 256:768], w[:, 128:256], w[:, 768:]], axis=1))
    gains = np.concatenate([np.tile(inp['att_k_norm'][0], 2), np.tile(inp['att_q_norm'][0], 8)])
    gains = np.ascontiguousarray(np.broadcast_to(gains[None, :], (128, 640))).astype(np.float32)
    sink = inp['att_sink'][0]
    sinkrow = np.ascontiguousarray(np.broadcast_to(np.repeat(sink, 128)[None, :], (128, 1024))).astype(np.float32)
    cvec = np.stack([col_layout(inp['c'][0], 8), col_layout(inp['c_ctx'], 8)], axis=2).reshape(128, 16)
    common = dict(
        ctxT=np.ascontiguousarray(ctx.T), cvec=np.ascontiguousarray(cvec),
        adaw=np.ascontiguousarray(inp['ada_w'][layer][:, 0:3072]), adab=col_layout(inp['ada_b'][layer][0:3072], 24),
        normg=col_layout(inp['norm_g'][layer, 0], 8), w_in=w_perm, gains=gains, sinkrow=sinkrow,
        ident=np.eye(128, dtype=np.float32), onesd=np.ones((128, 128), np.float32),
        convw=np.ascontiguousarray(inp['hy_conv_w'][0].reshape(3, 12, 128).transpose(2, 1, 0).reshape(128, 36)),
        convb=col_layout(inp['hy_conv_b'][0], 12))
    maps = []
    for c in range(NCORE):
        s0 = c * TLOC
        C, S = rope_tables(np.arange(s0 - HALO, s0 + TLOC + HALO).clip(0, SEQ - 1))
        m = dict(common)
        edge = np.ones((128, 2), np.float32); edge[:, 0] = 0.0 if c == 0 else 1.0; edge[:, 1] = 0.0 if c == NCORE - 1 else 1.0
        m.update(xT=np.ascontiguousarray(xTp[:, s0:s0 + TEXT]), ctab=C, stab=S, edge=edge,
                 masks=np.ascontiguousarray(band_masks(c).transpose(1, 0, 2).reshape(128, 512)))
        maps.append(m)
    return blobify(maps, BLOB_A)


NEXP = 32
NC_MOE = 8
NH_MOE = 16


def moe_passes(ncore, has_ctx):
    nlat_pass = (SEQ // ncore) // 1024
    passes = [[(0, 512, 0), (512, 512, 0)] for _ in range(nlat_pass)]
    if has_ctx:
        passes.append([(0, NCTX // ncore, 1)])
    return passes


def build_C(passes):
    TH = 1024
    nhalf = len(passes)
    P = Prog()
    Bk = Banks(P)
    di = lambda n, s, dt=F32: P.dram(n, s, dt, "ExternalInput")
    mT = di("mT", [nhalf, D, TH]); xT = di("xT", [nhalf, D, TH])
    bl = Blob(BLOB_C).dram(P)
    cvec = bl.ap("cvec"); adaw = di("adaw", [D, 4096]); adab = bl.ap("adab")
    normg = bl.ap("normg"); w_out = di("w_out", [D, D])
    rw = bl.ap("rw"); rb = bl.ap("rb")
    w_gu = di("w_gu", [NEXP, D, 2048]); bgu = bl.ap("bgu")
    w_dn = di("w_dn", [NEXP, D, D]); bdn = di("bdn", [NEXP, D])
    ident = bl.ap("ident"); onesd = bl.ap("onesd")
    outT = P.dram("outT", [nhalf, D, TH], F32, "ExternalOutput")
    gt_dram = P.dram("gt_scratch", [nhalf, NEXP, TH], F32, "Internal")

    ones_sb = P.sb([128, 128], F32, name="ones"); P.dma(SP, ones_sb[:], onesd[:, :], writes=[ones_sb])
    id_f = P.sb([128, 128], F32, name="idf"); P.dma(SP, id_f[:], ident[:, :], writes=[id_f])
    cv = P.sb([128, 8, 2], F32, name="cv"); P.dma(SP, cv[:], cvec.rearrange("p (k n) -> p k n", n=2), writes=[cv])
    adab_sb = P.sb([128, 32], F32, name="adab"); P.dma(SP, adab_sb[:], adab[:, :], writes=[adab_sb])
    g_sb = P.sb([128, 8], F32, name="normg"); P.dma(SP, g_sb[:], normg[:, :], writes=[g_sb])
    rw_sb = P.sb([128, 8, NEXP], F32, name="rw"); P.dma(SP, rw_sb[:], rw.rearrange("p (k e) -> p k e", e=NEXP), writes=[rw_sb])
    rb_sb = P.sb([128, NEXP], F32, name="rb"); P.dma(SP, rb_sb[:], rb[:, :], writes=[rb_sb])
    bgu_sb = P.sb([128, NEXP, 16], F32, name="bgu"); P.dma(SP, bgu_sb[:], bgu.rearrange("p (e o) -> p e o", o=16), writes=[bgu_sb])
    bdn_sb = P.sb([NEXP, D], F32, name="bdn"); P.dma(SP, bdn_sb[:], bdn[:, :], writes=[bdn_sb])
    ada = P.sb([128, 32, 2], F32, name="ada")
    with P.scope():
        wbuf = P.sb([128, 8, 1024], F32, name="adawbuf")
        emit_adaln(P, Bk, cv, adaw, adab_sb, wbuf, ada, 2, nparts=4)
    Acol = [P.sb([128, 8], F32, name=f"Acol{j}") for j in range(2)]
    Bcol = [P.sb([128, 8], F32, name=f"Bcol{j}") for j in range(2)]
    G0 = [P.sb([128, 8], F32, name=f"G0{j}") for j in range(2)]
    G1 = [P.sb([128, 8], F32, name=f"G1{j}") for j in range(2)]
    for j in range(2):
        P.I(DVE, "scalar_tensor_tensor", [ada, g_sb], [Acol[j]], out=Acol[j][:], in0=ada[:, 16:24, j], scalar=1.0, in1=g_sb[:], op0=ALU.add, op1=ALU.mult)
        P.I(DVE, "tensor_copy", [ada], [Bcol[j]], out=Bcol[j][:], in_=ada[:, 8:16, j])
        P.I(DVE, "tensor_copy", [ada], [G0[j]], out=G0[j][:], in_=ada[:, 0:8, j])
        P.I(DVE, "tensor_copy", [ada], [G1[j]], out=G1[j][:], in_=ada[:, 24:32, j])

    x1 = P.sb([128, 8, TH], F32, name="x1")
    hT = P.sb([128, 8, TH], BF16, name="hT")
    GT = P.sb([NEXP, TH], F32, name="GT")
    outs = []
    gu_cnt = [0]; dn_cnt = [0]; ch_cnt = [0]; st_cnt = [0]

    for hf in range(nhalf):
        tiles = passes[hf]
        with P.scope():
            mb = P.sb([128, 8, TH], BF16, name=f"mb{hf}")
            wo = P.sb([128, 8, D], BF16, name=f"wo{hf}")
            for kc in range(8):
                P.dma(POOL, mb[:, kc, :], mT[hf, kc * 128:(kc + 1) * 128, :], writes=[mb])
                P.dma(POOL, wo[:, kc, :], w_out[kc * 128:(kc + 1) * 128, :], writes=[wo])
                P.dma(SP if kc % 2 == 0 else ACT, x1[:, kc, :], xT[hf, kc * 128:(kc + 1) * 128, :], writes=[x1])
            for (c0, n, j) in tiles:
                for oc in range(8):
                    ps = Bk.f[oc % 2]
                    for kc in range(8):
                        P.I(PE, "matmul", [wo, mb], [ps], ps[:, 0:n], wo[:, kc, oc * 128:(oc + 1) * 128], mb[:, kc, c0:c0 + n], start=(kc == 0), stop=(kc == 7))
                    P.I(DVE, "scalar_tensor_tensor", [ps, G0[j], x1], [x1], out=x1[:, oc, c0:c0 + n], in0=ps[:, 0:n], scalar=G0[j][:, oc:oc + 1],
                        in1=x1[:, oc, c0:c0 + n], op0=ALU.mult, op1=ALU.add)
        with P.scope():
            sq_sb = P.sb([128, 8, 512], F32, name=f"sq{hf}")
            rs_sb = P.sb([128, 512], F32, name=f"rs{hf}")
            hf32 = P.sb([128, 8, 512], F32, name=f"hf32{hf}")
            lg = P.sb([128, NEXP], F32, name=f"lg{hf}"); mx = P.sb([128, 8], F32, name=f"mx{hf}")
            msk = P.sb([128, NEXP], F32, name=f"msk{hf}"); ex = P.sb([128, NEXP], F32, name=f"ex{hf}")
            sm = P.sb([128, 1], F32, name=f"sm{hf}"); nm = P.sb([128, 1], F32, name=f"nm{hf}")
            for (c0, n, j) in tiles:
                ps = Bk.f[6]
                for kc in range(8):
                    P.I(ACT, "activation", [x1], [sq_sb], out=sq_sb[:, kc, 0:n], in_=x1[:, kc, c0:c0 + n], func=AF.Square)
                for kc in range(8):
                    P.I(PE, "matmul", [ones_sb, sq_sb], [ps], ps[:, 0:n], ones_sb[:], sq_sb[:, kc, 0:n], start=(kc == 0), stop=(kc == 7))
                P.I(DVE, "tensor_scalar", [ps], [rs_sb], out=rs_sb[:, 0:n], in0=ps[:, 0:n], scalar1=1.0 / D, scalar2=EPS, op0=ALU.mult, op1=ALU.add)
                P.I(ACT, "activation", [rs_sb], [rs_sb], out=rs_sb[:, 0:n], in_=rs_sb[:, 0:n], func=AF.Ln)
                P.I(ACT, "activation", [rs_sb], [rs_sb], out=rs_sb[:, 0:n], in_=rs_sb[:, 0:n], func=AF.Exp, scale=-0.5)
                for kc in range(8):
                    P.I(DVE, "tensor_tensor", [x1, rs_sb, sq_sb], [sq_sb], out=sq_sb[:, kc, 0:n], in0=x1[:, kc, c0:c0 + n], in1=rs_sb[:, 0:n], op=ALU.mult)
                    P.I(ACT, "activation", [sq_sb, Acol[j], Bcol[j]], [hf32], out=hf32[:, kc, 0:n], in_=sq_sb[:, kc, 0:n], func=AF.Identity,
                        bias=Bcol[j][:, kc:kc + 1], scale=Acol[j][:, kc:kc + 1])
                    P.I(POOL, "tensor_copy", [hf32], [hT], out=hT[:, kc, c0:c0 + n], in_=hf32[:, kc, 0:n])
                s0 = 0
                while s0 < n:
                    m = min(128, n - s0)
                    pl = Bk.f[5]
                    for kc in range(8):
                        P.I(PE, "matmul", [hf32, rw_sb], [pl], pl[0:m, 0:NEXP], hf32[:, kc, s0:s0 + m], rw_sb[:, kc, :], start=(kc == 0), stop=(kc == 7))
                    P.I(DVE, "tensor_tensor", [pl, rb_sb], [lg], out=lg[0:m, :], in0=pl[0:m, 0:NEXP], in1=rb_sb[0:m, :], op=ALU.add)
                    P.I(DVE, "max", [lg], [mx], out=mx[0:m, :], in_=lg[0:m, :])
                    P.I(DVE, "tensor_scalar", [lg, mx], [msk], out=msk[0:m, :], in0=lg[0:m, :], scalar1=mx[0:m, 3:4], scalar2=None, op0=ALU.is_ge)
                    P.I(DVE, "tensor_scalar", [mx], [nm], out=nm[0:m, :], in0=mx[0:m, 0:1], scalar1=-1.0, scalar2=None, op0=ALU.mult)
                    P.I(ACT, "activation", [lg, nm], [ex], out=ex[0:m, :], in_=lg[0:m, :], func=AF.Exp, bias=nm[0:m, 0:1], scale=1.0)
                    P.I(DVE, "tensor_tensor", [ex, msk], [ex], out=ex[0:m, :], in0=ex[0:m, :], in1=msk[0:m, :], op=ALU.mult)
                    P.I(DVE, "reduce_sum", [ex], [sm], out=sm[0:m, :], in_=ex[0:m, :], axis=AX.X)
                    P.I(DVE, "reciprocal", [sm], [sm], out=sm[0:m, :], in_=sm[0:m, :])
                    P.I(DVE, "tensor_scalar", [ex, sm], [ex], out=ex[0:m, :], in0=ex[0:m, :], scalar1=sm[0:m, 0:1], scalar2=None, op0=ALU.mult)
                    pt = Bk.f[4]
                    P.I(PE, "transpose", [ex, id_f], [pt], pt[0:NEXP, 0:m], ex[0:m, :], id_f[0:m, 0:m])
                    P.I(ACT, "activation", [pt], [GT], out=GT[:, c0 + s0:c0 + s0 + m], in_=pt[0:NEXP, 0:m], func=AF.Copy)
                    s0 += m
        P.dma(SP, gt_dram[hf], GT[:, :], reads=[GT], writes=[("gtd", hf)])
        p3 = P.scope(); p3.__enter__()
        yT = P.sb([128, 8, TH], F32, name=f"yT{hf}")
        actT = P.sb([128, 8, TH], BF16, name=f"actT{hf}")
        gu_ring = [P.sb([128, 8, 512], BF16, name=f"gu{hf}_{i}") for i in range(3)]
        dn_ring = [P.sb([128, 8, 256], BF16, name=f"dn{hf}_{i}") for i in range(3)]
        stage = [P.sb([128, 8, 256], F32, name=f"stg{hf}_{i}") for i in range(3)]
        gbs = [P.sb([128, TH], F32, name=f"gb{hf}_{i}") for i in range(2)]
        g1 = [P.sb([128, 512], F32, name=f"g1{hf}_{i}") for i in range(2)]
        tt = [P.sb([128, 512], F32, name=f"tt{hf}_{i}") for i in range(2)]
        u1 = [P.sb([128, 512], F32, name=f"u1{hf}_{i}") for i in range(2)]
        for e in range(NEXP):
            gb_sb = gbs[e % 2]
            P.dma(SP, gb_sb[:, :], gt_dram[hf, e:e + 1, :].partition_broadcast(128), reads=[("gtd", hf)], writes=[gb_sb])
            for q in range(4):
                wb = gu_ring[gu_cnt[0] % 3]; gu_cnt[0] += 1
                src = w_gu[e].rearrange("(k p) n -> p k n", p=128)
                for part in range(2):
                    sg_ = stage[st_cnt[0] % 3]; st_cnt[0] += 1
                    P.dma(SP, sg_[:], src[:, :, part * 1024 + q * 256:part * 1024 + (q + 1) * 256], writes=[sg_])
                    if st_cnt[0] % 2:
                        P.I(POOL, "tensor_copy", [sg_], [wb], out=wb[:, :, part * 256:(part + 1) * 256], in_=sg_[:])
                    else:
                        P.I(ACT, "activation", [sg_], [wb], out=wb[:, :, part * 256:(part + 1) * 256], in_=sg_[:], func=AF.Copy)
                for o2 in range(2):
                    oc = q * 2 + o2
                    for (c0, n, j) in tiles:
                        i = ch_cnt[0] % 2; ch_cnt[0] += 1
                        pgt, put = Bk.f[i], Bk.f[2 + i]
                        for kc in range(8):
                            P.I(PE, "matmul", [wb, hT], [pgt], pgt[:, 0:n], wb[:, kc, o2 * 128:(o2 + 1) * 128], hT[:, kc, c0:c0 + n], start=(kc == 0), stop=(kc == 7))
                        for kc in range(8):
                            P.I(PE, "matmul", [wb, hT], [put], put[:, 0:n], wb[:, kc, 256 + o2 * 128:256 + (o2 + 1) * 128], hT[:, kc, c0:c0 + n], start=(kc == 0), stop=(kc == 7))
                        P.I(DVE, "tensor_scalar", [pgt, bgu_sb], [g1[i]], out=g1[i][:, 0:n], in0=pgt[:, 0:n], scalar1=bgu_sb[:, e, oc:oc + 1], scalar2=7.0, op0=ALU.add, op1=ALU.min)
                        P.I(ACT, "activation", [g1[i]], [tt[i]], out=tt[i][:, 0:n], in_=g1[i][:, 0:n], func=AF.Silu, scale=1.702)
                        P.I(DVE, "tensor_scalar", [put, bgu_sb], [u1[i]], out=u1[i][:, 0:n], in0=put[:, 0:n], scalar1=bgu_sb[:, e, 8 + oc:8 + oc + 1], scalar2=7.0, op0=ALU.add, op1=ALU.min)
                        P.I(POOL, "tensor_scalar", [u1[i]], [u1[i]], out=u1[i][:, 0:n], in0=u1[i][:, 0:n], scalar1=-7.0, scalar2=1.0, op0=ALU.max, op1=ALU.add)
                        P.I(POOL, "tensor_tensor", [tt[i], u1[i]], [tt[i]], out=tt[i][:, 0:n], in0=tt[i][:, 0:n], in1=u1[i][:, 0:n], op=ALU.mult)
                        P.I(DVE, "scalar_tensor_tensor", [tt[i], gb_sb], [actT], out=actT[:, oc, c0:c0 + n], in0=tt[i][:, 0:n], scalar=1.0 / 1.702, in1=gb_sb[:, c0:c0 + n],
                            op0=ALU.mult, op1=ALU.mult)
            for q in range(4):
                wd = dn_ring[dn_cnt[0] % 3]; dn_cnt[0] += 1
                sg_ = stage[st_cnt[0] % 3]; st_cnt[0] += 1
                P.dma(SP, sg_[:], w_dn[e].rearrange("(k p) n -> p k n", p=128)[:, :, q * 256:(q + 1) * 256], writes=[sg_])
                if st_cnt[0] % 2:
                    P.I(POOL, "tensor_copy", [sg_], [wd], out=wd[:], in_=sg_[:])
                else:
                    P.I(ACT, "activation", [sg_], [wd], out=wd[:], in_=sg_[:], func=AF.Copy)
                for o2 in range(2):
                    dc = q * 2 + o2
                    for (c0, n, j) in tiles:
                        i = ch_cnt[0] % 2; ch_cnt[0] += 1
                        pd = Bk.f[4 + i]
                        for kc in range(8):
                            P.I(PE, "matmul", [wd, actT], [pd], pd[:, 0:n], wd[:, kc, o2 * 128:(o2 + 1) * 128], actT[:, kc, c0:c0 + n], start=(kc == 0), stop=(kc == 7))
                        if e == 0:
                            P.I(DVE, "tensor_copy", [pd], [yT], out=yT[:, dc, c0:c0 + n], in_=pd[:, 0:n])
                        else:
                            P.I(DVE, "tensor_tensor", [pd, yT], [yT], out=yT[:, dc, c0:c0 + n], in0=pd[:, 0:n], in1=yT[:, dc, c0:c0 + n], op=ALU.add)
        for (c0, n, j) in tiles:
            for dc in range(8):
                pd = Bk.f[4 + dc % 2]
                P.I(PE, "matmul", [bdn_sb, GT], [pd], pd[:, 0:n], bdn_sb[:, dc * 128:(dc + 1) * 128], GT[:, c0:c0 + n], start=True, stop=True)
                P.I(DVE, "tensor_tensor", [pd, yT], [yT], out=yT[:, dc, c0:c0 + n], in0=pd[:, 0:n], in1=yT[:, dc, c0:c0 + n], op=ALU.add)
                P.I(DVE, "scalar_tensor_tensor", [yT, G1[j], x1], [x1], out=x1[:, dc, c0:c0 + n], in0=yT[:, dc, c0:c0 + n], scalar=G1[j][:, dc:dc + 1],
                    in1=x1[:, dc, c0:c0 + n], op0=ALU.mult, op1=ALU.add)
        for kc in range(8):
            outs.append(P.dma(SP if kc % 2 == 0 else ACT, outT[hf, kc * 128:(kc + 1) * 128, :], x1[:, kc, :], reads=[x1]))
        p3.__exit__(None, None, None)
    return P.finish(outs)


def prep_C(inp, layer, mT_full, xT_full, mcT=None, xcT=None):
    passes = moe_passes(NC_MOE, mcT is not None)
    nlp = (SEQ // NC_MOE) // 1024
    ncx = NCTX // NC_MOE
    aw = inp['ada_w'][layer]; ab = inp['ada_b'][layer]
    adaw = np.ascontiguousarray(np.concatenate([aw[:, 2048:3072], aw[:, 3072:6144]], axis=1))
    adab = col_layout(np.concatenate([ab[2048:3072], ab[3072:6144]]), 32)
    cvec = np.stack([col_layout(inp['c'][0], 8), col_layout(inp['c_ctx'], 8)], axis=2).reshape(128, 16)
    bgu = inp['moe_b_gu'][layer]
    bgu_l = np.ascontiguousarray(bgu.reshape(NEXP, 16, 128).transpose(2, 0, 1).reshape(128, NEXP * 16))
    common = dict(
        cvec=np.ascontiguousarray(cvec), adaw=adaw, adab=adab, normg=col_layout(inp['norm_g'][layer, 1], 8),
        w_out=np.ascontiguousarray(inp['w_out'][layer]),
        rw=np.ascontiguousarray(inp['router_w'][layer].reshape(8, 128, NEXP).transpose(1, 0, 2).reshape(128, 8 * NEXP)),
        rb=np.ascontiguousarray(np.broadcast_to(inp['router_b'][layer][None, :], (128, NEXP))).astype(np.float32),
        w_gu=inp['moe_w_gu'][layer], bgu=bgu_l, w_dn=inp['moe_w_dn'][layer], bdn=np.ascontiguousarray(inp['moe_b_dn'][layer]),
        ident=np.eye(128, dtype=np.float32), onesd=np.ones((128, 128), np.float32))
    maps = []
    for c in range(NC_MOE):
        ms = np.zeros((len(passes), D, 1024), np.float32); xs = np.zeros((len(passes), D, 1024), np.float32)
        for hf in range(nlp):
            t0 = (c * nlp + hf) * 1024
            ms[hf] = mT_full[:, t0:t0 + 1024]; xs[hf] = xT_full[:, t0:t0 + 1024]
        if mcT is not None:
            ms[nlp, :, 0:ncx] = mcT[:, c * ncx:(c + 1) * ncx]; xs[nlp, :, 0:ncx] = xcT[:, c * ncx:(c + 1) * ncx]
        d = dict(common); d.update(mT=ms, xT=xs)
        maps.append(d)
    return blobify(maps, BLOB_C), passes


def gather_C(results, has_ctx):
    nlp = (SEQ // NC_MOE) // 1024
    ncx = NCTX // NC_MOE
    xT = np.zeros((D, SEQ), np.float32); xcT = np.zeros((D, NCTX), np.float32) if has_ctx else None
    for c in range(NC_MOE):
        o = results[c]['outT']
        for hf in range(nlp):
            t0 = (c * nlp + hf) * 1024
            xT[:, t0:t0 + 1024] = o[hf]
        if has_ctx:
            xcT[:, c * ncx:(c + 1) * ncx] = o[nlp][:, 0:ncx]
    return xT, xcT


PI = float(np.pi)


def build_B(L):
    NBK = L // 128
    NK = 2 * NBK - 1
    HROW = 2 * L
    P = Prog()
    Bk = Banks(P)
    di = lambda n, s, dt=F32: P.dram(n, s, dt, "ExternalInput")
    vB = di("vB", [128, 64, NBK]); x1B = di("x1B", [128, 64, NBK]); x2B = di("x2B", [128, 64, NBK])
    zt = di("zt", [2, 33, L]); tn = di("tn", [2, 1, L])
    bl = Blob(BLOB_B).dram(P)
    w1 = bl.ap("w1"); w2 = bl.ap("w2"); w3 = bl.ap("w3"); w4s = bl.ap("w4s")
    fqb = bl.ap("fqb")
    negd = bl.ap("negd"); fbias = bl.ap("fbias")
    ident = bl.ap("ident"); onesd = bl.ap("onesd"); jmat = bl.ap("jmat")
    hyB = P.dram("hyB", [128, 64, NBK], F32, "ExternalOutput")
    Hd = P.dram("Hd_scratch", [128, HROW], BF16, "Internal")

    ones_sb = P.sb([128, 128], F32, name="ones"); P.dma(SP, ones_sb[:], onesd[:, :], writes=[ones_sb])
    id_f = P.sb([128, 128], F32, name="idf"); P.dma(SP, id_f[:], ident[:, :], writes=[id_f])
    j_b = P.sb([128, 128], BF16, name="jb"); P.dma(POOL, j_b[:], jmat[:, :], writes=[j_b])
    w1_sb = P.sb([33, 64], F32, name="w1"); P.dma(SP, w1_sb[:], w1[:, :], writes=[w1_sb])
    w2_sb = P.sb([64, 64], F32, name="w2"); P.dma(SP, w2_sb[:], w2[:, :], writes=[w2_sb])
    w3_sb = P.sb([64, 64], F32, name="w3"); P.dma(SP, w3_sb[:], w3[:, :], writes=[w3_sb])
    w4_sb = P.sb([64, 2, 128], F32, name="w4"); P.dma(SP, w4_sb[:], w4s.rearrange("k (s m) -> k s m", s=2), writes=[w4_sb])
    fq_sb = P.sb([64, 4], F32, name="fq"); P.dma(SP, fq_sb[:], fqb[:, :], writes=[fq_sb])
    fb_sb = P.sb([64, 3], F32, name="fqbias")
    P.I(DVE, "tensor_tensor", [fq_sb], [fb_sb], out=fb_sb[:], in0=fq_sb[:, 1:4], in1=fq_sb[:, 0:1].broadcast_to([64, 3]), op=ALU.mult)
    negd_sb = P.sb([128, 1], F32, name="negd"); P.dma(SP, negd_sb[:], negd[:, :], writes=[negd_sb], allow_slow_non_contiguous=True)
    fbias_sb = P.sb([128, 128], F32, name="fbias"); P.dma(SP, fbias_sb[:], fbias[:, :], writes=[fbias_sb])

    CH = min(512, L)
    NCH = L // CH
    abss = P.sb([128, 2 * NCH], F32, name="abss")
    with P.scope():
        z_sb = [P.sb([33, CH], F32, name=f"z{i}") for i in range(2)]
        tn_sb = [P.sb([128, CH], F32, name=f"tn{i}") for i in range(2)]
        a_sb = P.sb([64, CH], F32, name="a_sb"); t_sb = P.sb([64, CH], F32, name="t_sb")
        hd_sb = P.sb([64, CH], F32, name="hd_sb")
        dec_sb = P.sb([128, CH], F32, name="dec_sb"); hf_sb = P.sb([128, CH], F32, name="hf_sb")
        hb_sb = [P.sb([128, CH], BF16, name=f"hb{i}") for i in range(2)]
        it = 0
        for side in range(2):
            for c in range(NCH):
                zb = z_sb[it % 2]; tb = tn_sb[it % 2]; hb = hb_sb[it % 2]; it += 1
                P.dma(SP, zb[:], zt[side, :, c * CH:(c + 1) * CH], writes=[zb])
                P.dma(ACT, tb[:], tn[side, :, c * CH:(c + 1) * CH].partition_broadcast(128), writes=[tb])
                src, Kd, wl = zb, 33, [w1_sb, w2_sb, w3_sb]
                for l in range(3):
                    ps = Bk.f[l % 2]
                    P.I(PE, "matmul", [wl[l], src], [ps], ps[0:64, 0:CH], wl[l][:], src[0:Kd, :], start=True, stop=True)
                    P.I(DVE, "tensor_scalar", [ps, fq_sb, fb_sb], [a_sb], out=a_sb[:], in0=ps[0:64, 0:CH], scalar1=fq_sb[:, 0:1], scalar2=fb_sb[:, l:l + 1], op0=ALU.mult, op1=ALU.add)
                    for rep in range(2):
                        P.I(POOL, "tensor_scalar", [a_sb], [t_sb], out=t_sb[:], in0=a_sb[:], scalar1=PI, scalar2=-2 * PI, op0=ALU.is_gt, op1=ALU.mult)
                        P.I(DVE, "tensor_tensor", [a_sb, t_sb], [hd_sb], out=hd_sb[:], in0=a_sb[:], in1=t_sb[:], op=ALU.add)
                        P.I(POOL, "tensor_scalar", [a_sb], [t_sb], out=t_sb[:], in0=a_sb[:], scalar1=-PI, scalar2=2 * PI, op0=ALU.is_lt, op1=ALU.mult)
                        P.I(DVE, "tensor_tensor", [hd_sb, t_sb], [a_sb], out=a_sb[:], in0=hd_sb[:], in1=t_sb[:], op=ALU.add)
                    P.I(ACT, "activation", [a_sb], [hd_sb], out=hd_sb[:], in_=a_sb[:], func=AF.Sin)
                    src, Kd = hd_sb, 64
                p4 = Bk.f[2]
                P.I(PE, "matmul", [w4_sb, hd_sb], [p4], p4[:, 0:CH], w4_sb[:, side, :], hd_sb[:], start=True, stop=True)
                P.I(ACT, "activation", [tb, negd_sb], [dec_sb], out=dec_sb[:], in_=tb[:], func=AF.Exp, scale=negd_sb[:, 0:1])
                P.I(DVE, "tensor_tensor", [p4, dec_sb], [hf_sb], out=hf_sb[:], in0=p4[:, 0:CH], in1=dec_sb[:], op=ALU.mult)
                if side == 1 and c == NCH - 1:
                    P.I(DVE, "memset", [], [hf_sb], hf_sb[:, CH - 1:CH], 0.0)
                P.I(DVE, "tensor_reduce", [hf_sb], [abss], out=abss[:, side * NCH + c:side * NCH + c + 1], in_=hf_sb[:], axis=AX.X, op=ALU.add, apply_absolute_value=True)
                P.I(ACT, "activation", [hf_sb], [hb], out=hb[:], in_=hf_sb[:], func=AF.Copy)
                if side == 1:
                    n = CH - 1 if c == NCH - 1 else CH
                    P.dma(POOL, Hd[:, c * CH:c * CH + n], hb[:, 0:n], reads=[hb], writes=["Hd"])
                else:
                    P.dma(POOL, Hd[:, L - 1 + c * CH:L - 1 + (c + 1) * CH], hb[:], reads=[hb], writes=["Hd"])
        zpad = P.sb([128, 1], BF16, name="zpad"); P.I(DVE, "memset", [], [zpad], zpad[:], 0.0)
        P.dma(POOL, Hd[:, 2 * L - 1:2 * L], zpad[:], reads=[zpad], writes=["Hd"], allow_slow_non_contiguous=True)
    rn = P.sb([128, 1], F32, name="rn"); rnb = P.sb([128, 128], F32, name="rnb"); dg = P.sb([128, 128], F32, name="dg")
    P.I(DVE, "reduce_sum", [abss], [rn], out=rn[:], in_=abss[:], axis=AX.X)
    P.I(DVE, "reciprocal", [rn], [rn], out=rn[:], in_=rn[:])
    P.I(DVE, "tensor_scalar", [id_f, rn], [dg], out=dg[:], in0=id_f[:], scalar1=rn[:, 0:1], scalar2=None, op0=ALU.mult)
    pr = Bk.f[0]
    P.I(PE, "matmul", [ones_sb, dg], [pr], pr[:, 0:128], ones_sb[:], dg[:], start=True, stop=True)
    P.I(ACT, "activation", [pr], [rnb], out=rnb[:], in_=pr[:, 0:128], func=AF.Copy)

    v_sb = P.sb([128, 64, NBK], F32, name="v_sb"); x1_sb = P.sb([128, 64, NBK], F32, name="x1_sb"); x2_sb = P.sb([128, 64, NBK], F32, name="x2_sb")
    P.dma(SP, v_sb[:], vB[:, :, :], writes=[v_sb]); P.dma(ACT, x1_sb[:], x1B[:, :, :], writes=[x1_sb]); P.dma(SP, x2_sb[:], x2B[:, :, :], writes=[x2_sb])
    zb16 = P.sb([128, 64 * NBK], BF16, name="zb16")
    zrev = P.sb([128, 64, NBK], BF16, name="zrev")
    z1_sb = P.sb([128, 64, NBK], F32, name="z1_sb")
    t0_sb = [P.sb([128, NBK], F32, name=f"t0_{i}") for i in range(2)]
    t1_sb = [P.sb([128, NBK], F32, name=f"t1_{i}") for i in range(2)]
    KP = min(51, NK)
    NPIECE = (NK + KP - 1) // KP
    hs_ring = [P.sb([128, KP * 128], BF16, name=f"hs{i}") for i in range(3)]
    outs = []
    hs_cnt = [0]

    def make_zrev(src_f32):
        flat = src_f32[:].rearrange("p c j -> p (c j)")
        P.I(ACT, "activation", [src_f32], [zb16], out=zb16[:], in_=flat, func=AF.Copy)
        tot = 64 * NBK
        c0 = 0
        zr_flat = zrev[:].rearrange("p c j -> p (c j)")
        i = 0
        while c0 < tot:
            n = min(512, tot - c0)
            pz = Bk.f[4 + i % 2]; i += 1
            P.I(PE, "matmul", [j_b, zb16], [pz], pz[:, 0:n], j_b[:], zb16[:, c0:c0 + n], start=True, stop=True)
            P.I(ACT if i % 2 else DVE, "activation" if i % 2 else "tensor_copy", [pz], [zrev],
                **(dict(out=zr_flat[:, c0:c0 + n], in_=pz[:, 0:n], func=AF.Copy) if i % 2 else dict(out=zr_flat[:, c0:c0 + n], in_=pz[:, 0:n])))
            c0 += n

    mid = (NK // 2) // KP
    piece_order = [mid] + [p for p in range(NPIECE) if p != mid]

    def conv(o, zin_f32, gate_sb, dst_sb):
        for ch in range(64):
            row = o * 64 + ch
            py = Bk.f[ch % 4][:, 0:NBK] if False else Bk.f[ch % 2]
            first = True
            for pi in piece_order:
                kk0 = pi * KP; kk1 = min(NK, kk0 + KP)
                hs = hs_ring[hs_cnt[0] % 3]; hs_cnt[0] += 1
                ncol = (kk1 - kk0) * 128
                src = bass.AP(Hd.tensor, row * HROW + kk0 * 128, [[1, 128], [1, ncol]])
                P.dma(SP if hs_cnt[0] % 2 else ACT, hs[:, 0:ncol], src, reads=["Hd"], writes=[hs])
                ks = list(range(kk0, kk1))
                if pi == mid:
                    ks.remove(NBK - 1); ks = [NBK - 1] + ks
                for kk in ks:
                    k = kk - (NBK - 1)
                    a_lo = max(0, k); a_hi = min(NBK - 1, NBK - 1 + k)
                    last = (pi == piece_order[-1] and kk == ks[-1])
                    P.I(PE, "matmul", [hs, zrev], [py], py[:, a_lo:a_hi + 1], hs[:, (kk - kk0) * 128:(kk - kk0 + 1) * 128], zrev[:, ch, a_lo - k:a_hi - k + 1],
                        start=first, stop=last)
                    first = False
            i = ch % 2
            P.I(POOL, "tensor_scalar", [zin_f32, fbias_sb], [t0_sb[i]], out=t0_sb[i][:], in0=zin_f32[:, ch, :], scalar1=fbias_sb[:, row:row + 1], scalar2=None, op0=ALU.mult)
            P.I(DVE, "scalar_tensor_tensor", [py, rnb, t0_sb[i]], [t1_sb[i]], out=t1_sb[i][:], in0=py[:, 0:NBK], scalar=rnb[:, row:row + 1], in1=t0_sb[i][:], op0=ALU.mult, op1=ALU.add)
            P.I(POOL, "tensor_tensor", [t1_sb[i], gate_sb], [dst_sb], out=dst_sb[:, ch, :], in0=t1_sb[i][:], in1=gate_sb[:, ch, :], op=ALU.mult)

    make_zrev(v_sb)
    conv(0, v_sb, x1_sb, z1_sb)
    make_zrev(z1_sb)
    conv(1, z1_sb, x2_sb, v_sb)
    outs.append(P.dma(SP, hyB[:, :, :], v_sb[:], reads=[v_sb]))
    return P.finish(outs)


def hyena_tables(L):
    t = np.linspace(0.0, 1.0, L, dtype=np.float32)[:, None]
    bands = 16
    w_ang = (2.0 * np.pi * np.arange(L, dtype=np.float32)[:, None] / L).astype(np.float32)
    fr = np.linspace(1e-4, bands - 1, bands, dtype=np.float32)[None]
    z = np.concatenate([t, np.cos(fr * w_ang), -np.sin(fr * w_ang)], axis=-1).astype(np.float32)
    zt = np.stack([z.T, z[::-1].T]).astype(np.float32)
    tn = np.stack([t.T, t[::-1].T]).astype(np.float32)
    return np.ascontiguousarray(zt), np.ascontiguousarray(tn)


def prep_B(inp, uT_full, L):
    NBK = L // 128
    zt, tn = hyena_tables(L)
    max_decay = np.log(1e-2) / 0.3; min_decay = np.log(1e-2) / 1.5
    deltas = np.abs(np.linspace(min_decay, max_decay, 512, dtype=np.float32)).astype(np.float32)
    w4 = inp['hy_w4'][0].reshape(64, 2, 2, 512)
    fqb = np.stack([inp['hy_freq'][0], inp['hy_b1'][0], inp['hy_b2'][0], inp['hy_b3'][0]], axis=1).astype(np.float32)
    jm = np.eye(128, dtype=np.float32)[::-1].copy()
    maps = []
    for c in range(NCORE):
        chs = slice(64 * c, 64 * c + 64)

        def blk(rows):
            return np.ascontiguousarray(rows.reshape(64, NBK, 128).transpose(2, 0, 1))
        w4s = np.concatenate([w4[:, :, s, chs].reshape(64, 128) for s in range(2)], axis=1)
        fb = inp['hy_filter_bias'][0][:, chs].reshape(128)
        maps.append(dict(
            vB=blk(uT_full[0:512][chs]), x1B=blk(uT_full[512:1024][chs]), x2B=blk(uT_full[1024:1536][chs]),
            zt=zt, tn=tn, w1=np.ascontiguousarray(inp['hy_w1'][0]), w2=np.ascontiguousarray(inp['hy_w2'][0]),
            w3=np.ascontiguousarray(inp['hy_w3'][0]), w4s=np.ascontiguousarray(w4s), fqb=np.ascontiguousarray(fqb),
            negd=np.ascontiguousarray(-np.tile(deltas[chs], 2)[:, None]).astype(np.float32),
            fbias=np.ascontiguousarray(np.broadcast_to(fb[None, :], (128, 128))).astype(np.float32),
            ident=np.eye(128, dtype=np.float32), onesd=np.ones((128, 128), np.float32), jmat=jm))
    return blobify(maps, BLOB_B)


def gather_B(results, L):
    NBK = L // 128
    hyT = np.zeros((512, L), np.float32)
    for c in range(NCORE):
        hb = results[c]['hyB']
        hyT[64 * c:64 * c + 64] = hb.transpose(1, 2, 0).reshape(64, L)
    return hyT


DROWS = 5136


def build_D():
    P = Prog()
    Bk = Banks(P)
    di = lambda n, s, dt=F32: P.dram(n, s, dt, "ExternalInput")
    xT = di("xT", [D, TEXT]); ctxT = di("ctxT", [D, NCTX])
    bl = Blob(BLOB_D).dram(P)
    cvec = bl.ap("cvec"); adaw = di("adaw", [D, 3072]); adab = bl.ap("adab")
    normg = bl.ap("normg"); w_in = di("w_in", [D, 4112])
    onesd = bl.ap("onesd")
    convw = bl.ap("convw"); convb = bl.ap("convb"); edge = bl.ap("edge")
    hglb = bl.ap("hglb")
    dtb = bl.ap("dtb")
    outT = P.dram("outT", [DROWS, TLOC], F32, "ExternalOutput"); outcT = P.dram("outcT", [DROWS, NCTX], F32, "ExternalOutput")

    ones_sb = P.sb([128, 128], F32, name="ones"); P.dma(SP, ones_sb[:], onesd[:, :], writes=[ones_sb])
    cv = P.sb([128, 8, 2], F32, name="cv"); P.dma(SP, cv[:], cvec.rearrange("p (k n) -> p k n", n=2), writes=[cv])
    adab_sb = P.sb([128, 24], F32, name="adab"); P.dma(SP, adab_sb[:], adab[:, :], writes=[adab_sb])
    g_sb = P.sb([128, 8], F32, name="normg"); P.dma(SP, g_sb[:], normg[:, :], writes=[g_sb])
    cw_sb = P.sb([128, 8, 3], F32, name="cw"); P.dma(SP, cw_sb[:], convw.rearrange("p (o t) -> p o t", t=3), writes=[cw_sb])
    cb_sb = P.sb([128, 8], F32, name="cb"); P.dma(SP, cb_sb[:], convb[:, :], writes=[cb_sb])
    edge_sb = P.sb([128, 2], F32, name="edge"); P.dma(SP, edge_sb[:], edge[:, :], writes=[edge_sb])
    lb_sb = P.sb([128, 8], F32, name="hglb"); P.dma(SP, lb_sb[:], hglb[:, :], writes=[lb_sb])
    dtb_sb = P.sb([16, 1], F32, name="dtb"); P.dma(SP, dtb_sb[:], dtb[:, :], writes=[dtb_sb], allow_slow_non_contiguous=True)
    lbc = P.sb([128, 4], F32, name="lbc"); oml = P.sb([128, 4], F32, name="oml")
    P.I(DVE, "tensor_tensor", [lb_sb], [lbc], out=lbc[:], in0=lb_sb[:, 4:8], in1=lb_sb[:, 0:4], op=ALU.subtract)
    P.I(ACT, "activation", [lbc], [lbc], out=lbc[:], in_=lbc[:], func=AF.Sigmoid)
    P.I(DVE, "tensor_scalar", [lbc], [oml], out=oml[:], in0=lbc[:], scalar1=-1.0, scalar2=1.0, op0=ALU.mult, op1=ALU.add)
    w_sb = P.sb([128, 8, 4112], BF16, name="w_in")
    for kc in range(8):
        P.dma(POOL, w_sb[:, kc, :], w_in[kc * 128:(kc + 1) * 128, :], writes=[w_sb])
    ada = P.sb([128, 24, 2], F32, name="ada")
    with P.scope():
        wbuf = P.sb([128, 8, 1024], F32, name="adawbuf")
        emit_adaln(P, Bk, cv, adaw, adab_sb, wbuf, ada, 2)
    Acol = [P.sb([128, 8], F32, name=f"Acol{j}") for j in range(2)]
    Bcol = [P.sb([128, 8], F32, name=f"Bcol{j}") for j in range(2)]
    for j in range(2):
        P.I(DVE, "scalar_tensor_tensor", [ada, g_sb], [Acol[j]], out=Acol[j][:], in0=ada[:, 8:16, j], scalar=1.0, in1=g_sb[:], op0=ALU.add, op1=ALU.mult)
        P.I(DVE, "tensor_copy", [ada], [Bcol[j]], out=Bcol[j][:], in_=ada[:, 0:8, j])
    h_all = P.sb([128, 8, TEXT + NCTX], BF16, name="h_all")
    x_sb = P.sb([128, 8, TT], F32, name="x_sb"); sq_sb = P.sb([128, 8, TT], F32, name="sq_sb"); rs_sb = P.sb([128, TT], F32, name="rs_sb")
    for t in range(NTT):
        for kc in range(8):
            P.dma(SP if kc % 2 == 0 else ACT, x_sb[:, kc, :], xT[kc * 128:(kc + 1) * 128, t * TT:(t + 1) * TT], writes=[x_sb])
        emit_norm_mod(P, Bk, x_sb, ones_sb, Acol[0], Bcol[0], h_all, TT, sq_sb, rs_sb, hcol0=t * TT)
    for kc in range(8):
        P.dma(SP if kc % 2 == 0 else ACT, x_sb[:, kc, 0:NCTX], ctxT[kc * 128:(kc + 1) * 128, :], writes=[x_sb])
    emit_norm_mod(P, Bk, x_sb, ones_sb, Acol[1], Bcol[1], h_all, NCTX, sq_sb, rs_sb, hcol0=TEXT)

    u_sb = [P.sb([128, 512], F32, name=f"u_sb{i}") for i in range(2)]
    a1_sb = [P.sb([128, 512], F32, name=f"a1_sb{i}") for i in range(2)]
    a2_sb = [P.sb([128, 512], F32, name=f"a2_sb{i}") for i in range(2)]
    outs = []

    def tile(h0, n, out_dram, ocol0, zl, zr, el, er):
        a0 = h0 - (0 if zl else 1); a1 = h0 + n + (0 if zr else 1)
        w = a1 - a0
        off = 1 if zl else 0
        for oc in range(33):
            M = 128 if oc < 32 else 16
            i = oc % 2
            pu = Bk.f[2 + i]; ub = u_sb[i]; r1 = a1_sb[i]; r2 = a2_sb[i]
            for kc in range(8):
                P.I(PE, "matmul", [w_sb, h_all], [pu], pu[0:M, off:off + w], w_sb[:, kc, oc * 128:oc * 128 + M], h_all[:, kc, a0:a1], start=(kc == 0), stop=(kc == 7))
            ctr = pu[0:M, 1:n + 1]
            dq = SP if oc % 2 == 0 else POOL
            if oc < 8:
                if zl:
                    P.I(POOL, "memset", [], [ub], ub[:, 0:1], 0.0)
                if zr:
                    P.I(POOL, "memset", [], [ub], ub[:, n + 1:n + 2], 0.0)
                P.I(ACT, "activation", [pu], [ub], out=ub[:, off:off + w], in_=pu[:, off:off + w], func=AF.Copy)
                if el:
                    P.I(DVE, "tensor_scalar", [ub, edge_sb], [ub], out=ub[:, 0:1], in0=ub[:, 0:1], scalar1=edge_sb[:, 0:1], scalar2=None, op0=ALU.mult)
                if er:
                    P.I(DVE, "tensor_scalar", [ub, edge_sb], [ub], out=ub[:, n + 1:n + 2], in0=ub[:, n + 1:n + 2], scalar1=edge_sb[:, 1:2], scalar2=None, op0=ALU.mult)
                P.I(DVE, "tensor_scalar", [ub, cw_sb, cb_sb], [r1], out=r1[:, 0:n], in0=ub[:, 1:n + 1], scalar1=cw_sb[:, oc, 1:2], scalar2=cb_sb[:, oc:oc + 1], op0=ALU.mult, op1=ALU.add)
                P.I(DVE, "scalar_tensor_tensor", [ub, cw_sb, r1], [r1], out=r1[:, 0:n], in0=ub[:, 0:n], scalar=cw_sb[:, oc, 0:1], in1=r1[:, 0:n], op0=ALU.mult, op1=ALU.add)
                P.I(DVE, "scalar_tensor_tensor", [ub, cw_sb, r1], [r1], out=r1[:, 0:n], in0=ub[:, 2:n + 2], scalar=cw_sb[:, oc, 2:3], in1=r1[:, 0:n], op0=ALU.mult, op1=ALU.add)
                P.I(ACT, "activation", [r1], [r2], out=r2[:, 0:n], in_=r1[:, 0:n], func=AF.Silu)
                outs.append(P.dma(dq, out_dram[oc * 128:(oc + 1) * 128, ocol0:ocol0 + n], r2[:, 0:n], reads=[r2]))
            elif oc < 16:
                hd = (oc - 8) % 4
                P.I(ACT, "activation", [pu], [r1], out=r1[:, 0:n], in_=ctr, func=AF.Sigmoid)
                P.I(DVE, "tensor_scalar", [r1, oml, lbc], [r1], out=r1[:, 0:n], in0=r1[:, 0:n], scalar1=oml[:, hd:hd + 1], scalar2=lbc[:, hd:hd + 1], op0=ALU.mult, op1=ALU.add)
                P.I(DVE, "tensor_scalar", [r1], [r2], out=r2[:, 0:n], in0=r1[:, 0:n], scalar1=-1.0, scalar2=1.0, op0=ALU.mult, op1=ALU.add)
                outs.append(P.dma(dq, out_dram[1024 + (oc - 8) * 128:1024 + (oc - 7) * 128, ocol0:ocol0 + n], r2[:, 0:n], reads=[r2]))
                P.I(ACT, "activation", [r1], [ub], out=ub[:, 0:n], in_=r1[:, 0:n], func=AF.Ln)
                outs.append(P.dma(dq, out_dram[2048 + (oc - 8) * 128:2048 + (oc - 7) * 128, ocol0:ocol0 + n], ub[:, 0:n], reads=[ub]))
            elif oc < 20:
                P.I(ACT, "activation", [pu], [r1], out=r1[:, 0:n], in_=ctr, func=AF.Copy)
                outs.append(P.dma(dq, out_dram[3072 + (oc - 16) * 128:3072 + (oc - 15) * 128, ocol0:ocol0 + n], r1[:, 0:n], reads=[r1]))
            elif oc < 32:
                P.I(ACT, "activation", [pu], [r1], out=r1[:, 0:n], in_=ctr, func=AF.Silu)
                outs.append(P.dma(dq, out_dram[3584 + (oc - 20) * 128:3584 + (oc - 19) * 128, ocol0:ocol0 + n], r1[:, 0:n], reads=[r1]))
            else:
                P.I(ACT, "activation", [pu, dtb_sb], [r1], out=r1[0:16, 0:n], in_=pu[0:16, 1:n + 1], func=AF.Exp, bias=dtb_sb[:, 0:1], scale=1.0)
                P.I(ACT, "activation", [r1], [r2], out=r2[0:16, 0:n], in_=r1[0:16, 0:n], func=AF.Ln, bias=1.0, scale=1.0)
                outs.append(P.dma(dq, out_dram[5120:5136, ocol0:ocol0 + n], r2[0:16, 0:n], reads=[r2]))

    lo = 0
    while lo < TLOC:
        n = min(510, TLOC - lo)
        tile(HALO + lo, n, outT, lo, False, False, lo == 0, lo + n == TLOC)
        lo += n
    tile(TEXT, NCTX, outcT, 0, True, True, False, False)
    return P.finish(outs)


def prep_D(inp, xT_full, xcT):
    layer = 1
    xTp = np.concatenate([np.zeros((D, HALO), np.float32), xT_full, np.zeros((D, HALO), np.float32)], axis=1)
    w = inp['w_in_odd'][0]
    w_perm = np.ascontiguousarray(np.concatenate([w[:, 0:1024], w[:, 1040:2064], w[:, 2064:2576], w[:, 2576:3088], w[:, 3088:3600],
                                                   w[:, 3600:4112], w[:, 1024:1040]], axis=1))
    cvec = np.stack([col_layout(inp['c'][0], 8), col_layout(inp['c_ctx'], 8)], axis=2).reshape(128, 16)
    hl = inp['hg_lower_bounds']
    common = dict(
        ctxT=np.ascontiguousarray(xcT), cvec=np.ascontiguousarray(cvec),
        adaw=np.ascontiguousarray(inp['ada_w'][layer][:, 0:3072]), adab=col_layout(inp['ada_b'][layer][0:3072], 24),
        normg=col_layout(inp['norm_g'][layer, 0], 8), w_in=w_perm, onesd=np.ones((128, 128), np.float32),
        convw=np.ascontiguousarray(inp['ssd_conv_w'][0].reshape(3, 8, 128).transpose(2, 1, 0).reshape(128, 24)),
        convb=col_layout(inp['ssd_conv_b'][0], 8),
        hglb=np.ascontiguousarray(np.concatenate([col_layout(hl[0], 4), col_layout(hl[1], 4)], axis=1)),
        dtb=np.ascontiguousarray(inp['ssd_dt_bias'][0].reshape(16, 1)))
    maps = []
    for c in range(NCORE):
        s0 = c * TLOC
        edge = np.ones((128, 2), np.float32); edge[:, 0] = 0.0 if c == 0 else 1.0; edge[:, 1] = 0.0 if c == NCORE - 1 else 1.0
        m = dict(common); m.update(xT=np.ascontiguousarray(xTp[:, s0:s0 + TEXT]), edge=edge)
        maps.append(m)
    return blobify(maps, BLOB_D)


LS = NCTX + SEQ
NBLK = LS // 128
GB = 10


def build_E():
    P = Prog()
    Bk = Banks(P)
    di = lambda n, s, dt=F32: P.dram(n, s, dt, "ExternalInput")
    xtok = di("xtok", [2, 128, NBLK, 64]); Btok = di("Btok", [2, 128, NBLK, 128])
    BT = di("BT", [2, 128, LS]); CT = di("CT", [2, 128, LS]); dttok = di("dttok", [2, 128, NBLK])
    bl = Blob(BLOB_E).dram(P)
    ssdp = bl.ap("ssdp")
    qT = di("qT", [2, 128, LS]); kT = di("kT", [2, 128, LS]); gtok = di("gtok", [2, 128, NBLK, 128])
    vZ = di("vZ", [2, 128, NBLK, 5, 64])
    Ud = bl.ap("U"); U4d = bl.ap("U4"); Mnegd = bl.ap("Mneg")
    ident = bl.ap("ident"); onesd = bl.ap("onesd")
    ytok = P.dram("ytok", [2, 128, NBLK, 64], F32, "ExternalOutput"); otok = P.dram("otok", [2, 128, NBLK, 64], F32, "ExternalOutput")

    ones_sb = P.sb([128, 128], F32, name="ones"); P.dma(SP, ones_sb[:], onesd[:, :], writes=[ones_sb])
    U_sb = P.sb([128, 128], F32, name="U"); P.dma(SP, U_sb[:], Ud[:, :], writes=[U_sb])
    U4_sb = P.sb([128, 128], F32, name="U4"); P.dma(SP, U4_sb[:], U4d[:, :], writes=[U4_sb])
    Mn_sb = P.sb([128, 128], F32, name="Mneg"); P.dma(SP, Mn_sb[:], Mnegd[:, :], writes=[Mn_sb])
    id_b = P.sb([128, 128], BF16, name="idb"); P.dma(POOL, id_b[:], ident[:, :], writes=[id_b])
    sp_sb = P.sb([128, 4], F32, name="ssdp"); P.dma(SP, sp_sb[:], ssdp[:, :], writes=[sp_sb])
    acol = P.sb([128, 2], F32, name="acol")
    P.I(ACT, "activation", [sp_sb], [acol], out=acol[:], in_=sp_sb[:, 0:2], func=AF.Exp)
    P.I(DVE, "tensor_scalar", [acol], [acol], out=acol[:], in0=acol[:], scalar1=-1.0, scalar2=None, op0=ALU.mult)
    outs = []

    with P.scope():
        dt_sb = P.sb([128, 2, NBLK], F32, name="dt_sb")
        for d in range(2):
            P.dma(SP, dt_sb[:, d, :], dttok[d], writes=[dt_sb])
        xg = [P.sb([128, GB, 64], F32, name=f"xg{i}") for i in range(2)]
        Bg = [P.sb([128, GB, 128], BF16, name=f"Bg{i}") for i in range(2)]
        BTg = [P.sb([128, GB * 128], BF16, name=f"BTg{i}") for i in range(2)]
        CTg = [P.sb([128, GB * 128], F32, name=f"CTg{i}") for i in range(2)]
        CTh = [P.sb([128, GB * 128], BF16, name=f"CTh{i}") for i in range(2)]
        yg = [P.sb([128, GB, 64], F32, name=f"yg{i}") for i in range(2)]
        da = P.sb([128, 1], F32, name="da"); dab = P.sb([128, 128], F32, name="dab")
        cs_sb = P.sb([128, 1], F32, name="cs_sb"); tot_sb = P.sb([128, 1], F32, name="tot_sb")
        te = P.sb([128, 1], F32, name="te"); dec = P.sb([128, 1], F32, name="dec"); wcol = P.sb([128, 1], F32, name="wcol")
        xdt = P.sb([128, 64], BF16, name="xdt"); xw = P.sb([128, 64], BF16, name="xw")
        em = P.sb([128, 128], F32, name="em"); gt = P.sb([128, 128], BF16, name="gt")
        ecs = P.sb([128, 128], F32, name="ecs"); cp = P.sb([128, 128], BF16, name="cp")
        S = P.sb([128, 64], F32, name="S_ssd"); Sbf = P.sb([128, 64], BF16, name="Sbf_ssd")
        gi = 0
        for d in range(2):
            P.I(DVE, "memset", [], [S], S[:], 0.0)
            P.I(POOL, "memset", [], [Sbf], Sbf[:], 0.0)
            for g0 in range(0, NBLK, GB):
                i = gi % 2; gi += 1
                P.dma(SP, xg[i][:], xtok[d, :, g0:g0 + GB, :], writes=[xg[i]])
                P.dma(POOL, Bg[i][:], Btok[d, :, g0:g0 + GB, :], writes=[Bg[i]])
                P.dma(POOL, BTg[i][:], BT[d, :, g0 * 128:(g0 + GB) * 128], writes=[BTg[i]])
                P.dma(ACT, CTg[i][:], CT[d, :, g0 * 128:(g0 + GB) * 128], writes=[CTg[i]])
                P.dma(POOL, CTh[i][:], CT[d, :, g0 * 128:(g0 + GB) * 128], writes=[CTh[i]])
                for bb in range(GB):
                    b = g0 + bb
                    cols = slice(bb * 128, (bb + 1) * 128)
                    P.I(DVE, "tensor_scalar", [dt_sb, acol], [da], out=da[:], in0=dt_sb[:, d, b:b + 1], scalar1=acol[:, d:d + 1], scalar2=None, op0=ALU.mult)
                    P.I(DVE, "tensor_scalar", [ones_sb, da], [dab], out=dab[:], in0=ones_sb[:], scalar1=da[:, 0:1], scalar2=None, op0=ALU.mult)
                    pc, pr = Bk.f[0], Bk.f[1]
                    P.I(PE, "matmul", [U_sb, da], [pc], pc[:, 0:1], U_sb[:], da[:], start=True, stop=True)
                    P.I(PE, "matmul", [dab, U_sb], [pr], pr[:, 0:128], dab[:], U_sb[:], start=True, stop=True)
                    P.I(ACT, "activation", [pc], [cs_sb], out=cs_sb[:], in_=pc[:, 0:1], func=AF.Copy)
                    P.I(ACT, "activation", [pr], [tot_sb], out=tot_sb[:], in_=pr[:, 127:128], func=AF.Copy)
                    P.I(ACT, "activation", [cs_sb, tot_sb], [te], out=te[:], in_=cs_sb[:], func=AF.Exp, scale=-1.0, bias=tot_sb[:, 0:1])
                    P.I(ACT, "activation", [tot_sb], [dec], out=dec[:], in_=tot_sb[:], func=AF.Exp)
                    P.I(DVE, "tensor_scalar", [xg[i], dt_sb], [xdt], out=xdt[:], in0=xg[i][:, bb, :], scalar1=dt_sb[:, d, b:b + 1], scalar2=None, op0=ALU.mult)
                    P.I(DVE, "tensor_tensor", [dt_sb, te], [wcol], out=wcol[:], in0=dt_sb[:, d, b:b + 1], in1=te[:], op=ALU.mult)
                    P.I(DVE, "tensor_scalar", [xg[i], wcol], [xw], out=xw[:], in0=xg[i][:, bb, :], scalar1=wcol[:, 0:1], scalar2=None, op0=ALU.mult)
                    psc = Bk.f[2]
                    P.I(PE, "matmul", [Bg[i], xw], [psc], psc[:, 0:64], Bg[i][:, bb, :], xw[:], start=True, stop=True)
                    pss = Bk.f[3]
                    P.I(PE, "matmul", [BTg[i], CTh[i]], [pss], pss[:, 0:128], BTg[i][:, cols], CTh[i][:, cols], start=True, stop=True)
                    P.I(DVE, "scalar_tensor_tensor", [pr, cs_sb, Mn_sb], [em], out=em[:], in0=pr[:, 0:128], scalar=cs_sb[:, 0:1], in1=Mn_sb[:], op0=ALU.subtract, op1=ALU.add)
                    P.I(ACT, "activation", [em], [em], out=em[:], in_=em[:], func=AF.Exp)
                    P.I(DVE, "tensor_tensor", [pss, em], [gt], out=gt[:], in0=pss[:, 0:128], in1=em[:], op=ALU.mult)
                    P.I(ACT, "activation", [pr], [ecs], out=ecs[:], in_=pr[:, 0:128], func=AF.Exp)
                    P.I(DVE, "tensor_tensor", [CTg[i], ecs], [cp], out=cp[:], in0=CTg[i][:, cols], in1=ecs[:], op=ALU.mult)
                    py = Bk.f[4]
                    P.I(PE, "matmul", [gt, xdt], [py], py[:, 0:64], gt[:], xdt[:], start=True, stop=False)
                    P.I(PE, "matmul", [cp, Sbf], [py], py[:, 0:64], cp[:], Sbf[:], start=False, stop=True)
                    P.I(DVE, "scalar_tensor_tensor", [xg[i], sp_sb, py], [yg[i]], out=yg[i][:, bb, :], in0=xg[i][:, bb, :], scalar=sp_sb[:, 2 + d:3 + d], in1=py[:, 0:64], op0=ALU.mult, op1=ALU.add)
                    P.I(DVE, "scalar_tensor_tensor", [S, dec, psc], [S], out=S[:], in0=S[:], scalar=dec[:, 0:1], in1=psc[:, 0:64], op0=ALU.mult, op1=ALU.add)
                    P.I(ACT, "activation", [S], [Sbf], out=Sbf[:], in_=S[:], func=AF.Copy)
                outs.append(P.dma(SP, ytok[d, :, g0:g0 + GB, :], yg[i][:], reads=[yg[i]]))

    with P.scope():
        qg = [P.sb([128, GB * 128], F32, name=f"qg{i}") for i in range(2)]
        kg = [P.sb([128, GB * 128], F32, name=f"kg{i}") for i in range(2)]
        gg = [P.sb([128, GB, 128], F32, name=f"gg{i}") for i in range(2)]
        vg = [P.sb([128, GB, 5, 64], BF16, name=f"vg{i}") for i in range(2)]
        og = [P.sb([128, GB, 64], F32, name=f"og{i}") for i in range(2)]
        cum = P.sb([128, 128], F32, name="cum"); d1 = P.sb([128, 128], F32, name="d1"); d2 = P.sb([128, 128], F32, name="d2")
        e1 = P.sb([128, 128], F32, name="e1"); e2 = P.sb([128, 128], F32, name="e2"); e3 = P.sb([128, 128], F32, name="e3"); e4 = P.sb([128, 128], F32, name="e4")
        dec4 = P.sb([128, 4], F32, name="dec4")
        QiZ = P.sb([128, 4, 128], BF16, name="QiZ"); P.I(POOL, "memset", [], [QiZ], QiZ[:], 0.0)
        Qp = P.sb([128, 128], BF16, name="Qp"); Kp = P.sb([128, 128], BF16, name="Kp"); Kpp = P.sb([128, 128], BF16, name="Kpp")
        am = P.sb([128, 128], F32, name="am"); amb = P.sb([128, 128], BF16, name="amb"); Ktok = P.sb([128, 128], BF16, name="Ktok")
        S = P.sb([128, 64], F32, name="S_hg"); Sst = [P.sb([128, 4, 64], BF16, name=f"Sst{i}") for i in range(2)]
        c3 = lambda t: t[:].rearrange("p (c t) -> p c t", t=32)
        QiZd = bass.AP(QiZ[:].tensor, 0, [[QiZ[:].ap[0][0], 128], [128 + 32, 4], [1, 32]])
        gi = 0; blk = 0
        for d in range(2):
            P.I(DVE, "memset", [], [S], S[:], 0.0)
            P.I(POOL, "memset", [], [Sst[blk % 2]], Sst[blk % 2][:], 0.0)
            for g0 in range(0, NBLK, GB):
                i = gi % 2; gi += 1
                P.dma(SP, qg[i][:], qT[d, :, g0 * 128:(g0 + GB) * 128], writes=[qg[i]])
                P.dma(ACT, kg[i][:], kT[d, :, g0 * 128:(g0 + GB) * 128], writes=[kg[i]])
                P.dma(SP, gg[i][:], gtok[d, :, g0:g0 + GB, :], writes=[gg[i]])
                P.dma(POOL, vg[i][:], vZ[d, :, g0:g0 + GB, :, :], writes=[vg[i]])
                for bb in range(GB):
                    cols = slice(bb * 128, (bb + 1) * 128)
                    cur, nxt = Sst[blk % 2], Sst[(blk + 1) % 2]; blk += 1
                    pcum = Bk.f[0]
                    P.I(PE, "matmul", [gg[i], U4_sb], [pcum], pcum[:, 0:128], gg[i][:, bb, :], U4_sb[:], start=True, stop=True)
                    P.I(ACT, "activation", [pcum], [cum], out=cum[:], in_=pcum[:, 0:128], func=AF.Copy)
                    rmid = c3(cum)[:, :, 15:16].broadcast_to([128, 4, 32]); cend = c3(cum)[:, :, 31:32].broadcast_to([128, 4, 32])
                    P.I(DVE, "tensor_tensor", [cum], [d1], out=c3(d1), in0=c3(cum), in1=rmid, op=ALU.subtract)
                    P.I(DVE, "tensor_tensor", [cum], [d2], out=c3(d2), in0=c3(cum), in1=cend, op=ALU.subtract)
                    P.I(ACT, "activation", [cum], [e1], out=e1[:], in_=cum[:], func=AF.Exp)
                    P.I(ACT, "activation", [d1], [e2], out=e2[:], in_=d1[:], func=AF.Exp)
                    P.I(ACT, "activation", [d1], [e3], out=e3[:], in_=d1[:], func=AF.Exp, scale=-1.0)
                    P.I(ACT, "activation", [d2], [e4], out=e4[:], in_=d2[:], func=AF.Exp, scale=-1.0)
                    P.I(ACT, "activation", [cum], [dec4], out=dec4[:], in_=c3(cum)[:, :, 31], func=AF.Exp)
                    P.I(DVE, "tensor_tensor", [qg[i], e1], [QiZ], out=QiZd, in0=qg[i][:, cols].rearrange("p (c t) -> p c t", t=32), in1=c3(e1), op=ALU.mult)
                    P.I(DVE, "tensor_tensor", [qg[i], e2], [Qp], out=Qp[:], in0=qg[i][:, cols], in1=e2[:], op=ALU.mult)
                    P.I(DVE, "tensor_tensor", [kg[i], e3], [Kp], out=Kp[:], in0=kg[i][:, cols], in1=e3[:], op=ALU.mult)
                    P.I(DVE, "tensor_tensor", [kg[i], e4], [Kpp], out=Kpp[:], in0=kg[i][:, cols], in1=e4[:], op=ALU.mult)
                    pa = Bk.f[1]
                    P.I(PE, "matmul", [Kp, Qp], [pa], pa[:, 0:128], Kp[:], Qp[:], start=True, stop=True)
                    P.I(DVE, "tensor_scalar", [pa], [am], out=am[:], in0=pa[:, 0:128], scalar1=1e30, scalar2=-1e30, op0=ALU.min, op1=ALU.max)
                    P.I(DVE, "tensor_tensor", [am, U4_sb], [amb], out=amb[:], in0=am[:], in1=U4_sb[:], op=ALU.mult)
                    pt = Bk.h[0]
                    P.I(PE, "transpose", [Kpp, id_b], [pt], pt[:, 0:128], Kpp[:], id_b[:])
                    P.I(ACT, "activation", [pt], [Ktok], out=Ktok[:], in_=pt[:, 0:128], func=AF.Copy)
                    po = Bk.f[2]
                    P.I(PE, "matmul", [amb, vg[i]], [po], po[:, 0:64], amb[:], vg[i][:, bb, 4, :], start=True, stop=False)
                    for ci in range(4):
                        P.I(PE, "matmul", [QiZ, cur], [po], po[:, 0:64], QiZ[:, ci, :], cur[:, ci, :], start=False, stop=(ci == 3))
                        if True:
                            psc = Bk.f[3 + ci % 2]
                            P.I(PE, "matmul", [Ktok, vg[i]], [psc], psc[:, 0:64], Ktok[:], vg[i][:, bb, ci, :], start=True, stop=True)
                            P.I(DVE, "scalar_tensor_tensor", [S, dec4, psc], [S], out=S[:], in0=S[:], scalar=dec4[:, ci:ci + 1], in1=psc[:, 0:64], op0=ALU.mult, op1=ALU.add)
                            if ci < 3:
                                P.I(ACT, "activation", [S], [cur], out=cur[:, ci + 1, :], in_=S[:], func=AF.Copy)
                            else:
                                P.I(ACT, "activation", [S], [nxt], out=nxt[:, 0, :], in_=S[:], func=AF.Copy)
                    P.I(ACT, "activation", [po], [og[i]], out=og[i][:, bb, :], in_=po[:, 0:64], func=AF.Copy)
                outs.append(P.dma(SP, otok[d, :, g0:g0 + GB, :], og[i][:], reads=[og[i]]))
    return P.finish(outs)


def gather_D(results):
    full = np.concatenate([results[c]['outT'] for c in range(NCORE)], axis=1)
    return full, results[0]['outcT']


def prep_E(inp, dfull, dctx):
    def seq(rows_lat, rows_ctx, d):
        if d == 0:
            return np.concatenate([rows_ctx, rows_lat], axis=1)
        return np.concatenate([rows_ctx[:, ::-1], rows_lat[:, ::-1]], axis=1)

    def blk(a):
        return np.ascontiguousarray(a.T.reshape(NBLK, 128, a.shape[0]).transpose(1, 0, 2))
    s_ = np.arange(128)[:, None]; l_ = np.arange(128)[None, :]
    U = (s_ <= l_).astype(np.float32)
    U4 = ((s_ // 32 == l_ // 32) & (s_ <= l_)).astype(np.float32)
    Mneg = np.where(s_ <= l_, 0.0, MASKNEG).astype(np.float32)
    maps = []
    for c in range(NCORE):
        h = c; g = c // 4; hh = c // 2; vh = c % 2
        R = lambda r0, n, d: seq(dfull[r0:r0 + n], dctx[r0:r0 + n], d)
        m = dict(U=U, U4=U4, Mneg=Mneg, ident=np.eye(128, dtype=np.float32), onesd=np.ones((128, 128), np.float32))
        m['xtok'] = np.stack([blk(R(64 * h, 64, d)) for d in range(2)])
        m['Btok'] = np.stack([blk(R(512 + 128 * g, 128, d)) for d in range(2)])
        m['BT'] = np.stack([np.ascontiguousarray(R(512 + 128 * g, 128, d)) for d in range(2)])
        m['CT'] = np.stack([np.ascontiguousarray(R(768 + 128 * g, 128, d)) for d in range(2)])
        m['dttok'] = np.stack([np.ascontiguousarray(R(5120 + 8 * d + h, 1, d)[0].reshape(NBLK, 128).T) for d in range(2)])
        al = inp['ssd_A_log'][0]; dd = inp['ssd_D'][0]
        m['ssdp'] = np.ascontiguousarray(np.broadcast_to(np.array([al[0, h], al[1, h], dd[0, h], dd[1, h]], np.float32)[None, :], (128, 4)))
        m['qT'] = np.stack([np.ascontiguousarray(R(4096 + 128 * hh, 128, d)) for d in range(2)])
        m['kT'] = np.stack([np.ascontiguousarray(R(1024 + 512 * d + 128 * hh, 128, d)) for d in range(2)])
        m['gtok'] = np.stack([blk(R(2048 + 512 * d + 128 * hh, 128, d)) for d in range(2)])
        vz = []
        for d in range(2):
            v = blk(R(3072 + 128 * hh + 64 * vh, 64, d))
            z5 = np.zeros((128, NBLK, 5, 64), np.float32)
            z5[:, :, 4, :] = v
            for i in range(4):
                z5[32 * i:32 * i + 32, :, i, :] = v[32 * i:32 * i + 32]
            vz.append(z5)
        m['vZ'] = np.stack(vz)
        maps.append(m)
    return blobify(maps, BLOB_E)


def gather_E(results):
    yT = np.zeros((2, 512, SEQ), np.float32); oT = np.zeros((2, 512, SEQ), np.float32)
    for c in range(NCORE):
        h = c; hh = c // 2; vh = c % 2
        for d in range(2):
            for name, dst, r0 in (("ytok", yT, 64 * h), ("otok", oT, 128 * hh + 64 * vh)):
                a = results[c][name][d].transpose(1, 0, 2).reshape(LS, 64)[NCTX:]
                if d == 1:
                    a = a[::-1]
                dst[d, r0:r0 + 64] = a.T
    return yT, oT


def build_M():
    P = Prog()
    Bk = Banks(P)
    di = lambda n, s, dt=F32: P.dram(n, s, dt, "ExternalInput")
    yT = di("yT", [2, 512, TLOC]); oT = di("oT", [2, 512, TLOC]); szT = di("szT", [512, TLOC]); sgT = di("sgT", [512, TLOC])
    bl = Blob(BLOB_M).dram(P)
    nrm = bl.ap("nrm"); onesd = bl.ap("onesd")
    mT = P.dram("mT", [D, TLOC], F32, "ExternalOutput")
    ones_sb = P.sb([128, 128], F32, name="ones"); P.dma(SP, ones_sb[:], onesd[:, :], writes=[ones_sb])
    nrm_sb = P.sb([128, 8], F32, name="nrm"); P.dma(SP, nrm_sb[:], nrm[:, :], writes=[nrm_sb])
    a = [P.sb([128, 4, 512], F32, name=f"ma{i}") for i in range(2)]
    b = [P.sb([128, 4, 512], F32, name=f"mb{i}") for i in range(2)]
    g = [P.sb([128, 4, 512], F32, name=f"mg{i}") for i in range(2)]
    sq = P.sb([128, 4, 512], F32, name="msq"); rs = P.sb([128, 512], F32, name="mrs")
    outs = []
    it = 0
    for part in range(2):
        src = yT if part == 0 else oT
        gsrc = szT if part == 0 else sgT
        for t0 in range(0, TLOC, 512):
            i = it % 2; it += 1
            P.dma(SP, a[i][:], src[0, :, t0:t0 + 512].rearrange("(k p) t -> p k t", p=128), writes=[a[i]])
            P.dma(ACT, b[i][:], src[1, :, t0:t0 + 512].rearrange("(k p) t -> p k t", p=128), writes=[b[i]])
            P.dma(SP, g[i][:], gsrc[:, t0:t0 + 512].rearrange("(k p) t -> p k t", p=128), writes=[g[i]])
            P.I(DVE, "tensor_tensor", [a[i], b[i]], [a[i]], out=a[i][:], in0=a[i][:], in1=b[i][:], op=ALU.add)
            if part == 0:
                P.I(DVE, "tensor_tensor", [a[i], g[i]], [a[i]], out=a[i][:], in0=a[i][:], in1=g[i][:], op=ALU.mult)
            P.I(ACT, "activation", [a[i]], [sq], out=sq[:], in_=a[i][:], func=AF.Square)
            groups = [(0, 2), (2, 4)] if part == 0 else [(0, 1), (1, 2), (2, 3), (3, 4)]
            for (k0, k1) in groups:
                ps = Bk.f[k0 % 2]
                for kc in range(k0, k1):
                    P.I(PE, "matmul", [ones_sb, sq], [ps], ps[:, 0:512], ones_sb[:], sq[:, kc, :], start=(kc == k0), stop=(kc == k1 - 1))
                P.I(DVE, "tensor_scalar", [ps], [rs], out=rs[:], in0=ps[:, 0:512], scalar1=1.0 / (128 * (k1 - k0)), scalar2=EPS, op0=ALU.mult, op1=ALU.add)
                P.I(ACT, "activation", [rs], [rs], out=rs[:], in_=rs[:], func=AF.Ln)
                P.I(ACT, "activation", [rs], [rs], out=rs[:], in_=rs[:], func=AF.Exp, scale=-0.5)
                for kc in range(k0, k1):
                    P.I(DVE, "scalar_tensor_tensor", [a[i], nrm_sb, rs], [b[i]], out=b[i][:, kc, :], in0=a[i][:, kc, :], scalar=nrm_sb[:, part * 4 + kc:part * 4 + kc + 1],
                        in1=rs[:], op0=ALU.mult, op1=ALU.mult)
            if part == 1:
                P.I(DVE, "tensor_tensor", [b[i], g[i]], [b[i]], out=b[i][:], in0=b[i][:], in1=g[i][:], op=ALU.mult)
            outs.append(P.dma(POOL, mT[part * 512:(part + 1) * 512, t0:t0 + 512].rearrange("(k p) t -> p k t", p=128), b[i][:], reads=[b[i]]))
    return P.finish(outs)


def prep_M(inp, yT, oT, dfull):
    nrm = np.concatenate([col_layout(inp['ssd_norm'][0], 4), col_layout(inp['hg_norm'][0], 4)], axis=1)
    maps = []
    for c in range(NCORE):
        sl = slice(c * TLOC, (c + 1) * TLOC)
        maps.append(dict(yT=np.ascontiguousarray(yT[:, :, sl]), oT=np.ascontiguousarray(oT[:, :, sl]),
                         szT=np.ascontiguousarray(dfull[3584:4096, sl]), sgT=np.ascontiguousarray(dfull[4608:5120, sl]),
                         nrm=np.ascontiguousarray(nrm), onesd=np.ones((128, 128), np.float32)))
    return blobify(maps, BLOB_M)


def _run(nc, maps, tag=""):
    import time, sys
    t0 = time.time()
    r = run_bass_kernel_spmd(nc, maps, core_ids=list(range(len(maps)))).results
    print(f"[kernel] launch {tag}: {time.time() - t0:.1f}s", file=sys.stderr, flush=True)
    return r


def kernel(**inp):
    inp = {k: np.asarray(v) for k, v in inp.items()}
    x = inp['x'][0]; ctx = inp['ctx'][0]
    xT = np.ascontiguousarray(x.T); xcT = np.ascontiguousarray(ctx.T)
    rA = _run(build_A(), prep_A(inp), 'A')
    uT = np.concatenate([rA[c]['uT'] for c in range(NCORE)], axis=1)
    attT = np.concatenate([rA[c]['attT'] for c in range(NCORE)], axis=1)
    ucT = rA[0]['ucT']; attcT = rA[0]['attcT']
    hyT = gather_B(_run(build_B(SEQ), prep_B(inp, uT, SEQ), 'B'), SEQ)
    hycT = gather_B(_run(build_B(NCTX), prep_B(inp, ucT, NCTX), 'Bc'), NCTX)
    mT = np.concatenate([hyT, attT], axis=0); mcT = np.concatenate([hycT, attcT], axis=0)
    maps, passes = prep_C(inp, 0, mT, xT, mcT, xcT)
    xT, xcT = gather_C(_run(build_C(passes), maps, 'C0'), True)
    dfull, dctx = gather_D(_run(build_D(), prep_D(inp, xT, xcT), 'D'))
    yT, oT = gather_E(_run(build_E(), prep_E(inp, dfull, dctx), 'E'))
    rM = _run(build_M(), prep_M(inp, yT, oT, dfull), 'M')
    mT = np.concatenate([rM[c]['mT'] for c in range(NCORE)], axis=1)
    maps, passes = prep_C(inp, 1, mT, xT, None, None)
    xT, _ = gather_C(_run(build_C(passes), maps, 'C1'), False)
    return np.ascontiguousarray(xT.T)[None].astype(np.float32)
```

```python
import contextlib
import numpy as np
import concourse.bass as bass
import concourse.mybir as mybir
from concourse.bass_utils import run_bass_kernel_spmd

F32 = mybir.dt.float32
BF16 = mybir.dt.bfloat16
I32 = mybir.dt.int32
AF = mybir.ActivationFunctionType
ALU = mybir.AluOpType
AX = mybir.AxisListType

PE, DVE, ACT, POOL, SP = "tensor", "vector", "scalar", "gpsimd", "sync"
COMPUTE = (PE, DVE, ACT, POOL)
NDMASEM = 8
EPOCH_LEN = 20000


class Prog:
    def __init__(self):
        self.nc = bass.Bass("TRN2", target_bir_lowering=False)
        self.stack = contextlib.ExitStack()
        self.streams = {e: [] for e in (PE, DVE, ACT, POOL, SP)}
        self.cnt = {e: 0 for e in COMPUTE}
        self.dcnt = {e: 0 for e in (SP, ACT, POOL)}
        self.sem = {}
        self.dsem = {}
        self.waited = {}
        self.lastw = {}
        self.reads = {}
        self.ntens = 0
        self.out_tokens = []
        self.epoch = {e: 0 for e in COMPUTE}
        self.root_stack = self.stack
        for e in COMPUTE:
            self.sem[(e, 0)] = self.stack.enter_context(self.nc.semaphore("s_" + e + "_0"))
        for q in (SP, ACT, POOL):
            self.dsem[q] = [self.stack.enter_context(self.nc.semaphore(f"d_{q}_{i}")) for i in range(NDMASEM)]

    @contextlib.contextmanager
    def scope(self):
        outer = self.stack
        self.stack = contextlib.ExitStack()
        try:
            yield
        finally:
            self.barrier()
            self.stack.close()
            self.stack = outer

    def barrier(self):
        toks = [("c", e, (self.epoch[e], self.cnt[e])) for e in COMPUTE if self.cnt[e] > 0]
        for q in (SP, ACT, POOL):
            for k in range(max(0, self.dcnt[q] - NDMASEM), self.dcnt[q]):
                toks.append(("d", q, k))
        for st in (PE, DVE, ACT, POOL, SP):
            self._emit_waits(st, [t for t in toks if not (t[0] == "c" and t[1] == st)])

    def dram(self, name, shape, dtype, kind):
        return self.nc.dram_tensor(name, list(shape), dtype, kind=kind).ap()

    def sb(self, shape, dtype, name=None):
        self.ntens += 1
        name = "sb_" + (name or f"t{self.ntens}")
        return self.stack.enter_context(self.nc.sbuf_tensor(name, list(shape), dtype))

    def ps(self, shape, dtype=F32, name=None):
        self.ntens += 1
        name = "ps_" + (name or f"p{self.ntens}")
        return self.stack.enter_context(self.nc.psum_tensor(name, list(shape), dtype))

    def _key(self, t):
        if isinstance(t, str):
            return t
        if isinstance(t, tuple):
            return t
        th = getattr(t, "tensor", t)
        return getattr(th, "name", None) or id(th)

    def _tok_sem_val(self, tok):
        kind, e, i = tok
        if kind == "c":
            ep, idx = i
            return self.sem[(e, ep)], idx, ("c", e, ep)
        return self.dsem[e][i % NDMASEM], 16 * (i // NDMASEM + 1), ("d", e, i % NDMASEM)

    def _emit_waits(self, stream, toks):
        need = {}
        for tok in toks:
            if tok is None:
                continue
            s, v, k = self._tok_sem_val(tok)
            if tok[0] == "c" and tok[1] == stream and stream == PE:
                continue
            if k not in need or need[k][1] < v:
                need[k] = (s, v)
        for k, (s, v) in need.items():
            if self.waited.get((stream, k), 0) >= v:
                continue
            self.waited[(stream, k)] = v
            self.streams[stream].append(("wait", s, v))

    def _deps(self, reads, writes):
        toks = []
        for t in reads:
            k = self._key(t)
            toks.append(self.lastw.get(k))
        for t in writes:
            k = self._key(t)
            toks.append(self.lastw.get(k))
            toks.extend(self.reads.get(k, []))
        return toks

    def _commit(self, tok, reads, writes):
        for t in reads:
            k = self._key(t)
            self.reads.setdefault(k, []).append(tok)
            if len(self.reads[k]) > 24:
                best = {}
                for tk in self.reads[k]:
                    kk = (tk[0], tk[1], tk[2][0]) if tk[0] == "c" else tk
                    if kk not in best or best[kk][2] < tk[2]:
                        best[kk] = tk
                self.reads[k] = list(best.values())
        for t in writes:
            k = self._key(t)
            self.lastw[k] = tok
            self.reads[k] = []

    def op(self, eng, fn, reads=(), writes=()):
        self._emit_waits(eng, self._deps(reads, writes))
        if self.cnt[eng] >= EPOCH_LEN:
            self.epoch[eng] += 1
            self.cnt[eng] = 0
            self.sem[(eng, self.epoch[eng])] = self.root_stack.enter_context(self.nc.semaphore(f"s_{eng}_{self.epoch[eng]}"))
        self.cnt[eng] += 1
        tok = ("c", eng, (self.epoch[eng], self.cnt[eng]))
        self.streams[eng].append(("op", fn, self.sem[(eng, self.epoch[eng])], 1))
        self._commit(tok, reads, writes)
        return tok

    def I(self, eng, mname, reads, writes, *args, **kw):
        return self.op(eng, lambda e: getattr(e, mname)(*args, **kw), reads=reads, writes=writes)

    def dma(self, q, out, in_, reads=(), writes=(), **kw):
        k = self.dcnt[q]
        toks = self._deps(reads, writes)
        if k >= NDMASEM:
            toks.append(("d", q, k - NDMASEM))
        self._emit_waits(q, toks)
        self.dcnt[q] += 1
        tok = ("d", q, k)
        self.streams[q].append(("op", lambda e: e.dma_start(out=out, in_=in_, **kw), self.dsem[q][k % NDMASEM], 16))
        self._commit(tok, reads, writes)
        return tok

    def finish(self, final_toks):
        self._emit_waits(SP, final_toks)
        nc = self.nc
        streams = self.streams

        def run(engine, lst):
            for it in lst:
                if it[0] == "wait":
                    engine.wait_ge(it[1], it[2])
                else:
                    it[1](engine).then_inc(it[2], it[3])

        with nc.Block() as block:
            @block.sync
            def _(e):
                run(e, streams[SP])

            @block.tensor
            def _(e):
                run(e, streams[PE])

            @block.vector
            def _(e):
                run(e, streams[DVE])

            @block.scalar
            def _(e):
                run(e, streams[ACT])

            @block.gpsimd
            def _(e):
                run(e, streams[POOL])
        self.stack.close()
        return nc


D = 1024
SEQ = 16384
NCORE = 8
TLOC = SEQ // NCORE
HALO = 128
TEXT = TLOC + 2 * HALO
NCTX = 256
EPS = 1e-6
MASKNEG = -30000.0


def _bcast_free(ap2d, n):
    return ap2d.unsqueeze(2).broadcast_to([ap2d.shape[0], ap2d.shape[1], n])


class Blob:
    def __init__(self, items):
        self.items = {}
        o = 0
        for name, rows, cols in items:
            self.items[name] = (rows, o, cols); o += cols
        self.total = o
        self.t = None

    def dram(self, P):
        self.t = P.dram("blob", [128, self.total], F32, "ExternalInput")
        return self

    def ap(self, name):
        rows, o, cols = self.items[name]
        return self.t[0:rows, o:o + cols]

    def pack(self, d):
        out = np.zeros((128, self.total), np.float32)
        for name, (rows, o, cols) in self.items.items():
            out[0:rows, o:o + cols] = np.asarray(d[name], np.float32).reshape(rows, cols)
        return out


BLOB_A = [("cvec", 128, 16), ("adab", 128, 24), ("normg", 128, 8), ("gains", 128, 640), ("masks", 128, 512), ("sinkrow", 128, 1024),
          ("ident", 128, 128), ("onesd", 128, 128), ("convw", 128, 36), ("convb", 128, 12), ("edge", 128, 2)]
BLOB_C = [("cvec", 128, 16), ("adab", 128, 32), ("normg", 128, 8), ("rw", 128, 256), ("rb", 128, 32), ("bgu", 128, 512),
          ("ident", 128, 128), ("onesd", 128, 128)]
BLOB_B = [("w1", 33, 64), ("w2", 64, 64), ("w3", 64, 64), ("w4s", 64, 256), ("fqb", 64, 4), ("negd", 128, 1), ("fbias", 128, 128),
          ("ident", 128, 128), ("onesd", 128, 128), ("jmat", 128, 128)]
BLOB_D = [("cvec", 128, 16), ("adab", 128, 24), ("normg", 128, 8), ("onesd", 128, 128), ("convw", 128, 24), ("convb", 128, 8),
          ("edge", 128, 2), ("hglb", 128, 8), ("dtb", 16, 1)]
BLOB_E = [("ssdp", 128, 4), ("U", 128, 128), ("U4", 128, 128), ("Mneg", 128, 128), ("ident", 128, 128), ("onesd", 128, 128)]
BLOB_M = [("nrm", 128, 8), ("onesd", 128, 128)]


def blobify(maps, spec):
    bl = Blob(spec)
    out = []
    for m in maps:
        m2 = {k: v for k, v in m.items() if k not in bl.items}
        m2['blob'] = bl.pack(m)
        out.append(m2)
    return out


class Banks:
    def __init__(self, P, nb16=1):
        self.f = [P.ps([128, 512], F32, name=f"bank{i}") for i in range(8 - nb16)]
        self.h = [P.ps([128, 1024], BF16, name=f"bankh{i}") for i in range(nb16)]


def emit_adaln(P, B, cv_sb, adaw_dram, adab_sb, wbuf, out_sb, ncol, nparts=3):
    sc = P.sb([128, 8, ncol], F32, name="ada_silu")
    P.op(ACT, lambda e: e.activation(out=sc[:], in_=cv_sb[:], func=AF.Silu), reads=[cv_sb], writes=[sc])
    ps = B.f[0]
    for part in range(nparts):
        for kc in range(8):
            P.dma(SP if kc % 2 == 0 else ACT, wbuf[:, kc, :], adaw_dram[kc * 128:(kc + 1) * 128, part * 1024:(part + 1) * 1024],
                  writes=[wbuf])
        for o in range(8):
            oc = part * 8 + o
            for kc in range(8):
                P.op(PE, lambda e, o=o, kc=kc, oc=oc: e.matmul(ps[:, oc * ncol:(oc + 1) * ncol], wbuf[:, kc, o * 128:(o + 1) * 128],
                                                               sc[:, kc, :], start=(kc == 0), stop=(kc == 7)),
                     reads=[wbuf, sc], writes=[ps])
    P.op(DVE, lambda e: e.tensor_tensor(out=out_sb[:], in0=ps[:, 0:8 * nparts * ncol].rearrange("p (o n) -> p o n", n=ncol),
                                        in1=_bcast_free(adab_sb[:], ncol), op=ALU.add),
         reads=[ps, adab_sb], writes=[out_sb])


def emit_norm_mod(P, B, x_sb, ones_sb, A_col, B_col, h_out, ntok, tmp_sq, tmp_rs, hcol0=0):
    ps = B.f[1]
    for kc in range(8):
        P.op(ACT, lambda e, kc=kc: e.activation(out=tmp_sq[:, kc, 0:ntok], in_=x_sb[:, kc, 0:ntok], func=AF.Square),
             reads=[x_sb], writes=[tmp_sq])
    for kc in range(8):
        P.op(PE, lambda e, kc=kc: e.matmul(ps[:, 0:ntok], ones_sb[:], tmp_sq[:, kc, 0:ntok], start=(kc == 0), stop=(kc == 7)),
             reads=[ones_sb, tmp_sq], writes=[ps])
    P.op(DVE, lambda e: e.tensor_scalar(out=tmp_rs[:, 0:ntok], in0=ps[:, 0:ntok], scalar1=1.0 / D, scalar2=EPS,
                                        op0=ALU.mult, op1=ALU.add), reads=[ps], writes=[tmp_rs])
    P.op(ACT, lambda e: e.activation(out=tmp_rs[:, 0:ntok], in_=tmp_rs[:, 0:ntok], func=AF.Ln), reads=[tmp_rs], writes=[tmp_rs])
    P.op(ACT, lambda e: e.activation(out=tmp_rs[:, 0:ntok], in_=tmp_rs[:, 0:ntok], func=AF.Exp, scale=-0.5), reads=[tmp_rs], writes=[tmp_rs])
    for kc in range(8):
        P.op(DVE, lambda e, kc=kc: e.tensor_tensor(out=tmp_sq[:, kc, 0:ntok], in0=x_sb[:, kc, 0:ntok], in1=tmp_rs[:, 0:ntok],
                                                   op=ALU.mult), reads=[x_sb, tmp_rs, tmp_sq], writes=[tmp_sq])
        P.op(ACT, lambda e, kc=kc: e.activation(out=h_out[:, kc, hcol0:hcol0 + ntok], in_=tmp_sq[:, kc, 0:ntok], func=AF.Identity,
                                                bias=B_col[:, kc:kc + 1], scale=A_col[:, kc:kc + 1]),
             reads=[tmp_sq, A_col, B_col], writes=[h_out])


TT = 384
NTT = TEXT // TT


def build_A():
    P = Prog()
    Bk = Banks(P)
    di = lambda n, s, dt=F32: P.dram(n, s, dt, "ExternalInput")
    do = lambda n, s, dt=F32: P.dram(n, s, dt, "ExternalOutput")
    xT = di("xT", [D, TEXT]); ctxT = di("ctxT", [D, NCTX])
    bl = Blob(BLOB_A).dram(P)
    cvec = bl.ap("cvec"); adaw = di("adaw", [D, 3072]); adab = bl.ap("adab")
    normg = bl.ap("normg"); w_in = di("w_in", [D, 2304])
    gains = bl.ap("gains"); ctab = di("ctab", [TEXT, 64]); stab = di("stab", [TEXT, 64])
    masks = bl.ap("masks"); sinkrow = bl.ap("sinkrow")
    ident = bl.ap("ident"); onesd = bl.ap("onesd")
    convw = bl.ap("convw"); convb = bl.ap("convb"); edge = bl.ap("edge")
    uT = do("uT", [1536, TLOC]); ucT = do("ucT", [1536, NCTX])
    attT = do("attT", [512, TLOC]); attcT = do("attcT", [512, NCTX])

    ones_sb = P.sb([128, 128], F32, name="ones"); P.dma(SP, ones_sb[:], onesd[:, :], writes=[ones_sb])
    id_f = P.sb([128, 128], F32, name="idf"); P.dma(SP, id_f[:], ident[:, :], writes=[id_f])
    id_b = P.sb([128, 128], BF16, name="idb"); P.dma(POOL, id_b[:], ident[:, :], writes=[id_b])
    cv = P.sb([128, 8, 2], F32, name="cv"); P.dma(SP, cv[:], cvec.rearrange("p (k n) -> p k n", n=2), writes=[cv])
    adab_sb = P.sb([128, 24], F32, name="adab"); P.dma(SP, adab_sb[:], adab[:, :], writes=[adab_sb])
    g_sb = P.sb([128, 8], F32, name="normg"); P.dma(SP, g_sb[:], normg[:, :], writes=[g_sb])
    gains_sb = P.sb([128, 640], F32, name="gains"); P.dma(SP, gains_sb[:], gains[:, :], writes=[gains_sb])
    mask_sb = P.sb([128, 4, 128], BF16, name="masks")
    P.dma(POOL, mask_sb[:], masks.rearrange("k (m q) -> k m q", m=4), writes=[mask_sb])
    sink_sb = P.sb([128, 1024], F32, name="sink"); P.dma(SP, sink_sb[:], sinkrow[:, :], writes=[sink_sb])
    P.op(ACT, lambda e: e.activation(out=sink_sb[:], in_=sink_sb[:], func=AF.Exp), reads=[sink_sb], writes=[sink_sb])
    w_sb = P.sb([128, 8, 2304], BF16, name="w_in")
    for kc in range(8):
        P.dma(POOL, w_sb[:, kc, :], w_in[kc * 128:(kc + 1) * 128, :], writes=[w_sb])

    ada = P.sb([128, 24, 2], F32, name="ada")
    with P.scope():
        wbuf = P.sb([128, 8, 1024], F32, name="adawbuf")
        emit_adaln(P, Bk, cv, adaw, adab_sb, wbuf, ada, 2)
    Acol = [P.sb([128, 8], F32, name=f"Acol{j}") for j in range(2)]
    Bcol = [P.sb([128, 8], F32, name=f"Bcol{j}") for j in range(2)]
    for j in range(2):
        P.op(DVE, lambda e, j=j: e.scalar_tensor_tensor(out=Acol[j][:], in0=ada[:, 8:16, j], scalar=1.0, in1=g_sb[:],
                                                         op0=ALU.add, op1=ALU.mult), reads=[ada, g_sb], writes=[Acol[j]])
        P.op(DVE, lambda e, j=j: e.tensor_copy(out=Bcol[j][:], in_=ada[:, 0:8, j]), reads=[ada], writes=[Bcol[j]])

    kqT = P.sb([64, 10, TEXT + NCTX], BF16, name="kqT")
    vaug = P.sb([128, (TEXT + NCTX) // 128, 2, 65], BF16, name="vaug")
    P.op(POOL, lambda e: e.memset(vaug[:], 1.0), writes=[vaug])

    x_sb = P.sb([128, 8, TT], F32, name="x_sb")
    sq_sb = P.sb([128, 8, TT], F32, name="sq_sb")
    rs_sb = P.sb([128, TT], F32, name="rs_sb")
    h_all = P.sb([128, 8, TEXT + NCTX], BF16, name="h_all")
    cw_sb = P.sb([128, 12, 3], F32, name="cw"); P.dma(SP, cw_sb[:], convw.rearrange("p (o t) -> p o t", t=3), writes=[cw_sb])
    cb_sb = P.sb([128, 12], F32, name="cb"); P.dma(SP, cb_sb[:], convb[:, :], writes=[cb_sb])
    edge_sb = P.sb([128, 2], F32, name="edge"); P.dma(SP, edge_sb[:], edge[:, :], writes=[edge_sb])
    kqv = P.sb([128, 768], F32, name="kqv")
    sq2 = P.sb([128, 640], F32, name="sq2")
    ss = P.sb([128, 10], F32, name="ss")
    tmpr = P.sb([128, 640], F32, name="tmpr")
    kqb = P.sb([128, 640], BF16, name="kqb")
    ct_sb = P.sb([128, 64], F32, name="ct"); st_sb = P.sb([128, 64], F32, name="st")
    u_sb = [P.sb([128, 512], F32, name=f"u_sb{i}") for i in range(2)]
    acc_sb = [P.sb([128, 512], F32, name=f"acc_sb{i}") for i in range(2)]

    def proj_tile(src_dram, col0, ntok, j, tok_base, is_ctx):
        for kc in range(8):
            P.dma(SP if kc % 2 == 0 else ACT, x_sb[:, kc, 0:ntok], src_dram[kc * 128:(kc + 1) * 128, col0:col0 + ntok], writes=[x_sb])
        emit_norm_mod(P, Bk, x_sb, ones_sb, Acol[j], Bcol[j], h_all, ntok, sq_sb, rs_sb, hcol0=tok_base)
        for s in range(ntok // 128):
            pa, pb = Bk.f[2], Bk.f[3]
            for kc in range(8):
                P.op(PE, lambda e, kc=kc, s=s: e.matmul(pa[:, 0:512], h_all[:, kc, tok_base + s * 128:tok_base + (s + 1) * 128], w_sb[:, kc, 0:512],
                                                        start=(kc == 0), stop=(kc == 7)), reads=[h_all, w_sb], writes=[pa])
            for kc in range(8):
                P.op(PE, lambda e, kc=kc, s=s: e.matmul(pb[:, 0:256], h_all[:, kc, tok_base + s * 128:tok_base + (s + 1) * 128], w_sb[:, kc, 512:768],
                                                        start=(kc == 0), stop=(kc == 7)), reads=[h_all, w_sb], writes=[pb])
            P.op(ACT, lambda e: e.activation(out=kqv[:, 0:512], in_=pa[:, 0:512], func=AF.Copy), reads=[pa], writes=[kqv])
            P.op(ACT, lambda e: e.activation(out=kqv[:, 512:768], in_=pb[:, 0:256], func=AF.Copy), reads=[pb], writes=[kqv])
            tile_idx = (tok_base + s * 128) // 128
            P.op(POOL, lambda e, ti=tile_idx: e.tensor_copy(out=vaug[:, ti, :, 0:64], in_=kqv[:, 640:768].rearrange("p (g d) -> p g d", d=64)),
                 reads=[kqv], writes=[vaug])
            P.op(DVE, lambda e: e.tensor_tensor(out=sq2[:], in0=kqv[:, 0:640], in1=kqv[:, 0:640], op=ALU.mult), reads=[kqv], writes=[sq2])
            P.op(DVE, lambda e: e.tensor_reduce(out=ss[:], in_=sq2[:].rearrange("p (h d) -> p h d", d=64), axis=AX.X, op=ALU.add),
                 reads=[sq2], writes=[ss])
            P.op(DVE, lambda e: e.tensor_scalar(out=ss[:], in0=ss[:], scalar1=1.0 / 64, scalar2=EPS, op0=ALU.mult, op1=ALU.add),
                 reads=[ss], writes=[ss])
            P.op(ACT, lambda e: e.activation(out=ss[:], in_=ss[:], func=AF.Ln), reads=[ss], writes=[ss])
            P.op(ACT, lambda e: e.activation(out=ss[:], in_=ss[:], func=AF.Exp, scale=-0.5), reads=[ss], writes=[ss])
            P.op(DVE, lambda e: e.tensor_tensor(out=sq2[:].rearrange("p (h d) -> p h d", d=64), in0=kqv[:, 0:640].rearrange("p (h d) -> p h d", d=64),
                                                in1=_bcast_free(ss[:], 64), op=ALU.mult), reads=[kqv, ss], writes=[sq2])
            P.op(DVE, lambda e: e.tensor_tensor(out=sq2[:], in0=sq2[:], in1=gains_sb[:], op=ALU.mult), reads=[sq2, gains_sb], writes=[sq2])
            if not is_ctx:
                r0 = col0 + s * 128
                P.dma(SP, ct_sb[:], ctab[r0:r0 + 128, :], writes=[ct_sb])
                P.dma(ACT, st_sb[:], stab[r0:r0 + 128, :], writes=[st_sb])
                v5 = lambda t: t[:].rearrange("p (h b two s) -> p (h b) two s", b=2, two=2, s=16)
                stv = st_sb[:].rearrange("p (b two s) -> p b two s", two=2, s=16)
                ctv = ct_sb[:].rearrange("p (b two s) -> p b two s", two=2, s=16)

                def bc(tv, two):
                    a = tv[:, :, two, :]
                    return a.unsqueeze(1).broadcast_to([128, 10, 2, 16])
                u4 = sq2[:].rearrange("p (h b two s) -> p h b two s", b=2, two=2, s=16)
                t4 = tmpr[:].rearrange("p (h b two s) -> p h b two s", b=2, two=2, s=16)
                for two in range(2):
                    P.op(DVE, lambda e, two=two: e.tensor_tensor(out=t4[:, :, :, two, :], in0=u4[:, :, :, 1 - two, :], in1=bc(stv, two), op=ALU.mult),
                         reads=[sq2, st_sb], writes=[tmpr])
                for two in range(2):
                    P.op(DVE, lambda e, two=two: e.tensor_tensor(out=u4[:, :, :, two, :], in0=u4[:, :, :, two, :], in1=bc(ctv, two), op=ALU.mult),
                         reads=[sq2, ct_sb], writes=[sq2])
                P.op(DVE, lambda e: e.tensor_tensor(out=kqb[:], in0=sq2[:], in1=tmpr[:], op=ALU.add), reads=[sq2, tmpr], writes=[kqb])
            else:
                P.op(DVE, lambda e: e.tensor_copy(out=kqb[:], in_=sq2[:]), reads=[sq2], writes=[kqb])
            pt = Bk.h[0]
            for hh in range(10):
                P.op(PE, lambda e, hh=hh: e.transpose(pt[0:64, hh * 128:(hh + 1) * 128][:, 0:128] if False else pt[0:64, hh * 128 % 1024:(hh * 128 % 1024) + 128],
                                                      kqb[:, hh * 64:(hh + 1) * 64], id_b[:]),
                     reads=[kqb, id_b], writes=[pt])
                if hh == 7 or hh == 9:
                    h0 = 0 if hh == 7 else 8
                    nh = hh - h0 + 1
                    t0 = tok_base + s * 128
                    P.op(ACT, lambda e, h0=h0, nh=nh, t0=t0: e.activation(
                        out=kqT[:, h0:h0 + nh, t0:t0 + 128],
                        in_=pt[0:64, (h0 * 128) % 1024:(h0 * 128) % 1024 + nh * 128].rearrange("p (h t) -> p h t", t=128), func=AF.Copy),
                        reads=[pt], writes=[kqT])

    def hyena_tile(h0, n, out_dram, ocol0, zl, zr, el, er):
        a0 = h0 - (0 if zl else 1); a1 = h0 + n + (0 if zr else 1)
        w = a1 - a0
        off = 1 if zl else 0
        for oc in range(12):
            pu = Bk.f[4 + oc % 2]
            ub = u_sb[oc % 2]; ac = acc_sb[oc % 2]
            for kc in range(8):
                P.I(PE, "matmul", [w_sb, h_all], [pu], pu[:, off:off + w], w_sb[:, kc, 768 + oc * 128:768 + (oc + 1) * 128], h_all[:, kc, a0:a1],
                    start=(kc == 0), stop=(kc == 7))
            if zl:
                P.I(POOL, "memset", [], [ub], ub[:, 0:1], 0.0)
            if zr:
                P.I(POOL, "memset", [], [ub], ub[:, n + 1:n + 2], 0.0)
            P.I(ACT, "activation", [pu], [ub], out=ub[:, off:off + w], in_=pu[:, off:off + w], func=AF.Copy)
            if el:
                P.I(DVE, "tensor_scalar", [ub, edge_sb], [ub], out=ub[:, 0:1], in0=ub[:, 0:1], scalar1=edge_sb[:, 0:1], scalar2=None, op0=ALU.mult)
            if er:
                P.I(DVE, "tensor_scalar", [ub, edge_sb], [ub], out=ub[:, n + 1:n + 2], in0=ub[:, n + 1:n + 2], scalar1=edge_sb[:, 1:2], scalar2=None, op0=ALU.mult)
            P.I(DVE, "tensor_scalar", [ub, cw_sb, cb_sb], [ac], out=ac[:, 0:n], in0=ub[:, 1:n + 1], scalar1=cw_sb[:, oc, 1:2], scalar2=cb_sb[:, oc:oc + 1], op0=ALU.mult, op1=ALU.add)
            P.I(DVE, "scalar_tensor_tensor", [ub, cw_sb, ac], [ac], out=ac[:, 0:n], in0=ub[:, 0:n], scalar=cw_sb[:, oc, 0:1], in1=ac[:, 0:n], op0=ALU.mult, op1=ALU.add)
            P.I(DVE, "scalar_tensor_tensor", [ub, cw_sb, ac], [ac], out=ac[:, 0:n], in0=ub[:, 2:n + 2], scalar=cw_sb[:, oc, 2:3], in1=ac[:, 0:n], op0=ALU.mult, op1=ALU.add)
            outs.append(P.dma(POOL, out_dram[oc * 128:(oc + 1) * 128, ocol0:ocol0 + n], ac[:, 0:n], reads=[ac]))

    outs = []
    for t in range(NTT):
        proj_tile(xT, t * TT, TT, 0, t * TT, False)
    proj_tile(ctxT, 0, NCTX, 1, TEXT, True)
    lo = 0
    while lo < TLOC:
        n = min(510, TLOC - lo)
        hyena_tile(HALO + lo, n, uT, lo, False, False, lo == 0, lo + n == TLOC)
        lo += n
    hyena_tile(TEXT, NCTX, ucT, 0, True, True, False, False)

    NB = TLOC // 128
    ctx_tiles = [TEXT // 128, TEXT // 128 + 1]
    pT = [P.sb([128, 512], BF16, name=f"pT{i}") for i in range(2)]
    o_sb = P.sb([64, 512], F32, name="o_sb"); rden = P.sb([65, 512], F32, name="rden")
    of_sb = P.sb([64, 512], F32, name="of_sb")
    ones_b = P.sb([128, 64], F32, name="ones_b")
    P.op(POOL, lambda e: e.memset(ones_b[:], 1.0), writes=[ones_b])

    def attend(qtile, key_tiles, key_masks, out_dram, out_col0):
        for g in range(2):
            po = Bk.f[2]
            nkt = len(key_tiles)
            for ci, (kt, mk) in enumerate(zip(key_tiles, key_masks)):
                psc = Bk.f[ci % 2]
                pTb = pT[ci % 2]
                qv = kqT[:, 2 + 4 * g:2 + 4 * g + 4, qtile * 128:(qtile + 1) * 128]
                P.op(PE, lambda e, psc=psc, kt=kt, qv=qv, mk=mk, g=g: e.matmul(psc[:, 0:512], kqT[:, g, kt * 128:(kt + 1) * 128], qv,
                                                                         start=True, stop=(mk is None)), reads=[kqT], writes=[psc])
                if mk is not None:
                    for hh in range(4):
                        P.op(PE, lambda e, psc=psc, hh=hh, mk=mk: e.matmul(psc[:, hh * 128:(hh + 1) * 128], id_b[:], mask_sb[:, mk, :],
                                                                           start=False, stop=(hh == 3)), reads=[id_b, mask_sb], writes=[psc])
                P.op(ACT, lambda e, psc=psc, pTb=pTb: e.activation(out=pTb[:], in_=psc[:, 0:512], func=AF.Exp, scale=0.125),
                     reads=[psc], writes=[pTb])
                P.op(PE, lambda e, pTb=pTb, kt=kt, ci=ci, g=g: e.matmul(po[0:65, 0:512], vaug[:, kt, g, :], pTb[:], start=(ci == 0), stop=(ci == nkt - 1)),
                     reads=[vaug, pTb], writes=[po])
            P.op(DVE, lambda e, g=g: e.tensor_tensor(out=rden[64:65, :], in0=po[64:65, 0:512], in1=sink_sb[64:65, g * 512:(g + 1) * 512], op=ALU.add),
                 reads=[po, sink_sb], writes=[rden])
            P.op(DVE, lambda e: e.reciprocal(out=rden[64:65, :], in_=rden[64:65, :]), reads=[rden], writes=[rden])
            P.op(ACT, lambda e: e.activation(out=o_sb[:], in_=po[0:64, 0:512], func=AF.Copy), reads=[po], writes=[o_sb])
            pb = Bk.f[3]
            P.op(PE, lambda e: e.matmul(pb[0:64, 0:512], ones_b[64:65, :], rden[64:65, :], start=True, stop=True), reads=[ones_b, rden], writes=[pb])
            P.op(DVE, lambda e: e.tensor_tensor(out=of_sb[:], in0=o_sb[:], in1=pb[0:64, 0:512], op=ALU.mult), reads=[o_sb, pb], writes=[of_sb])
            for hh in range(4):
                r0 = (4 * g + hh) * 64
                outs.append(P.dma(POOL if hh % 2 == 0 else SP, out_dram[r0:r0 + 64, out_col0:out_col0 + 128], of_sb[:, hh * 128:(hh + 1) * 128], reads=[of_sb]))

    for b in range(NB):
        qt = b + 1
        mprev = 0 if b == 0 else 1
        mnext = 3 if b == NB - 1 else 2
        attend(qt, [qt - 1, qt, qt + 1] + ctx_tiles, [mprev, None, mnext, None, None], attT, b * 128)
    for cb in range(2):
        attend(ctx_tiles[cb], ctx_tiles, [None, None], attcT, cb * 128)
    return P.finish(outs)


def col_layout(v, nchunk):
    return np.ascontiguousarray(np.asarray(v, np.float32).reshape(nchunk, 128).T)


def rope_tables(tok_idx):
    inv = (10000.0 ** (-np.arange(16, dtype=np.float32) / 16)).astype(np.float32)
    t = np.asarray(tok_idx)
    row = (t // 64).astype(np.float32)[:, None] * inv
    col = (t % 64).astype(np.float32)[:, None] * inv
    cr, sr, cc, sc_ = np.cos(row), np.sin(row), np.cos(col), np.sin(col)
    C = np.concatenate([cr, cr, cc, cc], axis=1).astype(np.float32)
    S = np.concatenate([-sr, sr, -sc_, sc_], axis=1).astype(np.float32)
    return C, S


def band_masks(core):
    j = np.arange(128)[:, None]; i = np.arange(128)[None, :]
    prev = np.where(j >= i, 0.0, MASKNEG).astype(np.float32)
    nxt = np.where(j <= i, 0.0, MASKNEG).astype(np.float32)
    allneg = np.full((128, 128), MASKNEG, np.float32)
    return np.stack([allneg if core == 0 else prev, prev, nxt, allneg if core == NCORE - 1 else nxt])


def prep_A(inp, layer=0):
    x = inp['x'][0]; ctx = inp['ctx'][0]
    xT = np.ascontiguousarray(x.T)
    xTp = np.concatenate([np.zeros((D, HALO), np.float32), xT, np.zeros((D, HALO), np.float32)], axis=1)
    w = inp['w_in_even'][0]
    w_perm = np.ascontiguousarray(np.concatenate([w[:, 0:128], w[:, 256:768], w[:, 128:256], w[:, 768:]], axis=1))
    gains = np.concatenate([np.tile(inp['att_k_norm'][0], 2), np.tile(inp['att_q_norm'][0], 8)])
    gains = np.ascontiguousarray(np.broadcast_to(gains[None, :], (128, 640))).astype(np.float32)
    sink = inp['att_sink'][0]
    sinkrow = np.ascontiguousarray(np.broadcast_to(np.repeat(sink, 128)[None, :], (128, 1024))).astype(np.float32)
    cvec = np.stack([col_layout(inp['c'][0], 8), col_layout(inp['c_ctx'], 8)], axis=2).reshape(128, 16)
    common = dict(
        ctxT=np.ascontiguousarray(ctx.T), cvec=np.ascontiguousarray(cvec),
        adaw=np.ascontiguousarray(inp['ada_w'][layer][:, 0:3072]), adab=col_layout(inp['ada_b'][layer][0:3072], 24),
        normg=col_layout(inp['norm_g'][layer, 0], 8), w_in=w_perm, gains=gains, sinkrow=sinkrow,
        ident=np.eye(128, dtype=np.float32), onesd=np.ones((128, 128), np.float32),
        convw=np.ascontiguousarray(inp['hy_conv_w'][0].reshape(3, 12, 128).transpose(2, 1, 0).reshape(128, 36)),
        convb=col_layout(inp['hy_conv_b'][0], 12))
    maps = []
    for c in range(NCORE):
        s0 = c * TLOC
        C, S = rope_tables(np.arange(s0 - HALO, s0 + TLOC + HALO).clip(0, SEQ - 1))
        m = dict(common)
        edge = np.ones((128, 2), np.float32); edge[:, 0] = 0.0 if c == 0 else 1.0; edge[:, 1] = 0.0 if c == NCORE - 1 else 1.0
        m.update(xT=np.ascontiguousarray(xTp[:, s0:s0 + TEXT]), ctab=C, stab=S, edge=edge,
                 masks=np.ascontiguousarray(band_masks(c).transpose(1, 0, 2).reshape(128, 512)))
        maps.append(m)
    return blobify(maps, BLOB_A)


NEXP = 32
NC_MOE = 8
NH_MOE = 16


def moe_passes(ncore, has_ctx):
    nlat_pass = (SEQ // ncore) // 1024
    passes = [[(0, 512, 0), (512, 512, 0)] for _ in range(nlat_pass)]
    if has_ctx:
        passes.append([(0, NCTX // ncore, 1)])
    return passes


def build_C(passes):
    TH = 1024
    nhalf = len(passes)
    P = Prog()
    Bk = Banks(P)
    di = lambda n, s, dt=F32: P.dram(n, s, dt, "ExternalInput")
    mT = di("mT", [nhalf, D, TH]); xT = di("xT", [nhalf, D, TH])
    bl = Blob(BLOB_C).dram(P)
    cvec = bl.ap("cvec"); adaw = di("adaw", [D, 4096]); adab = bl.ap("adab")
    normg = bl.ap("normg"); w_out = di("w_out", [D, D])
    rw = bl.ap("rw"); rb = bl.ap("rb")
    w_gu = di("w_gu", [NEXP, D, 2048]); bgu = bl.ap("bgu")
    w_dn = di("w_dn", [NEXP, D, D]); bdn = di("bdn", [NEXP, D])
    ident = bl.ap("ident"); onesd = bl.ap("onesd")
    outT = P.dram("outT", [nhalf, D, TH], F32, "ExternalOutput")
    gt_dram = P.dram("gt_scratch", [nhalf, NEXP, TH], F32, "Internal")

    ones_sb = P.sb([128, 128], F32, name="ones"); P.dma(SP, ones_sb[:], onesd[:, :], writes=[ones_sb])
    id_f = P.sb([128, 128], F32, name="idf"); P.dma(SP, id_f[:], ident[:, :], writes=[id_f])
    cv = P.sb([128, 8, 2], F32, name="cv"); P.dma(SP, cv[:], cvec.rearrange("p (k n) -> p k n", n=2), writes=[cv])
    adab_sb = P.sb([128, 32], F32, name="adab"); P.dma(SP, adab_sb[:], adab[:, :], writes=[adab_sb])
    g_sb = P.sb([128, 8], F32, name="normg"); P.dma(SP, g_sb[:], normg[:, :], writes=[g_sb])
    rw_sb = P.sb([128, 8, NEXP], F32, name="rw"); P.dma(SP, rw_sb[:], rw.rearrange("p (k e) -> p k e", e=NEXP), writes=[rw_sb])
    rb_sb = P.sb([128, NEXP], F32, name="rb"); P.dma(SP, rb_sb[:], rb[:, :], writes=[rb_sb])
    bgu_sb = P.sb([128, NEXP, 16], F32, name="bgu"); P.dma(SP, bgu_sb[:], bgu.rearrange("p (e o) -> p e o", o=16), writes=[bgu_sb])
    bdn_sb = P.sb([NEXP, D], F32, name="bdn"); P.dma(SP, bdn_sb[:], bdn[:, :], writes=[bdn_sb])
    ada = P.sb([128, 32, 2], F32, name="ada")
    with P.scope():
        wbuf = P.sb([128, 8, 1024], F32, name="adawbuf")
        emit_adaln(P, Bk, cv, adaw, adab_sb, wbuf, ada, 2, nparts=4)
    Acol = [P.sb([128, 8], F32, name=f"Acol{j}") for j in range(2)]
    Bcol = [P.sb([128, 8], F32, name=f"Bcol{j}") for j in range(2)]
    G0 = [P.sb([128, 8], F32, name=f"G0{j}") for j in range(2)]
    G1 = [P.sb([128, 8], F32, name=f"G1{j}") for j in range(2)]
    for j in range(2):
        P.I(DVE, "scalar_tensor_tensor", [ada, g_sb], [Acol[j]], out=Acol[j][:], in0=ada[:, 16:24, j], scalar=1.0, in1=g_sb[:], op0=ALU.add, op1=ALU.mult)
        P.I(DVE, "tensor_copy", [ada], [Bcol[j]], out=Bcol[j][:], in_=ada[:, 8:16, j])
        P.I(DVE, "tensor_copy", [ada], [G0[j]], out=G0[j][:], in_=ada[:, 0:8, j])
        P.I(DVE, "tensor_copy", [ada], [G1[j]], out=G1[j][:], in_=ada[:, 24:32, j])

    x1 = P.sb([128, 8, TH], F32, name="x1")
    hT = P.sb([128, 8, TH], BF16, name="hT")
    GT = P.sb([NEXP, TH], F32, name="GT")
    outs = []
    gu_cnt = [0]; dn_cnt = [0]; ch_cnt = [0]; st_cnt = [0]; pend = [None]

    for hf in range(nhalf):
        tiles = passes[hf]
        with P.scope():
            mb = P.sb([128, 8, TH], BF16, name=f"mb{hf}")
            wo = P.sb([128, 8, D], BF16, name=f"wo{hf}")
            for kc in range(8):
                P.dma(POOL, mb[:, kc, :], mT[hf, kc * 128:(kc + 1) * 128, :], writes=[mb])
                P.dma(POOL, wo[:, kc, :], w_out[kc * 128:(kc + 1) * 128, :], writes=[wo])
                P.dma(SP if kc % 2 == 0 else ACT, x1[:, kc, :], xT[hf, kc * 128:(kc + 1) * 128, :], writes=[x1])
            for (c0, n, j) in tiles:
                for oc in range(8):
                    ps = Bk.f[oc % 2]
                    for kc in range(8):
                        P.I(PE, "matmul", [wo, mb], [ps], ps[:, 0:n], wo[:, kc, oc * 128:(oc + 1) * 128], mb[:, kc, c0:c0 + n], start=(kc == 0), stop=(kc == 7))
                    P.I(DVE, "scalar_tensor_tensor", [ps, G0[j], x1], [x1], out=x1[:, oc, c0:c0 + n], in0=ps[:, 0:n], scalar=G0[j][:, oc:oc + 1],
                        in1=x1[:, oc, c0:c0 + n], op0=ALU.mult, op1=ALU.add)
        with P.scope():
            sq_sb = P.sb([128, 8, 512], F32, name=f"sq{hf}")
            rs_sb = P.sb([128, 512], F32, name=f"rs{hf}")
            hf32 = P.sb([128, 8, 512], F32, name=f"hf32{hf}")
            lg = P.sb([128, NEXP], F32, name=f"lg{hf}"); mx = P.sb([128, 8], F32, name=f"mx{hf}")
            msk = P.sb([128, NEXP], F32, name=f"msk{hf}"); ex = P.sb([128, NEXP], F32, name=f"ex{hf}")
            sm = P.sb([128, 1], F32, name=f"sm{hf}"); nm = P.sb([128, 1], F32, name=f"nm{hf}")
            for (c0, n, j) in tiles:
                ps = Bk.f[6]
                for kc in range(8):
                    P.I(ACT, "activation", [x1], [sq_sb], out=sq_sb[:, kc, 0:n], in_=x1[:, kc, c0:c0 + n], func=AF.Square)
                for kc in range(8):
                    P.I(PE, "matmul", [ones_sb, sq_sb], [ps], ps[:, 0:n], ones_sb[:], sq_sb[:, kc, 0:n], start=(kc == 0), stop=(kc == 7))
                P.I(DVE, "tensor_scalar", [ps], [rs_sb], out=rs_sb[:, 0:n], in0=ps[:, 0:n], scalar1=1.0 / D, scalar2=EPS, op0=ALU.mult, op1=ALU.add)
                P.I(ACT, "activation", [rs_sb], [rs_sb], out=rs_sb[:, 0:n], in_=rs_sb[:, 0:n], func=AF.Ln)
                P.I(ACT, "activation", [rs_sb], [rs_sb], out=rs_sb[:, 0:n], in_=rs_sb[:, 0:n], func=AF.Exp, scale=-0.5)
                for kc in range(8):
                    P.I(DVE, "tensor_tensor", [x1, rs_sb, sq_sb], [sq_sb], out=sq_sb[:, kc, 0:n], in0=x1[:, kc, c0:c0 + n], in1=rs_sb[:, 0:n], op=ALU.mult)
                    P.I(ACT, "activation", [sq_sb, Acol[j], Bcol[j]], [hf32], out=hf32[:, kc, 0:n], in_=sq_sb[:, kc, 0:n], func=AF.Identity,
                        bias=Bcol[j][:, kc:kc + 1], scale=Acol[j][:, kc:kc + 1])
                    P.I(POOL, "tensor_copy", [hf32], [hT], out=hT[:, kc, c0:c0 + n], in_=hf32[:, kc, 0:n])
                s0 = 0
                while s0 < n:
                    m = min(128, n - s0)
                    pl = Bk.f[5]
                    for kc in range(8):
                        P.I(PE, "matmul", [hf32, rw_sb], [pl], pl[0:m, 0:NEXP], hf32[:, kc, s0:s0 + m], rw_sb[:, kc, :], start=(kc == 0), stop=(kc == 7))
                    P.I(DVE, "tensor_tensor", [pl, rb_sb], [lg], out=lg[0:m, :], in0=pl[0:m, 0:NEXP], in1=rb_sb[0:m, :], op=ALU.add)
                    P.I(DVE, "max", [lg], [mx], out=mx[0:m, :], in_=lg[0:m, :])
                    P.I(DVE, "tensor_scalar", [lg, mx], [msk], out=msk[0:m, :], in0=lg[0:m, :], scalar1=mx[0:m, 3:4], scalar2=None, op0=ALU.is_ge)
                    P.I(DVE, "tensor_scalar", [mx], [nm], out=nm[0:m, :], in0=mx[0:m, 0:1], scalar1=-1.0, scalar2=None, op0=ALU.mult)
                    P.I(ACT, "activation", [lg, nm], [ex], out=ex[0:m, :], in_=lg[0:m, :], func=AF.Exp, bias=nm[0:m, 0:1], scale=1.0)
                    P.I(DVE, "tensor_tensor", [ex, msk], [ex], out=ex[0:m, :], in0=ex[0:m, :], in1=msk[0:m, :], op=ALU.mult)
                    P.I(DVE, "reduce_sum", [ex], [sm], out=sm[0:m, :], in_=ex[0:m, :], axis=AX.X)
                    P.I(DVE, "reciprocal", [sm], [sm], out=sm[0:m, :], in_=sm[0:m, :])
                    P.I(DVE, "tensor_scalar", [ex, sm], [ex], out=ex[0:m, :], in0=ex[0:m, :], scalar1=sm[0:m, 0:1], scalar2=None, op0=ALU.mult)
                    pt = Bk.f[4]
                    P.I(PE, "transpose", [ex, id_f], [pt], pt[0:NEXP, 0:m], ex[0:m, :], id_f[0:m, 0:m])
                    P.I(ACT, "activation", [pt], [GT], out=GT[:, c0 + s0:c0 + s0 + m], in_=pt[0:NEXP, 0:m], func=AF.Copy)
                    s0 += m
        P.dma(SP, gt_dram[hf], GT[:, :], reads=[GT], writes=[("gtd", hf)])
        p3 = P.scope(); p3.__enter__()
        yT = P.sb([128, 8, TH], F32, name=f"yT{hf}")
        actT = P.sb([128, 8, TH], BF16, name=f"actT{hf}")
        gu_ring = [P.sb([128, 8, 512], BF16, name=f"gu{hf}_{i}") for i in range(3)]
        dn_ring = [P.sb([128, 8, 256], BF16, name=f"dn{hf}_{i}") for i in range(3)]
        stage = [P.sb([128, 8, 256], F32, name=f"stg{hf}_{i}") for i in range(3)]
        gbs = [P.sb([128, TH], F32, name=f"gb{hf}_{i}") for i in range(2)]
        g1 = [P.sb([128, 512], F32, name=f"g1{hf}_{i}") for i in range(2)]
        tt = [P.sb([128, 512], F32, name=f"tt{hf}_{i}") for i in range(2)]
        u1 = [P.sb([128, 512], F32, name=f"u1{hf}_{i}") for i in range(2)]
        for e in range(NEXP):
            gb_sb = gbs[e % 2]
            P.dma(SP, gb_sb[:, :], gt_dram[hf, e:e + 1, :].partition_broadcast(128), reads=[("gtd", hf)], writes=[gb_sb])
            for q in range(4):
                wb = gu_ring[gu_cnt[0] % 3]; gu_cnt[0] += 1
                src = w_gu[e].rearrange("(k p) n -> p k n", p=128)
                for part in range(2):
                    sg_ = stage[st_cnt[0] % 3]; st_cnt[0] += 1
                    P.dma(SP, sg_[:], src[:, :, part * 1024 + q * 256:part * 1024 + (q + 1) * 256], writes=[sg_])
                    if st_cnt[0] % 2:
                        P.I(POOL, "tensor_copy", [sg_], [wb], out=wb[:, :, part * 256:(part + 1) * 256], in_=sg_[:])
                    else:
                        P.I(ACT, "activation", [sg_], [wb], out=wb[:, :, part * 256:(part + 1) * 256], in_=sg_[:], func=AF.Copy)
                for o2 in range(2):
                    oc = q * 2 + o2
                    for (c0, n, j) in tiles:
                        i = ch_cnt[0] % 2; ch_cnt[0] += 1
                        pgt, put = Bk.f[i], Bk.f[2 + i]
                        for kc in range(8):
                            P.I(PE, "matmul", [wb, hT], [pgt], pgt[:, 0:n], wb[:, kc, o2 * 128:(o2 + 1) * 128], hT[:, kc, c0:c0 + n], start=(kc == 0), stop=(kc == 7))
                        for kc in range(8):
                            P.I(PE, "matmul", [wb, hT], [put], put[:, 0:n], wb[:, kc, 256 + o2 * 128:256 + (o2 + 1) * 128], hT[:, kc, c0:c0 + n], start=(kc == 0), stop=(kc == 7))
                        P.I(DVE, "tensor_scalar", [pgt, bgu_sb], [g1[i]], out=g1[i][:, 0:n], in0=pgt[:, 0:n], scalar1=bgu_sb[:, e, oc:oc + 1], scalar2=7.0, op0=ALU.add, op1=ALU.min)
                        P.I(ACT, "activation", [g1[i]], [tt[i]], out=tt[i][:, 0:n], in_=g1[i][:, 0:n], func=AF.Silu, scale=1.702)
                        P.I(DVE, "tensor_scalar", [put, bgu_sb], [u1[i]], out=u1[i][:, 0:n], in0=put[:, 0:n], scalar1=bgu_sb[:, e, 8 + oc:8 + oc + 1], scalar2=7.0, op0=ALU.add, op1=ALU.min)
                        P.I(POOL, "tensor_scalar", [u1[i]], [u1[i]], out=u1[i][:, 0:n], in0=u1[i][:, 0:n], scalar1=-7.0, scalar2=1.0, op0=ALU.max, op1=ALU.add)
                        P.I(POOL, "tensor_tensor", [u1[i], gb_sb], [u1[i]], out=u1[i][:, 0:n], in0=u1[i][:, 0:n], in1=gb_sb[:, c0:c0 + n], op=ALU.mult)
                        if pend[0] is not None:
                            pend[0]()
                        pend[0] = (lambda i=i, oc=oc, c0=c0, n=n: P.I(DVE, "scalar_tensor_tensor", [tt[i], u1[i]], [actT], out=actT[:, oc, c0:c0 + n], in0=tt[i][:, 0:n],
                                                                      scalar=1.0 / 1.702, in1=u1[i][:, 0:n], op0=ALU.mult, op1=ALU.mult))
            if pend[0] is not None:
                pend[0](); pend[0] = None
            for q in range(4):
                wd = dn_ring[dn_cnt[0] % 3]; dn_cnt[0] += 1
                sg_ = stage[st_cnt[0] % 3]; st_cnt[0] += 1
                P.dma(SP, sg_[:], w_dn[e].rearrange("(k p) n -> p k n", p=128)[:, :, q * 256:(q + 1) * 256], writes=[sg_])
                if st_cnt[0] % 2:
                    P.I(POOL, "tensor_copy", [sg_], [wd], out=wd[:], in_=sg_[:])
                else:
                    P.I(ACT, "activation", [sg_], [wd], out=wd[:], in_=sg_[:], func=AF.Copy)
                for o2 in range(2):
                    dc = q * 2 + o2
                    for (c0, n, j) in tiles:
                        i = ch_cnt[0] % 2; ch_cnt[0] += 1
                        pd = Bk.f[4 + i]
                        for kc in range(8):
                            P.I(PE, "matmul", [wd, actT], [pd], pd[:, 0:n], wd[:, kc, o2 * 128:(o2 + 1) * 128], actT[:, kc, c0:c0 + n], start=(kc == 0), stop=(kc == 7))
                        if e == 0:
                            P.I(DVE, "tensor_copy", [pd], [yT], out=yT[:, dc, c0:c0 + n], in_=pd[:, 0:n])
                        else:
                            P.I(DVE, "tensor_tensor", [pd, yT], [yT], out=yT[:, dc, c0:c0 + n], in0=pd[:, 0:n], in1=yT[:, dc, c0:c0 + n], op=ALU.add)
        for (c0, n, j) in tiles:
            for dc in range(8):
                pd = Bk.f[4 + dc % 2]
                P.I(PE, "matmul", [bdn_sb, GT], [pd], pd[:, 0:n], bdn_sb[:, dc * 128:(dc + 1) * 128], GT[:, c0:c0 + n], start=True, stop=True)
                P.I(DVE, "tensor_tensor", [pd, yT], [yT], out=yT[:, dc, c0:c0 + n], in0=pd[:, 0:n], in1=yT[:, dc, c0:c0 + n], op=ALU.add)
                P.I(DVE, "scalar_tensor_tensor", [yT, G1[j], x1], [x1], out=x1[:, dc, c0:c0 + n], in0=yT[:, dc, c0:c0 + n], scalar=G1[j][:, dc:dc + 1],
                    in1=x1[:, dc, c0:c0 + n], op0=ALU.mult, op1=ALU.add)
        for kc in range(8):
            outs.append(P.dma(SP if kc % 2 == 0 else ACT, outT[hf, kc * 128:(kc + 1) * 128, :], x1[:, kc, :], reads=[x1]))
        p3.__exit__(None, None, None)
    return P.finish(outs)


def prep_C(inp, layer, mT_full, xT_full, mcT=None, xcT=None):
    passes = moe_passes(NC_MOE, mcT is not None)
    nlp = (SEQ // NC_MOE) // 1024
    ncx = NCTX // NC_MOE
    aw = inp['ada_w'][layer]; ab = inp['ada_b'][layer]
    adaw = np.ascontiguousarray(np.concatenate([aw[:, 2048:3072], aw[:, 3072:6144]], axis=1))
    adab = col_layout(np.concatenate([ab[2048:3072], ab[3072:6144]]), 32)
    cvec = np.stack([col_layout(inp['c'][0], 8), col_layout(inp['c_ctx'], 8)], axis=2).reshape(128, 16)
    bgu = inp['moe_b_gu'][layer]
    bgu_l = np.ascontiguousarray(bgu.reshape(NEXP, 16, 128).transpose(2, 0, 1).reshape(128, NEXP * 16))
    common = dict(
        cvec=np.ascontiguousarray(cvec), adaw=adaw, adab=adab, normg=col_layout(inp['norm_g'][layer, 1], 8),
        w_out=np.ascontiguousarray(inp['w_out'][layer]),
        rw=np.ascontiguousarray(inp['router_w'][layer].reshape(8, 128, NEXP).transpose(1, 0, 2).reshape(128, 8 * NEXP)),
        rb=np.ascontiguousarray(np.broadcast_to(inp['router_b'][layer][None, :], (128, NEXP))).astype(np.float32),
        w_gu=inp['moe_w_gu'][layer], bgu=bgu_l, w_dn=inp['moe_w_dn'][layer], bdn=np.ascontiguousarray(inp['moe_b_dn'][layer]),
        ident=np.eye(128, dtype=np.float32), onesd=np.ones((128, 128), np.float32))
    maps = []
    for c in range(NC_MOE):
        ms = np.zeros((len(passes), D, 1024), np.float32); xs = np.zeros((len(passes), D, 1024), np.float32)
        for hf in range(nlp):
            t0 = (c * nlp + hf) * 1024
            ms[hf] = mT_full[:, t0:t0 + 1024]; xs[hf] = xT_full[:, t0:t0 + 1024]
        if mcT is not None:
            ms[nlp, :, 0:ncx] = mcT[:, c * ncx:(c + 1) * ncx]; xs[nlp, :, 0:ncx] = xcT[:, c * ncx:(c + 1) * ncx]
        d = dict(common); d.update(mT=ms, xT=xs)
        maps.append(d)
    return blobify(maps, BLOB_C), passes


def gather_C(results, has_ctx):
    nlp = (SEQ // NC_MOE) // 1024
    ncx = NCTX // NC_MOE
    xT = np.zeros((D, SEQ), np.float32); xcT = np.zeros((D, NCTX), np.float32) if has_ctx else None
    for c in range(NC_MOE):
        o = results[c]['outT']
        for hf in range(nlp):
            t0 = (c * nlp + hf) * 1024
            xT[:, t0:t0 + 1024] = o[hf]
        if has_ctx:
            xcT[:, c * ncx:(c + 1) * ncx] = o[nlp][:, 0:ncx]
    return xT, xcT


PI = float(np.pi)


def build_B(L):
    NBK = L // 128
    NK = 2 * NBK - 1
    HROW = 2 * L
    P = Prog()
    Bk = Banks(P)
    di = lambda n, s, dt=F32: P.dram(n, s, dt, "ExternalInput")
    vB = di("vB", [128, 64, NBK]); x1B = di("x1B", [128, 64, NBK]); x2B = di("x2B", [128, 64, NBK])
    zt = di("zt", [2, 33, L]); tn = di("tn", [2, 1, L])
    bl = Blob(BLOB_B).dram(P)
    w1 = bl.ap("w1"); w2 = bl.ap("w2"); w3 = bl.ap("w3"); w4s = bl.ap("w4s")
    fqb = bl.ap("fqb")
    negd = bl.ap("negd"); fbias = bl.ap("fbias")
    ident = bl.ap("ident"); onesd = bl.ap("onesd"); jmat = bl.ap("jmat")
    hyB = P.dram("hyB", [128, 64, NBK], F32, "ExternalOutput")
    Hd = P.dram("Hd_scratch", [128, HROW], BF16, "Internal")

    ones_sb = P.sb([128, 128], F32, name="ones"); P.dma(SP, ones_sb[:], onesd[:, :], writes=[ones_sb])
    id_f = P.sb([128, 128], F32, name="idf"); P.dma(SP, id_f[:], ident[:, :], writes=[id_f])
    j_b = P.sb([128, 128], BF16, name="jb"); P.dma(POOL, j_b[:], jmat[:, :], writes=[j_b])
    w1_sb = P.sb([33, 64], F32, name="w1"); P.dma(SP, w1_sb[:], w1[:, :], writes=[w1_sb])
    w2_sb = P.sb([64, 64], F32, name="w2"); P.dma(SP, w2_sb[:], w2[:, :], writes=[w2_sb])
    w3_sb = P.sb([64, 64], F32, name="w3"); P.dma(SP, w3_sb[:], w3[:, :], writes=[w3_sb])
    w4_sb = P.sb([64, 2, 128], F32, name="w4"); P.dma(SP, w4_sb[:], w4s.rearrange("k (s m) -> k s m", s=2), writes=[w4_sb])
    fq_sb = P.sb([64, 4], F32, name="fq"); P.dma(SP, fq_sb[:], fqb[:, :], writes=[fq_sb])
    fb_sb = P.sb([64, 3], F32, name="fqbias")
    P.I(DVE, "tensor_tensor", [fq_sb], [fb_sb], out=fb_sb[:], in0=fq_sb[:, 1:4], in1=fq_sb[:, 0:1].broadcast_to([64, 3]), op=ALU.mult)
    negd_sb = P.sb([128, 1], F32, name="negd"); P.dma(SP, negd_sb[:], negd[:, :], writes=[negd_sb], allow_slow_non_contiguous=True)
    fbias_sb = P.sb([128, 128], F32, name="fbias"); P.dma(SP, fbias_sb[:], fbias[:, :], writes=[fbias_sb])

    CH = min(512, L)
    NCH = L // CH
    abss = P.sb([128, 2 * NCH], F32, name="abss")
    with P.scope():
        z_sb = [P.sb([33, CH], F32, name=f"z{i}") for i in range(2)]
        tn_sb = [P.sb([128, CH], F32, name=f"tn{i}") for i in range(2)]
        a_sb = P.sb([64, CH], F32, name="a_sb"); t_sb = P.sb([64, CH], F32, name="t_sb")
        hd_sb = P.sb([64, CH], F32, name="hd_sb")
        dec_sb = P.sb([128, CH], F32, name="dec_sb"); hf_sb = P.sb([128, CH], F32, name="hf_sb")
        hb_sb = [P.sb([128, CH], BF16, name=f"hb{i}") for i in range(2)]
        it = 0
        for side in range(2):
            for c in range(NCH):
                zb = z_sb[it % 2]; tb = tn_sb[it % 2]; hb = hb_sb[it % 2]; it += 1
                P.dma(SP, zb[:], zt[side, :, c * CH:(c + 1) * CH], writes=[zb])
                P.dma(ACT, tb[:], tn[side, :, c * CH:(c + 1) * CH].partition_broadcast(128), writes=[tb])
                src, Kd, wl = zb, 33, [w1_sb, w2_sb, w3_sb]
                for l in range(3):
                    ps = Bk.f[l % 2]
                    P.I(PE, "matmul", [wl[l], src], [ps], ps[0:64, 0:CH], wl[l][:], src[0:Kd, :], start=True, stop=True)
                    P.I(DVE, "tensor_scalar", [ps, fq_sb, fb_sb], [a_sb], out=a_sb[:], in0=ps[0:64, 0:CH], scalar1=fq_sb[:, 0:1], scalar2=fb_sb[:, l:l + 1], op0=ALU.mult, op1=ALU.add)
                    for rep in range(2):
                        P.I(POOL, "tensor_scalar", [a_sb], [t_sb], out=t_sb[:], in0=a_sb[:], scalar1=PI, scalar2=-2 * PI, op0=ALU.is_gt, op1=ALU.mult)
                        P.I(DVE, "tensor_tensor", [a_sb, t_sb], [hd_sb], out=hd_sb[:], in0=a_sb[:], in1=t_sb[:], op=ALU.add)
                        P.I(POOL, "tensor_scalar", [a_sb], [t_sb], out=t_sb[:], in0=a_sb[:], scalar1=-PI, scalar2=2 * PI, op0=ALU.is_lt, op1=ALU.mult)
                        P.I(DVE, "tensor_tensor", [hd_sb, t_sb], [a_sb], out=a_sb[:], in0=hd_sb[:], in1=t_sb[:], op=ALU.add)
                    P.I(ACT, "activation", [a_sb], [hd_sb], out=hd_sb[:], in_=a_sb[:], func=AF.Sin)
                    src, Kd = hd_sb, 64
                p4 = Bk.f[2]
                P.I(PE, "matmul", [w4_sb, hd_sb], [p4], p4[:, 0:CH], w4_sb[:, side, :], hd_sb[:], start=True, stop=True)
                P.I(ACT, "activation", [tb, negd_sb], [dec_sb], out=dec_sb[:], in_=tb[:], func=AF.Exp, scale=negd_sb[:, 0:1])
                P.I(DVE, "tensor_tensor", [p4, dec_sb], [hf_sb], out=hf_sb[:], in0=p4[:, 0:CH], in1=dec_sb[:], op=ALU.mult)
                if side == 1 and c == NCH - 1:
                    P.I(DVE, "memset", [], [hf_sb], hf_sb[:, CH - 1:CH], 0.0)
                P.I(DVE, "tensor_reduce", [hf_sb], [abss], out=abss[:, side * NCH + c:side * NCH + c + 1], in_=hf_sb[:], axis=AX.X, op=ALU.add, apply_absolute_value=True)
                P.I(ACT, "activation", [hf_sb], [hb], out=hb[:], in_=hf_sb[:], func=AF.Copy)
                if side == 1:
                    n = CH - 1 if c == NCH - 1 else CH
                    P.dma(POOL, Hd[:, c * CH:c * CH + n], hb[:, 0:n], reads=[hb], writes=["Hd"])
                else:
                    P.dma(POOL, Hd[:, L - 1 + c * CH:L - 1 + (c + 1) * CH], hb[:], reads=[hb], writes=["Hd"])
        zpad = P.sb([128, 1], BF16, name="zpad"); P.I(DVE, "memset", [], [zpad], zpad[:], 0.0)
        P.dma(POOL, Hd[:, 2 * L - 1:2 * L], zpad[:], reads=[zpad], writes=["Hd"], allow_slow_non_contiguous=True)
    rn = P.sb([128, 1], F32, name="rn"); rnb = P.sb([128, 128], F32, name="rnb"); dg = P.sb([128, 128], F32, name="dg")
    P.I(DVE, "reduce_sum", [abss], [rn], out=rn[:], in_=abss[:], axis=AX.X)
    P.I(DVE, "reciprocal", [rn], [rn], out=rn[:], in_=rn[:])
    P.I(DVE, "tensor_scalar", [id_f, rn], [dg], out=dg[:], in0=id_f[:], scalar1=rn[:, 0:1], scalar2=None, op0=ALU.mult)
    pr = Bk.f[0]
    P.I(PE, "matmul", [ones_sb, dg], [pr], pr[:, 0:128], ones_sb[:], dg[:], start=True, stop=True)
    P.I(ACT, "activation", [pr], [rnb], out=rnb[:], in_=pr[:, 0:128], func=AF.Copy)

    v_sb = P.sb([128, 64, NBK], F32, name="v_sb"); x1_sb = P.sb([128, 64, NBK], F32, name="x1_sb"); x2_sb = P.sb([128, 64, NBK], F32, name="x2_sb")
    P.dma(SP, v_sb[:], vB[:, :, :], writes=[v_sb]); P.dma(ACT, x1_sb[:], x1B[:, :, :], writes=[x1_sb]); P.dma(SP, x2_sb[:], x2B[:, :, :], writes=[x2_sb])
    zb16 = P.sb([128, 64 * NBK], BF16, name="zb16")
    zrev = P.sb([128, 64, NBK], BF16, name="zrev")
    z1_sb = P.sb([128, 64, NBK], F32, name="z1_sb")
    t0_sb = [P.sb([128, NBK], F32, name=f"t0_{i}") for i in range(2)]
    t1_sb = [P.sb([128, NBK], F32, name=f"t1_{i}") for i in range(2)]
    KP = min(51, NK)
    NPIECE = (NK + KP - 1) // KP
    hs_ring = [P.sb([128, KP * 128], BF16, name=f"hs{i}") for i in range(3)]
    outs = []
    hs_cnt = [0]

    def make_zrev(src_f32):
        flat = src_f32[:].rearrange("p c j -> p (c j)")
        P.I(ACT, "activation", [src_f32], [zb16], out=zb16[:], in_=flat, func=AF.Copy)
        tot = 64 * NBK
        c0 = 0
        zr_flat = zrev[:].rearrange("p c j -> p (c j)")
        i = 0
        while c0 < tot:
            n = min(512, tot - c0)
            pz = Bk.f[4 + i % 2]; i += 1
            P.I(PE, "matmul", [j_b, zb16], [pz], pz[:, 0:n], j_b[:], zb16[:, c0:c0 + n], start=True, stop=True)
            P.I(ACT if i % 2 else DVE, "activation" if i % 2 else "tensor_copy", [pz], [zrev],
                **(dict(out=zr_flat[:, c0:c0 + n], in_=pz[:, 0:n], func=AF.Copy) if i % 2 else dict(out=zr_flat[:, c0:c0 + n], in_=pz[:, 0:n])))
            c0 += n

    mid = (NK // 2) // KP
    piece_order = [mid] + [p for p in range(NPIECE) if p != mid]

    def conv(o, zin_f32, gate_sb, dst_sb):
        for ch in range(64):
            row = o * 64 + ch
            py = Bk.f[ch % 4][:, 0:NBK] if False else Bk.f[ch % 2]
            first = True
            for pi in piece_order:
                kk0 = pi * KP; kk1 = min(NK, kk0 + KP)
                hs = hs_ring[hs_cnt[0] % 3]; hs_cnt[0] += 1
                ncol = (kk1 - kk0) * 128
                src = bass.AP(Hd.tensor, row * HROW + kk0 * 128, [[1, 128], [1, ncol]])
                P.dma(SP if hs_cnt[0] % 2 else ACT, hs[:, 0:ncol], src, reads=["Hd"], writes=[hs])
                ks = list(range(kk0, kk1))
                if pi == mid:
                    ks.remove(NBK - 1); ks = [NBK - 1] + ks
                for kk in ks:
                    k = kk - (NBK - 1)
                    a_lo = max(0, k); a_hi = min(NBK - 1, NBK - 1 + k)
                    last = (pi == piece_order[-1] and kk == ks[-1])
                    P.I(PE, "matmul", [hs, zrev], [py], py[:, a_lo:a_hi + 1], hs[:, (kk - kk0) * 128:(kk - kk0 + 1) * 128], zrev[:, ch, a_lo - k:a_hi - k + 1],
                        start=first, stop=last)
                    first = False
            i = ch % 2
            P.I(POOL, "tensor_scalar", [zin_f32, fbias_sb], [t0_sb[i]], out=t0_sb[i][:], in0=zin_f32[:, ch, :], scalar1=fbias_sb[:, row:row + 1], scalar2=None, op0=ALU.mult)
            P.I(DVE, "scalar_tensor_tensor", [py, rnb, t0_sb[i]], [t1_sb[i]], out=t1_sb[i][:], in0=py[:, 0:NBK], scalar=rnb[:, row:row + 1], in1=t0_sb[i][:], op0=ALU.mult, op1=ALU.add)
            P.I(POOL, "tensor_tensor", [t1_sb[i], gate_sb], [dst_sb], out=dst_sb[:, ch, :], in0=t1_sb[i][:], in1=gate_sb[:, ch, :], op=ALU.mult)

    make_zrev(v_sb)
    conv(0, v_sb, x1_sb, z1_sb)
    make_zrev(z1_sb)
    conv(1, z1_sb, x2_sb, v_sb)
    outs.append(P.dma(SP, hyB[:, :, :], v_sb[:], reads=[v_sb]))
    return P.finish(outs)


def hyena_tables(L):
    t = np.linspace(0.0, 1.0, L, dtype=np.float32)[:, None]
    bands = 16
    w_ang = (2.0 * np.pi * np.arange(L, dtype=np.float32)[:, None] / L).astype(np.float32)
    fr = np.linspace(1e-4, bands - 1, bands, dtype=np.float32)[None]
    z = np.concatenate([t, np.cos(fr * w_ang), -np.sin(fr * w_ang)], axis=-1).astype(np.float32)
    zt = np.stack([z.T, z[::-1].T]).astype(np.float32)
    tn = np.stack([t.T, t[::-1].T]).astype(np.float32)
    return np.ascontiguousarray(zt), np.ascontiguousarray(tn)


def prep_B(inp, uT_full, L):
    NBK = L // 128
    zt, tn = hyena_tables(L)
    max_decay = np.log(1e-2) / 0.3; min_decay = np.log(1e-2) / 1.5
    deltas = np.abs(np.linspace(min_decay, max_decay, 512, dtype=np.float32)).astype(np.float32)
    w4 = inp['hy_w4'][0].reshape(64, 2, 2, 512)
    fqb = np.stack([inp['hy_freq'][0], inp['hy_b1'][0], inp['hy_b2'][0], inp['hy_b3'][0]], axis=1).astype(np.float32)
    jm = np.eye(128, dtype=np.float32)[::-1].copy()
    maps = []
    for c in range(NCORE):
        chs = slice(64 * c, 64 * c + 64)

        def blk(rows):
            return np.ascontiguousarray(rows.reshape(64, NBK, 128).transpose(2, 0, 1))
        w4s = np.concatenate([w4[:, :, s, chs].reshape(64, 128) for s in range(2)], axis=1)
        fb = inp['hy_filter_bias'][0][:, chs].reshape(128)
        maps.append(dict(
            vB=blk(uT_full[0:512][chs]), x1B=blk(uT_full[512:1024][chs]), x2B=blk(uT_full[1024:1536][chs]),
            zt=zt, tn=tn, w1=np.ascontiguousarray(inp['hy_w1'][0]), w2=np.ascontiguousarray(inp['hy_w2'][0]),
            w3=np.ascontiguousarray(inp['hy_w3'][0]), w4s=np.ascontiguousarray(w4s), fqb=np.ascontiguousarray(fqb),
            negd=np.ascontiguousarray(-np.tile(deltas[chs], 2)[:, None]).astype(np.float32),
            fbias=np.ascontiguousarray(np.broadcast_to(fb[None, :], (128, 128))).astype(np.float32),
            ident=np.eye(128, dtype=np.float32), onesd=np.ones((128, 128), np.float32), jmat=jm))
    return blobify(maps, BLOB_B)


def gather_B(results, L):
    NBK = L // 128
    hyT = np.zeros((512, L), np.float32)
    for c in range(NCORE):
        hb = results[c]['hyB']
        hyT[64 * c:64 * c + 64] = hb.transpose(1, 2, 0).reshape(64, L)
    return hyT


DROWS = 5136


def build_D():
    P = Prog()
    Bk = Banks(P)
    di = lambda n, s, dt=F32: P.dram(n, s, dt, "ExternalInput")
    xT = di("xT", [D, TEXT]); ctxT = di("ctxT", [D, NCTX])
    bl = Blob(BLOB_D).dram(P)
    cvec = bl.ap("cvec"); adaw = di("adaw", [D, 3072]); adab = bl.ap("adab")
    normg = bl.ap("normg"); w_in = di("w_in", [D, 4112])
    onesd = bl.ap("onesd")
    convw = bl.ap("convw"); convb = bl.ap("convb"); edge = bl.ap("edge")
    hglb = bl.ap("hglb")
    dtb = bl.ap("dtb")
    outT = P.dram("outT", [DROWS, TLOC], F32, "ExternalOutput"); outcT = P.dram("outcT", [DROWS, NCTX], F32, "ExternalOutput")

    ones_sb = P.sb([128, 128], F32, name="ones"); P.dma(SP, ones_sb[:], onesd[:, :], writes=[ones_sb])
    cv = P.sb([128, 8, 2], F32, name="cv"); P.dma(SP, cv[:], cvec.rearrange("p (k n) -> p k n", n=2), writes=[cv])
    adab_sb = P.sb([128, 24], F32, name="adab"); P.dma(SP, adab_sb[:], adab[:, :], writes=[adab_sb])
    g_sb = P.sb([128, 8], F32, name="normg"); P.dma(SP, g_sb[:], normg[:, :], writes=[g_sb])
    cw_sb = P.sb([128, 8, 3], F32, name="cw"); P.dma(SP, cw_sb[:], convw.rearrange("p (o t) -> p o t", t=3), writes=[cw_sb])
    cb_sb = P.sb([128, 8], F32, name="cb"); P.dma(SP, cb_sb[:], convb[:, :], writes=[cb_sb])
    edge_sb = P.sb([128, 2], F32, name="edge"); P.dma(SP, edge_sb[:], edge[:, :], writes=[edge_sb])
    lb_sb = P.sb([128, 8], F32, name="hglb"); P.dma(SP, lb_sb[:], hglb[:, :], writes=[lb_sb])
    dtb_sb = P.sb([16, 1], F32, name="dtb"); P.dma(SP, dtb_sb[:], dtb[:, :], writes=[dtb_sb], allow_slow_non_contiguous=True)
    lbc = P.sb([128, 4], F32, name="lbc"); oml = P.sb([128, 4], F32, name="oml")
    P.I(DVE, "tensor_tensor", [lb_sb], [lbc], out=lbc[:], in0=lb_sb[:, 4:8], in1=lb_sb[:, 0:4], op=ALU.subtract)
    P.I(ACT, "activation", [lbc], [lbc], out=lbc[:], in_=lbc[:], func=AF.Sigmoid)
    P.I(DVE, "tensor_scalar", [lbc], [oml], out=oml[:], in0=lbc[:], scalar1=-1.0, scalar2=1.0, op0=ALU.mult, op1=ALU.add)
    w_sb = P.sb([128, 8, 4112], BF16, name="w_in")
    for kc in range(8):
        P.dma(POOL, w_sb[:, kc, :], w_in[kc * 128:(kc + 1) * 128, :], writes=[w_sb])
    ada = P.sb([128, 24, 2], F32, name="ada")
    with P.scope():
        wbuf = P.sb([128, 8, 1024], F32, name="adawbuf")
        emit_adaln(P, Bk, cv, adaw, adab_sb, wbuf, ada, 2)
    Acol = [P.sb([128, 8], F32, name=f"Acol{j}") for j in range(2)]
    Bcol = [P.sb([128, 8], F32, name=f"Bcol{j}") for j in range(2)]
    for j in range(2):
        P.I(DVE, "scalar_tensor_tensor", [ada, g_sb], [Acol[j]], out=Acol[j][:], in0=ada[:, 8:16, j], scalar=1.0, in1=g_sb[:], op0=ALU.add, op1=ALU.mult)
        P.I(DVE, "tensor_copy", [ada], [Bcol[j]], out=Bcol[j][:], in_=ada[:, 0:8, j])
    h_all = P.sb([128, 8, TEXT + NCTX], BF16, name="h_all")
    x_sb = P.sb([128, 8, TT], F32, name="x_sb"); sq_sb = P.sb([128, 8, TT], F32, name="sq_sb"); rs_sb = P.sb([128, TT], F32, name="rs_sb")
    for t in range(NTT):
        for kc in range(8):
            P.dma(SP if kc % 2 == 0 else ACT, x_sb[:, kc, :], xT[kc * 128:(kc + 1) * 128, t * TT:(t + 1) * TT], writes=[x_sb])
        emit_norm_mod(P, Bk, x_sb, ones_sb, Acol[0], Bcol[0], h_all, TT, sq_sb, rs_sb, hcol0=t * TT)
    for kc in range(8):
        P.dma(SP if kc % 2 == 0 else ACT, x_sb[:, kc, 0:NCTX], ctxT[kc * 128:(kc + 1) * 128, :], writes=[x_sb])
    emit_norm_mod(P, Bk, x_sb, ones_sb, Acol[1], Bcol[1], h_all, NCTX, sq_sb, rs_sb, hcol0=TEXT)

    u_sb = [P.sb([128, 512], F32, name=f"u_sb{i}") for i in range(2)]
    a1_sb = [P.sb([128, 512], F32, name=f"a1_sb{i}") for i in range(2)]
    a2_sb = [P.sb([128, 512], F32, name=f"a2_sb{i}") for i in range(2)]
    outs = []

    def tile(h0, n, out_dram, ocol0, zl, zr, el, er):
        a0 = h0 - (0 if zl else 1); a1 = h0 + n + (0 if zr else 1)
        w = a1 - a0
        off = 1 if zl else 0
        for oc in range(33):
            M = 128 if oc < 32 else 16
            i = oc % 2
            pu = Bk.f[2 + i]; ub = u_sb[i]; r1 = a1_sb[i]; r2 = a2_sb[i]
            for kc in range(8):
                P.I(PE, "matmul", [w_sb, h_all], [pu], pu[0:M, off:off + w], w_sb[:, kc, oc * 128:oc * 128 + M], h_all[:, kc, a0:a1], start=(kc == 0), stop=(kc == 7))
            ctr = pu[0:M, 1:n + 1]
            dq = SP if oc % 2 == 0 else POOL
            if oc < 8:
                if zl:
                    P.I(POOL, "memset", [], [ub], ub[:, 0:1], 0.0)
                if zr:
                    P.I(POOL, "memset", [], [ub], ub[:, n + 1:n + 2], 0.0)
                P.I(ACT, "activation", [pu], [ub], out=ub[:, off:off + w], in_=pu[:, off:off + w], func=AF.Copy)
                if el:
                    P.I(DVE, "tensor_scalar", [ub, edge_sb], [ub], out=ub[:, 0:1], in0=ub[:, 0:1], scalar1=edge_sb[:, 0:1], scalar2=None, op0=ALU.mult)
                if er:
                    P.I(DVE, "tensor_scalar", [ub, edge_sb], [ub], out=ub[:, n + 1:n + 2], in0=ub[:, n + 1:n + 2], scalar1=edge_sb[:, 1:2], scalar2=None, op0=ALU.mult)
                P.I(DVE, "tensor_scalar", [ub, cw_sb, cb_sb], [r1], out=r1[:, 0:n], in0=ub[:, 1:n + 1], scalar1=cw_sb[:, oc, 1:2], scalar2=cb_sb[:, oc:oc + 1], op0=ALU.mult, op1=ALU.add)
                P.I(DVE, "scalar_tensor_tensor", [ub, cw_sb, r1], [r1], out=r1[:, 0:n], in0=ub[:, 0:n], scalar=cw_sb[:, oc, 0:1], in1=r1[:, 0:n], op0=ALU.mult, op1=ALU.add)
                P.I(DVE, "scalar_tensor_tensor", [ub, cw_sb, r1], [r1], out=r1[:, 0:n], in0=ub[:, 2:n + 2], scalar=cw_sb[:, oc, 2:3], in1=r1[:, 0:n], op0=ALU.mult, op1=ALU.add)
                P.I(ACT, "activation", [r1], [r2], out=r2[:, 0:n], in_=r1[:, 0:n], func=AF.Silu)
                outs.append(P.dma(dq, out_dram[oc * 128:(oc + 1) * 128, ocol0:ocol0 + n], r2[:, 0:n], reads=[r2]))
            elif oc < 16:
                hd = (oc - 8) % 4
                P.I(ACT, "activation", [pu], [r1], out=r1[:, 0:n], in_=ctr, func=AF.Sigmoid)
                P.I(DVE, "tensor_scalar", [r1, oml, lbc], [r1], out=r1[:, 0:n], in0=r1[:, 0:n], scalar1=oml[:, hd:hd + 1], scalar2=lbc[:, hd:hd + 1], op0=ALU.mult, op1=ALU.add)
                P.I(DVE, "tensor_scalar", [r1], [r2], out=r2[:, 0:n], in0=r1[:, 0:n], scalar1=-1.0, scalar2=1.0, op0=ALU.mult, op1=ALU.add)
                outs.append(P.dma(dq, out_dram[1024 + (oc - 8) * 128:1024 + (oc - 7) * 128, ocol0:ocol0 + n], r2[:, 0:n], reads=[r2]))
                P.I(ACT, "activation", [r1], [ub], out=ub[:, 0:n], in_=r1[:, 0:n], func=AF.Ln)
                outs.append(P.dma(dq, out_dram[2048 + (oc - 8) * 128:2048 + (oc - 7) * 128, ocol0:ocol0 + n], ub[:, 0:n], reads=[ub]))
            elif oc < 20:
                P.I(ACT, "activation", [pu], [r1], out=r1[:, 0:n], in_=ctr, func=AF.Copy)
                outs.append(P.dma(dq, out_dram[3072 + (oc - 16) * 128:3072 + (oc - 15) * 128, ocol0:ocol0 + n], r1[:, 0:n], reads=[r1]))
            elif oc < 32:
                P.I(ACT, "activation", [pu], [r1], out=r1[:, 0:n], in_=ctr, func=AF.Silu)
                outs.append(P.dma(dq, out_dram[3584 + (oc - 20) * 128:3584 + (oc - 19) * 128, ocol0:ocol0 + n], r1[:, 0:n], reads=[r1]))
            else:
                P.I(ACT, "activation", [pu, dtb_sb], [r1], out=r1[0:16, 0:n], in_=pu[0:16, 1:n + 1], func=AF.Exp, bias=dtb_sb[:, 0:1], scale=1.0)
                P.I(ACT, "activation", [r1], [r2], out=r2[0:16, 0:n], in_=r1[0:16, 0:n], func=AF.Ln, bias=1.0, scale=1.0)
                outs.append(P.dma(dq, out_dram[5120:5136, ocol0:ocol0 + n], r2[0:16, 0:n], reads=[r2]))

    lo = 0
    while lo < TLOC:
        n = min(510, TLOC - lo)
        tile(HALO + lo, n, outT, lo, False, False, lo == 0, lo + n == TLOC)
        lo += n
    tile(TEXT, NCTX, outcT, 0, True, True, False, False)
    return P.finish(outs)


def prep_D(inp, xT_full, xcT):
    layer = 1
    xTp = np.concatenate([np.zeros((D, HALO), np.float32), xT_full, np.zeros((D, HALO), np.float32)], axis=1)
    w = inp['w_in_odd'][0]
    w_perm = np.ascontiguousarray(np.concatenate([w[:, 0:1024], w[:, 1040:2064], w[:, 2064:2576], w[:, 2576:3088], w[:, 3088:3600],
                                                   w[:, 3600:4112], w[:, 1024:1040]], axis=1))
    cvec = np.stack([col_layout(inp['c'][0], 8), col_layout(inp['c_ctx'], 8)], axis=2).reshape(128, 16)
    hl = inp['hg_lower_bounds']
    common = dict(
        ctxT=np.ascontiguousarray(xcT), cvec=np.ascontiguousarray(cvec),
        adaw=np.ascontiguousarray(inp['ada_w'][layer][:, 0:3072]), adab=col_layout(inp['ada_b'][layer][0:3072], 24),
        normg=col_layout(inp['norm_g'][layer, 0], 8), w_in=w_perm, onesd=np.ones((128, 128), np.float32),
        convw=np.ascontiguousarray(inp['ssd_conv_w'][0].reshape(3, 8, 128).transpose(2, 1, 0).reshape(128, 24)),
        convb=col_layout(inp['ssd_conv_b'][0], 8),
        hglb=np.ascontiguousarray(np.concatenate([col_layout(hl[0], 4), col_layout(hl[1], 4)], axis=1)),
        dtb=np.ascontiguousarray(inp['ssd_dt_bias'][0].reshape(16, 1)))
    maps = []
    for c in range(NCORE):
        s0 = c * TLOC
        edge = np.ones((128, 2), np.float32); edge[:, 0] = 0.0 if c == 0 else 1.0; edge[:, 1] = 0.0 if c == NCORE - 1 else 1.0
        m = dict(common); m.update(xT=np.ascontiguousarray(xTp[:, s0:s0 + TEXT]), edge=edge)
        maps.append(m)
    return blobify(maps, BLOB_D)


LS = NCTX + SEQ
NBLK = LS // 128
GB = 10


def build_E():
    P = Prog()
    Bk = Banks(P)
    di = lambda n, s, dt=F32: P.dram(n, s, dt, "ExternalInput")
    xtok = di("xtok", [2, 128, NBLK, 64]); Btok = di("Btok", [2, 128, NBLK, 128])
    BT = di("BT", [2, 128, LS]); CT = di("CT", [2, 128, LS]); dttok = di("dttok", [2, 128, NBLK])
    bl = Blob(BLOB_E).dram(P)
    ssdp = bl.ap("ssdp")
    qT = di("qT", [2, 128, LS]); kT = di("kT", [2, 128, LS]); gtok = di("gtok", [2, 128, NBLK, 128])
    vZ = di("vZ", [2, 128, NBLK, 5, 64])
    Ud = bl.ap("U"); U4d = bl.ap("U4"); Mnegd = bl.ap("Mneg")
    ident = bl.ap("ident"); onesd = bl.ap("onesd")
    ytok = P.dram("ytok", [2, 128, NBLK, 64], F32, "ExternalOutput"); otok = P.dram("otok", [2, 128, NBLK, 64], F32, "ExternalOutput")

    ones_sb = P.sb([128, 128], F32, name="ones"); P.dma(SP, ones_sb[:], onesd[:, :], writes=[ones_sb])
    U_sb = P.sb([128, 128], F32, name="U"); P.dma(SP, U_sb[:], Ud[:, :], writes=[U_sb])
    U4_sb = P.sb([128, 128], F32, name="U4"); P.dma(SP, U4_sb[:], U4d[:, :], writes=[U4_sb])
    Mn_sb = P.sb([128, 128], F32, name="Mneg"); P.dma(SP, Mn_sb[:], Mnegd[:, :], writes=[Mn_sb])
    id_b = P.sb([128, 128], BF16, name="idb"); P.dma(POOL, id_b[:], ident[:, :], writes=[id_b])
    sp_sb = P.sb([128, 4], F32, name="ssdp"); P.dma(SP, sp_sb[:], ssdp[:, :], writes=[sp_sb])
    acol = P.sb([128, 2], F32, name="acol")
    P.I(ACT, "activation", [sp_sb], [acol], out=acol[:], in_=sp_sb[:, 0:2], func=AF.Exp)
    P.I(DVE, "tensor_scalar", [acol], [acol], out=acol[:], in0=acol[:], scalar1=-1.0, scalar2=None, op0=ALU.mult)
    outs = []

    with P.scope():
        dt_sb = P.sb([128, 2, NBLK], F32, name="dt_sb")
        for d in range(2):
            P.dma(SP, dt_sb[:, d, :], dttok[d], writes=[dt_sb])
        xg = [P.sb([128, GB, 64], F32, name=f"xg{i}") for i in range(2)]
        Bg = [P.sb([128, GB, 128], BF16, name=f"Bg{i}") for i in range(2)]
        BTg = [P.sb([128, GB * 128], BF16, name=f"BTg{i}") for i in range(2)]
        CTg = [P.sb([128, GB * 128], F32, name=f"CTg{i}") for i in range(2)]
        CTh = [P.sb([128, GB * 128], BF16, name=f"CTh{i}") for i in range(2)]
        yg = [P.sb([128, GB, 64], F32, name=f"yg{i}") for i in range(2)]
        da = P.sb([128, 1], F32, name="da"); dab = P.sb([128, 128], F32, name="dab")
        cs_sb = P.sb([128, 1], F32, name="cs_sb"); tot_sb = P.sb([128, 1], F32, name="tot_sb")
        te = P.sb([128, 1], F32, name="te"); dec = P.sb([128, 1], F32, name="dec"); wcol = P.sb([128, 1], F32, name="wcol")
        xdt = P.sb([128, 64], BF16, name="xdt"); xw = P.sb([128, 64], BF16, name="xw")
        em = P.sb([128, 128], F32, name="em"); gt = P.sb([128, 128], BF16, name="gt")
        ecs = P.sb([128, 128], F32, name="ecs"); cp = P.sb([128, 128], BF16, name="cp")
        S = P.sb([128, 64], F32, name="S_ssd"); Sbf = P.sb([128, 64], BF16, name="Sbf_ssd")
        gi = 0
        for d in range(2):
            P.I(DVE, "memset", [], [S], S[:], 0.0)
            P.I(POOL, "memset", [], [Sbf], Sbf[:], 0.0)
            for g0 in range(0, NBLK, GB):
                i = gi % 2; gi += 1
                P.dma(SP, xg[i][:], xtok[d, :, g0:g0 + GB, :], writes=[xg[i]])
                P.dma(POOL, Bg[i][:], Btok[d, :, g0:g0 + GB, :], writes=[Bg[i]])
                P.dma(POOL, BTg[i][:], BT[d, :, g0 * 128:(g0 + GB) * 128], writes=[BTg[i]])
                P.dma(ACT, CTg[i][:], CT[d, :, g0 * 128:(g0 + GB) * 128], writes=[CTg[i]])
                P.dma(POOL, CTh[i][:], CT[d, :, g0 * 128:(g0 + GB) * 128], writes=[CTh[i]])
                for bb in range(GB):
                    b = g0 + bb
                    cols = slice(bb * 128, (bb + 1) * 128)
                    P.I(DVE, "tensor_scalar", [dt_sb, acol], [da], out=da[:], in0=dt_sb[:, d, b:b + 1], scalar1=acol[:, d:d + 1], scalar2=None, op0=ALU.mult)
                    P.I(DVE, "tensor_scalar", [ones_sb, da], [dab], out=dab[:], in0=ones_sb[:], scalar1=da[:, 0:1], scalar2=None, op0=ALU.mult)
                    pc, pr = Bk.f[0], Bk.f[1]
                    P.I(PE, "matmul", [U_sb, da], [pc], pc[:, 0:1], U_sb[:], da[:], start=True, stop=True)
                    P.I(PE, "matmul", [dab, U_sb], [pr], pr[:, 0:128], dab[:], U_sb[:], start=True, stop=True)
                    P.I(ACT, "activation", [pc], [cs_sb], out=cs_sb[:], in_=pc[:, 0:1], func=AF.Copy)
                    P.I(ACT, "activation", [pr], [tot_sb], out=tot_sb[:], in_=pr[:, 127:128], func=AF.Copy)
                    P.I(ACT, "activation", [cs_sb, tot_sb], [te], out=te[:], in_=cs_sb[:], func=AF.Exp, scale=-1.0, bias=tot_sb[:, 0:1])
                    P.I(ACT, "activation", [tot_sb], [dec], out=dec[:], in_=tot_sb[:], func=AF.Exp)
                    P.I(DVE, "tensor_scalar", [xg[i], dt_sb], [xdt], out=xdt[:], in0=xg[i][:, bb, :], scalar1=dt_sb[:, d, b:b + 1], scalar2=None, op0=ALU.mult)
                    P.I(DVE, "tensor_tensor", [dt_sb, te], [wcol], out=wcol[:], in0=dt_sb[:, d, b:b + 1], in1=te[:], op=ALU.mult)
                    P.I(DVE, "tensor_scalar", [xg[i], wcol], [xw], out=xw[:], in0=xg[i][:, bb, :], scalar1=wcol[:, 0:1], scalar2=None, op0=ALU.mult)
                    psc = Bk.f[2]
                    P.I(PE, "matmul", [Bg[i], xw], [psc], psc[:, 0:64], Bg[i][:, bb, :], xw[:], start=True, stop=True)
                    pss = Bk.f[3]
                    P.I(PE, "matmul", [BTg[i], CTh[i]], [pss], pss[:, 0:128], BTg[i][:, cols], CTh[i][:, cols], start=True, stop=True)
                    P.I(DVE, "scalar_tensor_tensor", [pr, cs_sb, Mn_sb], [em], out=em[:], in0=pr[:, 0:128], scalar=cs_sb[:, 0:1], in1=Mn_sb[:], op0=ALU.subtract, op1=ALU.add)
                    P.I(ACT, "activation", [em], [em], out=em[:], in_=em[:], func=AF.Exp)
                    P.I(DVE, "tensor_tensor", [pss, em], [gt], out=gt[:], in0=pss[:, 0:128], in1=em[:], op=ALU.mult)
                    P.I(ACT, "activation", [pr], [ecs], out=ecs[:], in_=pr[:, 0:128], func=AF.Exp)
                    P.I(DVE, "tensor_tensor", [CTg[i], ecs], [cp], out=cp[:], in0=CTg[i][:, cols], in1=ecs[:], op=ALU.mult)
                    py = Bk.f[4]
                    P.I(PE, "matmul", [gt, xdt], [py], py[:, 0:64], gt[:], xdt[:], start=True, stop=False)
                    P.I(PE, "matmul", [cp, Sbf], [py], py[:, 0:64], cp[:], Sbf[:], start=False, stop=True)
                    P.I(DVE, "scalar_tensor_tensor", [xg[i], sp_sb, py], [yg[i]], out=yg[i][:, bb, :], in0=xg[i][:, bb, :], scalar=sp_sb[:, 2 + d:3 + d], in1=py[:, 0:64], op0=ALU.mult, op1=ALU.add)
                    P.I(DVE, "scalar_tensor_tensor", [S, dec, psc], [S], out=S[:], in0=S[:], scalar=dec[:, 0:1], in1=psc[:, 0:64], op0=ALU.mult, op1=ALU.add)
                    P.I(ACT, "activation", [S], [Sbf], out=Sbf[:], in_=S[:], func=AF.Copy)
                outs.append(P.dma(SP, ytok[d, :, g0:g0 + GB, :], yg[i][:], reads=[yg[i]]))

    with P.scope():
        qg = [P.sb([128, GB * 128], F32, name=f"qg{i}") for i in range(2)]
        kg = [P.sb([128, GB * 128], F32, name=f"kg{i}") for i in range(2)]
        gg = [P.sb([128, GB, 128], F32, name=f"gg{i}") for i in range(2)]
        vg = [P.sb([128, GB, 5, 64], BF16, name=f"vg{i}") for i in range(2)]
        og = [P.sb([128, GB, 64], F32, name=f"og{i}") for i in range(2)]
        cum = P.sb([128, 128], F32, name="cum"); d1 = P.sb([128, 128], F32, name="d1"); d2 = P.sb([128, 128], F32, name="d2")
        e1 = P.sb([128, 128], F32, name="e1"); e2 = P.sb([128, 128], F32, name="e2"); e3 = P.sb([128, 128], F32, name="e3"); e4 = P.sb([128, 128], F32, name="e4")
        dec4 = P.sb([128, 4], F32, name="dec4")
        QiZ = P.sb([128, 4, 128], BF16, name="QiZ"); P.I(POOL, "memset", [], [QiZ], QiZ[:], 0.0)
        Qp = P.sb([128, 128], BF16, name="Qp"); Kp = P.sb([128, 128], BF16, name="Kp"); Kpp = P.sb([128, 128], BF16, name="Kpp")
        am = P.sb([128, 128], F32, name="am"); amb = P.sb([128, 128], BF16, name="amb"); Ktok = P.sb([128, 128], BF16, name="Ktok")
        S = P.sb([128, 64], F32, name="S_hg"); Sst = [P.sb([128, 4, 64], BF16, name=f"Sst{i}") for i in range(2)]
        c3 = lambda t: t[:].rearrange("p (c t) -> p c t", t=32)
        QiZd = bass.AP(QiZ[:].tensor, 0, [[QiZ[:].ap[0][0], 128], [128 + 32, 4], [1, 32]])
        gi = 0; blk = 0
        for d in range(2):
            P.I(DVE, "memset", [], [S], S[:], 0.0)
            P.I(POOL, "memset", [], [Sst[blk % 2]], Sst[blk % 2][:], 0.0)
            for g0 in range(0, NBLK, GB):
                i = gi % 2; gi += 1
                P.dma(SP, qg[i][:], qT[d, :, g0 * 128:(g0 + GB) * 128], writes=[qg[i]])
                P.dma(ACT, kg[i][:], kT[d, :, g0 * 128:(g0 + GB) * 128], writes=[kg[i]])
                P.dma(SP, gg[i][:], gtok[d, :, g0:g0 + GB, :], writes=[gg[i]])
                P.dma(POOL, vg[i][:], vZ[d, :, g0:g0 + GB, :, :], writes=[vg[i]])
                for bb in range(GB):
                    cols = slice(bb * 128, (bb + 1) * 128)
                    cur, nxt = Sst[blk % 2], Sst[(blk + 1) % 2]; blk += 1
                    pcum = Bk.f[0]
                    P.I(PE, "matmul", [gg[i], U4_sb], [pcum], pcum[:, 0:128], gg[i][:, bb, :], U4_sb[:], start=True, stop=True)
                    P.I(ACT, "activation", [pcum], [cum], out=cum[:], in_=pcum[:, 0:128], func=AF.Copy)
                    rmid = c3(cum)[:, :, 15:16].broadcast_to([128, 4, 32]); cend = c3(cum)[:, :, 31:32].broadcast_to([128, 4, 32])
                    P.I(DVE, "tensor_tensor", [cum], [d1], out=c3(d1), in0=c3(cum), in1=rmid, op=ALU.subtract)
                    P.I(DVE, "tensor_tensor", [cum], [d2], out=c3(d2), in0=c3(cum), in1=cend, op=ALU.subtract)
                    P.I(ACT, "activation", [cum], [e1], out=e1[:], in_=cum[:], func=AF.Exp)
                    P.I(ACT, "activation", [d1], [e2], out=e2[:], in_=d1[:], func=AF.Exp)
                    P.I(ACT, "activation", [d1], [e3], out=e3[:], in_=d1[:], func=AF.Exp, scale=-1.0)
                    P.I(ACT, "activation", [d2], [e4], out=e4[:], in_=d2[:], func=AF.Exp, scale=-1.0)
                    P.I(ACT, "activation", [cum], [dec4], out=dec4[:], in_=c3(cum)[:, :, 31], func=AF.Exp)
                    P.I(DVE, "tensor_tensor", [qg[i], e1], [QiZ], out=QiZd, in0=qg[i][:, cols].rearrange("p (c t) -> p c t", t=32), in1=c3(e1), op=ALU.mult)
                    P.I(DVE, "tensor_tensor", [qg[i], e2], [Qp], out=Qp[:], in0=qg[i][:, cols], in1=e2[:], op=ALU.mult)
                    P.I(DVE, "tensor_tensor", [kg[i], e3], [Kp], out=Kp[:], in0=kg[i][:, cols], in1=e3[:], op=ALU.mult)
                    P.I(DVE, "tensor_tensor", [kg[i], e4], [Kpp], out=Kpp[:], in0=kg[i][:, cols], in1=e4[:], op=ALU.mult)
                    pa = Bk.f[1]
                    P.I(PE, "matmul", [Kp, Qp], [pa], pa[:, 0:128], Kp[:], Qp[:], start=True, stop=True)
                    P.I(DVE, "tensor_scalar", [pa], [am], out=am[:], in0=pa[:, 0:128], scalar1=1e30, scalar2=-1e30, op0=ALU.min, op1=ALU.max)
                    P.I(DVE, "tensor_tensor", [am, U4_sb], [amb], out=amb[:], in0=am[:], in1=U4_sb[:], op=ALU.mult)
                    pt = Bk.h[0]
                    P.I(PE, "transpose", [Kpp, id_b], [pt], pt[:, 0:128], Kpp[:], id_b[:])
                    P.I(ACT, "activation", [pt], [Ktok], out=Ktok[:], in_=pt[:, 0:128], func=AF.Copy)
                    po = Bk.f[2]
                    P.I(PE, "matmul", [amb, vg[i]], [po], po[:, 0:64], amb[:], vg[i][:, bb, 4, :], start=True, stop=False)
                    for ci in range(4):
                        P.I(PE, "matmul", [QiZ, cur], [po], po[:, 0:64], QiZ[:, ci, :], cur[:, ci, :], start=False, stop=(ci == 3))
                        if True:
                            psc = Bk.f[3 + ci % 2]
                            P.I(PE, "matmul", [Ktok, vg[i]], [psc], psc[:, 0:64], Ktok[:], vg[i][:, bb, ci, :], start=True, stop=True)
                            P.I(DVE, "scalar_tensor_tensor", [S, dec4, psc], [S], out=S[:], in0=S[:], scalar=dec4[:, ci:ci + 1], in1=psc[:, 0:64], op0=ALU.mult, op1=ALU.add)
                            if ci < 3:
                                P.I(ACT, "activation", [S], [cur], out=cur[:, ci + 1, :], in_=S[:], func=AF.Copy)
                            else:
                                P.I(ACT, "activation", [S], [nxt], out=nxt[:, 0, :], in_=S[:], func=AF.Copy)
                    P.I(ACT, "activation", [po], [og[i]], out=og[i][:, bb, :], in_=po[:, 0:64], func=AF.Copy)
                outs.append(P.dma(SP, otok[d, :, g0:g0 + GB, :], og[i][:], reads=[og[i]]))
    return P.finish(outs)


def gather_D(results):
    full = np.concatenate([results[c]['outT'] for c in range(NCORE)], axis=1)
    return full, results[0]['outcT']


def prep_E(inp, dfull, dctx):
    def seq(rows_lat, rows_ctx, d):
        if d == 0:
            return np.concatenate([rows_ctx, rows_lat], axis=1)
        return np.concatenate([rows_ctx[:, ::-1], rows_lat[:, ::-1]], axis=1)

    def blk(a):
        return np.ascontiguousarray(a.T.reshape(NBLK, 128, a.shape[0]).transpose(1, 0, 2))
    s_ = np.arange(128)[:, None]; l_ = np.arange(128)[None, :]
    U = (s_ <= l_).astype(np.float32)
    U4 = ((s_ // 32 == l_ // 32) & (s_ <= l_)).astype(np.float32)
    Mneg = np.where(s_ <= l_, 0.0, MASKNEG).astype(np.float32)
    maps = []
    for c in range(NCORE):
        h = c; g = c // 4; hh = c // 2; vh = c % 2
        R = lambda r0, n, d: seq(dfull[r0:r0 + n], dctx[r0:r0 + n], d)
        m = dict(U=U, U4=U4, Mneg=Mneg, ident=np.eye(128, dtype=np.float32), onesd=np.ones((128, 128), np.float32))
        m['xtok'] = np.stack([blk(R(64 * h, 64, d)) for d in range(2)])
        m['Btok'] = np.stack([blk(R(512 + 128 * g, 128, d)) for d in range(2)])
        m['BT'] = np.stack([np.ascontiguousarray(R(512 + 128 * g, 128, d)) for d in range(2)])
        m['CT'] = np.stack([np.ascontiguousarray(R(768 + 128 * g, 128, d)) for d in range(2)])
        m['dttok'] = np.stack([np.ascontiguousarray(R(5120 + 8 * d + h, 1, d)[0].reshape(NBLK, 128).T) for d in range(2)])
        al = inp['ssd_A_log'][0]; dd = inp['ssd_D'][0]
        m['ssdp'] = np.ascontiguousarray(np.broadcast_to(np.array([al[0, h], al[1, h], dd[0, h], dd[1, h]], np.float32)[None, :], (128, 4)))
        m['qT'] = np.stack([np.ascontiguousarray(R(4096 + 128 * hh, 128, d)) for d in range(2)])
        m['kT'] = np.stack([np.ascontiguousarray(R(1024 + 512 * d + 128 * hh, 128, d)) for d in range(2)])
        m['gtok'] = np.stack([blk(R(2048 + 512 * d + 128 * hh, 128, d)) for d in range(2)])
        vz = []
        for d in range(2):
            v = blk(R(3072 + 128 * hh + 64 * vh, 64, d))
            z5 = np.zeros((128, NBLK, 5, 64), np.float32)
            z5[:, :, 4, :] = v
            for i in range(4):
                z5[32 * i:32 * i + 32, :, i, :] = v[32 * i:32 * i + 32]
            vz.append(z5)
        m['vZ'] = np.stack(vz)
        maps.append(m)
    return blobify(maps, BLOB_E)


def gather_E(results):
    yT = np.zeros((2, 512, SEQ), np.float32); oT = np.zeros((2, 512, SEQ), np.float32)
    for c in range(NCORE):
        h = c; hh = c // 2; vh = c % 2
        for d in range(2):
            for name, dst, r0 in (("ytok", yT, 64 * h), ("otok", oT, 128 * hh + 64 * vh)):
                a = results[c][name][d].transpose(1, 0, 2).reshape(LS, 64)[NCTX:]
                if d == 1:
                    a = a[::-1]
                dst[d, r0:r0 + 64] = a.T
    return yT, oT


def build_M():
    P = Prog()
    Bk = Banks(P)
    di = lambda n, s, dt=F32: P.dram(n, s, dt, "ExternalInput")
    yT = di("yT", [2, 512, TLOC]); oT = di("oT", [2, 512, TLOC]); szT = di("szT", [512, TLOC]); sgT = di("sgT", [512, TLOC])
    bl = Blob(BLOB_M).dram(P)
    nrm = bl.ap("nrm"); onesd = bl.ap("onesd")
    mT = P.dram("mT", [D, TLOC], F32, "ExternalOutput")
    ones_sb = P.sb([128, 128], F32, name="ones"); P.dma(SP, ones_sb[:], onesd[:, :], writes=[ones_sb])
    nrm_sb = P.sb([128, 8], F32, name="nrm"); P.dma(SP, nrm_sb[:], nrm[:, :], writes=[nrm_sb])
    a = [P.sb([128, 4, 512], F32, name=f"ma{i}") for i in range(2)]
    b = [P.sb([128, 4, 512], F32, name=f"mb{i}") for i in range(2)]
    g = [P.sb([128, 4, 512], F32, name=f"mg{i}") for i in range(2)]
    sq = P.sb([128, 4, 512], F32, name="msq"); rs = P.sb([128, 512], F32, name="mrs")
    outs = []
    it = 0
    for part in range(2):
        src = yT if part == 0 else oT
        gsrc = szT if part == 0 else sgT
        for t0 in range(0, TLOC, 512):
            i = it % 2; it += 1
            P.dma(SP, a[i][:], src[0, :, t0:t0 + 512].rearrange("(k p) t -> p k t", p=128), writes=[a[i]])
            P.dma(ACT, b[i][:], src[1, :, t0:t0 + 512].rearrange("(k p) t -> p k t", p=128), writes=[b[i]])
            P.dma(SP, g[i][:], gsrc[:, t0:t0 + 512].rearrange("(k p) t -> p k t", p=128), writes=[g[i]])
            P.I(DVE, "tensor_tensor", [a[i], b[i]], [a[i]], out=a[i][:], in0=a[i][:], in1=b[i][:], op=ALU.add)
            if part == 0:
                P.I(DVE, "tensor_tensor", [a[i], g[i]], [a[i]], out=a[i][:], in0=a[i][:], in1=g[i][:], op=ALU.mult)
            P.I(ACT, "activation", [a[i]], [sq], out=sq[:], in_=a[i][:], func=AF.Square)
            groups = [(0, 2), (2, 4)] if part == 0 else [(0, 1), (1, 2), (2, 3), (3, 4)]
            for (k0, k1) in groups:
                ps = Bk.f[k0 % 2]
                for kc in range(k0, k1):
                    P.I(PE, "matmul", [ones_sb, sq], [ps], ps[:, 0:512], ones_sb[:], sq[:, kc, :], start=(kc == k0), stop=(kc == k1 - 1))
                P.I(DVE, "tensor_scalar", [ps], [rs], out=rs[:], in0=ps[:, 0:512], scalar1=1.0 / (128 * (k1 - k0)), scalar2=EPS, op0=ALU.mult, op1=ALU.add)
                P.I(ACT, "activation", [rs], [rs], out=rs[:], in_=rs[:], func=AF.Ln)
                P.I(ACT, "activation", [rs], [rs], out=rs[:], in_=rs[:], func=AF.Exp, scale=-0.5)
                for kc in range(k0, k1):
                    P.I(DVE, "scalar_tensor_tensor", [a[i], nrm_sb, rs], [b[i]], out=b[i][:, kc, :], in0=a[i][:, kc, :], scalar=nrm_sb[:, part * 4 + kc:part * 4 + kc + 1],
                        in1=rs[:], op0=ALU.mult, op1=ALU.mult)
            if part == 1:
                P.I(DVE, "tensor_tensor", [b[i], g[i]], [b[i]], out=b[i][:], in0=b[i][:], in1=g[i][:], op=ALU.mult)
            outs.append(P.dma(POOL, mT[part * 512:(part + 1) * 512, t0:t0 + 512].rearrange("(k p) t -> p k t", p=128), b[i][:], reads=[b[i]]))
    return P.finish(outs)


def prep_M(inp, yT, oT, dfull):
    nrm = np.concatenate([col_layout(inp['ssd_norm'][0], 4), col_layout(inp['hg_norm'][0], 4)], axis=1)
    maps = []
    for c in range(NCORE):
        sl = slice(c * TLOC, (c + 1) * TLOC)
        maps.append(dict(yT=np.ascontiguousarray(yT[:, :, sl]), oT=np.ascontiguousarray(oT[:, :, sl]),
                         szT=np.ascontiguousarray(dfull[3584:4096, sl]), sgT=np.ascontiguousarray(dfull[4608:5120, sl]),
                         nrm=np.ascontiguousarray(nrm), onesd=np.ones((128, 128), np.float32)))
    return blobify(maps, BLOB_M)


def _run(nc, maps, tag=""):
    import time, sys
    t0 = time.time()
    r = run_bass_kernel_spmd(nc, maps, core_ids=list(range(len(maps)))).results
    print(f"[kernel] launch {tag}: {time.time() - t0:.1f}s", file=sys.stderr, flush=True)
    return r


def kernel(**inp):
    inp = {k: np.asarray(v) for k, v in inp.items()}
    x = inp['x'][0]; ctx = inp['ctx'][0]
    xT = np.ascontiguousarray(x.T); xcT = np.ascontiguousarray(ctx.T)
    rA = _run(build_A(), prep_A(inp), 'A')
    uT = np.concatenate([rA[c]['uT'] for c in range(NCORE)], axis=1)
    attT = np.concatenate([rA[c]['attT'] for c in range(NCORE)], axis=1)
    ucT = rA[0]['ucT']; attcT = rA[0]['attcT']
    hyT = gather_B(_run(build_B(SEQ), prep_B(inp, uT, SEQ), 'B'), SEQ)
    hycT = gather_B(_run(build_B(NCTX), prep_B(inp, ucT, NCTX), 'Bc'), NCTX)
    mT = np.concatenate([hyT, attT], axis=0); mcT = np.concatenate([hycT, attcT], axis=0)
    maps, passes = prep_C(inp, 0, mT, xT, mcT, xcT)
    xT, xcT = gather_C(_run(build_C(passes), maps, 'C0'), True)
    dfull, dctx = gather_D(_run(build_D(), prep_D(inp, xT, xcT), 'D'))
    yT, oT = gather_E(_run(build_E(), prep_E(inp, dfull, dctx), 'E'))
    rM = _run(build_M(), prep_M(inp, yT, oT, dfull), 'M')
    mT = np.concatenate([rM[c]['mT'] for c in range(NCORE)], axis=1)
    maps, passes = prep_C(inp, 1, mT, xT, None, None)
    xT, _ = gather_C(_run(build_C(passes), maps, 'C1'), False)
    return np.ascontiguousarray(xT.T)[None].astype(np.float32)
```

```python
import contextlib
import numpy as np
import concourse.bass as bass
import concourse.mybir as mybir
from concourse.bass_utils import run_bass_kernel_spmd

F32 = mybir.dt.float32
BF16 = mybir.dt.bfloat16
I32 = mybir.dt.int32
AF = mybir.ActivationFunctionType
ALU = mybir.AluOpType
AX = mybir.AxisListType

PE, DVE, ACT, POOL, SP = "tensor", "vector", "scalar", "gpsimd", "sync"
COMPUTE = (PE, DVE, ACT, POOL)
NDMASEM = 8
EPOCH_LEN = 20000


class Prog:
    def __init__(self):
        self.nc = bass.Bass("TRN2", target_bir_lowering=False)
        self.stack = contextlib.ExitStack()
        self.streams = {e: [] for e in (PE, DVE, ACT, POOL, SP)}
        self.cnt = {e: 0 for e in COMPUTE}
        self.dcnt = {e: 0 for e in (SP, ACT, POOL)}
        self.sem = {}
        self.dsem = {}
        self.waited = {}
        self.lastw = {}
        self.reads = {}
        self.ntens = 0
        self.out_tokens = []
        self.epoch = {e: 0 for e in COMPUTE}
        self.root_stack = self.stack
        for e in COMPUTE:
            self.sem[(e, 0)] = self.stack.enter_context(self.nc.semaphore("s_" + e + "_0"))
        for q in (SP, ACT, POOL):
            self.dsem[q] = [self.stack.enter_context(self.nc.semaphore(f"d_{q}_{i}")) for i in range(NDMASEM)]

    @contextlib.contextmanager
    def scope(self):
        outer = self.stack
        self.stack = contextlib.ExitStack()
        try:
            yield
        finally:
            self.barrier()
            self.stack.close()
            self.stack = outer

    def barrier(self):
        toks = [("c", e, (self.epoch[e], self.cnt[e])) for e in COMPUTE if self.cnt[e] > 0]
        for q in (SP, ACT, POOL):
            for k in range(max(0, self.dcnt[q] - NDMASEM), self.dcnt[q]):
                toks.append(("d", q, k))
        for st in (PE, DVE, ACT, POOL, SP):
            self._emit_waits(st, [t for t in toks if not (t[0] == "c" and t[1] == st)])

    def dram(self, name, shape, dtype, kind):
        return self.nc.dram_tensor(name, list(shape), dtype, kind=kind).ap()

    def sb(self, shape, dtype, name=None):
        self.ntens += 1
        name = "sb_" + (name or f"t{self.ntens}")
        return self.stack.enter_context(self.nc.sbuf_tensor(name, list(shape), dtype))

    def ps(self, shape, dtype=F32, name=None):
        self.ntens += 1
        name = "ps_" + (name or f"p{self.ntens}")
        return self.stack.enter_context(self.nc.psum_tensor(name, list(shape), dtype))

    def _key(self, t):
        if isinstance(t, str):
            return t
        if isinstance(t, tuple):
            return t
        th = getattr(t, "tensor", t)
        return getattr(th, "name", None) or id(th)

    def _tok_sem_val(self, tok):
        kind, e, i = tok
        if kind == "c":
            ep, idx = i
            return self.sem[(e, ep)], idx, ("c", e, ep)
        return self.dsem[e][i % NDMASEM], 16 * (i // NDMASEM + 1), ("d", e, i % NDMASEM)

    def _emit_waits(self, stream, toks):
        need = {}
        for tok in toks:
            if tok is None:
                continue
            s, v, k = self._tok_sem_val(tok)
            if tok[0] == "c" and tok[1] == stream and stream == PE:
                continue
            if k not in need or need[k][1] < v:
                need[k] = (s, v)
        for k, (s, v) in need.items():
            if self.waited.get((stream, k), 0) >= v:
                continue
            self.waited[(stream, k)] = v
            self.streams[stream].append(("wait", s, v))

    def _deps(self, reads, writes):
        toks = []
        for t in reads:
            k = self._key(t)
            toks.append(self.lastw.get(k))
        for t in writes:
            k = self._key(t)
            toks.append(self.lastw.get(k))
            toks.extend(self.reads.get(k, []))
        return toks

    def _commit(self, tok, reads, writes):
        for t in reads:
            k = self._key(t)
            self.reads.setdefault(k, []).append(tok)
            if len(self.reads[k]) > 24:
                best = {}
                for tk in self.reads[k]:
                    kk = (tk[0], tk[1], tk[2][0]) if tk[0] == "c" else tk
                    if kk not in best or best[kk][2] < tk[2]:
                        best[kk] = tk
                self.reads[k] = list(best.values())
        for t in writes:
            k = self._key(t)
            self.lastw[k] = tok
            self.reads[k] = []

    def op(self, eng, fn, reads=(), writes=()):
        self._emit_waits(eng, self._deps(reads, writes))
        if self.cnt[eng] >= EPOCH_LEN:
            self.epoch[eng] += 1
            self.cnt[eng] = 0
            self.sem[(eng, self.epoch[eng])] = self.root_stack.enter_context(self.nc.semaphore(f"s_{eng}_{self.epoch[eng]}"))
        self.cnt[eng] += 1
        tok = ("c", eng, (self.epoch[eng], self.cnt[eng]))
        self.streams[eng].append(("op", fn, self.sem[(eng, self.epoch[eng])], 1))
        self._commit(tok, reads, writes)
        return tok

    def I(self, eng, mname, reads, writes, *args, **kw):
        return self.op(eng, lambda e: getattr(e, mname)(*args, **kw), reads=reads, writes=writes)

    def dma(self, q, out, in_, reads=(), writes=(), **kw):
        k = self.dcnt[q]
        toks = self._deps(reads, writes)
        if k >= NDMASEM:
            toks.append(("d", q, k - NDMASEM))
        self._emit_waits(q, toks)
        self.dcnt[q] += 1
        tok = ("d", q, k)
        self.streams[q].append(("op", lambda e: e.dma_start(out=out, in_=in_, **kw), self.dsem[q][k % NDMASEM], 16))
        self._commit(tok, reads, writes)
        return tok

    def finish(self, final_toks):
        self._emit_waits(SP, final_toks)
        nc = self.nc
        streams = self.streams

        def run(engine, lst):
            for it in lst:
                if it[0] == "wait":
                    engine.wait_ge(it[1], it[2])
                else:
                    it[1](engine).then_inc(it[2], it[3])

        with nc.Block() as block:
            @block.sync
            def _(e):
                run(e, streams[SP])

            @block.tensor
            def _(e):
                run(e, streams[PE])

            @block.vector
            def _(e):
                run(e, streams[DVE])

            @block.scalar
            def _(e):
                run(e, streams[ACT])

            @block.gpsimd
            def _(e):
                run(e, streams[POOL])
        self.stack.close()
        return nc


D = 1024
SEQ = 16384
NCORE = 8
TLOC = SEQ // NCORE
HALO = 128
TEXT = TLOC + 2 * HALO
NCTX = 256
EPS = 1e-6
MASKNEG = -30000.0


def _bcast_free(ap2d, n):
    return ap2d.unsqueeze(2).broadcast_to([ap2d.shape[0], ap2d.shape[1], n])


class Blob:
    def __init__(self, items):
        self.items = {}
        o = 0
        for name, rows, cols in items:
            self.items[name] = (rows, o, cols); o += cols
        self.total = o
        self.t = None

    def dram(self, P):
        self.t = P.dram("blob", [128, self.total], F32, "ExternalInput")
        return self

    def ap(self, name):
        rows, o, cols = self.items[name]
        return self.t[0:rows, o:o + cols]

    def pack(self, d):
        out = np.zeros((128, self.total), np.float32)
        for name, (rows, o, cols) in self.items.items():
            out[0:rows, o:o + cols] = np.asarray(d[name], np.float32).reshape(rows, cols)
        return out


BLOB_A = [("cvec", 128, 16), ("adab", 128, 24), ("normg", 128, 8), ("gains", 128, 640), ("masks", 128, 512), ("sinkrow", 128, 1024),
          ("ident", 128, 128), ("onesd", 128, 128), ("convw", 128, 36), ("convb", 128, 12), ("edge", 128, 2)]
BLOB_C = [("cvec", 128, 16), ("adab", 128, 32), ("normg", 128, 8), ("rw", 128, 256), ("rb", 128, 32), ("bgu", 128, 512),
          ("ident", 128, 128), ("onesd", 128, 128)]
BLOB_B = [("w1", 33, 64), ("w2", 64, 64), ("w3", 64, 64), ("w4s", 64, 256), ("fqb", 64, 4), ("negd", 128, 1), ("fbias", 128, 128),
          ("ident", 128, 128), ("onesd", 128, 128), ("jmat", 128, 128)]
BLOB_D = [("cvec", 128, 16), ("adab", 128, 24), ("normg", 128, 8), ("onesd", 128, 128), ("convw", 128, 24), ("convb", 128, 8),
          ("edge", 128, 2), ("hglb", 128, 8), ("dtb", 16, 1)]
BLOB_E = [("ssdp", 128, 4), ("U", 128, 128), ("U4", 128, 128), ("Mneg", 128, 128), ("ident", 128, 128), ("onesd", 128, 128)]
BLOB_M = [("nrm", 128, 8), ("onesd", 128, 128)]


def blobify(maps, spec):
    bl = Blob(spec)
    out = []
    for m in maps:
        m2 = {k: v for k, v in m.items() if k not in bl.items}
        m2['blob'] = bl.pack(m)
        out.append(m2)
    return out


class Banks:
    def __init__(self, P, nb16=1):
        self.f = [P.ps([128, 512], F32, name=f"bank{i}") for i in range(8 - nb16)]
        self.h = [P.ps([128, 1024], BF16, name=f"bankh{i}") for i in range(nb16)]


def emit_adaln(P, B, cv_sb, adaw_dram, adab_sb, wbuf, out_sb, ncol, nparts=3):
    sc = P.sb([128, 8, ncol], F32, name="ada_silu")
    P.op(ACT, lambda e: e.activation(out=sc[:], in_=cv_sb[:], func=AF.Silu), reads=[cv_sb], writes=[sc])
    ps = B.f[0]
    for part in range(nparts):
        for kc in range(8):
            P.dma(SP if kc % 2 == 0 else ACT, wbuf[:, kc, :], adaw_dram[kc * 128:(kc + 1) * 128, part * 1024:(part + 1) * 1024],
                  writes=[wbuf])
        for o in range(8):
            oc = part * 8 + o
            for kc in range(8):
                P.op(PE, lambda e, o=o, kc=kc, oc=oc: e.matmul(ps[:, oc * ncol:(oc + 1) * ncol], wbuf[:, kc, o * 128:(o + 1) * 128],
                                                               sc[:, kc, :], start=(kc == 0), stop=(kc == 7)),
                     reads=[wbuf, sc], writes=[ps])
    P.op(DVE, lambda e: e.tensor_tensor(out=out_sb[:], in0=ps[:, 0:8 * nparts * ncol].rearrange("p (o n) -> p o n", n=ncol),
                                        in1=_bcast_free(adab_sb[:], ncol), op=ALU.add),
         reads=[ps, adab_sb], writes=[out_sb])


def emit_norm_mod(P, B, x_sb, ones_sb, A_col, B_col, h_out, ntok, tmp_sq, tmp_rs, hcol0=0):
    ps = B.f[1]
    for kc in range(8):
        P.op(ACT, lambda e, kc=kc: e.activation(out=tmp_sq[:, kc, 0:ntok], in_=x_sb[:, kc, 0:ntok], func=AF.Square),
             reads=[x_sb], writes=[tmp_sq])
    for kc in range(8):
        P.op(PE, lambda e, kc=kc: e.matmul(ps[:, 0:ntok], ones_sb[:], tmp_sq[:, kc, 0:ntok], start=(kc == 0), stop=(kc == 7)),
             reads=[ones_sb, tmp_sq], writes=[ps])
    P.op(DVE, lambda e: e.tensor_scalar(out=tmp_rs[:, 0:ntok], in0=ps[:, 0:ntok], scalar1=1.0 / D, scalar2=EPS,
                                        op0=ALU.mult, op1=ALU.add), reads=[ps], writes=[tmp_rs])
    P.op(ACT, lambda e: e.activation(out=tmp_rs[:, 0:ntok], in_=tmp_rs[:, 0:ntok], func=AF.Ln), reads=[tmp_rs], writes=[tmp_rs])
    P.op(ACT, lambda e: e.activation(out=tmp_rs[:, 0:ntok], in_=tmp_rs[:, 0:ntok], func=AF.Exp, scale=-0.5), reads=[tmp_rs], writes=[tmp_rs])
    for kc in range(8):
        P.op(DVE, lambda e, kc=kc: e.tensor_tensor(out=tmp_sq[:, kc, 0:ntok], in0=x_sb[:, kc, 0:ntok], in1=tmp_rs[:, 0:ntok],
                                                   op=ALU.mult), reads=[x_sb, tmp_rs, tmp_sq], writes=[tmp_sq])
        P.op(ACT, lambda e, kc=kc: e.activation(out=h_out[:, kc, hcol0:hcol0 + ntok], in_=tmp_sq[:, kc, 0:ntok], func=AF.Identity,
                                                bias=B_col[:, kc:kc + 1], scale=A_col[:, kc:kc + 1]),
             reads=[tmp_sq, A_col, B_col], writes=[h_out])


TT = 384
NTT = TEXT // TT


def build_A():
    P = Prog()
    Bk = Banks(P)
    di = lambda n, s, dt=F32: P.dram(n, s, dt, "ExternalInput")
    do = lambda n, s, dt=F32: P.dram(n, s, dt, "ExternalOutput")
    xT = di("xT", [D, TEXT]); ctxT = di("ctxT", [D, NCTX])
    bl = Blob(BLOB_A).dram(P)
    cvec = bl.ap("cvec"); adaw = di("adaw", [D, 3072]); adab = bl.ap("adab")
    normg = bl.ap("normg"); w_in = di("w_in", [D, 2304])
    gains = bl.ap("gains"); ctab = di("ctab", [TEXT, 64]); stab = di("stab", [TEXT, 64])
    masks = bl.ap("masks"); sinkrow = bl.ap("sinkrow")
    ident = bl.ap("ident"); onesd = bl.ap("onesd")
    convw = bl.ap("convw"); convb = bl.ap("convb"); edge = bl.ap("edge")
    uT = do("uT", [1536, TLOC]); ucT = do("ucT", [1536, NCTX])
    attT = do("attT", [512, TLOC]); attcT = do("attcT", [512, NCTX])

    ones_sb = P.sb([128, 128], F32, name="ones"); P.dma(SP, ones_sb[:], onesd[:, :], writes=[ones_sb])
    id_f = P.sb([128, 128], F32, name="idf"); P.dma(SP, id_f[:], ident[:, :], writes=[id_f])
    id_b = P.sb([128, 128], BF16, name="idb"); P.dma(POOL, id_b[:], ident[:, :], writes=[id_b])
    cv = P.sb([128, 8, 2], F32, name="cv"); P.dma(SP, cv[:], cvec.rearrange("p (k n) -> p k n", n=2), writes=[cv])
    adab_sb = P.sb([128, 24], F32, name="adab"); P.dma(SP, adab_sb[:], adab[:, :], writes=[adab_sb])
    g_sb = P.sb([128, 8], F32, name="normg"); P.dma(SP, g_sb[:], normg[:, :], writes=[g_sb])
    gains_sb = P.sb([128, 640], F32, name="gains"); P.dma(SP, gains_sb[:], gains[:, :], writes=[gains_sb])
    mask_sb = P.sb([128, 4, 128], BF16, name="masks")
    P.dma(POOL, mask_sb[:], masks.rearrange("k (m q) -> k m q", m=4), writes=[mask_sb])
    sink_sb = P.sb([128, 1024], F32, name="sink"); P.dma(SP, sink_sb[:], sinkrow[:, :], writes=[sink_sb])
    P.op(ACT, lambda e: e.activation(out=sink_sb[:], in_=sink_sb[:], func=AF.Exp), reads=[sink_sb], writes=[sink_sb])
    w_sb = P.sb([128, 8, 2304], BF16, name="w_in")
    for kc in range(8):
        P.dma(POOL, w_sb[:, kc, :], w_in[kc * 128:(kc + 1) * 128, :], writes=[w_sb])

    ada = P.sb([128, 24, 2], F32, name="ada")
    with P.scope():
        wbuf = P.sb([128, 8, 1024], F32, name="adawbuf")
        emit_adaln(P, Bk, cv, adaw, adab_sb, wbuf, ada, 2)
    Acol = [P.sb([128, 8], F32, name=f"Acol{j}") for j in range(2)]
    Bcol = [P.sb([128, 8], F32, name=f"Bcol{j}") for j in range(2)]
    for j in range(2):
        P.op(DVE, lambda e, j=j: e.scalar_tensor_tensor(out=Acol[j][:], in0=ada[:, 8:16, j], scalar=1.0, in1=g_sb[:],
                                                         op0=ALU.add, op1=ALU.mult), reads=[ada, g_sb], writes=[Acol[j]])
        P.op(DVE, lambda e, j=j: e.tensor_copy(out=Bcol[j][:], in_=ada[:, 0:8, j]), reads=[ada], writes=[Bcol[j]])

    kqT = P.sb([64, 10, TEXT + NCTX], BF16, name="kqT")
    vaug = P.sb([128, (TEXT + NCTX) // 128, 2, 65], BF16, name="vaug")
    P.op(POOL, lambda e: e.memset(vaug[:], 1.0), writes=[vaug])

    x_sb = P.sb([128, 8, TT], F32, name="x_sb")
    sq_sb = P.sb([128, 8, TT], F32, name="sq_sb")
    rs_sb = P.sb([128, TT], F32, name="rs_sb")
    h_all = P.sb([128, 8, TEXT + NCTX], BF16, name="h_all")
    cw_sb = P.sb([128, 12, 3], F32, name="cw"); P.dma(SP, cw_sb[:], convw.rearrange("p (o t) -> p o t", t=3), writes=[cw_sb])
    cb_sb = P.sb([128, 12], F32, name="cb"); P.dma(SP, cb_sb[:], convb[:, :], writes=[cb_sb])
    edge_sb = P.sb([128, 2], F32, name="edge"); P.dma(SP, edge_sb[:], edge[:, :], writes=[edge_sb])
    kqv = P.sb([128, 768], F32, name="kqv")
    sq2 = P.sb([128, 640], F32, name="sq2")
    ss = P.sb([128, 10], F32, name="ss")
    tmpr = P.sb([128, 640], F32, name="tmpr")
    kqb = P.sb([128, 640], BF16, name="kqb")
    ct_sb = P.sb([128, 64], F32, name="ct"); st_sb = P.sb([128, 64], F32, name="st")
    u_sb = [P.sb([128, 512], F32, name=f"u_sb{i}") for i in range(2)]
    acc_sb = [P.sb([128, 512], F32, name=f"acc_sb{i}") for i in range(2)]

    def proj_tile(src_dram, col0, ntok, j, tok_base, is_ctx):
        for kc in range(8):
            P.dma(SP if kc % 2 == 0 else ACT, x_sb[:, kc, 0:ntok], src_dram[kc * 128:(kc + 1) * 128, col0:col0 + ntok], writes=[x_sb])
        emit_norm_mod(P, Bk, x_sb, ones_sb, Acol[j], Bcol[j], h_all, ntok, sq_sb, rs_sb, hcol0=tok_base)
        for s in range(ntok // 128):
            pa, pb = Bk.f[2], Bk.f[3]
            for kc in range(8):
                P.op(PE, lambda e, kc=kc, s=s: e.matmul(pa[:, 0:512], h_all[:, kc, tok_base + s * 128:tok_base + (s + 1) * 128], w_sb[:, kc, 0:512],
                                                        start=(kc == 0), stop=(kc == 7)), reads=[h_all, w_sb], writes=[pa])
            for kc in range(8):
                P.op(PE, lambda e, kc=kc, s=s: e.matmul(pb[:, 0:256], h_all[:, kc, tok_base + s * 128:tok_base + (s + 1) * 128], w_sb[:, kc, 512:768],
                                                        start=(kc == 0), stop=(kc == 7)), reads=[h_all, w_sb], writes=[pb])
            P.op(ACT, lambda e: e.activation(out=kqv[:, 0:512], in_=pa[:, 0:512], func=AF.Copy), reads=[pa], writes=[kqv])
            P.op(ACT, lambda e: e.activation(out=kqv[:, 512:768], in_=pb[:, 0:256], func=AF.Copy), reads=[pb], writes=[kqv])
            tile_idx = (tok_base + s * 128) // 128
            P.op(POOL, lambda e, ti=tile_idx: e.tensor_copy(out=vaug[:, ti, :, 0:64], in_=kqv[:, 640:768].rearrange("p (g d) -> p g d", d=64)),
                 reads=[kqv], writes=[vaug])
            P.op(DVE, lambda e: e.tensor_tensor(out=sq2[:], in0=kqv[:, 0:640], in1=kqv[:, 0:640], op=ALU.mult), reads=[kqv], writes=[sq2])
            P.op(DVE, lambda e: e.tensor_reduce(out=ss[:], in_=sq2[:].rearrange("p (h d) -> p h d", d=64), axis=AX.X, op=ALU.add),
                 reads=[sq2], writes=[ss])
            P.op(DVE, lambda e: e.tensor_scalar(out=ss[:], in0=ss[:], scalar1=1.0 / 64, scalar2=EPS, op0=ALU.mult, op1=ALU.add),
                 reads=[ss], writes=[ss])
            P.op(ACT, lambda e: e.activation(out=ss[:], in_=ss[:], func=AF.Ln), reads=[ss], writes=[ss])
            P.op(ACT, lambda e: e.activation(out=ss[:], in_=ss[:], func=AF.Exp, scale=-0.5), reads=[ss], writes=[ss])
            P.op(DVE, lambda e: e.tensor_tensor(out=sq2[:].rearrange("p (h d) -> p h d", d=64), in0=kqv[:, 0:640].rearrange("p (h d) -> p h d", d=64),
                                                in1=_bcast_free(ss[:], 64), op=ALU.mult), reads=[kqv, ss], writes=[sq2])
            P.op(DVE, lambda e: e.tensor_tensor(out=sq2[:], in0=sq2[:], in1=gains_sb[:], op=ALU.mult), reads=[sq2, gains_sb], writes=[sq2])
            if not is_ctx:
                r0 = col0 + s * 128
                P.dma(SP, ct_sb[:], ctab[r0:r0 + 128, :], writes=[ct_sb])
                P.dma(ACT, st_sb[:], stab[r0:r0 + 128, :], writes=[st_sb])
                v5 = lambda t: t[:].rearrange("p (h b two s) -> p (h b) two s", b=2, two=2, s=16)
                stv = st_sb[:].rearrange("p (b two s) -> p b two s", two=2, s=16)
                ctv = ct_sb[:].rearrange("p (b two s) -> p b two s", two=2, s=16)

                def bc(tv, two):
                    a = tv[:, :, two, :]
                    return a.unsqueeze(1).broadcast_to([128, 10, 2, 16])
                u4 = sq2[:].rearrange("p (h b two s) -> p h b two s", b=2, two=2, s=16)
                t4 = tmpr[:].rearrange("p (h b two s) -> p h b two s", b=2, two=2, s=16)
                for two in range(2):
                    P.op(DVE, lambda e, two=two: e.tensor_tensor(out=t4[:, :, :, two, :], in0=u4[:, :, :, 1 - two, :], in1=bc(stv, two), op=ALU.mult),
                         reads=[sq2, st_sb], writes=[tmpr])
                for two in range(2):
                    P.op(DVE, lambda e, two=two: e.tensor_tensor(out=u4[:, :, :, two, :], in0=u4[:, :, :, two, :], in1=bc(ctv, two), op=ALU.mult),
                         reads=[sq2, ct_sb], writes=[sq2])
                P.op(DVE, lambda e: e.tensor_tensor(out=kqb[:], in0=sq2[:], in1=tmpr[:], op=ALU.add), reads=[sq2, tmpr], writes=[kqb])
            else:
                P.op(DVE, lambda e: e.tensor_copy(out=kqb[:], in_=sq2[:]), reads=[sq2], writes=[kqb])
            pt = Bk.h[0]
            for hh in range(10):
                P.op(PE, lambda e, hh=hh: e.transpose(pt[0:64, hh * 128:(hh + 1) * 128][:, 0:128] if False else pt[0:64, hh * 128 % 1024:(hh * 128 % 1024) + 128],
                                                      kqb[:, hh * 64:(hh + 1) * 64], id_b[:]),
                     reads=[kqb, id_b], writes=[pt])
                if hh == 7 or hh == 9:
                    h0 = 0 if hh == 7 else 8
                    nh = hh - h0 + 1
                    t0 = tok_base + s * 128
                    P.op(ACT, lambda e, h0=h0, nh=nh, t0=t0: e.activation(
                        out=kqT[:, h0:h0 + nh, t0:t0 + 128],
                        in_=pt[0:64, (h0 * 128) % 1024:(h0 * 128) % 1024 + nh * 128].rearrange("p (h t) -> p h t", t=128), func=AF.Copy),
                        reads=[pt], writes=[kqT])

    def hyena_tile(h0, n, out_dram, ocol0, zl, zr, el, er):
        a0 = h0 - (0 if zl else 1); a1 = h0 + n + (0 if zr else 1)
        w = a1 - a0
        off = 1 if zl else 0
        for oc in range(12):
            pu = Bk.f[4 + oc % 2]
            ub = u_sb[oc % 2]; ac = acc_sb[oc % 2]
            for kc in range(8):
                P.I(PE, "matmul", [w_sb, h_all], [pu], pu[:, off:off + w], w_sb[:, kc, 768 + oc * 128:768 + (oc + 1) * 128], h_all[:, kc, a0:a1],
                    start=(kc == 0), stop=(kc == 7))
            if zl:
                P.I(POOL, "memset", [], [ub], ub[:, 0:1], 0.0)
            if zr:
                P.I(POOL, "memset", [], [ub], ub[:, n + 1:n + 2], 0.0)
            P.I(ACT, "activation", [pu], [ub], out=ub[:, off:off + w], in_=pu[:, off:off + w], func=AF.Copy)
            if el:
                P.I(DVE, "tensor_scalar", [ub, edge_sb], [ub], out=ub[:, 0:1], in0=ub[:, 0:1], scalar1=edge_sb[:, 0:1], scalar2=None, op0=ALU.mult)
            if er:
                P.I(DVE, "tensor_scalar", [ub, edge_sb], [ub], out=ub[:, n + 1:n + 2], in0=ub[:, n + 1:n + 2], scalar1=edge_sb[:, 1:2], scalar2=None, op0=ALU.mult)
            P.I(DVE, "tensor_scalar", [ub, cw_sb, cb_sb], [ac], out=ac[:, 0:n], in0=ub[:, 1:n + 1], scalar1=cw_sb[:, oc, 1:2], scalar2=cb_sb[:, oc:oc + 1], op0=ALU.mult, op1=ALU.add)
            P.I(DVE, "scalar_tensor_tensor", [ub, cw_sb, ac], [ac], out=ac[:, 0:n], in0=ub[:, 0:n], scalar=cw_sb[:, oc, 0:1], in1=ac[:, 0:n], op0=ALU.mult, op1=ALU.add)
            P.I(DVE, "scalar_tensor_tensor", [ub, cw_sb, ac], [ac], out=ac[:, 0:n], in0=ub[:, 2:n + 2], scalar=cw_sb[:, oc, 2:3], in1=ac[:, 0:n], op0=ALU.mult, op1=ALU.add)
            outs.append(P.dma(POOL, out_dram[oc * 128:(oc + 1) * 128, ocol0:ocol0 + n], ac[:, 0:n], reads=[ac]))

    outs = []
    for t in range(NTT):
        proj_tile(xT, t * TT, TT, 0, t * TT, False)
    proj_tile(ctxT, 0, NCTX, 1, TEXT, True)
    lo = 0
    while lo < TLOC:
        n = min(510, TLOC - lo)
        hyena_tile(HALO + lo, n, uT, lo, False, False, lo == 0, lo + n == TLOC)
        lo += n
    hyena_tile(TEXT, NCTX, ucT, 0, True, True, False, False)

    NB = TLOC // 128
    ctx_tiles = [TEXT // 128, TEXT // 128 + 1]
    pT = [P.sb([128, 512], BF16, name=f"pT{i}") for i in range(2)]
    o_sb = P.sb([64, 512], F32, name="o_sb"); rden = P.sb([65, 512], F32, name="rden")
    of_sb = P.sb([64, 512], F32, name="of_sb")
    ones_b = P.sb([128, 64], F32, name="ones_b")
    P.op(POOL, lambda e: e.memset(ones_b[:], 1.0), writes=[ones_b])

    def attend(qtile, key_tiles, key_masks, out_dram, out_col0):
        for g in range(2):
            po = Bk.f[2]
            nkt = len(key_tiles)
            for ci, (kt, mk) in enumerate(zip(key_tiles, key_masks)):
                psc = Bk.f[ci % 2]
                pTb = pT[ci % 2]
                qv = kqT[:, 2 + 4 * g:2 + 4 * g + 4, qtile * 128:(qtile + 1) * 128]
                P.op(PE, lambda e, psc=psc, kt=kt, qv=qv, mk=mk, g=g: e.matmul(psc[:, 0:512], kqT[:, g, kt * 128:(kt + 1) * 128], qv,
                                                                         start=True, stop=(mk is None)), reads=[kqT], writes=[psc])
                if mk is not None:
                    for hh in range(4):
                        P.op(PE, lambda e, psc=psc, hh=hh, mk=mk: e.matmul(psc[:, hh * 128:(hh + 1) * 128], id_b[:], mask_sb[:, mk, :],
                                                                           start=False, stop=(hh == 3)), reads=[id_b, mask_sb], writes=[psc])
                P.op(ACT, lambda e, psc=psc, pTb=pTb: e.activation(out=pTb[:], in_=psc[:, 0:512], func=AF.Exp, scale=0.125),
                     reads=[psc], writes=[pTb])
                P.op(PE, lambda e, pTb=pTb, kt=kt, ci=ci, g=g: e.matmul(po[0:65, 0:512], vaug[:, kt, g, :], pTb[:], start=(ci == 0), stop=(ci == nkt - 1)),
                     reads=[vaug, pTb], writes=[po])
            P.op(DVE, lambda e, g=g: e.tensor_tensor(out=rden[64:65, :], in0=po[64:65, 0:512], in1=sink_sb[64:65, g * 512:(g + 1) * 512], op=ALU.add),
                 reads=[po, sink_sb], writes=[rden])
            P.op(DVE, lambda e: e.reciprocal(out=rden[64:65, :], in_=rden[64:65, :]), reads=[rden], writes=[rden])
            P.op(ACT, lambda e: e.activation(out=o_sb[:], in_=po[0:64, 0:512], func=AF.Copy), reads=[po], writes=[o_sb])
            pb = Bk.f[3]
            P.op(PE, lambda e: e.matmul(pb[0:64, 0:512], ones_b[64:65, :], rden[64:65, :], start=True, stop=True), reads=[ones_b, rden], writes=[pb])
            P.op(DVE, lambda e: e.tensor_tensor(out=of_sb[:], in0=o_sb[:], in1=pb[0:64, 0:512], op=ALU.mult), reads=[o_sb, pb], writes=[of_sb])
            for hh in range(4):
                r0 = (4 * g + hh) * 64
                outs.append(P.dma(POOL if hh % 2 == 0 else SP, out_dram[r0:r0 + 64, out_col0:out_col0 + 128], of_sb[:, hh * 128:(hh + 1) * 128], reads=[of_sb]))

    for b in range(NB):
        qt = b + 1
        mprev = 0 if b == 0 else 1
        mnext = 3 if b == NB - 1 else 2
        attend(qt, [qt - 1, qt, qt + 1] + ctx_tiles, [mprev, None, mnext, None, None], attT, b * 128)
    for cb in range(2):
        attend(ctx_tiles[cb], ctx_tiles, [None, None], attcT, cb * 128)
    return P.finish(outs)


def col_layout(v, nchunk):
    return np.ascontiguousarray(np.asarray(v, np.float32).reshape(nchunk, 128).T)


def rope_tables(tok_idx):
    inv = (10000.0 ** (-np.arange(16, dtype=np.float32) / 16)).astype(np.float32)
    t = np.asarray(tok_idx)
    row = (t // 64).astype(np.float32)[:, None] * inv
    col = (t % 64).astype(np.float32)[:, None] * inv
    cr, sr, cc, sc_ = np.cos(row), np.sin(row), np.cos(col), np.sin(col)
    C = np.concatenate([cr, cr, cc, cc], axis=1).astype(np.float32)
    S = np.concatenate([-sr, sr, -sc_, sc_], axis=1).astype(np.float32)
    return C, S


def band_masks(core):
    j = np.arange(128)[:, None]; i = np.arange(128)[None, :]
    prev = np.where(j >= i, 0.0, MASKNEG).astype(np.float32)
    nxt = np.where(j <= i, 0.0, MASKNEG).astype(np.float32)
    allneg = np.full((128, 128), MASKNEG, np.float32)
    return np.stack([allneg if core == 0 else prev, prev, nxt, allneg if core == NCORE - 1 else nxt])


def prep_A(inp, layer=0):
    x = inp['x'][0]; ctx = inp['ctx'][0]
    xT = np.ascontiguousarray(x.T)
    xTp = np.concatenate([np.zeros((D, HALO), np.float32), xT, np.zeros((D, HALO), np.float32)], axis=1)
    w = inp['w_in_even'][0]
    w_perm = np.ascontiguousarray(np.concatenate([w[:, 0:128], w[:, 256:768], w[:, 128:256], w[:, 768:]], axis=1))
    gains = np.concatenate([np.tile(inp['att_k_norm'][0], 2), np.tile(inp['att_q_norm'][0], 8)])
    gains = np.ascontiguousarray(np.broadcast_to(gains[None, :], (128, 640))).astype(np.float32)
    sink = inp['att_sink'][0]
    sinkrow = np.ascontiguousarray(np.broadcast_to(np.repeat(sink, 128)[None, :], (128, 1024))).astype(np.float32)
    cvec = np.stack([col_layout(inp['c'][0], 8), col_layout(inp['c_ctx'], 8)], axis=2).reshape(128, 16)
    common = dict(
        ctxT=np.ascontiguousarray(ctx.T), cvec=np.ascontiguousarray(cvec),
        adaw=np.ascontiguousarray(inp['ada_w'][layer][:, 0:3072]), adab=col_layout(inp['ada_b'][layer][0:3072], 24),
        normg=col_layout(inp['norm_g'][layer, 0], 8), w_in=w_perm, gains=gains, sinkrow=sinkrow,
        ident=np.eye(128, dtype=np.float32), onesd=np.ones((128, 128), np.float32),
        convw=np.ascontiguousarray(inp['hy_conv_w'][0].reshape(3, 12, 128).transpose(2, 1, 0).reshape(128, 36)),
        convb=col_layout(inp['hy_conv_b'][0], 12))
    maps = []
    for c in range(NCORE):
        s0 = c * TLOC
        C, S = rope_tables(np.arange(s0 - HALO, s0 + TLOC + HALO).clip(0, SEQ - 1))
        m = dict(common)
        edge = np.ones((128, 2), np.float32); edge[:, 0] = 0.0 if c == 0 else 1.0; edge[:, 1] = 0.0 if c == NCORE - 1 else 1.0
        m.update(xT=np.ascontiguousarray(xTp[:, s0:s0 + TEXT]), ctab=C, stab=S, edge=edge,
                 masks=np.ascontiguousarray(band_masks(c).transpose(1, 0, 2).reshape(128, 512)))
        maps.append(m)
    return blobify(maps, BLOB_A)


NEXP = 32
NC_MOE = 8
NH_MOE = 16


def moe_passes(ncore, has_ctx):
    nlat_pass = (SEQ // ncore) // 1024
    passes = [[(0, 512, 0), (512, 512, 0)] for _ in range(nlat_pass)]
    if has_ctx:
        passes.append([(0, NCTX // ncore, 1)])
    return passes


def build_C(passes):
    TH = 1024
    nhalf = len(passes)
    P = Prog()
    Bk = Banks(P)
    di = lambda n, s, dt=F32: P.dram(n, s, dt, "ExternalInput")
    mT = di("mT", [nhalf, D, TH]); xT = di("xT", [nhalf, D, TH])
    bl = Blob(BLOB_C).dram(P)
    cvec = bl.ap("cvec"); adaw = di("adaw", [D, 4096]); adab = bl.ap("adab")
    normg = bl.ap("normg"); w_out = di("w_out", [D, D])
    rw = bl.ap("rw"); rb = bl.ap("rb")
    w_gu = di("w_gu", [NEXP, 4, 2, 128, 2048]); bgu = bl.ap("bgu")
    w_dn = di("w_dn", [NEXP, 4, 128, 2048]); bdn = di("bdn", [NEXP, D])
    ident = bl.ap("ident"); onesd = bl.ap("onesd")
    outT = P.dram("outT", [nhalf, D, TH], F32, "ExternalOutput")
    gt_dram = P.dram("gt_scratch", [nhalf, NEXP, TH], F32, "Internal")

    ones_sb = P.sb([128, 128], F32, name="ones"); P.dma(SP, ones_sb[:], onesd[:, :], writes=[ones_sb])
    id_f = P.sb([128, 128], F32, name="idf"); P.dma(SP, id_f[:], ident[:, :], writes=[id_f])
    cv = P.sb([128, 8, 2], F32, name="cv"); P.dma(SP, cv[:], cvec.rearrange("p (k n) -> p k n", n=2), writes=[cv])
    adab_sb = P.sb([128, 32], F32, name="adab"); P.dma(SP, adab_sb[:], adab[:, :], writes=[adab_sb])
    g_sb = P.sb([128, 8], F32, name="normg"); P.dma(SP, g_sb[:], normg[:, :], writes=[g_sb])
    rw_sb = P.sb([128, 8, NEXP], F32, name="rw"); P.dma(SP, rw_sb[:], rw.rearrange("p (k e) -> p k e", e=NEXP), writes=[rw_sb])
    rb_sb = P.sb([128, NEXP], F32, name="rb"); P.dma(SP, rb_sb[:], rb[:, :], writes=[rb_sb])
    bgu_sb = P.sb([128, NEXP, 16], F32, name="bgu"); P.dma(SP, bgu_sb[:], bgu.rearrange("p (e o) -> p e o", o=16), writes=[bgu_sb])
    bdn_sb = P.sb([NEXP, D], F32, name="bdn"); P.dma(SP, bdn_sb[:], bdn[:, :], writes=[bdn_sb])
    ada = P.sb([128, 32, 2], F32, name="ada")
    with P.scope():
        wbuf = P.sb([128, 8, 1024], F32, name="adawbuf")
        emit_adaln(P, Bk, cv, adaw, adab_sb, wbuf, ada, 2, nparts=4)
    Acol = [P.sb([128, 8], F32, name=f"Acol{j}") for j in range(2)]
    Bcol = [P.sb([128, 8], F32, name=f"Bcol{j}") for j in range(2)]
    G0 = [P.sb([128, 8], F32, name=f"G0{j}") for j in range(2)]
    G1 = [P.sb([128, 8], F32, name=f"G1{j}") for j in range(2)]
    for j in range(2):
        P.I(DVE, "scalar_tensor_tensor", [ada, g_sb], [Acol[j]], out=Acol[j][:], in0=ada[:, 16:24, j], scalar=1.0, in1=g_sb[:], op0=ALU.add, op1=ALU.mult)
        P.I(DVE, "tensor_copy", [ada], [Bcol[j]], out=Bcol[j][:], in_=ada[:, 8:16, j])
        P.I(DVE, "tensor_copy", [ada], [G0[j]], out=G0[j][:], in_=ada[:, 0:8, j])
        P.I(DVE, "tensor_copy", [ada], [G1[j]], out=G1[j][:], in_=ada[:, 24:32, j])

    x1 = P.sb([128, 8, TH], F32, name="x1")
    hT = P.sb([128, 8, TH], BF16, name="hT")
    GT = P.sb([NEXP, TH], F32, name="GT")
    outs = []
    gu_cnt = [0]; dn_cnt = [0]; ch_cnt = [0]; st_cnt = [0]; pend = [None]

    for hf in range(nhalf):
        tiles = passes[hf]
        with P.scope():
            mb = P.sb([128, 8, TH], BF16, name=f"mb{hf}")
            wo = P.sb([128, 8, D], BF16, name=f"wo{hf}")
            for kc in range(8):
                P.dma(POOL, mb[:, kc, :], mT[hf, kc * 128:(kc + 1) * 128, :], writes=[mb])
                P.dma(POOL, wo[:, kc, :], w_out[kc * 128:(kc + 1) * 128, :], writes=[wo])
                P.dma(SP if kc % 2 == 0 else ACT, x1[:, kc, :], xT[hf, kc * 128:(kc + 1) * 128, :], writes=[x1])
            for (c0, n, j) in tiles:
                for oc in range(8):
                    ps = Bk.f[oc % 2]
                    for kc in range(8):
                        P.I(PE, "matmul", [wo, mb], [ps], ps[:, 0:n], wo[:, kc, oc * 128:(oc + 1) * 128], mb[:, kc, c0:c0 + n], start=(kc == 0), stop=(kc == 7))
                    P.I(DVE, "scalar_tensor_tensor", [ps, G0[j], x1], [x1], out=x1[:, oc, c0:c0 + n], in0=ps[:, 0:n], scalar=G0[j][:, oc:oc + 1],
                        in1=x1[:, oc, c0:c0 + n], op0=ALU.mult, op1=ALU.add)
        with P.scope():
            sq_sb = P.sb([128, 8, 512], F32, name=f"sq{hf}")
            rs_sb = P.sb([128, 512], F32, name=f"rs{hf}")
            hf32 = P.sb([128, 8, 512], F32, name=f"hf32{hf}")
            lg = P.sb([128, NEXP], F32, name=f"lg{hf}"); mx = P.sb([128, 8], F32, name=f"mx{hf}")
            msk = P.sb([128, NEXP], F32, name=f"msk{hf}"); ex = P.sb([128, NEXP], F32, name=f"ex{hf}")
            sm = P.sb([128, 1], F32, name=f"sm{hf}"); nm = P.sb([128, 1], F32, name=f"nm{hf}")
            for (c0, n, j) in tiles:
                ps = Bk.f[6]
                for kc in range(8):
                    P.I(ACT, "activation", [x1], [sq_sb], out=sq_sb[:, kc, 0:n], in_=x1[:, kc, c0:c0 + n], func=AF.Square)
                for kc in range(8):
                    P.I(PE, "matmul", [ones_sb, sq_sb], [ps], ps[:, 0:n], ones_sb[:], sq_sb[:, kc, 0:n], start=(kc == 0), stop=(kc == 7))
                P.I(DVE, "tensor_scalar", [ps], [rs_sb], out=rs_sb[:, 0:n], in0=ps[:, 0:n], scalar1=1.0 / D, scalar2=EPS, op0=ALU.mult, op1=ALU.add)
                P.I(ACT, "activation", [rs_sb], [rs_sb], out=rs_sb[:, 0:n], in_=rs_sb[:, 0:n], func=AF.Ln)
                P.I(ACT, "activation", [rs_sb], [rs_sb], out=rs_sb[:, 0:n], in_=rs_sb[:, 0:n], func=AF.Exp, scale=-0.5)
                for kc in range(8):
                    P.I(DVE, "tensor_tensor", [x1, rs_sb, sq_sb], [sq_sb], out=sq_sb[:, kc, 0:n], in0=x1[:, kc, c0:c0 + n], in1=rs_sb[:, 0:n], op=ALU.mult)
                    P.I(ACT, "activation", [sq_sb, Acol[j], Bcol[j]], [hf32], out=hf32[:, kc, 0:n], in_=sq_sb[:, kc, 0:n], func=AF.Identity,
                        bias=Bcol[j][:, kc:kc + 1], scale=Acol[j][:, kc:kc + 1])
                    P.I(POOL, "tensor_copy", [hf32], [hT], out=hT[:, kc, c0:c0 + n], in_=hf32[:, kc, 0:n])
                s0 = 0
                while s0 < n:
                    m = min(128, n - s0)
                    pl = Bk.f[5]
                    for kc in range(8):
                        P.I(PE, "matmul", [hf32, rw_sb], [pl], pl[0:m, 0:NEXP], hf32[:, kc, s0:s0 + m], rw_sb[:, kc, :], start=(kc == 0), stop=(kc == 7))
                    P.I(DVE, "tensor_tensor", [pl, rb_sb], [lg], out=lg[0:m, :], in0=pl[0:m, 0:NEXP], in1=rb_sb[0:m, :], op=ALU.add)
                    P.I(DVE, "max", [lg], [mx], out=mx[0:m, :], in_=lg[0:m, :])
                    P.I(DVE, "tensor_scalar", [lg, mx], [msk], out=msk[0:m, :], in0=lg[0:m, :], scalar1=mx[0:m, 3:4], scalar2=None, op0=ALU.is_ge)
                    P.I(DVE, "tensor_scalar", [mx], [nm], out=nm[0:m, :], in0=mx[0:m, 0:1], scalar1=-1.0, scalar2=None, op0=ALU.mult)
                    P.I(ACT, "activation", [lg, nm], [ex], out=ex[0:m, :], in_=lg[0:m, :], func=AF.Exp, bias=nm[0:m, 0:1], scale=1.0)
                    P.I(DVE, "tensor_tensor", [ex, msk], [ex], out=ex[0:m, :], in0=ex[0:m, :], in1=msk[0:m, :], op=ALU.mult)
                    P.I(DVE, "reduce_sum", [ex], [sm], out=sm[0:m, :], in_=ex[0:m, :], axis=AX.X)
                    P.I(DVE, "reciprocal", [sm], [sm], out=sm[0:m, :], in_=sm[0:m, :])
                    P.I(DVE, "tensor_scalar", [ex, sm], [ex], out=ex[0:m, :], in0=ex[0:m, :], scalar1=sm[0:m, 0:1], scalar2=None, op0=ALU.mult)
                    pt = Bk.f[4]
                    P.I(PE, "transpose", [ex, id_f], [pt], pt[0:NEXP, 0:m], ex[0:m, :], id_f[0:m, 0:m])
                    P.I(ACT, "activation", [pt], [GT], out=GT[:, c0 + s0:c0 + s0 + m], in_=pt[0:NEXP, 0:m], func=AF.Copy)
                    s0 += m
        P.dma(SP, gt_dram[hf], GT[:, :], reads=[GT], writes=[("gtd", hf)])
        p3 = P.scope(); p3.__enter__()
        yT = P.sb([128, 8, TH], F32, name=f"yT{hf}")
        actT = P.sb([128, 8, TH], BF16, name=f"actT{hf}")
        gu_ring = [P.sb([128, 8, 512], BF16, name=f"gu{hf}_{i}") for i in range(3)]
        dn_ring = [P.sb([128, 8, 256], BF16, name=f"dn{hf}_{i}") for i in range(3)]
        stage = [P.sb([128, 8, 256], F32, name=f"stg{hf}_{i}") for i in range(3)]
        gbs = [P.sb([128, TH], F32, name=f"gb{hf}_{i}") for i in range(2)]
        g1 = [P.sb([128, 512], F32, name=f"g1{hf}_{i}") for i in range(2)]
        tt = [P.sb([128, 512], F32, name=f"tt{hf}_{i}") for i in range(2)]
        u1 = [P.sb([128, 512], F32, name=f"u1{hf}_{i}") for i in range(2)]
        for e in range(NEXP):
            gb_sb = gbs[e % 2]
            P.dma(SP, gb_sb[:, :], gt_dram[hf, e:e + 1, :].partition_broadcast(128), reads=[("gtd", hf)], writes=[gb_sb])
            for q in range(4):
                wb = gu_ring[gu_cnt[0] % 3]; gu_cnt[0] += 1
                for part in range(2):
                    sg_ = stage[st_cnt[0] % 3]; st_cnt[0] += 1
                    P.dma(SP, sg_[:], w_gu[e, q, part].rearrange("p (k n) -> p k n", n=256), writes=[sg_])
                    if st_cnt[0] % 2:
                        P.I(POOL, "tensor_copy", [sg_], [wb], out=wb[:, :, part * 256:(part + 1) * 256], in_=sg_[:])
                    else:
                        P.I(ACT, "activation", [sg_], [wb], out=wb[:, :, part * 256:(part + 1) * 256], in_=sg_[:], func=AF.Copy)
                for o2 in range(2):
                    oc = q * 2 + o2
                    for (c0, n, j) in tiles:
                        i = ch_cnt[0] % 2; ch_cnt[0] += 1
                        pgt, put = Bk.f[i], Bk.f[2 + i]
                        for kc in range(8):
                            P.I(PE, "matmul", [wb, hT], [pgt], pgt[:, 0:n], wb[:, kc, o2 * 128:(o2 + 1) * 128], hT[:, kc, c0:c0 + n], start=(kc == 0), stop=(kc == 7))
                        for kc in range(8):
                            P.I(PE, "matmul", [wb, hT], [put], put[:, 0:n], wb[:, kc, 256 + o2 * 128:256 + (o2 + 1) * 128], hT[:, kc, c0:c0 + n], start=(kc == 0), stop=(kc == 7))
                        P.I(DVE, "tensor_scalar", [pgt, bgu_sb], [g1[i]], out=g1[i][:, 0:n], in0=pgt[:, 0:n], scalar1=bgu_sb[:, e, oc:oc + 1], scalar2=7.0, op0=ALU.add, op1=ALU.min)
                        P.I(ACT, "activation", [g1[i]], [tt[i]], out=tt[i][:, 0:n], in_=g1[i][:, 0:n], func=AF.Silu, scale=1.702)
                        P.I(DVE, "tensor_scalar", [put, bgu_sb], [u1[i]], out=u1[i][:, 0:n], in0=put[:, 0:n], scalar1=bgu_sb[:, e, 8 + oc:8 + oc + 1], scalar2=7.0, op0=ALU.add, op1=ALU.min)
                        P.I(POOL, "tensor_scalar", [u1[i]], [u1[i]], out=u1[i][:, 0:n], in0=u1[i][:, 0:n], scalar1=-7.0, scalar2=1.0, op0=ALU.max, op1=ALU.add)
                        P.I(POOL, "tensor_tensor", [u1[i], gb_sb], [u1[i]], out=u1[i][:, 0:n], in0=u1[i][:, 0:n], in1=gb_sb[:, c0:c0 + n], op=ALU.mult)
                        if pend[0] is not None:
                            pend[0]()
                        pend[0] = (lambda i=i, oc=oc, c0=c0, n=n: P.I(DVE, "scalar_tensor_tensor", [tt[i], u1[i]], [actT], out=actT[:, oc, c0:c0 + n], in0=tt[i][:, 0:n],
                                                                      scalar=1.0 / 1.702, in1=u1[i][:, 0:n], op0=ALU.mult, op1=ALU.mult))
            if pend[0] is not None:
                pend[0](); pend[0] = None
            for q in range(4):
                wd = dn_ring[dn_cnt[0] % 3]; dn_cnt[0] += 1
                sg_ = stage[st_cnt[0] % 3]; st_cnt[0] += 1
                P.dma(SP, sg_[:], w_dn[e, q].rearrange("p (k n) -> p k n", n=256), writes=[sg_])
                if st_cnt[0] % 2:
                    P.I(POOL, "tensor_copy", [sg_], [wd], out=wd[:], in_=sg_[:])
                else:
                    P.I(ACT, "activation", [sg_], [wd], out=wd[:], in_=sg_[:], func=AF.Copy)
                for o2 in range(2):
                    dc = q * 2 + o2
                    for (c0, n, j) in tiles:
                        i = ch_cnt[0] % 2; ch_cnt[0] += 1
                        pd = Bk.f[4 + i]
                        for kc in range(8):
                            P.I(PE, "matmul", [wd, actT], [pd], pd[:, 0:n], wd[:, kc, o2 * 128:(o2 + 1) * 128], actT[:, kc, c0:c0 + n], start=(kc == 0), stop=(kc == 7))
                        if e == 0:
                            P.I(DVE, "tensor_copy", [pd], [yT], out=yT[:, dc, c0:c0 + n], in_=pd[:, 0:n])
                        else:
                            P.I(DVE, "tensor_tensor", [pd, yT], [yT], out=yT[:, dc, c0:c0 + n], in0=pd[:, 0:n], in1=yT[:, dc, c0:c0 + n], op=ALU.add)
        for (c0, n, j) in tiles:
            for dc in range(8):
                pd = Bk.f[4 + dc % 2]
                P.I(PE, "matmul", [bdn_sb, GT], [pd], pd[:, 0:n], bdn_sb[:, dc * 128:(dc + 1) * 128], GT[:, c0:c0 + n], start=True, stop=True)
                P.I(DVE, "tensor_tensor", [pd, yT], [yT], out=yT[:, dc, c0:c0 + n], in0=pd[:, 0:n], in1=yT[:, dc, c0:c0 + n], op=ALU.add)
                P.I(DVE, "scalar_tensor_tensor", [yT, G1[j], x1], [x1], out=x1[:, dc, c0:c0 + n], in0=yT[:, dc, c0:c0 + n], scalar=G1[j][:, dc:dc + 1],
                    in1=x1[:, dc, c0:c0 + n], op0=ALU.mult, op1=ALU.add)
        for kc in range(8):
            outs.append(P.dma(SP if kc % 2 == 0 else ACT, outT[hf, kc * 128:(kc + 1) * 128, :], x1[:, kc, :], reads=[x1]))
        p3.__exit__(None, None, None)
    return P.finish(outs)


def prep_C(inp, layer, mT_full, xT_full, mcT=None, xcT=None):
    passes = moe_passes(NC_MOE, mcT is not None)
    nlp = (SEQ // NC_MOE) // 1024
    ncx = NCTX // NC_MOE
    aw = inp['ada_w'][layer]; ab = inp['ada_b'][layer]
    adaw = np.ascontiguousarray(np.concatenate([aw[:, 2048:3072], aw[:, 3072:6144]], axis=1))
    adab = col_layout(np.concatenate([ab[2048:3072], ab[3072:6144]]), 32)
    cvec = np.stack([col_layout(inp['c'][0], 8), col_layout(inp['c_ctx'], 8)], axis=2).reshape(128, 16)
    bgu = inp['moe_b_gu'][layer]
    bgu_l = np.ascontiguousarray(bgu.reshape(NEXP, 16, 128).transpose(2, 0, 1).reshape(128, NEXP * 16))
    common = dict(
        cvec=np.ascontiguousarray(cvec), adaw=adaw, adab=adab, normg=col_layout(inp['norm_g'][layer, 1], 8),
        w_out=np.ascontiguousarray(inp['w_out'][layer]),
        rw=np.ascontiguousarray(inp['router_w'][layer].reshape(8, 128, NEXP).transpose(1, 0, 2).reshape(128, 8 * NEXP)),
        rb=np.ascontiguousarray(np.broadcast_to(inp['router_b'][layer][None, :], (128, NEXP))).astype(np.float32),
        w_gu=np.ascontiguousarray(inp['moe_w_gu'][layer].reshape(NEXP, 8, 128, 2, 4, 256).transpose(0, 4, 3, 2, 1, 5)).reshape(NEXP, 4, 2, 128, 2048),
        bgu=bgu_l,
        w_dn=np.ascontiguousarray(inp['moe_w_dn'][layer].reshape(NEXP, 8, 128, 4, 256).transpose(0, 3, 2, 1, 4)).reshape(NEXP, 4, 128, 2048),
        bdn=np.ascontiguousarray(inp['moe_b_dn'][layer]),
        ident=np.eye(128, dtype=np.float32), onesd=np.ones((128, 128), np.float32))
    maps = []
    for c in range(NC_MOE):
        ms = np.zeros((len(passes), D, 1024), np.float32); xs = np.zeros((len(passes), D, 1024), np.float32)
        for hf in range(nlp):
            t0 = (c * nlp + hf) * 1024
            ms[hf] = mT_full[:, t0:t0 + 1024]; xs[hf] = xT_full[:, t0:t0 + 1024]
        if mcT is not None:
            ms[nlp, :, 0:ncx] = mcT[:, c * ncx:(c + 1) * ncx]; xs[nlp, :, 0:ncx] = xcT[:, c * ncx:(c + 1) * ncx]
        d = dict(common); d.update(mT=ms, xT=xs)
        maps.append(d)
    return blobify(maps, BLOB_C), passes


def gather_C(results, has_ctx):
    nlp = (SEQ // NC_MOE) // 1024
    ncx = NCTX // NC_MOE
    xT = np.zeros((D, SEQ), np.float32); xcT = np.zeros((D, NCTX), np.float32) if has_ctx else None
    for c in range(NC_MOE):
        o = results[c]['outT']
        for hf in range(nlp):
            t0 = (c * nlp + hf) * 1024
            xT[:, t0:t0 + 1024] = o[hf]
        if has_ctx:
            xcT[:, c * ncx:(c + 1) * ncx] = o[nlp][:, 0:ncx]
    return xT, xcT


PI = float(np.pi)


def build_B(L):
    NBK = L // 128
    NK = 2 * NBK - 1
    HROW = 2 * L
    P = Prog()
    Bk = Banks(P)
    di = lambda n, s, dt=F32: P.dram(n, s, dt, "ExternalInput")
    vB = di("vB", [128, 64, NBK]); x1B = di("x1B", [128, 64, NBK]); x2B = di("x2B", [128, 64, NBK])
    zt = di("zt", [2, 33, L]); tn = di("tn", [2, 1, L])
    bl = Blob(BLOB_B).dram(P)
    w1 = bl.ap("w1"); w2 = bl.ap("w2"); w3 = bl.ap("w3"); w4s = bl.ap("w4s")
    fqb = bl.ap("fqb")
    negd = bl.ap("negd"); fbias = bl.ap("fbias")
    ident = bl.ap("ident"); onesd = bl.ap("onesd"); jmat = bl.ap("jmat")
    hyB = P.dram("hyB", [128, 64, NBK], F32, "ExternalOutput")
    Hd = P.dram("Hd_scratch", [128, HROW], BF16, "Internal")

    ones_sb = P.sb([128, 128], F32, name="ones"); P.dma(SP, ones_sb[:], onesd[:, :], writes=[ones_sb])
    id_f = P.sb([128, 128], F32, name="idf"); P.dma(SP, id_f[:], ident[:, :], writes=[id_f])
    j_b = P.sb([128, 128], BF16, name="jb"); P.dma(POOL, j_b[:], jmat[:, :], writes=[j_b])
    w1_sb = P.sb([33, 64], F32, name="w1"); P.dma(SP, w1_sb[:], w1[:, :], writes=[w1_sb])
    w2_sb = P.sb([64, 64], F32, name="w2"); P.dma(SP, w2_sb[:], w2[:, :], writes=[w2_sb])
    w3_sb = P.sb([64, 64], F32, name="w3"); P.dma(SP, w3_sb[:], w3[:, :], writes=[w3_sb])
    w4_sb = P.sb([64, 2, 128], F32, name="w4"); P.dma(SP, w4_sb[:], w4s.rearrange("k (s m) -> k s m", s=2), writes=[w4_sb])
    fq_sb = P.sb([64, 4], F32, name="fq"); P.dma(SP, fq_sb[:], fqb[:, :], writes=[fq_sb])
    fb_sb = P.sb([64, 3], F32, name="fqbias")
    P.I(DVE, "tensor_tensor", [fq_sb], [fb_sb], out=fb_sb[:], in0=fq_sb[:, 1:4], in1=fq_sb[:, 0:1].broadcast_to([64, 3]), op=ALU.mult)
    negd_sb = P.sb([128, 1], F32, name="negd"); P.dma(SP, negd_sb[:], negd[:, :], writes=[negd_sb], allow_slow_non_contiguous=True)
    fbias_sb = P.sb([128, 128], F32, name="fbias"); P.dma(SP, fbias_sb[:], fbias[:, :], writes=[fbias_sb])

    CH = min(512, L)
    NCH = L // CH
    abss = P.sb([128, 2 * NCH], F32, name="abss")
    with P.scope():
        z_sb = [P.sb([33, CH], F32, name=f"z{i}") for i in range(2)]
        tn_sb = [P.sb([128, CH], F32, name=f"tn{i}") for i in range(2)]
        a_sb = P.sb([64, CH], F32, name="a_sb"); t_sb = P.sb([64, CH], F32, name="t_sb")
        hd_sb = P.sb([64, CH], F32, name="hd_sb")
        dec_sb = P.sb([128, CH], F32, name="dec_sb"); hf_sb = P.sb([128, CH], F32, name="hf_sb")
        hb_sb = [P.sb([128, CH], BF16, name=f"hb{i}") for i in range(2)]
        it = 0
        for side in range(2):
            for c in range(NCH):
                zb = z_sb[it % 2]; tb = tn_sb[it % 2]; hb = hb_sb[it % 2]; it += 1
                P.dma(SP, zb[:], zt[side, :, c * CH:(c + 1) * CH], writes=[zb])
                P.dma(ACT, tb[:], tn[side, :, c * CH:(c + 1) * CH].partition_broadcast(128), writes=[tb])
                src, Kd, wl = zb, 33, [w1_sb, w2_sb, w3_sb]
                for l in range(3):
                    ps = Bk.f[l % 2]
                    P.I(PE, "matmul", [wl[l], src], [ps], ps[0:64, 0:CH], wl[l][:], src[0:Kd, :], start=True, stop=True)
                    P.I(DVE, "tensor_scalar", [ps, fq_sb, fb_sb], [a_sb], out=a_sb[:], in0=ps[0:64, 0:CH], scalar1=fq_sb[:, 0:1], scalar2=fb_sb[:, l:l + 1], op0=ALU.mult, op1=ALU.add)
                    for rep in range(2):
                        P.I(POOL, "tensor_scalar", [a_sb], [t_sb], out=t_sb[:], in0=a_sb[:], scalar1=PI, scalar2=-2 * PI, op0=ALU.is_gt, op1=ALU.mult)
                        P.I(DVE, "tensor_tensor", [a_sb, t_sb], [hd_sb], out=hd_sb[:], in0=a_sb[:], in1=t_sb[:], op=ALU.add)
                        P.I(POOL, "tensor_scalar", [a_sb], [t_sb], out=t_sb[:], in0=a_sb[:], scalar1=-PI, scalar2=2 * PI, op0=ALU.is_lt, op1=ALU.mult)
                        P.I(DVE, "tensor_tensor", [hd_sb, t_sb], [a_sb], out=a_sb[:], in0=hd_sb[:], in1=t_sb[:], op=ALU.add)
                    P.I(ACT, "activation", [a_sb], [hd_sb], out=hd_sb[:], in_=a_sb[:], func=AF.Sin)
                    src, Kd = hd_sb, 64
                p4 = Bk.f[2]
                P.I(PE, "matmul", [w4_sb, hd_sb], [p4], p4[:, 0:CH], w4_sb[:, side, :], hd_sb[:], start=True, stop=True)
                P.I(ACT, "activation", [tb, negd_sb], [dec_sb], out=dec_sb[:], in_=tb[:], func=AF.Exp, scale=negd_sb[:, 0:1])
                P.I(DVE, "tensor_tensor", [p4, dec_sb], [hf_sb], out=hf_sb[:], in0=p4[:, 0:CH], in1=dec_sb[:], op=ALU.mult)
                if side == 1 and c == NCH - 1:
                    P.I(DVE, "memset", [], [hf_sb], hf_sb[:, CH - 1:CH], 0.0)
                P.I(DVE, "tensor_reduce", [hf_sb], [abss], out=abss[:, side * NCH + c:side * NCH + c + 1], in_=hf_sb[:], axis=AX.X, op=ALU.add, apply_absolute_value=True)
                P.I(ACT, "activation", [hf_sb], [hb], out=hb[:], in_=hf_sb[:], func=AF.Copy)
                if side == 1:
                    n = CH - 1 if c == NCH - 1 else CH
                    P.dma(POOL, Hd[:, c * CH:c * CH + n], hb[:, 0:n], reads=[hb], writes=["Hd"])
                else:
                    P.dma(POOL, Hd[:, L - 1 + c * CH:L - 1 + (c + 1) * CH], hb[:], reads=[hb], writes=["Hd"])
        zpad = P.sb([128, 1], BF16, name="zpad"); P.I(DVE, "memset", [], [zpad], zpad[:], 0.0)
        P.dma(POOL, Hd[:, 2 * L - 1:2 * L], zpad[:], reads=[zpad], writes=["Hd"], allow_slow_non_contiguous=True)
    rn = P.sb([128, 1], F32, name="rn"); rnb = P.sb([128, 128], F32, name="rnb"); dg = P.sb([128, 128], F32, name="dg")
    P.I(DVE, "reduce_sum", [abss], [rn], out=rn[:], in_=abss[:], axis=AX.X)
    P.I(DVE, "reciprocal", [rn], [rn], out=rn[:], in_=rn[:])
    P.I(DVE, "tensor_scalar", [id_f, rn], [dg], out=dg[:], in0=id_f[:], scalar1=rn[:, 0:1], scalar2=None, op0=ALU.mult)
    pr = Bk.f[0]
    P.I(PE, "matmul", [ones_sb, dg], [pr], pr[:, 0:128], ones_sb[:], dg[:], start=True, stop=True)
    P.I(ACT, "activation", [pr], [rnb], out=rnb[:], in_=pr[:, 0:128], func=AF.Copy)

    v_sb = P.sb([128, 64, NBK], F32, name="v_sb"); x1_sb = P.sb([128, 64, NBK], F32, name="x1_sb"); x2_sb = P.sb([128, 64, NBK], F32, name="x2_sb")
    P.dma(SP, v_sb[:], vB[:, :, :], writes=[v_sb]); P.dma(ACT, x1_sb[:], x1B[:, :, :], writes=[x1_sb]); P.dma(SP, x2_sb[:], x2B[:, :, :], writes=[x2_sb])
    zb16 = P.sb([128, 64 * NBK], BF16, name="zb16")
    zrev = P.sb([128, 64, NBK], BF16, name="zrev")
    z1_sb = P.sb([128, 64, NBK], F32, name="z1_sb")
    t0_sb = [P.sb([128, NBK], F32, name=f"t0_{i}") for i in range(2)]
    t1_sb = [P.sb([128, NBK], F32, name=f"t1_{i}") for i in range(2)]
    KP = min(51, NK)
    NPIECE = (NK + KP - 1) // KP
    hs_ring = [P.sb([128, KP * 128], BF16, name=f"hs{i}") for i in range(3)]
    outs = []
    hs_cnt = [0]

    def make_zrev(src_f32):
        flat = src_f32[:].rearrange("p c j -> p (c j)")
        P.I(ACT, "activation", [src_f32], [zb16], out=zb16[:], in_=flat, func=AF.Copy)
        tot = 64 * NBK
        c0 = 0
        zr_flat = zrev[:].rearrange("p c j -> p (c j)")
        i = 0
        while c0 < tot:
            n = min(512, tot - c0)
            pz = Bk.f[4 + i % 2]; i += 1
            P.I(PE, "matmul", [j_b, zb16], [pz], pz[:, 0:n], j_b[:], zb16[:, c0:c0 + n], start=True, stop=True)
            P.I(ACT if i % 2 else DVE, "activation" if i % 2 else "tensor_copy", [pz], [zrev],
                **(dict(out=zr_flat[:, c0:c0 + n], in_=pz[:, 0:n], func=AF.Copy) if i % 2 else dict(out=zr_flat[:, c0:c0 + n], in_=pz[:, 0:n])))
            c0 += n

    mid = (NK // 2) // KP
    piece_order = [mid] + [p for p in range(NPIECE) if p != mid]

    def conv(o, zin_f32, gate_sb, dst_sb):
        for ch in range(64):
            row = o * 64 + ch
            py = Bk.f[ch % 4][:, 0:NBK] if False else Bk.f[ch % 2]
            first = True
            for pi in piece_order:
                kk0 = pi * KP; kk1 = min(NK, kk0 + KP)
                hs = hs_ring[hs_cnt[0] % 3]; hs_cnt[0] += 1
                ncol = (kk1 - kk0) * 128
                src = bass.AP(Hd.tensor, row * HROW + kk0 * 128, [[1, 128], [1, ncol]])
                P.dma(SP if hs_cnt[0] % 2 else ACT, hs[:, 0:ncol], src, reads=["Hd"], writes=[hs])
                ks = list(range(kk0, kk1))
                if pi == mid:
                    ks.remove(NBK - 1); ks = [NBK - 1] + ks
                for kk in ks:
                    k = kk - (NBK - 1)
                    a_lo = max(0, k); a_hi = min(NBK - 1, NBK - 1 + k)
                    last = (pi == piece_order[-1] and kk == ks[-1])
                    P.I(PE, "matmul", [hs, zrev], [py], py[:, a_lo:a_hi + 1], hs[:, (kk - kk0) * 128:(kk - kk0 + 1) * 128], zrev[:, ch, a_lo - k:a_hi - k + 1],
                        start=first, stop=last)
                    first = False
            i = ch % 2
            P.I(POOL, "tensor_scalar", [zin_f32, fbias_sb], [t0_sb[i]], out=t0_sb[i][:], in0=zin_f32[:, ch, :], scalar1=fbias_sb[:, row:row + 1], scalar2=None, op0=ALU.mult)
            P.I(DVE, "scalar_tensor_tensor", [py, rnb, t0_sb[i]], [t1_sb[i]], out=t1_sb[i][:], in0=py[:, 0:NBK], scalar=rnb[:, row:row + 1], in1=t0_sb[i][:], op0=ALU.mult, op1=ALU.add)
            P.I(POOL, "tensor_tensor", [t1_sb[i], gate_sb], [dst_sb], out=dst_sb[:, ch, :], in0=t1_sb[i][:], in1=gate_sb[:, ch, :], op=ALU.mult)

    make_zrev(v_sb)
    conv(0, v_sb, x1_sb, z1_sb)
    make_zrev(z1_sb)
    conv(1, z1_sb, x2_sb, v_sb)
    outs.append(P.dma(SP, hyB[:, :, :], v_sb[:], reads=[v_sb]))
    return P.finish(outs)


def hyena_tables(L):
    t = np.linspace(0.0, 1.0, L, dtype=np.float32)[:, None]
    bands = 16
    w_ang = (2.0 * np.pi * np.arange(L, dtype=np.float32)[:, None] / L).astype(np.float32)
    fr = np.linspace(1e-4, bands - 1, bands, dtype=np.float32)[None]
    z = np.concatenate([t, np.cos(fr * w_ang), -np.sin(fr * w_ang)], axis=-1).astype(np.float32)
    zt = np.stack([z.T, z[::-1].T]).astype(np.float32)
    tn = np.stack([t.T, t[::-1].T]).astype(np.float32)
    return np.ascontiguousarray(zt), np.ascontiguousarray(tn)


def prep_B(inp, uT_full, L):
    NBK = L // 128
    zt, tn = hyena_tables(L)
    max_decay = np.log(1e-2) / 0.3; min_decay = np.log(1e-2) / 1.5
    deltas = np.abs(np.linspace(min_decay, max_decay, 512, dtype=np.float32)).astype(np.float32)
    w4 = inp['hy_w4'][0].reshape(64, 2, 2, 512)
    fqb = np.stack([inp['hy_freq'][0], inp['hy_b1'][0], inp['hy_b2'][0], inp['hy_b3'][0]], axis=1).astype(np.float32)
    jm = np.eye(128, dtype=np.float32)[::-1].copy()
    maps = []
    for c in range(NCORE):
        chs = slice(64 * c, 64 * c + 64)

        def blk(rows):
            return np.ascontiguousarray(rows.reshape(64, NBK, 128).transpose(2, 0, 1))
        w4s = np.concatenate([w4[:, :, s, chs].reshape(64, 128) for s in range(2)], axis=1)
        fb = inp['hy_filter_bias'][0][:, chs].reshape(128)
        maps.append(dict(
            vB=blk(uT_full[0:512][chs]), x1B=blk(uT_full[512:1024][chs]), x2B=blk(uT_full[1024:1536][chs]),
            zt=zt, tn=tn, w1=np.ascontiguousarray(inp['hy_w1'][0]), w2=np.ascontiguousarray(inp['hy_w2'][0]),
            w3=np.ascontiguousarray(inp['hy_w3'][0]), w4s=np.ascontiguousarray(w4s), fqb=np.ascontiguousarray(fqb),
            negd=np.ascontiguousarray(-np.tile(deltas[chs], 2)[:, None]).astype(np.float32),
            fbias=np.ascontiguousarray(np.broadcast_to(fb[None, :], (128, 128))).astype(np.float32),
            ident=np.eye(128, dtype=np.float32), onesd=np.ones((128, 128), np.float32), jmat=jm))
    return blobify(maps, BLOB_B)


def gather_B(results, L):
    NBK = L // 128
    hyT = np.zeros((512, L), np.float32)
    for c in range(NCORE):
        hb = results[c]['hyB']
        hyT[64 * c:64 * c + 64] = hb.transpose(1, 2, 0).reshape(64, L)
    return hyT


DROWS = 5136


def build_D():
    P = Prog()
    Bk = Banks(P)
    di = lambda n, s, dt=F32: P.dram(n, s, dt, "ExternalInput")
    xT = di("xT", [D, TEXT]); ctxT = di("ctxT", [D, NCTX])
    bl = Blob(BLOB_D).dram(P)
    cvec = bl.ap("cvec"); adaw = di("adaw", [D, 3072]); adab = bl.ap("adab")
    normg = bl.ap("normg"); w_in = di("w_in", [D, 4112])
    onesd = bl.ap("onesd")
    convw = bl.ap("convw"); convb = bl.ap("convb"); edge = bl.ap("edge")
    hglb = bl.ap("hglb")
    dtb = bl.ap("dtb")
    outT = P.dram("outT", [DROWS, TLOC], F32, "ExternalOutput"); outcT = P.dram("outcT", [DROWS, NCTX], F32, "ExternalOutput")

    ones_sb = P.sb([128, 128], F32, name="ones"); P.dma(SP, ones_sb[:], onesd[:, :], writes=[ones_sb])
    cv = P.sb([128, 8, 2], F32, name="cv"); P.dma(SP, cv[:], cvec.rearrange("p (k n) -> p k n", n=2), writes=[cv])
    adab_sb = P.sb([128, 24], F32, name="adab"); P.dma(SP, adab_sb[:], adab[:, :], writes=[adab_sb])
    g_sb = P.sb([128, 8], F32, name="normg"); P.dma(SP, g_sb[:], normg[:, :], writes=[g_sb])
    cw_sb = P.sb([128, 8, 3], F32, name="cw"); P.dma(SP, cw_sb[:], convw.rearrange("p (o t) -> p o t", t=3), writes=[cw_sb])
    cb_sb = P.sb([128, 8], F32, name="cb"); P.dma(SP, cb_sb[:], convb[:, :], writes=[cb_sb])
    edge_sb = P.sb([128, 2], F32, name="edge"); P.dma(SP, edge_sb[:], edge[:, :], writes=[edge_sb])
    lb_sb = P.sb([128, 8], F32, name="hglb"); P.dma(SP, lb_sb[:], hglb[:, :], writes=[lb_sb])
    dtb_sb = P.sb([16, 1], F32, name="dtb"); P.dma(SP, dtb_sb[:], dtb[:, :], writes=[dtb_sb], allow_slow_non_contiguous=True)
    lbc = P.sb([128, 4], F32, name="lbc"); oml = P.sb([128, 4], F32, name="oml")
    P.I(DVE, "tensor_tensor", [lb_sb], [lbc], out=lbc[:], in0=lb_sb[:, 4:8], in1=lb_sb[:, 0:4], op=ALU.subtract)
    P.I(ACT, "activation", [lbc], [lbc], out=lbc[:], in_=lbc[:], func=AF.Sigmoid)
    P.I(DVE, "tensor_scalar", [lbc], [oml], out=oml[:], in0=lbc[:], scalar1=-1.0, scalar2=1.0, op0=ALU.mult, op1=ALU.add)
    w_sb = P.sb([128, 8, 4112], BF16, name="w_in")
    for kc in range(8):
        P.dma(POOL, w_sb[:, kc, :], w_in[kc * 128:(kc + 1) * 128, :], writes=[w_sb])
    ada = P.sb([128, 24, 2], F32, name="ada")
    with P.scope():
        wbuf = P.sb([128, 8, 1024], F32, name="adawbuf")
        emit_adaln(P, Bk, cv, adaw, adab_sb, wbuf, ada, 2)
    Acol = [P.sb([128, 8], F32, name=f"Acol{j}") for j in range(2)]
    Bcol = [P.sb([128, 8], F32, name=f"Bcol{j}") for j in range(2)]
    for j in range(2):
        P.I(DVE, "scalar_tensor_tensor", [ada, g_sb], [Acol[j]], out=Acol[j][:], in0=ada[:, 8:16, j], scalar=1.0, in1=g_sb[:], op0=ALU.add, op1=ALU.mult)
        P.I(DVE, "tensor_copy", [ada], [Bcol[j]], out=Bcol[j][:], in_=ada[:, 0:8, j])
    h_all = P.sb([128, 8, TEXT + NCTX], BF16, name="h_all")
    x_sb = P.sb([128, 8, TT], F32, name="x_sb"); sq_sb = P.sb([128, 8, TT], F32, name="sq_sb"); rs_sb = P.sb([128, TT], F32, name="rs_sb")
    for t in range(NTT):
        for kc in range(8):
            P.dma(SP if kc % 2 == 0 else ACT, x_sb[:, kc, :], xT[kc * 128:(kc + 1) * 128, t * TT:(t + 1) * TT], writes=[x_sb])
        emit_norm_mod(P, Bk, x_sb, ones_sb, Acol[0], Bcol[0], h_all, TT, sq_sb, rs_sb, hcol0=t * TT)
    for kc in range(8):
        P.dma(SP if kc % 2 == 0 else ACT, x_sb[:, kc, 0:NCTX], ctxT[kc * 128:(kc + 1) * 128, :], writes=[x_sb])
    emit_norm_mod(P, Bk, x_sb, ones_sb, Acol[1], Bcol[1], h_all, NCTX, sq_sb, rs_sb, hcol0=TEXT)

    u_sb = [P.sb([128, 512], F32, name=f"u_sb{i}") for i in range(2)]
    a1_sb = [P.sb([128, 512], F32, name=f"a1_sb{i}") for i in range(2)]
    a2_sb = [P.sb([128, 512], F32, name=f"a2_sb{i}") for i in range(2)]
    outs = []

    def tile(h0, n, out_dram, ocol0, zl, zr, el, er):
        a0 = h0 - (0 if zl else 1); a1 = h0 + n + (0 if zr else 1)
        w = a1 - a0
        off = 1 if zl else 0
        for oc in range(33):
            M = 128 if oc < 32 else 16
            i = oc % 2
            pu = Bk.f[2 + i]; ub = u_sb[i]; r1 = a1_sb[i]; r2 = a2_sb[i]
            for kc in range(8):
                P.I(PE, "matmul", [w_sb, h_all], [pu], pu[0:M, off:off + w], w_sb[:, kc, oc * 128:oc * 128 + M], h_all[:, kc, a0:a1], start=(kc == 0), stop=(kc == 7))
            ctr = pu[0:M, 1:n + 1]
            dq = SP if oc % 2 == 0 else POOL
            if oc < 8:
                if zl:
                    P.I(POOL, "memset", [], [ub], ub[:, 0:1], 0.0)
                if zr:
                    P.I(POOL, "memset", [], [ub], ub[:, n + 1:n + 2], 0.0)
                P.I(ACT, "activation", [pu], [ub], out=ub[:, off:off + w], in_=pu[:, off:off + w], func=AF.Copy)
                if el:
                    P.I(DVE, "tensor_scalar", [ub, edge_sb], [ub], out=ub[:, 0:1], in0=ub[:, 0:1], scalar1=edge_sb[:, 0:1], scalar2=None, op0=ALU.mult)
                if er:
                    P.I(DVE, "tensor_scalar", [ub, edge_sb], [ub], out=ub[:, n + 1:n + 2], in0=ub[:, n + 1:n + 2], scalar1=edge_sb[:, 1:2], scalar2=None, op0=ALU.mult)
                P.I(DVE, "tensor_scalar", [ub, cw_sb, cb_sb], [r1], out=r1[:, 0:n], in0=ub[:, 1:n + 1], scalar1=cw_sb[:, oc, 1:2], scalar2=cb_sb[:, oc:oc + 1], op0=ALU.mult, op1=ALU.add)
                P.I(DVE, "scalar_tensor_tensor", [ub, cw_sb, r1], [r1], out=r1[:, 0:n], in0=ub[:, 0:n], scalar=cw_sb[:, oc, 0:1], in1=r1[:, 0:n], op0=ALU.mult, op1=ALU.add)
                P.I(DVE, "scalar_tensor_tensor", [ub, cw_sb, r1], [r1], out=r1[:, 0:n], in0=ub[:, 2:n + 2], scalar=cw_sb[:, oc, 2:3], in1=r1[:, 0:n], op0=ALU.mult, op1=ALU.add)
                P.I(ACT, "activation", [r1], [r2], out=r2[:, 0:n], in_=r1[:, 0:n], func=AF.Silu)
                outs.append(P.dma(dq, out_dram[oc * 128:(oc + 1) * 128, ocol0:ocol0 + n], r2[:, 0:n], reads=[r2]))
            elif oc < 16:
                hd = (oc - 8) % 4
                P.I(ACT, "activation", [pu], [r1], out=r1[:, 0:n], in_=ctr, func=AF.Sigmoid)
                P.I(DVE, "tensor_scalar", [r1, oml, lbc], [r1], out=r1[:, 0:n], in0=r1[:, 0:n], scalar1=oml[:, hd:hd + 1], scalar2=lbc[:, hd:hd + 1], op0=ALU.mult, op1=ALU.add)
                P.I(DVE, "tensor_scalar", [r1], [r2], out=r2[:, 0:n], in0=r1[:, 0:n], scalar1=-1.0, scalar2=1.0, op0=ALU.mult, op1=ALU.add)
                outs.append(P.dma(dq, out_dram[1024 + (oc - 8) * 128:1024 + (oc - 7) * 128, ocol0:ocol0 + n], r2[:, 0:n], reads=[r2]))
                P.I(ACT, "activation", [r1], [ub], out=ub[:, 0:n], in_=r1[:, 0:n], func=AF.Ln)
                outs.append(P.dma(dq, out_dram[2048 + (oc - 8) * 128:2048 + (oc - 7) * 128, ocol0:ocol0 + n], ub[:, 0:n], reads=[ub]))
            elif oc < 20:
                P.I(ACT, "activation", [pu], [r1], out=r1[:, 0:n], in_=ctr, func=AF.Copy)
                outs.append(P.dma(dq, out_dram[3072 + (oc - 16) * 128:3072 + (oc - 15) * 128, ocol0:ocol0 + n], r1[:, 0:n], reads=[r1]))
            elif oc < 32:
                P.I(ACT, "activation", [pu], [r1], out=r1[:, 0:n], in_=ctr, func=AF.Silu)
                outs.append(P.dma(dq, out_dram[3584 + (oc - 20) * 128:3584 + (oc - 19) * 128, ocol0:ocol0 + n], r1[:, 0:n], reads=[r1]))
            else:
                P.I(ACT, "activation", [pu, dtb_sb], [r1], out=r1[0:16, 0:n], in_=pu[0:16, 1:n + 1], func=AF.Exp, bias=dtb_sb[:, 0:1], scale=1.0)
                P.I(ACT, "activation", [r1], [r2], out=r2[0:16, 0:n], in_=r1[0:16, 0:n], func=AF.Ln, bias=1.0, scale=1.0)
                outs.append(P.dma(dq, out_dram[5120:5136, ocol0:ocol0 + n], r2[0:16, 0:n], reads=[r2]))

    lo = 0
    while lo < TLOC:
        n = min(510, TLOC - lo)
        tile(HALO + lo, n, outT, lo, False, False, lo == 0, lo + n == TLOC)
        lo += n
    tile(TEXT, NCTX, outcT, 0, True, True, False, False)
    return P.finish(outs)


def prep_D(inp, xT_full, xcT):
    layer = 1
    xTp = np.concatenate([np.zeros((D, HALO), np.float32), xT_full, np.zeros((D, HALO), np.float32)], axis=1)
    w = inp['w_in_odd'][0]
    w_perm = np.ascontiguousarray(np.concatenate([w[:, 0:1024], w[:, 1040:2064], w[:, 2064:2576], w[:, 2576:3088], w[:, 3088:3600],
                                                   w[:, 3600:4112], w[:, 1024:1040]], axis=1))
    cvec = np.stack([col_layout(inp['c'][0], 8), col_layout(inp['c_ctx'], 8)], axis=2).reshape(128, 16)
    hl = inp['hg_lower_bounds']
    common = dict(
        ctxT=np.ascontiguousarray(xcT), cvec=np.ascontiguousarray(cvec),
        adaw=np.ascontiguousarray(inp['ada_w'][layer][:, 0:3072]), adab=col_layout(inp['ada_b'][layer][0:3072], 24),
        normg=col_layout(inp['norm_g'][layer, 0], 8), w_in=w_perm, onesd=np.ones((128, 128), np.float32),
        convw=np.ascontiguousarray(inp['ssd_conv_w'][0].reshape(3, 8, 128).transpose(2, 1, 0).reshape(128, 24)),
        convb=col_layout(inp['ssd_conv_b'][0], 8),
        hglb=np.ascontiguousarray(np.concatenate([col_layout(hl[0], 4), col_layout(hl[1], 4)], axis=1)),
        dtb=np.ascontiguousarray(inp['ssd_dt_bias'][0].reshape(16, 1)))
    maps = []
    for c in range(NCORE):
        s0 = c * TLOC
        edge = np.ones((128, 2), np.float32); edge[:, 0] = 0.0 if c == 0 else 1.0; edge[:, 1] = 0.0 if c == NCORE - 1 else 1.0
        m = dict(common); m.update(xT=np.ascontiguousarray(xTp[:, s0:s0 + TEXT]), edge=edge)
        maps.append(m)
    return blobify(maps, BLOB_D)


LS = NCTX + SEQ
NBLK = LS // 128
GB = 10


def build_E():
    P = Prog()
    Bk = Banks(P)
    di = lambda n, s, dt=F32: P.dram(n, s, dt, "ExternalInput")
    xtok = di("xtok", [2, 128, NBLK, 64]); Btok = di("Btok", [2, 128, NBLK, 128])
    BT = di("BT", [2, 128, LS]); CT = di("CT", [2, 128, LS]); dttok = di("dttok", [2, 128, NBLK])
    bl = Blob(BLOB_E).dram(P)
    ssdp = bl.ap("ssdp")
    qT = di("qT", [2, 128, LS]); kT = di("kT", [2, 128, LS]); gtok = di("gtok", [2, 128, NBLK, 128])
    vZ = di("vZ", [2, 128, NBLK, 5, 64])
    Ud = bl.ap("U"); U4d = bl.ap("U4"); Mnegd = bl.ap("Mneg")
    ident = bl.ap("ident"); onesd = bl.ap("onesd")
    ytok = P.dram("ytok", [2, 128, NBLK, 64], F32, "ExternalOutput"); otok = P.dram("otok", [2, 128, NBLK, 64], F32, "ExternalOutput")

    ones_sb = P.sb([128, 128], F32, name="ones"); P.dma(SP, ones_sb[:], onesd[:, :], writes=[ones_sb])
    U_sb = P.sb([128, 128], F32, name="U"); P.dma(SP, U_sb[:], Ud[:, :], writes=[U_sb])
    U4_sb = P.sb([128, 128], F32, name="U4"); P.dma(SP, U4_sb[:], U4d[:, :], writes=[U4_sb])
    Mn_sb = P.sb([128, 128], F32, name="Mneg"); P.dma(SP, Mn_sb[:], Mnegd[:, :], writes=[Mn_sb])
    id_b = P.sb([128, 128], BF16, name="idb"); P.dma(POOL, id_b[:], ident[:, :], writes=[id_b])
    sp_sb = P.sb([128, 4], F32, name="ssdp"); P.dma(SP, sp_sb[:], ssdp[:, :], writes=[sp_sb])
    acol = P.sb([128, 2], F32, name="acol")
    P.I(ACT, "activation", [sp_sb], [acol], out=acol[:], in_=sp_sb[:, 0:2], func=AF.Exp)
    P.I(DVE, "tensor_scalar", [acol], [acol], out=acol[:], in0=acol[:], scalar1=-1.0, scalar2=None, op0=ALU.mult)
    outs = []

    with P.scope():
        dt_sb = P.sb([128, 2, NBLK], F32, name="dt_sb")
        for d in range(2):
            P.dma(SP, dt_sb[:, d, :], dttok[d], writes=[dt_sb])
        xg = [P.sb([128, GB, 64], F32, name=f"xg{i}") for i in range(2)]
        Bg = [P.sb([128, GB, 128], BF16, name=f"Bg{i}") for i in range(2)]
        BTg = [P.sb([128, GB * 128], BF16, name=f"BTg{i}") for i in range(2)]
        CTg = [P.sb([128, GB * 128], F32, name=f"CTg{i}") for i in range(2)]
        CTh = [P.sb([128, GB * 128], BF16, name=f"CTh{i}") for i in range(2)]
        yg = [P.sb([128, GB, 64], F32, name=f"yg{i}") for i in range(2)]
        da = P.sb([128, 1], F32, name="da"); dab = P.sb([128, 128], F32, name="dab")
        cs_sb = P.sb([128, 1], F32, name="cs_sb"); tot_sb = P.sb([128, 1], F32, name="tot_sb")
        te = P.sb([128, 1], F32, name="te"); dec = P.sb([128, 1], F32, name="dec"); wcol = P.sb([128, 1], F32, name="wcol")
        xdt = P.sb([128, 64], BF16, name="xdt"); xw = P.sb([128, 64], BF16, name="xw")
        em = P.sb([128, 128], F32, name="em"); gt = P.sb([128, 128], BF16, name="gt")
        ecs = P.sb([128, 128], F32, name="ecs"); cp = P.sb([128, 128], BF16, name="cp")
        S = P.sb([128, 64], F32, name="S_ssd"); Sbf = P.sb([128, 64], BF16, name="Sbf_ssd")
        gi = 0
        for d in range(2):
            P.I(DVE, "memset", [], [S], S[:], 0.0)
            P.I(POOL, "memset", [], [Sbf], Sbf[:], 0.0)
            for g0 in range(0, NBLK, GB):
                i = gi % 2; gi += 1
                P.dma(SP, xg[i][:], xtok[d, :, g0:g0 + GB, :], writes=[xg[i]])
                P.dma(POOL, Bg[i][:], Btok[d, :, g0:g0 + GB, :], writes=[Bg[i]])
                P.dma(POOL, BTg[i][:], BT[d, :, g0 * 128:(g0 + GB) * 128], writes=[BTg[i]])
                P.dma(ACT, CTg[i][:], CT[d, :, g0 * 128:(g0 + GB) * 128], writes=[CTg[i]])
                P.dma(POOL, CTh[i][:], CT[d, :, g0 * 128:(g0 + GB) * 128], writes=[CTh[i]])
                for bb in range(GB):
                    b = g0 + bb
                    cols = slice(bb * 128, (bb + 1) * 128)
                    P.I(DVE, "tensor_scalar", [dt_sb, acol], [da], out=da[:], in0=dt_sb[:, d, b:b + 1], scalar1=acol[:, d:d + 1], scalar2=None, op0=ALU.mult)
                    P.I(DVE, "tensor_scalar", [ones_sb, da], [dab], out=dab[:], in0=ones_sb[:], scalar1=da[:, 0:1], scalar2=None, op0=ALU.mult)
                    pc, pr = Bk.f[0], Bk.f[1]
                    P.I(PE, "matmul", [U_sb, da], [pc], pc[:, 0:1], U_sb[:], da[:], start=True, stop=True)
                    P.I(PE, "matmul", [dab, U_sb], [pr], pr[:, 0:128], dab[:], U_sb[:], start=True, stop=True)
                    P.I(ACT, "activation", [pc], [cs_sb], out=cs_sb[:], in_=pc[:, 0:1], func=AF.Copy)
                    P.I(ACT, "activation", [pr], [tot_sb], out=tot_sb[:], in_=pr[:, 127:128], func=AF.Copy)
                    P.I(ACT, "activation", [cs_sb, tot_sb], [te], out=te[:], in_=cs_sb[:], func=AF.Exp, scale=-1.0, bias=tot_sb[:, 0:1])
                    P.I(ACT, "activation", [tot_sb], [dec], out=dec[:], in_=tot_sb[:], func=AF.Exp)
                    P.I(DVE, "tensor_scalar", [xg[i], dt_sb], [xdt], out=xdt[:], in0=xg[i][:, bb, :], scalar1=dt_sb[:, d, b:b + 1], scalar2=None, op0=ALU.mult)
                    P.I(DVE, "tensor_tensor", [dt_sb, te], [wcol], out=wcol[:], in0=dt_sb[:, d, b:b + 1], in1=te[:], op=ALU.mult)
                    P.I(DVE, "tensor_scalar", [xg[i], wcol], [xw], out=xw[:], in0=xg[i][:, bb, :], scalar1=wcol[:, 0:1], scalar2=None, op0=ALU.mult)
                    psc = Bk.f[2]
                    P.I(PE, "matmul", [Bg[i], xw], [psc], psc[:, 0:64], Bg[i][:, bb, :], xw[:], start=True, stop=True)
                    pss = Bk.f[3]
                    P.I(PE, "matmul", [BTg[i], CTh[i]], [pss], pss[:, 0:128], BTg[i][:, cols], CTh[i][:, cols], start=True, stop=True)
                    P.I(DVE, "scalar_tensor_tensor", [pr, cs_sb, Mn_sb], [em], out=em[:], in0=pr[:, 0:128], scalar=cs_sb[:, 0:1], in1=Mn_sb[:], op0=ALU.subtract, op1=ALU.add)
                    P.I(ACT, "activation", [em], [em], out=em[:], in_=em[:], func=AF.Exp)
                    P.I(DVE, "tensor_tensor", [pss, em], [gt], out=gt[:], in0=pss[:, 0:128], in1=em[:], op=ALU.mult)
                    P.I(ACT, "activation", [pr], [ecs], out=ecs[:], in_=pr[:, 0:128], func=AF.Exp)
                    P.I(DVE, "tensor_tensor", [CTg[i], ecs], [cp], out=cp[:], in0=CTg[i][:, cols], in1=ecs[:], op=ALU.mult)
                    py = Bk.f[4]
                    P.I(PE, "matmul", [gt, xdt], [py], py[:, 0:64], gt[:], xdt[:], start=True, stop=False)
                    P.I(PE, "matmul", [cp, Sbf], [py], py[:, 0:64], cp[:], Sbf[:], start=False, stop=True)
                    P.I(DVE, "scalar_tensor_tensor", [xg[i], sp_sb, py], [yg[i]], out=yg[i][:, bb, :], in0=xg[i][:, bb, :], scalar=sp_sb[:, 2 + d:3 + d], in1=py[:, 0:64], op0=ALU.mult, op1=ALU.add)
                    P.I(DVE, "scalar_tensor_tensor", [S, dec, psc], [S], out=S[:], in0=S[:], scalar=dec[:, 0:1], in1=psc[:, 0:64], op0=ALU.mult, op1=ALU.add)
                    P.I(ACT, "activation", [S], [Sbf], out=Sbf[:], in_=S[:], func=AF.Copy)
                outs.append(P.dma(SP, ytok[d, :, g0:g0 + GB, :], yg[i][:], reads=[yg[i]]))

    with P.scope():
        qg = [P.sb([128, GB * 128], F32, name=f"qg{i}") for i in range(2)]
        kg = [P.sb([128, GB * 128], F32, name=f"kg{i}") for i in range(2)]
        gg = [P.sb([128, GB, 128], F32, name=f"gg{i}") for i in range(2)]
        vg = [P.sb([128, GB, 5, 64], BF16, name=f"vg{i}") for i in range(2)]
        og = [P.sb([128, GB, 64], F32, name=f"og{i}") for i in range(2)]
        cum = P.sb([128, 128], F32, name="cum"); d1 = P.sb([128, 128], F32, name="d1"); d2 = P.sb([128, 128], F32, name="d2")
        e1 = P.sb([128, 128], F32, name="e1"); e2 = P.sb([128, 128], F32, name="e2"); e3 = P.sb([128, 128], F32, name="e3"); e4 = P.sb([128, 128], F32, name="e4")
        dec4 = P.sb([128, 4], F32, name="dec4")
        QiZ = P.sb([128, 4, 128], BF16, name="QiZ"); P.I(POOL, "memset", [], [QiZ], QiZ[:], 0.0)
        Qp = P.sb([128, 128], BF16, name="Qp"); Kp = P.sb([128, 128], BF16, name="Kp"); Kpp = P.sb([128, 128], BF16, name="Kpp")
        am = P.sb([128, 128], F32, name="am"); amb = P.sb([128, 128], BF16, name="amb"); Ktok = P.sb([128, 128], BF16, name="Ktok")
        S = P.sb([128, 64], F32, name="S_hg"); Sst = [P.sb([128, 4, 64], BF16, name=f"Sst{i}") for i in range(2)]
        c3 = lambda t: t[:].rearrange("p (c t) -> p c t", t=32)
        QiZd = bass.AP(QiZ[:].tensor, 0, [[QiZ[:].ap[0][0], 128], [128 + 32, 4], [1, 32]])
        gi = 0; blk = 0
        for d in range(2):
            P.I(DVE, "memset", [], [S], S[:], 0.0)
            P.I(POOL, "memset", [], [Sst[blk % 2]], Sst[blk % 2][:], 0.0)
            for g0 in range(0, NBLK, GB):
                i = gi % 2; gi += 1
                P.dma(SP, qg[i][:], qT[d, :, g0 * 128:(g0 + GB) * 128], writes=[qg[i]])
                P.dma(ACT, kg[i][:], kT[d, :, g0 * 128:(g0 + GB) * 128], writes=[kg[i]])
                P.dma(SP, gg[i][:], gtok[d, :, g0:g0 + GB, :], writes=[gg[i]])
                P.dma(POOL, vg[i][:], vZ[d, :, g0:g0 + GB, :, :], writes=[vg[i]])
                for bb in range(GB):
                    cols = slice(bb * 128, (bb + 1) * 128)
                    cur, nxt = Sst[blk % 2], Sst[(blk + 1) % 2]; blk += 1
                    pcum = Bk.f[0]
                    P.I(PE, "matmul", [gg[i], U4_sb], [pcum], pcum[:, 0:128], gg[i][:, bb, :], U4_sb[:], start=True, stop=True)
                    P.I(ACT, "activation", [pcum], [cum], out=cum[:], in_=pcum[:, 0:128], func=AF.Copy)
                    rmid = c3(cum)[:, :, 15:16].broadcast_to([128, 4, 32]); cend = c3(cum)[:, :, 31:32].broadcast_to([128, 4, 32])
                    P.I(DVE, "tensor_tensor", [cum], [d1], out=c3(d1), in0=c3(cum), in1=rmid, op=ALU.subtract)
                    P.I(DVE, "tensor_tensor", [cum], [d2], out=c3(d2), in0=c3(cum), in1=cend, op=ALU.subtract)
                    P.I(ACT, "activation", [cum], [e1], out=e1[:], in_=cum[:], func=AF.Exp)
                    P.I(ACT, "activation", [d1], [e2], out=e2[:], in_=d1[:], func=AF.Exp)
                    P.I(ACT, "activation", [d1], [e3], out=e3[:], in_=d1[:], func=AF.Exp, scale=-1.0)
                    P.I(ACT, "activation", [d2], [e4], out=e4[:], in_=d2[:], func=AF.Exp, scale=-1.0)
                    P.I(ACT, "activation", [cum], [dec4], out=dec4[:], in_=c3(cum)[:, :, 31], func=AF.Exp)
                    P.I(DVE, "tensor_tensor", [qg[i], e1], [QiZ], out=QiZd, in0=qg[i][:, cols].rearrange("p (c t) -> p c t", t=32), in1=c3(e1), op=ALU.mult)
                    P.I(DVE, "tensor_tensor", [qg[i], e2], [Qp], out=Qp[:], in0=qg[i][:, cols], in1=e2[:], op=ALU.mult)
                    P.I(DVE, "tensor_tensor", [kg[i], e3], [Kp], out=Kp[:], in0=kg[i][:, cols], in1=e3[:], op=ALU.mult)
                    P.I(DVE, "tensor_tensor", [kg[i], e4], [Kpp], out=Kpp[:], in0=kg[i][:, cols], in1=e4[:], op=ALU.mult)
                    pa = Bk.f[1]
                    P.I(PE, "matmul", [Kp, Qp], [pa], pa[:, 0:128], Kp[:], Qp[:], start=True, stop=True)
                    P.I(DVE, "tensor_scalar", [pa], [am], out=am[:], in0=pa[:, 0:128], scalar1=1e30, scalar2=-1e30, op0=ALU.min, op1=ALU.max)
                    P.I(DVE, "tensor_tensor", [am, U4_sb], [amb], out=amb[:], in0=am[:], in1=U4_sb[:], op=ALU.mult)
                    pt = Bk.h[0]
                    P.I(PE, "transpose", [Kpp, id_b], [pt], pt[:, 0:128], Kpp[:], id_b[:])
                    P.I(ACT, "activation", [pt], [Ktok], out=Ktok[:], in_=pt[:, 0:128], func=AF.Copy)
                    po = Bk.f[2]
                    P.I(PE, "matmul", [amb, vg[i]], [po], po[:, 0:64], amb[:], vg[i][:, bb, 4, :], start=True, stop=False)
                    for ci in range(4):
                        P.I(PE, "matmul", [QiZ, cur], [po], po[:, 0:64], QiZ[:, ci, :], cur[:, ci, :], start=False, stop=(ci == 3))
                        if True:
                            psc = Bk.f[3 + ci % 2]
                            P.I(PE, "matmul", [Ktok, vg[i]], [psc], psc[:, 0:64], Ktok[:], vg[i][:, bb, ci, :], start=True, stop=True)
                            P.I(DVE, "scalar_tensor_tensor", [S, dec4, psc], [S], out=S[:], in0=S[:], scalar=dec4[:, ci:ci + 1], in1=psc[:, 0:64], op0=ALU.mult, op1=ALU.add)
                            if ci < 3:
                                P.I(ACT, "activation", [S], [cur], out=cur[:, ci + 1, :], in_=S[:], func=AF.Copy)
                            else:
                                P.I(ACT, "activation", [S], [nxt], out=nxt[:, 0, :], in_=S[:], func=AF.Copy)
                    P.I(ACT, "activation", [po], [og[i]], out=og[i][:, bb, :], in_=po[:, 0:64], func=AF.Copy)
                outs.append(P.dma(SP, otok[d, :, g0:g0 + GB, :], og[i][:], reads=[og[i]]))
    return P.finish(outs)


def gather_D(results):
    full = np.concatenate([results[c]['outT'] for c in range(NCORE)], axis=1)
    return full, results[0]['outcT']


def prep_E(inp, dfull, dctx):
    def seq(rows_lat, rows_ctx, d):
        if d == 0:
            return np.concatenate([rows_ctx, rows_lat], axis=1)
        return np.concatenate([rows_ctx[:, ::-1], rows_lat[:, ::-1]], axis=1)

    def blk(a):
        return np.ascontiguousarray(a.T.reshape(NBLK, 128, a.shape[0]).transpose(1, 0, 2))
    s_ = np.arange(128)[:, None]; l_ = np.arange(128)[None, :]
    U = (s_ <= l_).astype(np.float32)
    U4 = ((s_ // 32 == l_ // 32) & (s_ <= l_)).astype(np.float32)
    Mneg = np.where(s_ <= l_, 0.0, MASKNEG).astype(np.float32)
    maps = []
    for c in range(NCORE):
        h = c; g = c // 4; hh = c // 2; vh = c % 2
        R = lambda r0, n, d: seq(dfull[r0:r0 + n], dctx[r0:r0 + n], d)
        m = dict(U=U, U4=U4, Mneg=Mneg, ident=np.eye(128, dtype=np.float32), onesd=np.ones((128, 128), np.float32))
        m['xtok'] = np.stack([blk(R(64 * h, 64, d)) for d in range(2)])
        m['Btok'] = np.stack([blk(R(512 + 128 * g, 128, d)) for d in range(2)])
        m['BT'] = np.stack([np.ascontiguousarray(R(512 + 128 * g, 128, d)) for d in range(2)])
        m['CT'] = np.stack([np.ascontiguousarray(R(768 + 128 * g, 128, d)) for d in range(2)])
        m['dttok'] = np.stack([np.ascontiguousarray(R(5120 + 8 * d + h, 1, d)[0].reshape(NBLK, 128).T) for d in range(2)])
        al = inp['ssd_A_log'][0]; dd = inp['ssd_D'][0]
        m['ssdp'] = np.ascontiguousarray(np.broadcast_to(np.array([al[0, h], al[1, h], dd[0, h], dd[1, h]], np.float32)[None, :], (128, 4)))
        m['qT'] = np.stack([np.ascontiguousarray(R(4096 + 128 * hh, 128, d)) for d in range(2)])
        m['kT'] = np.stack([np.ascontiguousarray(R(1024 + 512 * d + 128 * hh, 128, d)) for d in range(2)])
        m['gtok'] = np.stack([blk(R(2048 + 512 * d + 128 * hh, 128, d)) for d in range(2)])
        vz = []
        for d in range(2):
            v = blk(R(3072 + 128 * hh + 64 * vh, 64, d))
            z5 = np.zeros((128, NBLK, 5, 64), np.float32)
            z5[:, :, 4, :] = v
            for i in range(4):
                z5[32 * i:32 * i + 32, :, i, :] = v[32 * i:32 * i + 32]
            vz.append(z5)
        m['vZ'] = np.stack(vz)
        maps.append(m)
    return blobify(maps, BLOB_E)


def gather_E(results):
    yT = np.zeros((2, 512, SEQ), np.float32); oT = np.zeros((2, 512, SEQ), np.float32)
    for c in range(NCORE):
        h = c; hh = c // 2; vh = c % 2
        for d in range(2):
            for name, dst, r0 in (("ytok", yT, 64 * h), ("otok", oT, 128 * hh + 64 * vh)):
                a = results[c][name][d].transpose(1, 0, 2).reshape(LS, 64)[NCTX:]
                if d == 1:
                    a = a[::-1]
                dst[d, r0:r0 + 64] = a.T
    return yT, oT


def build_M():
    P = Prog()
    Bk = Banks(P)
    di = lambda n, s, dt=F32: P.dram(n, s, dt, "ExternalInput")
    yT = di("yT", [2, 512, TLOC]); oT = di("oT", [2, 512, TLOC]); szT = di("szT", [512, TLOC]); sgT = di("sgT", [512, TLOC])
    bl = Blob(BLOB_M).dram(P)
    nrm = bl.ap("nrm"); onesd = bl.ap("onesd")
    mT = P.dram("mT", [D, TLOC], F32, "ExternalOutput")
    ones_sb = P.sb([128, 128], F32, name="ones"); P.dma(SP, ones_sb[:], onesd[:, :], writes=[ones_sb])
    nrm_sb = P.sb([128, 8], F32, name="nrm"); P.dma(SP, nrm_sb[:], nrm[:, :], writes=[nrm_sb])
    a = [P.sb([128, 4, 512], F32, name=f"ma{i}") for i in range(2)]
    b = [P.sb([128, 4, 512], F32, name=f"mb{i}") for i in range(2)]
    g = [P.sb([128, 4, 512], F32, name=f"mg{i}") for i in range(2)]
    sq = P.sb([128, 4, 512], F32, name="msq"); rs = P.sb([128, 512], F32, name="mrs")
    outs = []
    it = 0
    for part in range(2):
        src = yT if part == 0 else oT
        gsrc = szT if part == 0 else sgT
        for t0 in range(0, TLOC, 512):
            i = it % 2; it += 1
            P.dma(SP, a[i][:], src[0, :, t0:t0 + 512].rearrange("(k p) t -> p k t", p=128), writes=[a[i]])
            P.dma(ACT, b[i][:], src[1, :, t0:t0 + 512].rearrange("(k p) t -> p k t", p=128), writes=[b[i]])
            P.dma(SP, g[i][:], gsrc[:, t0:t0 + 512].rearrange("(k p) t -> p k t", p=128), writes=[g[i]])
            P.I(DVE, "tensor_tensor", [a[i], b[i]], [a[i]], out=a[i][:], in0=a[i][:], in1=b[i][:], op=ALU.add)
            if part == 0:
                P.I(DVE, "tensor_tensor", [a[i], g[i]], [a[i]], out=a[i][:], in0=a[i][:], in1=g[i][:], op=ALU.mult)
            P.I(ACT, "activation", [a[i]], [sq], out=sq[:], in_=a[i][:], func=AF.Square)
            groups = [(0, 2), (2, 4)] if part == 0 else [(0, 1), (1, 2), (2, 3), (3, 4)]
            for (k0, k1) in groups:
                ps = Bk.f[k0 % 2]
                for kc in range(k0, k1):
                    P.I(PE, "matmul", [ones_sb, sq], [ps], ps[:, 0:512], ones_sb[:], sq[:, kc, :], start=(kc == k0), stop=(kc == k1 - 1))
                P.I(DVE, "tensor_scalar", [ps], [rs], out=rs[:], in0=ps[:, 0:512], scalar1=1.0 / (128 * (k1 - k0)), scalar2=EPS, op0=ALU.mult, op1=ALU.add)
                P.I(ACT, "activation", [rs], [rs], out=rs[:], in_=rs[:], func=AF.Ln)
                P.I(ACT, "activation", [rs], [rs], out=rs[:], in_=rs[:], func=AF.Exp, scale=-0.5)
                for kc in range(k0, k1):
                    P.I(DVE, "scalar_tensor_tensor", [a[i], nrm_sb, rs], [b[i]], out=b[i][:, kc, :], in0=a[i][:, kc, :], scalar=nrm_sb[:, part * 4 + kc:part * 4 + kc + 1],
                        in1=rs[:], op0=ALU.mult, op1=ALU.mult)
            if part == 1:
                P.I(DVE, "tensor_tensor", [b[i], g[i]], [b[i]], out=b[i][:], in0=b[i][:], in1=g[i][:], op=ALU.mult)
            outs.append(P.dma(POOL, mT[part * 512:(part + 1) * 512, t0:t0 + 512].rearrange("(k p) t -> p k t", p=128), b[i][:], reads=[b[i]]))
    return P.finish(outs)


def prep_M(inp, yT, oT, dfull):
    nrm = np.concatenate([col_layout(inp['ssd_norm'][0], 4), col_layout(inp['hg_norm'][0], 4)], axis=1)
    maps = []
    for c in range(NCORE):
        sl = slice(c * TLOC, (c + 1) * TLOC)
        maps.append(dict(yT=np.ascontiguousarray(yT[:, :, sl]), oT=np.ascontiguousarray(oT[:, :, sl]),
                         szT=np.ascontiguousarray(dfull[3584:4096, sl]), sgT=np.ascontiguousarray(dfull[4608:5120, sl]),
                         nrm=np.ascontiguousarray(nrm), onesd=np.ones((128, 128), np.float32)))
    return blobify(maps, BLOB_M)


def _run(nc, maps, tag=""):
    import time, sys
    t0 = time.time()
    r = run_bass_kernel_spmd(nc, maps, core_ids=list(range(len(maps)))).results
    print(f"[kernel] launch {tag}: {time.time() - t0:.1f}s", file=sys.stderr, flush=True)
    return r


def kernel(**inp):
    inp = {k: np.asarray(v) for k, v in inp.items()}
    x = inp['x'][0]; ctx = inp['ctx'][0]
    xT = np.ascontiguousarray(x.T); xcT = np.ascontiguousarray(ctx.T)
    rA = _run(build_A(), prep_A(inp), 'A')
    uT = np.concatenate([rA[c]['uT'] for c in range(NCORE)], axis=1)
    attT = np.concatenate([rA[c]['attT'] for c in range(NCORE)], axis=1)
    ucT = rA[0]['ucT']; attcT = rA[0]['attcT']
    hyT = gather_B(_run(build_B(SEQ), prep_B(inp, uT, SEQ), 'B'), SEQ)
    hycT = gather_B(_run(build_B(NCTX), prep_B(inp, ucT, NCTX), 'Bc'), NCTX)
    mT = np.concatenate([hyT, attT], axis=0); mcT = np.concatenate([hycT, attcT], axis=0)
    maps, passes = prep_C(inp, 0, mT, xT, mcT, xcT)
    xT, xcT = gather_C(_run(build_C(passes), maps, 'C0'), True)
    dfull, dctx = gather_D(_run(build_D(), prep_D(inp, xT, xcT), 'D'))
    yT, oT = gather_E(_run(build_E(), prep_E(inp, dfull, dctx), 'E'))
    rM = _run(build_M(), prep_M(inp, yT, oT, dfull), 'M')
    mT = np.concatenate([rM[c]['mT'] for c in range(NCORE)], axis=1)
    maps, passes = prep_C(inp, 1, mT, xT, None, None)
    xT, _ = gather_C(_run(build_C(passes), maps, 'C1'), False)
    return np.ascontiguousarray(xT.T)[None].astype(np.float32)
```
